# Optimizing a Trainium2 kernel written in Bass

```python
import jax, jax.numpy as jnp
from jax import lax
import numpy as np

D_MODEL = 2048
BATCH = 4
SEQ = 2048
DEPTH = 1
DEC_BATCH = 128
DEC_SEQ = 8
PAST_LEN = 16384
PAGE_SIZE = 128

RET_HEADS = 8
RET_DK = D_MODEL // RET_HEADS
RET_DV = 2 * D_MODEL // RET_HEADS
RET_CHUNK = 128
ROPE_BASE = 10000.0
D_CONV = D_MODEL
CONV_WIDTH = 31
N_GROUPS = 4
EXPERTS_PER_GROUP = 8
N_EXPERTS = N_GROUPS * EXPERTS_PER_GROUP
TOP_K_FINE = 2
D_EXPERT = D_MODEL // 2
MOE_BLOCK = 128
EPS = 1e-6

W_Q = RET_HEADS * RET_DK
W_K = RET_HEADS * RET_DK
W_V = RET_HEADS * RET_DV
W_G = RET_HEADS * RET_DV
W_CA = D_CONV
W_CB = D_CONV
W_GATE_RET = D_MODEL
W_GATE_CONV = D_MODEL
IN_COLS = W_Q + W_K + W_V + W_G + W_CA + W_CB + W_GATE_RET + W_GATE_CONV
SPLIT_POINTS = (W_Q, W_Q + W_K, W_Q + W_K + W_V, W_Q + W_K + W_V + W_G,
                W_Q + W_K + W_V + W_G + W_CA, W_Q + W_K + W_V + W_G + W_CA + W_CB,
                W_Q + W_K + W_V + W_G + W_CA + W_CB + W_GATE_RET)

kernel_name = "retnet_conformer_hmoe_decode_step"


def rmsnorm(x, w):
    xf = x.astype(jnp.float32)
    y = xf * lax.rsqrt(jnp.mean(xf * xf, axis=-1, keepdims=True) + EPS) * w.astype(jnp.float32)
    return y.astype(x.dtype)


def rotary(x, pos):
    half = x.shape[-1] // 2
    inv = ROPE_BASE ** (-jnp.arange(half, dtype=jnp.float32) / half)
    ang = pos[:, None] * inv[None, :]
    cos = jnp.cos(ang)[None, :, None, :]
    sin = jnp.sin(ang)[None, :, None, :]
    x1, x2 = x[..., :half], x[..., half:]
    return jnp.concatenate([x1 * cos - x2 * sin, x1 * sin + x2 * cos], axis=-1)


def retention(q, k, v, s0):
    b, l, h, _ = q.shape
    dv = v.shape[-1]
    c = RET_CHUNK if l % RET_CHUNK == 0 else l
    n = l // c
    log_g = jnp.log1p(-jnp.exp2(-5.0 - jnp.arange(h, dtype=jnp.float32)))
    idx = jnp.arange(c, dtype=jnp.float32)
    diff = idx[:, None] - idx[None, :]
    decay = jnp.where(diff[None] >= 0.0,
                      jnp.exp(jnp.maximum(diff, 0.0)[None] * log_g[:, None, None]), 0.0)
    q_dec = jnp.exp((idx[:, None] + 1.0) * log_g[None, :])
    k_dec = jnp.exp((c - 1.0 - idx)[:, None] * log_g[None, :])
    c_dec = jnp.exp(c * log_g)

    def to_chunks(t):
        return t.reshape(b, n, c, h, t.shape[-1]).swapaxes(0, 1)

    def step(s, inp):
        qc, kc, vc = inp
        att = jnp.einsum('bihd,bjhd->bhij', qc, kc) * decay
        o = (jnp.einsum('bhij,bjhe->bihe', att, vc)
             + jnp.einsum('bihd,bhde->bihe', qc * q_dec[:, :, None], s))
        s = s * c_dec[:, None, None] + jnp.einsum('bjhd,bjhe->bhde', kc * k_dec[:, :, None], vc)
        return s, o

    s, o = lax.scan(step, s0, (to_chunks(q), to_chunks(k), to_chunks(v)))
    return o.swapaxes(0, 1).reshape(b, l, h, dv), s


def conv_module(a, bgate, buf, conv_w, conv_b, ln_w, ln_b, w_out):
    u = a * jax.nn.sigmoid(bgate)
    u_pad = jnp.concatenate([buf.astype(u.dtype), u], axis=1)
    new_buf = u_pad[:, -(CONV_WIDTH - 1):]
    c = lax.conv_general_dilated(u_pad, conv_w[:, None, :].astype(u.dtype), (1,), 'VALID',
                                 dimension_numbers=('NWC', 'WIO', 'NWC'),
                                 feature_group_count=D_CONV)
    cf = c.astype(jnp.float32) + conv_b.astype(jnp.float32)
    mu = jnp.mean(cf, axis=-1, keepdims=True)
    var = jnp.mean(jnp.square(cf - mu), axis=-1, keepdims=True)
    cn = (cf - mu) * lax.rsqrt(var + EPS) * ln_w.astype(jnp.float32) + ln_b.astype(jnp.float32)
    return jnp.matmul(jax.nn.silu(cn).astype(a.dtype), w_out), new_buf


def hier_moe(x, w_coarse, b_coarse, w_fine, b_fine, w_gate, w_up, w_down):
    n_tok, d = x.shape
    lc = jnp.matmul(x, w_coarse).astype(jnp.float32) + b_coarse.astype(jnp.float32)
    pc = jax.nn.softmax(lc, axis=-1)
    grp = jnp.argmax(lc, axis=-1).astype(jnp.int32)
    p_grp = jnp.take_along_axis(pc, grp[:, None], axis=-1)
    lf = (jnp.matmul(x, w_fine).astype(jnp.float32) + b_fine.astype(jnp.float32)
          ).reshape(n_tok, N_GROUPS, EXPERTS_PER_GROUP)
    lf_g = jnp.take_along_axis(lf, grp[:, None, None], axis=1)[:, 0]
    top_v, top_i = lax.top_k(lf_g, TOP_K_FINE)
    gate = p_grp * jax.nn.softmax(top_v, axis=-1)
    expert = grp[:, None] * EXPERTS_PER_GROUP + top_i.astype(jnp.int32)

    n_asg = n_tok * TOP_K_FINE
    e_flat = expert.reshape(-1)
    t_flat = jnp.repeat(jnp.arange(n_tok, dtype=jnp.int32), TOP_K_FINE)
    g_flat = gate.reshape(-1)
    order = jnp.argsort(e_flat)
    e_s, t_s, g_s = e_flat[order], t_flat[order], g_flat[order]
    counts = jax.ops.segment_sum(jnp.ones_like(e_flat), e_flat, num_segments=N_EXPERTS)
    start = jnp.cumsum(counts) - counts
    padded = (counts + MOE_BLOCK - 1) // MOE_BLOCK * MOE_BLOCK
    pend = jnp.cumsum(padded)
    pstart = pend - padded
    row = pstart[e_s] + jnp.arange(n_asg, dtype=jnp.int32) - start[e_s]
    n_blocks = -(-n_asg // MOE_BLOCK) + N_EXPERTS
    n_rows = n_blocks * MOE_BLOCK
    tok_rows = jnp.full((n_rows,), n_tok, jnp.int32).at[row].set(t_s)
    gate_rows = jnp.zeros((n_rows,), jnp.float32).at[row].set(g_s)
    blk_exp = jnp.minimum(
        jnp.searchsorted(pend, jnp.arange(n_blocks, dtype=jnp.int32) * MOE_BLOCK, side='right'),
        N_EXPERTS - 1).astype(jnp.int32)
    x_pad = jnp.concatenate([x, jnp.zeros((1, d), x.dtype)], axis=0)

    def expert_block(args):
        idx, e = args
        xb = x_pad[idx]
        hb = jax.nn.silu(jnp.matmul(xb, w_gate[e])) * jnp.matmul(xb, w_up[e])
        return jnp.matmul(hb, w_down[e])

    out = lax.map(expert_block, (tok_rows.reshape(n_blocks, MOE_BLOCK), blk_exp))
    out = out.reshape(n_rows, d) * gate_rows[:, None].astype(x.dtype)
    return jax.ops.segment_sum(out, tok_rows, num_segments=n_tok + 1)[:n_tok]


def trunk_layer(x, s_ret, s_conv, pos0, norm_mix, w_in, w_ret_o, conv_w, conv_b, conv_ln_w,
                conv_ln_b, w_conv_o, w_o, norm_ffn, w_coarse, b_coarse, w_fine, b_fine,
                w_gate, w_up, w_down):
    b, l, _ = x.shape
    h = rmsnorm(x, norm_mix)
    z = jnp.matmul(h, w_in)
    q, k, v, g, ca, cb, g_ret, g_conv = jnp.split(z, SPLIT_POINTS, axis=-1)
    pos = (pos0 + jnp.arange(l)).astype(jnp.float32)
    q = rotary(q.reshape(b, l, RET_HEADS, RET_DK).astype(jnp.float32), pos)
    k = rotary(k.reshape(b, l, RET_HEADS, RET_DK).astype(jnp.float32), pos) * (RET_DK ** -0.5)
    v = v.reshape(b, l, RET_HEADS, RET_DV).astype(jnp.float32)
    o, s_ret_new = retention(q, k, v, s_ret.astype(jnp.float32))
    o = o * lax.rsqrt(jnp.mean(o * o, axis=-1, keepdims=True) + EPS)
    o = (jax.nn.silu(g.reshape(b, l, RET_HEADS, RET_DV).astype(jnp.float32)) * o
         ).astype(x.dtype).reshape(b, l, RET_HEADS * RET_DV)
    ret_out = jnp.matmul(o, w_ret_o)
    conv_out, s_conv_new = conv_module(ca, cb, s_conv, conv_w, conv_b, conv_ln_w, conv_ln_b, w_conv_o)
    merged = jax.nn.sigmoid(g_ret) * ret_out + jax.nn.sigmoid(g_conv) * conv_out
    x = x + jnp.matmul(merged, w_o)
    h2 = rmsnorm(x, norm_ffn)
    x = x + hier_moe(h2.reshape(b * l, -1), w_coarse, b_coarse, w_fine, b_fine,
                     w_gate, w_up, w_down).reshape(b, l, -1)
    return x, s_ret_new.astype(s_ret.dtype), s_conv_new


def setup_inputs(seed: int = 0) -> dict:
    key = jax.random.key(seed)
    ks = jax.random.split(key, 24)

    def nrm(k, shape, scale):
        return jax.random.normal(k, shape, jnp.float32) * scale

    return {
        "x_prompt": nrm(ks[0], (BATCH, SEQ, D_MODEL), 1.0),
        "x_sample": nrm(ks[1], (DEC_BATCH, DEC_SEQ, D_MODEL), 1.0),
        "state_ret": nrm(ks[2], (DEPTH, DEC_BATCH, RET_HEADS, RET_DK, RET_DV), 0.1),
        "state_conv": nrm(ks[3], (DEPTH, DEC_BATCH, CONV_WIDTH - 1, D_CONV), 0.5),
        "norm_mix": 1.0 + nrm(ks[4], (DEPTH, D_MODEL), 0.01),
        "w_in": nrm(ks[5], (DEPTH, D_MODEL, IN_COLS), D_MODEL ** -0.5),
        "w_ret_o": nrm(ks[6], (DEPTH, RET_HEADS * RET_DV, D_MODEL), (RET_HEADS * RET_DV) ** -0.5),
        "conv_w": nrm(ks[7], (DEPTH, CONV_WIDTH, D_CONV), CONV_WIDTH ** -0.5),
        "conv_b": nrm(ks[8], (DEPTH, D_CONV), 0.01),
        "conv_ln_w": 1.0 + nrm(ks[9], (DEPTH, D_CONV), 0.01),
        "conv_ln_b": nrm(ks[10], (DEPTH, D_CONV), 0.01),
        "w_conv_o": nrm(ks[11], (DEPTH, D_CONV, D_MODEL), D_CONV ** -0.5),
        "w_o": nrm(ks[12], (DEPTH, D_MODEL, D_MODEL), D_MODEL ** -0.5),
        "norm_ffn": 1.0 + nrm(ks[13], (DEPTH, D_MODEL), 0.01),
        "w_coarse": nrm(ks[14], (DEPTH, D_MODEL, N_GROUPS), D_MODEL ** -0.5),
        "b_coarse": nrm(ks[15], (DEPTH, N_GROUPS), 0.01),
        "w_fine": nrm(ks[16], (DEPTH, D_MODEL, N_EXPERTS), D_MODEL ** -0.5),
        "b_fine": nrm(ks[17], (DEPTH, N_EXPERTS), 0.01),
        "w_gate": nrm(ks[18], (DEPTH, N_EXPERTS, D_MODEL, D_EXPERT), D_MODEL ** -0.5),
        "w_up": nrm(ks[19], (DEPTH, N_EXPERTS, D_MODEL, D_EXPERT), D_MODEL ** -0.5),
        "w_down": nrm(ks[20], (DEPTH, N_EXPERTS, D_EXPERT, D_MODEL), D_EXPERT ** -0.5),
        "norm_f": 1.0 + nrm(ks[21], (D_MODEL,), 0.01),
    }


def reference(x_prompt, x_sample, state_ret, state_conv, norm_mix, w_in, w_ret_o, conv_w, conv_b,
              conv_ln_w, conv_ln_b, w_conv_o, w_o, norm_ffn, w_coarse, b_coarse, w_fine, b_fine,
              w_gate, w_up, w_down, norm_f):
    yp, ys = x_prompt, x_sample
    bp = x_prompt.shape[0]
    ret_p, conv_p, ret_s, conv_s = [], [], [], []
    for l in range(DEPTH):
        lw = (norm_mix[l], w_in[l], w_ret_o[l], conv_w[l], conv_b[l], conv_ln_w[l], conv_ln_b[l],
              w_conv_o[l], w_o[l], norm_ffn[l], w_coarse[l], b_coarse[l], w_fine[l], b_fine[l],
              w_gate[l], w_up[l], w_down[l])
        zero_ret = jnp.zeros((bp, RET_HEADS, RET_DK, RET_DV), state_ret.dtype)
        zero_conv = jnp.zeros((bp, CONV_WIDTH - 1, D_CONV), x_prompt.dtype)
        yp, rp, cp = trunk_layer(yp, zero_ret, zero_conv, 0, *lw)
        ys, rs, cs = trunk_layer(ys, state_ret[l], state_conv[l], PAST_LEN, *lw)
        ret_p.append(rp)
        conv_p.append(cp)
        ret_s.append(rs)
        conv_s.append(cs)
    yp = rmsnorm(yp, norm_f)
    ys = rmsnorm(ys, norm_f)
    return (yp, ys, jnp.stack(ret_p, 0), jnp.stack(conv_p, 0), jnp.stack(ret_s, 0), jnp.stack(conv_s, 0))
```

```python
import contextlib
import numpy as np
import concourse.bass as bass
import concourse.mybir as mybir
from concourse.bass_utils import run_bass_kernel_spmd

F32, BF16, I32, U8 = mybir.dt.float32, mybir.dt.bfloat16, mybir.dt.int32, mybir.dt.uint8
F32R = mybir.dt.float32r
ALU = mybir.AluOpType
AF = mybir.ActivationFunctionType

ENGS = ("pe", "act", "dve", "pool", "sp")
DSIZE = {F32: 4, BF16: 2, I32: 4, U8: 1}

D = 2048
H = 8
DK = 256
DV = 512
NTILE = 9
TOK = 1152
PAST_LEN = 16384
CW = 31
NE = 32
DE = 1024
EPS = 1e-6
Q0, K0, V0, G0, CA0, CB0, GR0, GC0 = 0, 2048, 4096, 8192, 12288, 14336, 16384, 18432
IN_COLS = 20480
BLKS = ((0, 512), (512, 1024), (1024, 1152))
LOG_G = [float(np.log1p(-2.0 ** (-5.0 - h))) for h in range(H)]
CDEC_P = [float(np.exp(128 * LOG_G[h])) for h in range(H)]
CDEC_S = [float(np.exp(8 * LOG_G[h])) for h in range(H)]


class Tile:
    def __init__(self, name, h):
        self.name, self.h = name, h
        self.w = None
        self.r = []
        self.sem = None

    def __getitem__(self, idx):
        return self.h[idx]


class Op:
    __slots__ = ("eng", "fn", "deps", "is_dma", "sem", "count", "need_inc", "val")

    def __init__(self, eng, fn, is_dma=False):
        self.eng, self.fn, self.is_dma = eng, fn, is_dma
        self.deps = []
        self.sem = None
        self.count = 0
        self.need_inc = False
        self.val = 0


class Prog:
    def __init__(self, nc):
        self.nc = nc
        self.ops = {e: [] for e in ENGS}
        self.dma_counts = {}
        self.last_dma = {}
        self.stack = contextlib.ExitStack()
        self.out_ops = []
        self.pending = {e: [] for e in ENGS}

    def sb(self, name, shape, dt):
        h = self.stack.enter_context(self.nc.sbuf_tensor(name, list(shape), dt))
        return Tile(name, h)

    def ps(self, name, shape, dt=F32):
        h = self.stack.enter_context(self.nc.psum_tensor(name, list(shape), dt))
        return Tile(name, h)

    def dram(self, name, shape, dt, kind):
        h = self.nc.dram_tensor(name, list(shape), dt, kind=kind)
        return Tile(name, h.ap())

    def _deps(self, o, reads, writes):
        deps = []
        for t in reads:
            if t.w is not None:
                deps.append((t.w, "raw"))
        for t in writes:
            if t.w is not None:
                deps.append((t.w, "waw"))
            for r in t.r:
                deps.append((r, "war"))
        seen = set()
        for d, kind in deps:
            if d is o or id(d) in seen:
                continue
            if (not d.is_dma) and d.eng == o.eng and not o.is_dma:
                if kind != "raw" or o.eng == "pe":
                    continue
            seen.add(id(d))
            o.deps.append(d)
        for d in self.pending[o.eng]:
            if id(d) not in seen and d is not o:
                seen.add(id(d))
                o.deps.append(d)
        self.pending[o.eng] = []
        for t in reads:
            t.r.append(o)
        for t in writes:
            t.w = o
            t.r = []

    def op(self, eng, fn, reads=(), writes=()):
        o = Op(eng, fn)
        self._deps(o, reads, writes)
        self.ops[eng].append(o)
        return o

    def dma(self, eng, fn, reads=(), writes=(), semtile=None, is_output=False):
        o = Op(eng, fn, is_dma=True)
        if semtile.sem is None:
            semtile.sem = "ds_%s" % semtile.name
        o.sem = semtile.sem
        self.dma_counts[o.sem] = self.dma_counts.get(o.sem, 0) + 16
        o.count = self.dma_counts[o.sem]
        self.last_dma[o.sem] = o
        self._deps(o, reads, writes)
        self.ops[eng].append(o)
        if is_output:
            self.out_ops.append(o)
        return o

    def barrier(self):
        deps = []
        for e in ENGS:
            for o in reversed(self.ops[e]):
                if not o.is_dma:
                    deps.append(o)
                    break
        deps += list(self.last_dma.values())
        for e in ENGS:
            self.pending[e] = self.pending[e] + [d for d in deps if d.is_dma or d.eng != e]

    def emit(self):
        nc = self.nc
        for e in ENGS:
            for o in self.ops[e]:
                for d in o.deps:
                    if not d.is_dma:
                        d.need_inc = True
        for e in ENGS:
            c = 0
            for o in self.ops[e]:
                if (not o.is_dma) and o.need_inc:
                    c += 1
                    o.val = c
        sems = {}
        for e in ENGS:
            sems["eng_" + e] = self.stack.enter_context(nc.semaphore("eng_" + e))
        for s in self.dma_counts:
            sems[s] = self.stack.enter_context(nc.semaphore(s))
        self.nsems = len(sems)
        final = {}
        for o in self.out_ops:
            final[o.sem] = max(final.get(o.sem, 0), o.count)

        def emit_eng(ename, eng):
            waited = {}
            for o in self.ops[ename]:
                for d in o.deps:
                    if d.is_dma:
                        s, v = d.sem, d.count
                    else:
                        s, v = "eng_" + d.eng, d.val
                    if waited.get(s, 0) >= v:
                        continue
                    waited[s] = v
                    eng.wait_ge(sems[s], v)
                ins = o.fn(eng)
                if o.is_dma:
                    ins.then_inc(sems[o.sem], 16)
                elif o.need_inc:
                    ins.then_inc(sems["eng_" + ename], 1)
            if ename == "sp":
                for s, v in final.items():
                    if waited.get(s, 0) < v:
                        eng.wait_ge(sems[s], v)

        with nc.Block() as block:
            @block.sync
            def _(e):
                emit_eng("sp", e)

            @block.scalar
            def _(e):
                emit_eng("act", e)

            @block.vector
            def _(e):
                emit_eng("dve", e)

            @block.gpsimd
            def _(e):
                emit_eng("pool", e)

            @block.tensor
            def _(e):
                emit_eng("pe", e)
        self.stack.close()


class Arena:
    def __init__(self, P, nbytes):
        self.P = P
        self.t = P.stack.enter_context(P.nc.sbuf_tensor("arena", [128, nbytes], U8))
        self.nbytes = nbytes

    def view(self, name, off, shape, dt):
        n = int(np.prod(shape)) * DSIZE[dt]
        assert off + n <= self.nbytes, (name, off, n)
        assert off % 4 == 0
        ap = self.t[:, off:off + n].bitcast(dt)
        if len(shape) == 2:
            ap = ap.rearrange("p (a b) -> p a b", a=shape[0])
        elif len(shape) == 3:
            ap = ap.rearrange("p (a b c) -> p a b c", a=shape[0], b=shape[1])
        return Tile(name, ap)


def build(n_experts=NE, stages="ARCMOE", debug=False):
    nc = bass.Bass("TRN2", target_bir_lowering=False)
    P = Prog(nc)
    IN, OUT = "ExternalInput", "ExternalOutput"
    x_own = P.dram("x_own", [TOK, D], F32, IN)
    x_prev = P.dram("x_prev", [1024, D], F32, IN)
    sret = P.dram("sret", [16, H, DK, DV], F32, IN)
    sconv = P.dram("sconv", [16 * 30, D], F32, IN)
    w_in = P.dram("w_in", [D, IN_COLS], F32, IN)
    w_ret_o = P.dram("w_ret_o", [H * DV, D], F32, IN)
    conv_w = P.dram("conv_w", [CW, D], F32, IN)
    vecs = P.dram("vecs", [128, 6, 16], F32, IN)
    norm_f = P.dram("norm_f", [1, D], F32, IN)
    w_conv_o = P.dram("w_conv_o", [D, D], F32, IN)
    w_o = P.dram("w_o", [D, D], F32, IN)
    w_rt = P.dram("w_rt", [D, 36], F32, IN)
    b_rt = P.dram("b_rt", [1, 36], F32, IN)
    w_gate = P.dram("w_gate", [n_experts, D, DE], F32, IN)
    w_up = P.dram("w_up", [n_experts, D, DE], F32, IN)
    w_down = P.dram("w_down", [n_experts, DE, D], F32, IN)
    cs_own_d = P.dram("cs_own", [128, 2, TOK], F32, IN)
    cs_prev_d = P.dram("cs_prev", [128, 2, 1024], F32, IN)
    dec_d = P.dram("dec", [H, 128, 4, 128], F32, IN)
    masks_d = P.dram("masks", [128, 2, 128], F32, IN)
    ident_d = P.dram("ident", [128, 128], F32, IN)
    rowmask_d = P.dram("rowmask", [128, 16], F32, IN)

    y_out = P.dram("y_out", [TOK, D], F32, OUT)
    retp_out = P.dram("retp_out", [H, DK, DV], F32, OUT)
    convp_out = P.dram("convp_out", [30, D], F32, OUT)
    rets_out = P.dram("rets_out", [16, H, DK, DV], F32, OUT)
    convs_out = P.dram("convs_out", [16, 30, D], F32, OUT)
    sinit = P.dram("sinit_scr", [H, DK, DV], F32, "Internal")
    ospill = P.dram("ospill_scr", [TOK, H * DV], BF16, OUT if debug else "Internal")

    ident_f = P.sb("ident_f", [128, 128], F32)
    ident_b = P.sb("ident_b", [128, 128], BF16)
    rowmask = P.sb("rowmask_s", [128, 16], F32)
    vec_s = P.sb("vec_s", [128, 6, 16], F32)
    hTh = P.sb("hTh", [128, 16, 128], BF16)
    small = P.sb("small", [128, 16], F32)
    epsb = P.sb("epsb", [128, 1], F32)

    AR = Arena(P, 184 * 1024)
    W = [AR.view("W%d" % i, i * 16384, [16, 512], BF16) for i in range(3)]
    OFF_HT = 49152
    OFF_HEAD = OFF_HT + 36864
    OFF_ROT = OFF_HEAD + 32256
    OFF_S = OFF_ROT + 8192
    OFF_S0 = OFF_S + 6144
    OFF_QK = OFF_S0 + 20480
    OFF_OF = OFF_QK + 3072 + 2048 + 4096
    OFF_END = OFF_OF + 2048
    cs_own = AR.view("cs_own_s", OFF_END, [2, TOK], F32)
    dec_h = AR.view("dec_h", OFF_END + 9216, [4, 128], F32)
    masks = AR.view("masks_s", OFF_END + 11264, [2, 128], F32)
    junk = AR.view("junk", OFF_END + 12800, [2048], BF16)

    pT = [P.ps("pT%d" % i, [128, 512]) for i in range(2)]
    pM = [P.ps("pM%d" % i, [128, 512]) for i in range(4)]
    pS = [P.ps("pS%d" % i, [128, 512]) for i in range(2)]
    cnt = {"m": 0, "t": 0, "s": 0, "w": 0}

    def nextM():
        cnt["m"] += 1
        return pM[cnt["m"] % 4]

    def nextT():
        cnt["t"] += 1
        return pT[cnt["t"] % 2]

    def nextS():
        cnt["s"] += 1
        return pS[cnt["s"] % 2]

    def nextW():
        cnt["w"] += 1
        return W[cnt["w"] % 3]

    def ld(dst, src_ap, eng="sp"):
        P.dma(eng, lambda e: e.dma_start(out=dst[:], in_=src_ap), writes=[dst], semtile=dst)

    ld(ident_f, ident_d[:])
    ld(cs_own, cs_own_d[:])
    ld(masks, masks_d[:])
    ld(rowmask, rowmask_d[:])
    ld(vec_s, vecs[:])
    P.op("dve", lambda e: e.tensor_copy(out=ident_b[:], in_=ident_f[:]), reads=[ident_f], writes=[ident_b])
    P.op("dve", lambda e: e.memset(epsb[:], EPS), writes=[epsb])

    def load_w(slot, src2d, c0=0):
        kc = src2d.shape[0] // 128
        ncols = src2d.shape[1]
        src = src2d.rearrange("(kc p) n -> p kc n", p=128)
        P.dma("pool", lambda e: e.dma_start(out=slot[:, 0:kc, c0:c0 + ncols], in_=src), writes=[slot], semtile=slot)

    def norm_to_T(x_dram, n_tiles, dstT, wrow, xin, xb):
        for t in range(n_tiles):
            xi = xin[t % 2]
            P.dma("sp", lambda e, xi=xi, t=t: e.dma_start(out=xi[:], in_=x_dram[t * 128:(t + 1) * 128, :]), writes=[xi], semtile=xi)
            ss = small
            P.op("act", lambda e, xi=xi: e.activation(out=junk[:], in_=xi[:], func=AF.Square, accum_out=small[:, 0:1]),
                 reads=[xi], writes=[junk, small])
            P.op("act", lambda e: e.activation(out=small[:, 1:2], in_=small[:, 0:1], func=AF.Sqrt, bias=epsb[:], scale=1.0 / D),
                 reads=[small, epsb], writes=[small])
            P.op("dve", lambda e: e.reciprocal(out=small[:, 2:3], in_=small[:, 1:2]), reads=[small], writes=[small])
            P.op("dve", lambda e, xi=xi: e.tensor_scalar(out=xb[:], in0=xi[:], scalar1=small[:, 2:3], scalar2=None, op0=ALU.mult),
                 reads=[xi, small], writes=[xb])
            for g in range(2):
                pt = nextT()
                ptb = pt[:].bitcast(BF16)
                for j in range(8):
                    kc = g * 8 + j
                    P.op("pe", lambda e, ptb=ptb, j=j, kc=kc: e.transpose(out=ptb[:, j * 128:(j + 1) * 128], in_=xb[:, kc * 128:(kc + 1) * 128], identity=ident_b[:]),
                         reads=[xb, ident_b], writes=[pt])
                P.op("dve", lambda e, ptb=ptb, g=g, t=t: e.tensor_tensor(
                    out=dstT[:, g * 8:(g + 1) * 8, t * 128:(t + 1) * 128],
                    in0=ptb.rearrange("p (a b) -> p a b", a=8),
                    in1=vec_s[:, wrow, g * 8:(g + 1) * 8].unsqueeze(2).to_broadcast([128, 8, 128]), op=ALU.mult),
                    reads=[pt, vec_s], writes=[dstT])

    def fm_proj(slot, c0, srcT, blks, evac):
        for bi, (t0, t1) in enumerate(blks):
            ps = nextM()
            for kc in range(16):
                P.op("pe", lambda e, ps=ps, kc=kc, t0=t0, t1=t1: e.matmul(ps[:, 0:t1 - t0], lhsT=slot[:, kc, c0:c0 + 128], rhs=srcT[:, kc, t0:t1], start=(kc == 0), stop=(kc == 15)),
                     reads=[slot, srcT], writes=[ps])
            evac(ps, bi, (t0, t1))

    def tm_proj(slot, ncols, srcT, tile, evac):
        ps = nextM()
        for kc in range(16):
            P.op("pe", lambda e, ps=ps, kc=kc: e.matmul(ps[:, 0:ncols], lhsT=srcT[:, kc, tile * 128:(tile + 1) * 128], rhs=slot[:, kc, 0:ncols], start=(kc == 0), stop=(kc == 15)),
                 reads=[slot, srcT], writes=[ps])
        evac(ps)

    def rotary_pair(p1, p2, n, cs, t0, decsel, out_fn, rot):
        cos = cs[:, 0, t0:t0 + n]
        sin = cs[:, 1, t0:t0 + n]
        ta, tb, tc, td = rot
        P.op("dve", lambda e: e.tensor_tensor(out=ta[:, 0:n], in0=p1[:, 0:n], in1=cos, op=ALU.mult), reads=[p1, cs], writes=[ta])
        P.op("dve", lambda e: e.tensor_tensor(out=tb[:, 0:n], in0=p2[:, 0:n], in1=sin, op=ALU.mult), reads=[p2, cs], writes=[tb])
        P.op("dve", lambda e: e.tensor_tensor(out=tc[:, 0:n], in0=p1[:, 0:n], in1=sin, op=ALU.mult), reads=[p1, cs], writes=[tc])
        P.op("dve", lambda e: e.tensor_tensor(out=td[:, 0:n], in0=p2[:, 0:n], in1=cos, op=ALU.mult), reads=[p2, cs], writes=[td])
        P.op("pool", lambda e: e.tensor_tensor(out=ta[:, 0:n], in0=ta[:, 0:n], in1=tb[:, 0:n], op=ALU.subtract), reads=[ta, tb], writes=[ta])
        P.op("pool", lambda e: e.tensor_tensor(out=tc[:, 0:n], in0=tc[:, 0:n], in1=td[:, 0:n], op=ALU.add), reads=[tc, td], writes=[tc])
        out_fn(0, ta)
        out_fn(1, tc)

    rot = [AR.view("rot%d" % i, OFF_ROT + i * 2048, [512], F32) for i in range(4)]
    xin = [AR.view("xin%d" % i, OFF_HEAD + i * 8192, [2048], F32) for i in range(2)]
    xb = AR.view("xb", OFF_HEAD + 16384, [2048], BF16)

    def dec_load(h):
        P.dma("sp", lambda e: e.dma_start(out=dec_h[:], in_=dec_d[h]), writes=[dec_h], semtile=dec_h)

    if "A" in stages:
        hTp = AR.view("hTp", OFF_HT, [16, 1024], BF16)
        cs_prev = AR.view("cs_prev", OFF_S0, [2, 1024], F32)
        ld(cs_prev, cs_prev_d[:])
        norm_to_T(x_prev, 8, hTp, 0, xin, xb)
        P.op("pool", lambda e: e.tensor_copy(out=hTh[:], in_=hTp[:, :, 896:1024]), reads=[hTp], writes=[hTh])
        P.barrier()
        kTa = AR.view("kTa", OFF_HEAD, [2, 1024], BF16)
        ktokA = AR.view("ktokA", OFF_HEAD + 4096, [8, 256], BF16)
        vA = AR.view("vA", OFF_HEAD + 8192, [8, 512], BF16)
        sstage = AR.view("sstageA", OFF_S, [512], F32)
        for h in range(H):
            dec_load(h)
            wk = nextW()
            load_w(wk, w_in[:, K0 + h * 256:K0 + (h + 1) * 256])
            wv = nextW()
            load_w(wv, w_in[:, V0 + h * 512:V0 + (h + 1) * 512])
            for bi, (t0, t1) in enumerate(((0, 512), (512, 1024))):
                pp = []
                for dc in range(2):
                    ps = nextM()
                    for kc in range(16):
                        P.op("pe", lambda e, ps=ps, kc=kc, dc=dc, t0=t0, t1=t1, wk=wk: e.matmul(ps[:, 0:512], lhsT=wk[:, kc, dc * 128:(dc + 1) * 128], rhs=hTp[:, kc, t0:t1], start=(kc == 0), stop=(kc == 15)),
                             reads=[wk, hTp], writes=[ps])
                    pp.append(ps)

                def outk(which, src, t0=t0):
                    P.op("dve", lambda e: e.tensor_tensor(out=kTa[:, which, t0:t0 + 512].rearrange("p (a b) -> p a b", a=4),
                                                          in0=src[:, 0:512].rearrange("p (a b) -> p a b", a=4),
                                                          in1=dec_h[:, 1, :].unsqueeze(1).to_broadcast([128, 4, 128]), op=ALU.mult),
                         reads=[src, dec_h], writes=[kTa])
                rotary_pair(pp[0], pp[1], 512, cs_prev, t0, None, outk, rot)
            for n in range(8):
                pt = nextT()
                ptb = pt[:].bitcast(BF16)
                for dc in range(2):
                    P.op("pe", lambda e, ptb=ptb, dc=dc, n=n: e.transpose(out=ptb[:, dc * 128:(dc + 1) * 128], in_=kTa[:, dc, n * 128:(n + 1) * 128], identity=ident_b[:]),
                         reads=[kTa, ident_b], writes=[pt])
                sc = CDEC_P[h] * float(np.exp(128 * (7 - n) * LOG_G[h]))
                P.op("act", lambda e, ptb=ptb, n=n, sc=sc: e.activation(out=ktokA[:, n, :], in_=ptb[:, 0:256], func=AF.Copy, scale=sc),
                     reads=[pt], writes=[ktokA])
                tm_proj(wv, 512, hTp, n, lambda ps, n=n: P.op("act", lambda e: e.activation(out=vA[:, n, :], in_=ps[:, 0:512], func=AF.Copy), reads=[ps], writes=[vA]))
            for dc in range(2):
                ps = nextS()
                for n in range(8):
                    P.op("pe", lambda e, ps=ps, n=n, dc=dc: e.matmul(ps[:, 0:512], lhsT=ktokA[:, n, dc * 128:(dc + 1) * 128], rhs=vA[:, n, :], start=(n == 0), stop=(n == 7)),
                         reads=[ktokA, vA], writes=[ps])
                P.op("act", lambda e, ps=ps: e.activation(out=sstage[:], in_=ps[:, 0:512], func=AF.Copy), reads=[ps], writes=[sstage])
                P.dma("sp", lambda e, dc=dc, h=h: e.dma_start(out=sinit[h, dc * 128:(dc + 1) * 128, :], in_=sstage[:]), reads=[sstage], writes=[sinit], semtile=sstage)
        P.barrier()

    hT = AR.view("hT", OFF_HT, [16, TOK], BF16)
    if "R" in stages:
        norm_to_T(x_own, NTILE, hT, 0, xin, xb)
        P.barrier()
        qT = AR.view("qT", OFF_HEAD, [2, TOK], BF16)
        kT = AR.view("kT", OFF_HEAD + 4608, [2, TOK], BF16)
        ktok = AR.view("ktok", OFF_HEAD + 9216, [NTILE, 256], BF16)
        vv = AR.view("vv", OFF_HEAD + 13824, [NTILE, 512], BF16)
        sg = AR.view("sg", OFF_HEAD + 23040, [NTILE, 512], BF16)
        Sf = AR.view("Sf", OFF_S, [2, 512], F32)
        Sb = AR.view("Sb", OFF_S + 4096, [2, 512], BF16)
        S0 = [AR.view("S0_%d" % i, OFF_S0 + i * 4096, [2, 512], F32) for i in range(3)]
        So = [AR.view("So_%d" % i, OFF_S0 + 12288 + i * 4096, [2, 512], F32) for i in range(2)]
        S0b = [AR.view("S0b%d" % i, OFF_QK + 3072 + 2048 + i * 2048, [2, 512], BF16) for i in range(2)]
        Qz = [AR.view("Qz%d" % i, OFF_QK + i * 512, [2, 128], BF16) for i in range(2)]
        Kz = [AR.view("Kz%d" % i, OFF_QK + 2048 + i * 512, [256], BF16) for i in range(2)]
        ofs = [AR.view("of%d" % i, OFF_OF + i * 1024, [512], BF16) for i in range(2)]
        attm = [AR.view("attm%d" % i, OFF_END + 12288 + i * 256, [128], BF16) for i in range(2)]
        for h in range(H):
            dec_load(h)
            wqk = nextW()
            load_w(wqk, w_in[:, Q0 + h * 256:Q0 + (h + 1) * 256], 0)
            load_w(wqk, w_in[:, K0 + h * 256:K0 + (h + 1) * 256], 256)
            wv = nextW()
            load_w(wv, w_in[:, V0 + h * 512:V0 + (h + 1) * 512])
            wg = nextW()
            load_w(wg, w_in[:, G0 + h * 512:G0 + (h + 1) * 512])
            if "A" in stages:
                P.dma("sp", lambda e, h=h: e.dma_start(out=Sf[:], in_=sinit[h].rearrange("(dc p) e -> p dc e", p=128)), reads=[sinit], writes=[Sf], semtile=Sf)
            else:
                P.op("dve", lambda e: e.memset(Sf[:], 0.0), writes=[Sf])
            P.op("act", lambda e: e.activation(out=Sb[:], in_=Sf[:], func=AF.Copy), reads=[Sf], writes=[Sb])
            for which, dstT in ((0, qT), (1, kT)):
                for bi, (t0, t1) in enumerate(BLKS):
                    n = t1 - t0
                    pp = []
                    for dc in range(2):
                        ps = nextM()
                        c0 = which * 256 + dc * 128
                        for kc in range(16):
                            P.op("pe", lambda e, ps=ps, kc=kc, c0=c0, t0=t0, t1=t1, wqk=wqk: e.matmul(ps[:, 0:t1 - t0], lhsT=wqk[:, kc, c0:c0 + 128], rhs=hT[:, kc, t0:t1], start=(kc == 0), stop=(kc == 15)),
                                 reads=[wqk, hT], writes=[ps])
                        pp.append(ps)
                    drow = which + (2 if bi == 2 else 0)

                    def outqk(dcw, src, t0=t0, n=n, drow=drow, dstT=dstT, bi=bi, which=which):
                        a = n // 128
                        P.op("dve", lambda e: e.tensor_tensor(out=dstT[:, dcw, t0:t0 + n].rearrange("p (a b) -> p a b", a=a),
                                                              in0=src[:, 0:n].rearrange("p (a b) -> p a b", a=a),
                                                              in1=dec_h[:, drow, :].unsqueeze(1).to_broadcast([128, a, 128]), op=ALU.mult),
                             reads=[src, dec_h], writes=[dstT])
                    rotary_pair(pp[0], pp[1], n, cs_own, t0, None, outqk, rot)
            for t in range(NTILE):
                pt = nextT()
                ptb = pt[:].bitcast(BF16)
                for dc in range(2):
                    P.op("pe", lambda e, ptb=ptb, dc=dc, t=t: e.transpose(out=ptb[:, dc * 128:(dc + 1) * 128], in_=kT[:, dc, t * 128:(t + 1) * 128], identity=ident_b[:]),
                         reads=[kT, ident_b], writes=[pt])
                sc = CDEC_P[h] if t < 8 else CDEC_S[h]
                P.op("act", lambda e, ptb=ptb, t=t, sc=sc: e.activation(out=ktok[:, t, :], in_=ptb[:, 0:256], func=AF.Copy, scale=sc),
                     reads=[pt], writes=[ktok])
                tm_proj(wv, 512, hT, t, lambda ps, t=t: P.op("act", lambda e: e.activation(out=vv[:, t, :], in_=ps[:, 0:512], func=AF.Copy), reads=[ps], writes=[vv]))
                tm_proj(wg, 512, hT, t, lambda ps, t=t: P.op("act", lambda e: e.activation(out=sg[:, t, :], in_=ps[:, 0:512], func=AF.Silu), reads=[ps], writes=[sg]))
            for t in range(NTILE):
                tc0, tc1 = t * 128, (t + 1) * 128
                pa = nextM()
                for dc in range(2):
                    P.op("pe", lambda e, pa=pa, dc=dc, tc0=tc0, tc1=tc1: e.matmul(pa[:, 0:128], lhsT=kT[:, dc, tc0:tc1], rhs=qT[:, dc, tc0:tc1], start=(dc == 0), stop=(dc == 1)),
                         reads=[kT, qT], writes=[pa])
                am = attm[t % 2]
                mrow = 0 if t < 8 else 1
                P.op("dve", lambda e, pa=pa, am=am, mrow=mrow: e.tensor_tensor(out=am[:], in0=pa[:, 0:128], in1=masks[:, mrow, :], op=ALU.mult),
                     reads=[pa, masks], writes=[am])
                po = nextM()
                P.op("pe", lambda e, po=po, am=am, t=t: e.matmul(po[:, 0:512], lhsT=am[:], rhs=vv[:, t, :], start=True, stop=False),
                     reads=[am, vv], writes=[po])
                if t < 8:
                    for dc in range(2):
                        P.op("pe", lambda e, po=po, dc=dc, tc0=tc0, tc1=tc1: e.matmul(po[:, 0:512], lhsT=qT[:, dc, tc0:tc1], rhs=Sb[:, dc, :], start=False, stop=(dc == 1)),
                             reads=[qT, Sb], writes=[po])
                    for dc in range(2):
                        ps = nextS()
                        P.op("pe", lambda e, ps=ps, dc=dc, t=t: e.matmul(ps[:, 0:512], lhsT=ktok[:, t, dc * 128:(dc + 1) * 128], rhs=vv[:, t, :], start=True, stop=True),
                             reads=[ktok, vv], writes=[ps])
                        P.op("dve", lambda e, ps=ps, dc=dc, h=h: e.scalar_tensor_tensor(out=Sf[:, dc, :], in0=Sf[:, dc, :], scalar=CDEC_P[h], in1=ps[:, 0:512], op0=ALU.mult, op1=ALU.add),
                             reads=[Sf, ps], writes=[Sf])
                    if t < 7:
                        P.op("act", lambda e: e.activation(out=Sb[:], in_=Sf[:], func=AF.Copy), reads=[Sf], writes=[Sb])
                    else:
                        P.dma("sp", lambda e, h=h: e.dma_start(out=retp_out[h].rearrange("(dc p) e -> p dc e", p=128), in_=Sf[:]), reads=[Sf], semtile=Sf, is_output=True)
                else:
                    for bb in range(16):
                        s0 = S0[bb % 3]
                        P.dma("sp", lambda e, s0=s0, bb=bb, h=h: e.dma_start(out=s0[:], in_=sret[bb, h].rearrange("(dc p) e -> p dc e", p=128)), writes=[s0], semtile=s0)
                        qz = Qz[bb % 2]
                        s0b = S0b[bb % 2]
                        P.op("act", lambda e, s0=s0, s0b=s0b: e.activation(out=s0b[:], in_=s0[:], func=AF.Copy), reads=[s0], writes=[s0b])
                        P.op("pool", lambda e, qz=qz: e.memset(qz[:], 0.0), writes=[qz])
                        P.op("pool", lambda e, qz=qz, bb=bb: e.tensor_copy(out=qz[:, :, bb * 8:(bb + 1) * 8], in_=qT[:, :, 1024 + bb * 8:1024 + (bb + 1) * 8]), reads=[qT], writes=[qz])
                        for dc in range(2):
                            P.op("pe", lambda e, po=po, dc=dc, s0b=s0b, qz=qz, bb=bb: e.matmul(po[:, 0:512], lhsT=qz[:, dc, :], rhs=s0b[:, dc, :], start=False, stop=(dc == 1 and bb == 15)),
                                 reads=[qz, s0b], writes=[po])
                        kz = Kz[bb % 2]
                        P.op("dve", lambda e, kz=kz, bb=bb, t=t: e.tensor_scalar(out=kz[:], in0=ktok[:, t, :], scalar1=rowmask[:, bb:bb + 1], scalar2=None, op0=ALU.mult),
                             reads=[ktok, rowmask], writes=[kz])
                        so = So[bb % 2]
                        for dc in range(2):
                            ps = nextS()
                            P.op("pe", lambda e, ps=ps, dc=dc, kz=kz, t=t: e.matmul(ps[:, 0:512], lhsT=kz[:, dc * 128:(dc + 1) * 128], rhs=vv[:, t, :], start=True, stop=True),
                                 reads=[kz, vv], writes=[ps])
                            P.op("dve", lambda e, ps=ps, dc=dc, s0=s0, so=so, h=h: e.scalar_tensor_tensor(out=so[:, dc, :], in0=s0[:, dc, :], scalar=CDEC_S[h], in1=ps[:, 0:512], op0=ALU.mult, op1=ALU.add),
                                 reads=[s0, ps], writes=[so])
                        P.dma("sp", lambda e, so=so, bb=bb, h=h: e.dma_start(out=rets_out[bb, h].rearrange("(dc p) e -> p dc e", p=128), in_=so[:]), reads=[so], semtile=so, is_output=True)
                P.op("act", lambda e, po=po: e.activation(out=junk[:, 0:512], in_=po[:, 0:512], func=AF.Square, accum_out=small[:, 4:5]),
                     reads=[po], writes=[junk, small])
                P.op("act", lambda e: e.activation(out=small[:, 5:6], in_=small[:, 4:5], func=AF.Sqrt, bias=epsb[:], scale=1.0 / DV),
                     reads=[small, epsb], writes=[small])
                P.op("dve", lambda e: e.reciprocal(out=small[:, 6:7], in_=small[:, 5:6]), reads=[small], writes=[small])
                of = ofs[t % 2]
                P.op("dve", lambda e, po=po, of=of, t=t: e.scalar_tensor_tensor(out=of[:], in0=po[:, 0:512], scalar=small[:, 6:7], in1=sg[:, t, :], op0=ALU.mult, op1=ALU.mult),
                     reads=[po, small, sg], writes=[of])
                P.dma("sp", lambda e, of=of, t=t, h=h: e.dma_start(out=ospill[t * 128:(t + 1) * 128, h * 512:(h + 1) * 512], in_=of[:]), reads=[of], writes=[ospill], semtile=of, is_output=debug)
        P.barrier()

    OFF_Z = OFF_HEAD
    OFF_Y = OFF_Z + 36864
    OFF_X = OFF_Y + 36864
    dbg_fm = P.dram("dbg_fm", [128, 16, TOK], BF16, OUT) if debug else None
    if "C" in stages:
        cf = AR.view("cf", OFF_Z, [16, TOK], BF16)
        fmY = AR.view("fmY", OFF_Y, [16, TOK], BF16)
        uP = AR.view("uP", OFF_Y, [1056], F32)
        uPb = AR.view("uPb", OFF_Y + 4224, [1056], BF16)
        uS = AR.view("uS", OFF_Y + 6400, [16, 38], F32)
        uSb = AR.view("uSb", OFF_Y + 8832, [16, 38], BF16)
        dg = AR.view("dg", OFF_Y + 10048, [31, 128], BF16)
        cwT = AR.view("cwT", OFF_Y + 17984, [16, 31], F32)
        sgt = [AR.view("sgt%d" % i, OFF_Y + 19968 + i * 2048, [512], F32) for i in range(2)]
        scs = AR.view("scs", OFF_Y + 24064, [4, 128], F32)
        cwrow = AR.view("cwrow", OFF_Y + 26112, [2048], F32)
        strow = [AR.view("strow%d" % i, OFF_Y + 34304 + i * 512, [128], F32) for i in range(2)]
        P.dma("sp", lambda e: e.dma_start(out=cwrow[0:CW, :], in_=conv_w[:]), writes=[cwrow], semtile=cwrow)
        for c in range(16):
            pt = nextT()
            P.op("pe", lambda e, pt=pt, c=c: e.transpose(out=pt[:, 0:CW], in_=cwrow[0:CW, c * 128:(c + 1) * 128], identity=ident_f[0:CW, 0:CW]),
                 reads=[cwrow, ident_f], writes=[pt])
            P.op("act", lambda e, pt=pt, c=c: e.activation(out=cwT[:, c, :], in_=pt[:, 0:CW], func=AF.Copy), reads=[pt], writes=[cwT])
        cpy = Tile("cpy", None)
        P.dma("sp", lambda e: e.dma_start(out=convs_out[:, 0:22, :], in_=sconv[:].rearrange("(b w) d -> b w d", w=30)[:, 8:30, :]), semtile=cpy, is_output=True)
        for c in range(16):
            if c % 4 == 0:
                wa = nextW()
                load_w(wa, w_in[:, CA0 + c * 128:CA0 + (c + 4) * 128])
                wb_ = nextW()
                load_w(wb_, w_in[:, CB0 + c * 128:CB0 + (c + 4) * 128])
            cc = (c % 4) * 128
            pst = nextT()
            for a in range(4):
                rows = 128 if a < 3 else 96
                st = strow[a % 2]
                P.dma("sp", lambda e, st=st, a=a, rows=rows, c=c: e.dma_start(out=st[0:rows, :], in_=sconv[a * 128:a * 128 + rows, c * 128:(c + 1) * 128]), writes=[st], semtile=st)
                P.op("pe", lambda e, pst=pst, st=st, a=a, rows=rows: e.transpose(out=pst[:, a * 128:a * 128 + rows], in_=st[0:rows, :], identity=ident_f[0:rows, 0:rows]),
                     reads=[st, ident_f], writes=[pst])
            P.op("act", lambda e, pst=pst: e.activation(out=uS[:, :, 0:30], in_=pst[:, 0:480].rearrange("p (b w) -> p b w", w=30), func=AF.Copy), reads=[pst], writes=[uS])
            segs = ((hTh, 0, 128, "h"), (hT, 0, 512, "p0"), (hT, 512, 1024, "p1"), (hT, 1024, 1152, "s"))
            for si, (src, t0, t1, kind) in enumerate(segs):
                n = t1 - t0
                pa_ = nextM()
                pb_ = nextM()
                for (pp_, wsl) in ((pa_, wa), (pb_, wb_)):
                    for kc in range(16):
                        P.op("pe", lambda e, pp_=pp_, wsl=wsl, kc=kc, cc=cc, src=src, t0=t0, t1=t1: e.matmul(pp_[:, 0:t1 - t0], lhsT=wsl[:, kc, cc:cc + 128], rhs=src[:, kc, t0:t1], start=(kc == 0), stop=(kc == 15)),
                             reads=[wsl, src], writes=[pp_])
                sgx = sgt[si % 2]
                P.op("act", lambda e, pb_=pb_, sgx=sgx, n=n: e.activation(out=sgx[:, 0:n], in_=pb_[:, 0:n], func=AF.Sigmoid), reads=[pb_], writes=[sgx])
                if kind == "h":
                    P.op("dve", lambda e, pa_=pa_, sgx=sgx: e.tensor_tensor(out=uP[:, 0:30], in0=pa_[:, 98:128], in1=sgx[:, 98:128], op=ALU.mult), reads=[pa_, sgx], writes=[uP])
                elif kind == "s":
                    P.op("dve", lambda e, pa_=pa_, sgx=sgx: e.tensor_tensor(out=uS[:, :, 30:38], in0=pa_[:, 0:128].rearrange("p (b i) -> p b i", i=8), in1=sgx[:, 0:128].rearrange("p (b i) -> p b i", i=8), op=ALU.mult),
                         reads=[pa_, sgx], writes=[uS])
                else:
                    P.op("dve", lambda e, pa_=pa_, sgx=sgx, t0=t0: e.tensor_tensor(out=uP[:, 30 + t0:30 + t0 + 512], in0=pa_[:, 0:512], in1=sgx[:, 0:512], op=ALU.mult), reads=[pa_, sgx], writes=[uP])
            P.op("pool", lambda e: e.tensor_copy(out=uPb[:, 0:1054], in_=uP[:, 0:1054]), reads=[uP], writes=[uPb])
            P.op("pool", lambda e: e.tensor_copy(out=uSb[:], in_=uS[:]), reads=[uS], writes=[uSb])
            pt = nextT()
            P.op("pe", lambda e, pt=pt: e.transpose(out=pt[0:30, 0:128], in_=uP[:, 1024:1054], identity=ident_f[:]), reads=[uP, ident_f], writes=[pt])
            P.op("act", lambda e, pt=pt: e.activation(out=scs[0:30, 0, :], in_=pt[0:30, 0:128], func=AF.Copy), reads=[pt], writes=[scs])
            P.dma("sp", lambda e, c=c: e.dma_start(out=convp_out[:, c * 128:(c + 1) * 128], in_=scs[0:30, 0, :]), reads=[scs], semtile=scs, is_output=True)
            P.op("act", lambda e: e.activation(out=scs[:, 1, :].rearrange("p (b i) -> p b i", i=8), in_=uS[:, :, 30:38], func=AF.Copy), reads=[uS], writes=[scs])
            pt2 = nextT()
            P.op("pe", lambda e, pt2=pt2: e.transpose(out=pt2[:, 0:128], in_=scs[:, 1, :], identity=ident_f[:]), reads=[scs, ident_f], writes=[pt2])
            P.op("act", lambda e, pt2=pt2: e.activation(out=scs[:, 2, :], in_=pt2[:, 0:128], func=AF.Copy), reads=[pt2], writes=[scs])
            for bb in range(16):
                P.dma("sp", lambda e, bb=bb, c=c: e.dma_start(out=convs_out[bb, 22:30, c * 128:(c + 1) * 128], in_=scs[bb * 8:(bb + 1) * 8, 2, :]), reads=[scs], semtile=scs, is_output=True)
            for tap in range(CW):
                P.op("pool", lambda e, tap=tap, c=c: e.tensor_scalar(out=dg[:, tap, :], in0=ident_f[:], scalar1=cwT[:, c, tap:tap + 1], scalar2=None, op0=ALU.mult),
                     reads=[ident_f, cwT], writes=[dg])
            for (t0, n, kind) in ((0, 512, "p"), (512, 512, "p"), (1024, 128, "s")):
                pc = nextM()
                for tap in range(CW):
                    if kind == "p":
                        P.op("pe", lambda e, pc=pc, tap=tap, t0=t0: e.matmul(pc[:, 0:512], lhsT=dg[:, tap, :], rhs=uPb[:, t0 + tap:t0 + tap + 512], start=(tap == 0), stop=(tap == CW - 1)),
                             reads=[dg, uPb], writes=[pc])
                    else:
                        P.op("pe", lambda e, pc=pc, tap=tap: e.matmul(pc[:, 0:128], lhsT=dg[:, tap, :], rhs=uSb[:, :, tap:tap + 8], start=(tap == 0), stop=(tap == CW - 1)),
                             reads=[dg, uSb], writes=[pc])
                P.op("act", lambda e, pc=pc, t0=t0, n=n, c=c: e.activation(out=cf[:, c, t0:t0 + n], in_=pc[:, 0:n], func=AF.Identity, bias=vec_s[:, 2, c:c + 1]),
                     reads=[pc, vec_s], writes=[cf])
        P.barrier()
        ones_b = AR.view("ones_b", OFF_Y, [128], BF16)
        sq = [AR.view("sq%d" % i, OFF_Y + 256 + i * 1024, [512], BF16) for i in range(2)]
        mu_t = AR.view("mu_t", OFF_Y + 2304, [TOK], F32)
        rs_t = AR.view("rs_t", OFF_Y + 2304 + 4608, [TOK], F32)
        lt = [AR.view("lt%d" % i, OFF_Y + 11520 + i * 2048, [512], F32) for i in range(2)]
        epsw = AR.view("epsw", OFF_Y + 15616, [1], F32)
        P.op("dve", lambda e: e.memset(ones_b[:], 1.0), writes=[ones_b])
        P.op("dve", lambda e: e.memset(epsw[:], EPS), writes=[epsw])
        for (t0, t1) in BLKS:
            n = t1 - t0
            p1 = nextM()
            p2 = nextM()
            for c in range(16):
                P.op("pe", lambda e, p1=p1, c=c, t0=t0, t1=t1: e.matmul(p1[:, 0:t1 - t0], lhsT=ones_b[:], rhs=cf[:, c, t0:t1], start=(c == 0), stop=(c == 15)),
                     reads=[ones_b, cf], writes=[p1])
                sqx = sq[c % 2]
                P.op("act", lambda e, sqx=sqx, c=c, t0=t0, t1=t1: e.activation(out=sqx[:, 0:t1 - t0], in_=cf[:, c, t0:t1], func=AF.Square), reads=[cf], writes=[sqx])
                P.op("pe", lambda e, p2=p2, sqx=sqx, c=c, n=n: e.matmul(p2[:, 0:n], lhsT=ones_b[:], rhs=sqx[:, 0:n], start=(c == 0), stop=(c == 15)),
                     reads=[ones_b, sqx], writes=[p2])
            P.op("act", lambda e, p1=p1, t0=t0, n=n: e.activation(out=mu_t[:, t0:t0 + n], in_=p1[:, 0:n], func=AF.Copy, scale=1.0 / D), reads=[p1], writes=[mu_t])
            l0 = lt[0]
            P.op("dve", lambda e, t0=t0, n=n, l0=l0: e.tensor_tensor(out=l0[:, 0:n], in0=mu_t[:, t0:t0 + n], in1=mu_t[:, t0:t0 + n], op=ALU.mult), reads=[mu_t], writes=[l0])
            P.op("dve", lambda e, p2=p2, n=n, l0=l0: e.scalar_tensor_tensor(out=l0[:, 0:n], in0=p2[:, 0:n], scalar=1.0 / D, in1=l0[:, 0:n], op0=ALU.mult, op1=ALU.subtract), reads=[p2, l0], writes=[l0])
            P.op("act", lambda e, n=n, l0=l0: e.activation(out=l0[:, 0:n], in_=l0[:, 0:n], func=AF.Sqrt, bias=epsw[:], scale=1.0), reads=[l0, epsw], writes=[l0])
            P.op("dve", lambda e, t0=t0, n=n, l0=l0: e.reciprocal(out=rs_t[:, t0:t0 + n], in_=l0[:, 0:n]), reads=[l0], writes=[rs_t])
        for c in range(16):
            for (t0, t1) in BLKS:
                n = t1 - t0
                lx = lt[(c * 3 + (t0 // 512)) % 2]
                P.op("dve", lambda e, lx=lx, c=c, t0=t0, t1=t1: e.tensor_tensor(out=lx[:, 0:t1 - t0], in0=cf[:, c, t0:t1], in1=mu_t[:, t0:t1], op=ALU.subtract), reads=[cf, mu_t], writes=[lx])
                P.op("pool", lambda e, lx=lx, t0=t0, t1=t1: e.tensor_tensor(out=lx[:, 0:t1 - t0], in0=lx[:, 0:t1 - t0], in1=rs_t[:, t0:t1], op=ALU.mult), reads=[lx, rs_t], writes=[lx])
                P.op("act", lambda e, lx=lx, c=c, t0=t0, t1=t1: e.activation(out=cf[:, c, t0:t1], in_=lx[:, 0:t1 - t0], func=AF.Silu, bias=vec_s[:, 4, c:c + 1], scale=vec_s[:, 3, c:c + 1]),
                     reads=[lx, vec_s], writes=[cf])
        P.barrier()
        for c4 in range(4):
            wsl = nextW()
            load_w(wsl, w_in[:, GC0 + c4 * 512:GC0 + (c4 + 1) * 512])
            for j in range(4):
                c = c4 * 4 + j
                fm_proj(wsl, j * 128, hT, BLKS, lambda ps, bi, tt, c=c: P.op("act", lambda e: e.activation(out=fmY[:, c, tt[0]:tt[1]], in_=ps[:, 0:tt[1] - tt[0]], func=AF.Sigmoid), reads=[ps], writes=[fmY]))
        for c4 in range(4):
            wsl = nextW()
            load_w(wsl, w_conv_o[:, c4 * 512:(c4 + 1) * 512])
            for j in range(4):
                c = c4 * 4 + j
                fm_proj(wsl, j * 128, cf, BLKS, lambda ps, bi, tt, c=c: P.op("dve", lambda e: e.tensor_tensor(out=fmY[:, c, tt[0]:tt[1]], in0=ps[:, 0:tt[1] - tt[0]], in1=fmY[:, c, tt[0]:tt[1]], op=ALU.mult), reads=[ps, fmY], writes=[fmY]))
        P.barrier()
        fmZ = AR.view("fmZ", OFF_Z, [16, TOK], BF16)
        for c4 in range(4):
            wsl = nextW()
            load_w(wsl, w_in[:, GR0 + c4 * 512:GR0 + (c4 + 1) * 512])
            for j in range(4):
                c = c4 * 4 + j
                fm_proj(wsl, j * 128, hT, BLKS, lambda ps, bi, tt, c=c: P.op("act", lambda e: e.activation(out=fmZ[:, c, tt[0]:tt[1]], in_=ps[:, 0:tt[1] - tt[0]], func=AF.Sigmoid), reads=[ps], writes=[fmZ]))
        P.barrier()

    if "M" in stages:
        oT = AR.view("oT", OFF_HT, [32, 512], BF16)
        orow = [AR.view("orow%d" % i, OFF_X + i * 8192, [4096], BF16) for i in range(2)]
        mt = [AR.view("mt%d" % i, OFF_X + 16384 + i * 2048, [512], F32) for i in range(2)]
        Wr = [Tile("Wr%d" % i, W[i][:].rearrange("p a b -> p (a b)").rearrange("p (a b) -> p a b", a=32)) for i in range(3)]
        for bi, (t0, t1) in enumerate(BLKS):
            n = t1 - t0
            for ti in range(n // 128):
                t = t0 // 128 + ti
                orw = orow[t % 2]
                P.dma("sp", lambda e, orw=orw, t=t: e.dma_start(out=orw[:], in_=ospill[t * 128:(t + 1) * 128, :]), reads=[ospill], writes=[orw], semtile=orw)
                for g in range(4):
                    pt = nextT()
                    ptb = pt[:].bitcast(BF16)
                    for j in range(8):
                        kc = g * 8 + j
                        P.op("pe", lambda e, ptb=ptb, j=j, kc=kc, orw=orw: e.transpose(out=ptb[:, j * 128:(j + 1) * 128], in_=orw[:, kc * 128:(kc + 1) * 128], identity=ident_b[:]),
                             reads=[orw, ident_b], writes=[pt])
                    P.op("act", lambda e, ptb=ptb, g=g, ti=ti: e.activation(out=oT[:, g * 8:(g + 1) * 8, ti * 128:(ti + 1) * 128], in_=ptb.rearrange("p (a b) -> p a b", a=8), func=AF.Copy),
                         reads=[pt], writes=[oT])
            for c2 in range(8):
                wsl = Wr[(bi * 8 + c2) % 3]
                src = w_ret_o[:, c2 * 256:(c2 + 1) * 256].rearrange("(kc p) n -> p kc n", p=128)
                P.dma("pool", lambda e, wsl=wsl, src=src: e.dma_start(out=wsl[:], in_=src), writes=[wsl], semtile=wsl)
                for j in range(2):
                    c = c2 * 2 + j
                    ps = nextM()
                    for kc in range(32):
                        P.op("pe", lambda e, ps=ps, kc=kc, wsl=wsl, j=j, n=n: e.matmul(ps[:, 0:n], lhsT=wsl[:, kc, j * 128:(j + 1) * 128], rhs=oT[:, kc, 0:n], start=(kc == 0), stop=(kc == 31)),
                             reads=[wsl, oT], writes=[ps])
                    mx = mt[c % 2]
                    P.op("dve", lambda e, ps=ps, mx=mx, c=c, t0=t0, t1=t1: e.tensor_tensor(out=mx[:, 0:t1 - t0], in0=ps[:, 0:t1 - t0], in1=fmZ[:, c, t0:t1], op=ALU.mult), reads=[ps, fmZ], writes=[mx])
                    P.op("pool", lambda e, mx=mx, c=c, t0=t0, t1=t1: e.tensor_tensor(out=fmY[:, c, t0:t1], in0=mx[:, 0:t1 - t0], in1=fmY[:, c, t0:t1], op=ALU.add), reads=[mx, fmY], writes=[fmY])
        if debug and stages.endswith("M"):
            P.dma("sp", lambda e: e.dma_start(out=dbg_fm[:], in_=fmY[:]), reads=[fmY], semtile=fmY, is_output=True)
        P.barrier()

    if "O" in stages:
        yacc = AR.view("yacc", OFF_HT, [NTILE, D], F32)
        xt = [AR.view("xt%d" % i, OFF_X + i * 2048, [512], F32) for i in range(2)]
        junk2 = AR.view("junk2", OFF_X + 4096, [2048], BF16)
        xb2 = AR.view("xb2", OFF_X + 8192, [2048], BF16)
        for cb in range(4):
            wsl = nextW()
            load_w(wsl, w_o[:, cb * 512:(cb + 1) * 512])
            for t in range(NTILE):
                xx = xt[t % 2]
                P.dma("sp", lambda e, xx=xx, t=t, cb=cb: e.dma_start(out=xx[:], in_=x_own[t * 128:(t + 1) * 128, cb * 512:(cb + 1) * 512]), writes=[xx], semtile=xx)
                ps = nextM()
                for kc in range(16):
                    P.op("pe", lambda e, ps=ps, kc=kc, t=t, wsl=wsl: e.matmul(ps[:, 0:512], lhsT=fmY[:, kc, t * 128:(t + 1) * 128], rhs=wsl[:, kc, :], start=(kc == 0), stop=(kc == 15)),
                         reads=[fmY, wsl], writes=[ps])
                P.op("dve", lambda e, ps=ps, xx=xx, t=t, cb=cb: e.tensor_tensor(out=yacc[:, t, cb * 512:(cb + 1) * 512], in0=ps[:, 0:512], in1=xx[:], op=ALU.add), reads=[ps, xx], writes=[yacc])
        P.barrier()
        if stages.endswith("O"):
            for t in range(NTILE):
                P.dma("sp", lambda e, t=t: e.dma_start(out=y_out[t * 128:(t + 1) * 128, :], in_=yacc[:, t, :]), reads=[yacc], semtile=yacc, is_output=True)
        h2T = AR.view("h2T", OFF_Y, [16, TOK], BF16)
        for t in range(NTILE):
            P.op("act", lambda e, t=t: e.activation(out=junk2[:], in_=yacc[:, t, :], func=AF.Square, accum_out=small[:, 0:1]), reads=[yacc], writes=[junk2, small])
            P.op("act", lambda e: e.activation(out=small[:, 1:2], in_=small[:, 0:1], func=AF.Sqrt, bias=epsb[:], scale=1.0 / D), reads=[small, epsb], writes=[small])
            P.op("dve", lambda e: e.reciprocal(out=small[:, 2:3], in_=small[:, 1:2]), reads=[small], writes=[small])
            P.op("dve", lambda e, t=t: e.tensor_scalar(out=xb2[:], in0=yacc[:, t, :], scalar1=small[:, 2:3], scalar2=None, op0=ALU.mult), reads=[yacc, small], writes=[xb2])
            for g in range(2):
                pt = nextT()
                ptb = pt[:].bitcast(BF16)
                for j in range(8):
                    kc = g * 8 + j
                    P.op("pe", lambda e, ptb=ptb, j=j, kc=kc: e.transpose(out=ptb[:, j * 128:(j + 1) * 128], in_=xb2[:, kc * 128:(kc + 1) * 128], identity=ident_b[:]), reads=[xb2, ident_b], writes=[pt])
                P.op("dve", lambda e, ptb=ptb, g=g, t=t: e.tensor_tensor(out=h2T[:, g * 8:(g + 1) * 8, t * 128:(t + 1) * 128], in0=ptb.rearrange("p (a b) -> p a b", a=8),
                                                                     in1=vec_s[:, 1, g * 8:(g + 1) * 8].unsqueeze(2).to_broadcast([128, 8, 128]), op=ALU.mult), reads=[pt, vec_s], writes=[h2T])
        P.barrier()

    if "E" in stages:
        hpT = AR.view("hpT", OFF_X, [8, TOK], BF16)
        GT = AR.view("GT", OFF_X + 18432, [TOK], F32)
        gbc = AR.view("gbc", OFF_X + 23040, [TOK], F32)
        rt = AR.view("rt", OFF_X + 27648, [256], F32)
        NB_BIAS = 0
        wrt = AR.view("wrt", 0, [16, 36], BF16)
        brt = AR.view("brt", 2048, [36], F32)
        selE = AR.view("selE", 2304, [128], F32)
        Gtok = AR.view("Gtok", 2816, [32], F32)
        P.dma("pool", lambda e: e.dma_start(out=wrt[:], in_=w_rt[:].rearrange("(kc p) n -> p kc n", p=128)), writes=[wrt], semtile=wrt)
        P.dma("sp", lambda e: e.dma_start(out=brt[:], in_=b_rt[:].partition_broadcast(128)), writes=[brt], semtile=brt)
        L = rt[:, 0:36]
        for t in range(NTILE):
            ps = nextM()
            for kc in range(16):
                P.op("pe", lambda e, ps=ps, kc=kc, t=t: e.matmul(ps[:, 0:36], lhsT=h2T[:, kc, t * 128:(t + 1) * 128], rhs=wrt[:, kc, :], start=(kc == 0), stop=(kc == 15)), reads=[h2T, wrt], writes=[ps])
            dv = lambda fn, rd=(), wr=(): P.op("dve", fn, reads=[rt] + list(rd), writes=[rt] + list(wr))
            dv(lambda e, ps=ps: e.tensor_tensor(out=rt[:, 0:36], in0=ps[:, 0:36], in1=brt[:], op=ALU.add), rd=[ps, brt])
            dv(lambda e: e.tensor_reduce(out=rt[:, 40:41], in_=rt[:, 0:4], axis=mybir.AxisListType.X, op=ALU.max))
            dv(lambda e: e.tensor_scalar(out=rt[:, 44:48], in0=rt[:, 0:4], scalar1=rt[:, 40:41], scalar2=None, op0=ALU.is_equal))
            dv(lambda e: e.tensor_scalar(out=rt[:, 41:42], in0=rt[:, 40:41], scalar1=-1.0, scalar2=None, op0=ALU.mult))
            P.op("act", lambda e: e.activation(out=rt[:, 48:52], in_=rt[:, 0:4], func=AF.Exp, bias=rt[:, 41:42], scale=1.0, accum_out=rt[:, 42:43]), reads=[rt], writes=[rt])
            dv(lambda e: e.reciprocal(out=rt[:, 43:44], in_=rt[:, 42:43]))
            dv(lambda e: e.tensor_scalar(out=rt[:, 52:56], in0=rt[:, 44:48], scalar1=-1.0, scalar2=1e30, op0=ALU.add, op1=ALU.mult))
            dv(lambda e: e.tensor_tensor(out=rt[:, 64:96].rearrange("p (g x) -> p g x", g=4), in0=rt[:, 4:36].rearrange("p (g x) -> p g x", g=4),
                                         in1=rt[:, 52:56].unsqueeze(2).to_broadcast([128, 4, 8]), op=ALU.add))
            dv(lambda e: e.tensor_reduce(out=rt[:, 56:57], in_=rt[:, 64:96], axis=mybir.AxisListType.X, op=ALU.max))
            dv(lambda e: e.tensor_scalar(out=rt[:, 96:128], in0=rt[:, 64:96], scalar1=rt[:, 56:57], scalar2=None, op0=ALU.is_equal))
            dv(lambda e: e.scalar_tensor_tensor(out=rt[:, 128:160], in0=rt[:, 96:128], scalar=-1e30, in1=rt[:, 64:96], op0=ALU.mult, op1=ALU.add))
            dv(lambda e: e.tensor_reduce(out=rt[:, 57:58], in_=rt[:, 128:160], axis=mybir.AxisListType.X, op=ALU.max))
            dv(lambda e: e.tensor_scalar(out=rt[:, 160:192], in0=rt[:, 128:160], scalar1=rt[:, 57:58], scalar2=None, op0=ALU.is_equal))
            dv(lambda e: e.tensor_scalar(out=rt[:, 58:59], in0=rt[:, 56:57], scalar1=-1.0, scalar2=None, op0=ALU.mult))
            P.op("act", lambda e: e.activation(out=rt[:, 59:60], in_=rt[:, 57:58], func=AF.Exp, bias=rt[:, 58:59], scale=1.0), reads=[rt], writes=[rt])
            dv(lambda e: e.tensor_scalar(out=rt[:, 60:61], in0=rt[:, 59:60], scalar1=1.0, scalar2=None, op0=ALU.add))
            dv(lambda e: e.reciprocal(out=rt[:, 61:62], in_=rt[:, 60:61]))
            dv(lambda e: e.tensor_tensor(out=rt[:, 62:63], in0=rt[:, 61:62], in1=rt[:, 43:44], op=ALU.mult))
            dv(lambda e: e.tensor_tensor(out=rt[:, 63:64], in0=rt[:, 62:63], in1=rt[:, 59:60], op=ALU.mult))
            dv(lambda e: e.tensor_scalar(out=rt[:, 192:224], in0=rt[:, 96:128], scalar1=rt[:, 62:63], scalar2=None, op0=ALU.mult))
            P.op("dve", lambda e: e.scalar_tensor_tensor(out=Gtok[:], in0=rt[:, 160:192], scalar=rt[:, 63:64], in1=rt[:, 192:224], op0=ALU.mult, op1=ALU.add), reads=[rt], writes=[Gtok])
            pt = nextT()
            P.op("pe", lambda e, pt=pt: e.transpose(out=pt[0:32, 0:128], in_=Gtok[:], identity=ident_f[:]), reads=[Gtok, ident_f], writes=[pt])
            P.op("act", lambda e, pt=pt, t=t: e.activation(out=GT[0:32, t * 128:(t + 1) * 128], in_=pt[0:32, 0:128], func=AF.Copy), reads=[pt], writes=[GT])
        P.barrier()
        ES = [Tile("ES%d" % i, AR.t[:, 5120 + i * 8192:5120 + (i + 1) * 8192].bitcast(BF16)) for i in range(4)]
        ecnt = {"n": 0}

        def nextE():
            ecnt["n"] += 1
            return ES[ecnt["n"] % 4]
        sgl = [AR.view("sgl%d" % i, 3072 + i * 1024, [512], BF16) for i in range(2)]
        tl = [AR.view("tl%d" % i, 37888 + i * 2048, [512], F32) for i in range(2)]
        for ex in range(n_experts):
            P.op("dve", lambda e, ex=ex: e.tensor_copy(out=selE[0:32, :], in_=ident_f[0:32, ex:ex + 1].to_broadcast([32, 128])), reads=[ident_f], writes=[selE])
            for (t0, t1) in BLKS:
                ps = nextM()
                P.op("pe", lambda e, ps=ps, t0=t0, t1=t1: e.matmul(ps[:, 0:t1 - t0], lhsT=selE[0:32, :], rhs=GT[0:32, t0:t1], start=True, stop=True), reads=[selE, GT], writes=[ps])
                P.op("act", lambda e, ps=ps, t0=t0, t1=t1: e.activation(out=gbc[:, t0:t1], in_=ps[:, 0:t1 - t0], func=AF.Copy), reads=[ps], writes=[gbc])
            for fq in range(4):
                wg_ = nextE()
                wu_ = nextE()
                for (wsl, wsrc) in ((wg_, w_gate), (wu_, w_up)):
                    src = wsrc[ex, :, fq * 256:(fq + 1) * 256].rearrange("(kc p) n -> p kc n", p=128)
                    P.dma("pool", lambda e, wsl=wsl, src=src: e.dma_start(out=wsl[:].rearrange("p (a b) -> p a b", a=16), in_=src), writes=[wsl], semtile=wsl)
                for fc in range(2):
                    f = fq * 2 + fc
                    for bi, (t0, t1) in enumerate(BLKS):
                        n = t1 - t0
                        pg = nextM()
                        pu = nextM()
                        for (pp_, wsl) in ((pg, wg_), (pu, wu_)):
                            for kc in range(16):
                                P.op("pe", lambda e, pp_=pp_, wsl=wsl, kc=kc, fc=fc, t0=t0, t1=t1: e.matmul(pp_[:, 0:t1 - t0], lhsT=wsl[:].rearrange("p (a b) -> p a b", a=16)[:, kc, fc * 128:(fc + 1) * 128], rhs=h2T[:, kc, t0:t1], start=(kc == 0), stop=(kc == 15)),
                                     reads=[wsl, h2T], writes=[pp_])
                        sx = sgl[(f * 3 + bi) % 2]
                        tx = tl[(f * 3 + bi) % 2]
                        P.op("act", lambda e, pg=pg, sx=sx, n=n: e.activation(out=sx[:, 0:n], in_=pg[:, 0:n], func=AF.Silu), reads=[pg], writes=[sx])
                        P.op("dve", lambda e, pu=pu, sx=sx, tx=tx, n=n: e.tensor_tensor(out=tx[:, 0:n], in0=pu[:, 0:n], in1=sx[:, 0:n], op=ALU.mult), reads=[pu, sx], writes=[tx])
                        P.op("pool", lambda e, tx=tx, f=f, t0=t0, t1=t1: e.tensor_tensor(out=hpT[:, f, t0:t1], in0=tx[:, 0:t1 - t0], in1=gbc[:, t0:t1], op=ALU.mult), reads=[tx, gbc], writes=[hpT])
            for cb in range(4):
                wd_ = nextE()
                src = w_down[ex, :, cb * 512:(cb + 1) * 512].rearrange("(kc p) n -> p kc n", p=128)
                P.dma("pool", lambda e, wd_=wd_, src=src: e.dma_start(out=wd_[:].rearrange("p (a b) -> p a b", a=8), in_=src), writes=[wd_], semtile=wd_)
                for t in range(NTILE):
                    ps = nextM()
                    for fc in range(8):
                        P.op("pe", lambda e, ps=ps, fc=fc, t=t, wd_=wd_: e.matmul(ps[:, 0:512], lhsT=hpT[:, fc, t * 128:(t + 1) * 128], rhs=wd_[:].rearrange("p (a b) -> p a b", a=8)[:, fc, :], start=(fc == 0), stop=(fc == 7)),
                             reads=[hpT, wd_], writes=[ps])
                    P.op("dve", lambda e, ps=ps, t=t, cb=cb: e.tensor_tensor(out=yacc[:, t, cb * 512:(cb + 1) * 512], in0=ps[:, 0:512], in1=yacc[:, t, cb * 512:(cb + 1) * 512], op=ALU.add), reads=[ps, yacc], writes=[yacc])
        P.barrier()
        nf = AR.view("nf", OFF_X, [D], F32)
        junk3 = AR.view("junk3", OFF_X + 8192, [2048], BF16)
        P.dma("sp", lambda e: e.dma_start(out=nf[:], in_=norm_f[:].partition_broadcast(128)), writes=[nf], semtile=nf)
        for t in range(NTILE):
            P.op("act", lambda e, t=t: e.activation(out=junk3[:], in_=yacc[:, t, :], func=AF.Square, accum_out=small[:, 0:1]), reads=[yacc], writes=[junk3, small])
            P.op("act", lambda e: e.activation(out=small[:, 1:2], in_=small[:, 0:1], func=AF.Sqrt, bias=epsb[:], scale=1.0 / D), reads=[small, epsb], writes=[small])
            P.op("dve", lambda e: e.reciprocal(out=small[:, 2:3], in_=small[:, 1:2]), reads=[small], writes=[small])
            P.op("dve", lambda e, t=t: e.scalar_tensor_tensor(out=yacc[:, t, :], in0=yacc[:, t, :], scalar=small[:, 2:3], in1=nf[:], op0=ALU.mult, op1=ALU.mult), reads=[yacc, small, nf], writes=[yacc])
            P.dma("sp", lambda e, t=t: e.dma_start(out=y_out[t * 128:(t + 1) * 128, :], in_=yacc[:, t, :]), reads=[yacc], semtile=yacc, is_output=True)

    P.emit()
    return nc, P


def _tables(half):
    pos = np.concatenate([half * 1024 + np.arange(1024), np.tile(PAST_LEN + np.arange(8), 16)]).astype(np.float32)
    ppos = np.arange(1024).astype(np.float32)
    inv = (np.float32(10000.0) ** (-np.arange(128, dtype=np.float32) / np.float32(128))).astype(np.float32)

    def cs(p):
        ang = (p[None, :] * inv[:, None]).astype(np.float32)
        return np.stack([np.cos(ang.astype(np.float64)), np.sin(ang.astype(np.float64))], axis=1).astype(np.float32)

    dec = np.zeros((H, 128, 4, 128), np.float32)
    i = np.arange(128, dtype=np.float64)
    i8 = (np.arange(128) % 8).astype(np.float64)
    for h in range(H):
        lg = LOG_G[h]
        dec[h, :, 0, :] = np.exp((i + 1) * lg)[None, :]
        dec[h, :, 1, :] = (np.exp(-(i + 1) * lg) * DK ** -0.5)[None, :]
        dec[h, :, 2, :] = np.exp((i8 + 1) * lg)[None, :]
        dec[h, :, 3, :] = (np.exp(-(i8 + 1) * lg) * DK ** -0.5)[None, :]
    j = np.arange(128)
    mp = (j[:, None] <= j[None, :]).astype(np.float32)
    ms = ((j[:, None] <= j[None, :]) & (j[:, None] // 8 == j[None, :] // 8)).astype(np.float32)
    masks = np.stack([mp, ms], axis=1)
    rowmask = (j[:, None] // 8 == np.arange(16)[None, :]).astype(np.float32)
    return dict(cs_own=cs(pos), cs_prev=cs(ppos), dec=dec, masks=np.ascontiguousarray(masks),
                ident=np.eye(128, dtype=np.float32), rowmask=rowmask)


def make_in_maps(inp, n_experts=NE, cores=range(8)):
    f = lambda a: np.ascontiguousarray(np.asarray(a, dtype=np.float32))
    xp = f(inp["x_prompt"])
    xs = f(inp["x_sample"])
    sr = f(inp["state_ret"])[0]
    sc = f(inp["state_conv"])[0]

    def kc_layout(v):
        return f(v).reshape(16, 128).T

    vecs = np.stack([kc_layout(inp["norm_mix"][0]), kc_layout(inp["norm_ffn"][0]), kc_layout(inp["conv_b"][0]),
                     kc_layout(inp["conv_ln_w"][0]), kc_layout(inp["conv_ln_b"][0]), np.zeros((128, 16), np.float32)], axis=1)
    shared = dict(
        w_in=f(inp["w_in"])[0], w_ret_o=f(inp["w_ret_o"])[0], conv_w=f(inp["conv_w"])[0], vecs=np.ascontiguousarray(vecs),
        norm_f=f(inp["norm_f"]).reshape(1, D), w_conv_o=f(inp["w_conv_o"])[0], w_o=f(inp["w_o"])[0],
        w_rt=np.ascontiguousarray(np.concatenate([f(inp["w_coarse"])[0], f(inp["w_fine"])[0]], axis=1)),
        b_rt=np.concatenate([f(inp["b_coarse"])[0], f(inp["b_fine"])[0]]).reshape(1, 36),
        w_gate=f(inp["w_gate"])[0][:n_experts], w_up=f(inp["w_up"])[0][:n_experts], w_down=f(inp["w_down"])[0][:n_experts],
    )
    tabs = [_tables(0), _tables(1)]
    maps = []
    for c in cores:
        b, half = c // 2, c % 2
        m = dict(shared)
        m.update(tabs[half])
        m["x_own"] = np.ascontiguousarray(np.concatenate([xp[b, half * 1024:(half + 1) * 1024], xs[16 * c:16 * (c + 1)].reshape(128, D)], axis=0))
        m["x_prev"] = np.ascontiguousarray(xp[b, 0:1024]) if half == 1 else np.zeros((1024, D), np.float32)
        m["sret"] = np.ascontiguousarray(sr[16 * c:16 * (c + 1)])
        m["sconv"] = np.ascontiguousarray(sc[16 * c:16 * (c + 1)].reshape(16 * 30, D))
        maps.append(m)
    return maps


_CACHE = {}


def kernel(**inputs):
    if "nc" not in _CACHE:
        _CACHE["nc"] = build()[0]
    nc = _CACHE["nc"]
    maps = make_in_maps(inputs)
    res = run_bass_kernel_spmd(nc, maps, core_ids=list(range(8)))
    r = res.results
    yp = np.zeros((4, 2048, D), np.float32)
    ys = np.zeros((128, 8, D), np.float32)
    retp = np.zeros((1, 4, H, DK, DV), np.float32)
    convp = np.zeros((1, 4, 30, D), np.float32)
    rets = np.zeros((1, 128, H, DK, DV), np.float32)
    convs = np.zeros((1, 128, 30, D), np.float32)
    for c in range(8):
        b, half = c // 2, c % 2
        yp[b, half * 1024:(half + 1) * 1024] = r[c]["y_out"][:1024]
        ys[16 * c:16 * (c + 1)] = r[c]["y_out"][1024:].reshape(16, 8, D)
        rets[0, 16 * c:16 * (c + 1)] = r[c]["rets_out"]
        convs[0, 16 * c:16 * (c + 1)] = r[c]["convs_out"]
        if half == 1:
            retp[0, b] = r[c]["retp_out"]
            convp[0, b] = r[c]["convp_out"]
    return (yp, ys, retp, convp, rets, convs)
```

```python
import contextlib
import numpy as np
import concourse.bass as bass
import concourse.mybir as mybir
from concourse.bass_utils import run_bass_kernel_spmd

F32, BF16, I32, U8 = mybir.dt.float32, mybir.dt.bfloat16, mybir.dt.int32, mybir.dt.uint8
F32R = mybir.dt.float32r
ALU = mybir.AluOpType
AF = mybir.ActivationFunctionType

ENGS = ("pe", "act", "dve", "pool", "sp")
DSIZE = {F32: 4, BF16: 2, I32: 4, U8: 1}

D = 2048
H = 8
DK = 256
DV = 512
NTILE = 9
TOK = 1152
PAST_LEN = 16384
CW = 31
NE = 32
DE = 1024
EPS = 1e-6
Q0, K0, V0, G0, CA0, CB0, GR0, GC0 = 0, 2048, 4096, 8192, 12288, 14336, 16384, 18432
IN_COLS = 20480
BLKS = ((0, 512), (512, 1024), (1024, 1152))
LOG_G = [float(np.log1p(-2.0 ** (-5.0 - h))) for h in range(H)]
CDEC_P = [float(np.exp(128 * LOG_G[h])) for h in range(H)]
CDEC_S = [float(np.exp(8 * LOG_G[h])) for h in range(H)]


class Tile:
    def __init__(self, name, h):
        self.name, self.h = name, h
        self.w = None
        self.r = []
        self.sem = None

    def __getitem__(self, idx):
        return self.h[idx]


class Op:
    __slots__ = ("eng", "fn", "deps", "is_dma", "sem", "count", "need_inc", "val")

    def __init__(self, eng, fn, is_dma=False):
        self.eng, self.fn, self.is_dma = eng, fn, is_dma
        self.deps = []
        self.sem = None
        self.count = 0
        self.need_inc = False
        self.val = 0


class Prog:
    def __init__(self, nc):
        self.nc = nc
        self.ops = {e: [] for e in ENGS}
        self.dma_counts = {}
        self.last_dma = {}
        self.stack = contextlib.ExitStack()
        self.out_ops = []
        self.pending = {e: [] for e in ENGS}

    def sb(self, name, shape, dt):
        h = self.stack.enter_context(self.nc.sbuf_tensor(name, list(shape), dt))
        return Tile(name, h)

    def ps(self, name, shape, dt=F32):
        h = self.stack.enter_context(self.nc.psum_tensor(name, list(shape), dt))
        return Tile(name, h)

    def dram(self, name, shape, dt, kind):
        h = self.nc.dram_tensor(name, list(shape), dt, kind=kind)
        return Tile(name, h.ap())

    def _deps(self, o, reads, writes):
        deps = []
        for t in reads:
            if t.w is not None:
                deps.append((t.w, "raw"))
        for t in writes:
            if t.w is not None:
                deps.append((t.w, "waw"))
            for r in t.r:
                deps.append((r, "war"))
        seen = set()
        for d, kind in deps:
            if d is o or id(d) in seen:
                continue
            if (not d.is_dma) and d.eng == o.eng and not o.is_dma:
                if kind != "raw" or o.eng == "pe":
                    continue
            seen.add(id(d))
            o.deps.append(d)
        for d in self.pending[o.eng]:
            if id(d) not in seen and d is not o:
                seen.add(id(d))
                o.deps.append(d)
        self.pending[o.eng] = []
        for t in reads:
            t.r.append(o)
        for t in writes:
            t.w = o
            t.r = []

    def op(self, eng, fn, reads=(), writes=()):
        o = Op(eng, fn)
        self._deps(o, reads, writes)
        self.ops[eng].append(o)
        return o

    def dma(self, eng, fn, reads=(), writes=(), semtile=None, is_output=False):
        o = Op(eng, fn, is_dma=True)
        if semtile.sem is None:
            semtile.sem = "ds_%s" % semtile.name
        o.sem = semtile.sem
        self.dma_counts[o.sem] = self.dma_counts.get(o.sem, 0) + 16
        o.count = self.dma_counts[o.sem]
        self.last_dma[o.sem] = o
        self._deps(o, reads, writes)
        self.ops[eng].append(o)
        if is_output:
            self.out_ops.append(o)
        return o

    def barrier(self):
        deps = []
        for e in ENGS:
            for o in reversed(self.ops[e]):
                if not o.is_dma:
                    deps.append(o)
                    break
        deps += list(self.last_dma.values())
        for e in ENGS:
            self.pending[e] = self.pending[e] + [d for d in deps if d.is_dma or d.eng != e]

    def emit(self):
        nc = self.nc
        for e in ENGS:
            for o in self.ops[e]:
                for d in o.deps:
                    if not d.is_dma:
                        d.need_inc = True
        for e in ENGS:
            c = 0
            for o in self.ops[e]:
                if (not o.is_dma) and o.need_inc:
                    c += 1
                    o.val = c
        sems = {}
        for e in ENGS:
            sems["eng_" + e] = self.stack.enter_context(nc.semaphore("eng_" + e))
        for s in self.dma_counts:
            sems[s] = self.stack.enter_context(nc.semaphore(s))
        self.nsems = len(sems)
        final = {}
        for o in self.out_ops:
            final[o.sem] = max(final.get(o.sem, 0), o.count)

        def emit_eng(ename, eng):
            waited = {}
            for o in self.ops[ename]:
                for d in o.deps:
                    if d.is_dma:
                        s, v = d.sem, d.count
                    else:
                        s, v = "eng_" + d.eng, d.val
                    if waited.get(s, 0) >= v:
                        continue
                    waited[s] = v
                    eng.wait_ge(sems[s], v)
                ins = o.fn(eng)
                if o.is_dma:
                    ins.then_inc(sems[o.sem], 16)
                elif o.need_inc:
                    ins.then_inc(sems["eng_" + ename], 1)
            if ename == "sp":
                for s, v in final.items():
                    if waited.get(s, 0) < v:
                        eng.wait_ge(sems[s], v)

        with nc.Block() as block:
            @block.sync
            def _(e):
                emit_eng("sp", e)

            @block.scalar
            def _(e):
                emit_eng("act", e)

            @block.vector
            def _(e):
                emit_eng("dve", e)

            @block.gpsimd
            def _(e):
                emit_eng("pool", e)

            @block.tensor
            def _(e):
                emit_eng("pe", e)
        self.stack.close()


class Arena:
    def __init__(self, P, nbytes):
        self.P = P
        self.t = P.stack.enter_context(P.nc.sbuf_tensor("arena", [128, nbytes], U8))
        self.nbytes = nbytes

    def view(self, name, off, shape, dt):
        n = int(np.prod(shape)) * DSIZE[dt]
        assert off + n <= self.nbytes, (name, off, n)
        assert off % 4 == 0
        ap = self.t[:, off:off + n].bitcast(dt)
        if len(shape) == 2:
            ap = ap.rearrange("p (a b) -> p a b", a=shape[0])
        elif len(shape) == 3:
            ap = ap.rearrange("p (a b c) -> p a b c", a=shape[0], b=shape[1])
        return Tile(name, ap)


def build(n_experts=NE, stages="ARCMOE", debug=False):
    nc = bass.Bass("TRN2", target_bir_lowering=False)
    P = Prog(nc)
    IN, OUT = "ExternalInput", "ExternalOutput"
    x_own = P.dram("x_own", [TOK, D], F32, IN)
    x_prev = P.dram("x_prev", [1024, D], F32, IN)
    sret = P.dram("sret", [16, H, DK, DV], F32, IN)
    sconv = P.dram("sconv", [16 * 30, D], F32, IN)
    w_in = P.dram("w_in", [D, IN_COLS], F32, IN)
    w_ret_o = P.dram("w_ret_o", [H * DV, D], F32, IN)
    conv_w = P.dram("conv_w", [CW, D], F32, IN)
    vecs = P.dram("vecs", [128, 6, 16], F32, IN)
    norm_f = P.dram("norm_f", [1, D], F32, IN)
    w_conv_o = P.dram("w_conv_o", [D, D], F32, IN)
    w_o = P.dram("w_o", [D, D], F32, IN)
    w_rt = P.dram("w_rt", [D, 36], F32, IN)
    b_rt = P.dram("b_rt", [1, 36], F32, IN)
    w_gate = P.dram("w_gate", [n_experts, D, DE], F32, IN)
    w_up = P.dram("w_up", [n_experts, D, DE], F32, IN)
    w_down = P.dram("w_down", [n_experts, DE, D], F32, IN)
    cs_own_d = P.dram("cs_own", [128, 2, TOK], F32, IN)
    cs_prev_d = P.dram("cs_prev", [128, 2, 1024], F32, IN)
    dec_d = P.dram("dec", [H, 128, 4, 128], F32, IN)
    masks_d = P.dram("masks", [128, 2, 128], F32, IN)
    ident_d = P.dram("ident", [128, 128], F32, IN)
    rowmask_d = P.dram("rowmask", [128, 16], F32, IN)
    mconst_d = P.dram("mconst", [128, 3, 128], F32, IN)

    y_out = P.dram("y_out", [TOK, D], F32, OUT)
    retp_out = P.dram("retp_out", [H, DK, DV], F32, OUT)
    convp_out = P.dram("convp_out", [30, D], F32, OUT)
    rets_out = P.dram("rets_out", [16, H, DK, DV], F32, OUT)
    convs_out = P.dram("convs_out", [16, 30, D], F32, OUT)
    sinit = P.dram("sinit_scr", [H, DK, DV], F32, "Internal")
    ospill = P.dram("ospill_scr", [TOK, H * DV], BF16, OUT if debug else "Internal")

    ident_f = P.sb("ident_f", [128, 128], F32)
    ident_b = P.sb("ident_b", [128, 128], BF16)
    rowmask = P.sb("rowmask_s", [128, 16], F32)
    vec_s = P.sb("vec_s", [128, 6, 16], F32)
    hTh = P.sb("hTh", [128, 16, 128], BF16)
    small = P.sb("small", [128, 16], F32)
    epsb = P.sb("epsb", [128, 1], F32)

    AR = Arena(P, 184 * 1024)
    W = [AR.view("W%d" % i, i * 16384, [16, 512], BF16) for i in range(3)]
    OFF_HT = 49152
    OFF_HEAD = OFF_HT + 36864
    OFF_ROT = OFF_HEAD + 32256
    OFF_S = OFF_ROT + 8192
    OFF_S0 = OFF_S + 6144
    OFF_QK = OFF_S0 + 20480
    OFF_OF = OFF_QK + 3072 + 2048 + 4096
    OFF_END = OFF_OF + 2048
    cs_own = AR.view("cs_own_s", OFF_END, [2, TOK], F32)
    dec_h = AR.view("dec_h", OFF_END + 9216, [4, 128], F32)
    masks = AR.view("masks_s", OFF_END + 11264, [2, 128], F32)
    junk = AR.view("junk", OFF_END + 12800, [2048], BF16)

    pT = [P.ps("pT%d" % i, [128, 512]) for i in range(2)]
    pM = [P.ps("pM%d" % i, [128, 512]) for i in range(4)]
    pS = [P.ps("pS%d" % i, [128, 512]) for i in range(2)]
    cnt = {"m": 0, "t": 0, "s": 0, "w": 0}

    def nextM():
        cnt["m"] += 1
        return pM[cnt["m"] % 4]

    def nextT():
        cnt["t"] += 1
        return pT[cnt["t"] % 2]

    def nextS():
        cnt["s"] += 1
        return pS[cnt["s"] % 2]

    def nextW():
        cnt["w"] += 1
        return W[cnt["w"] % 3]

    def ld(dst, src_ap, eng="sp"):
        P.dma(eng, lambda e: e.dma_start(out=dst[:], in_=src_ap), writes=[dst], semtile=dst)

    ld(ident_f, ident_d[:])
    ld(cs_own, cs_own_d[:])
    ld(masks, masks_d[:])
    ld(rowmask, rowmask_d[:])
    ld(vec_s, vecs[:])
    P.op("dve", lambda e: e.tensor_copy(out=ident_b[:], in_=ident_f[:]), reads=[ident_f], writes=[ident_b])
    P.op("dve", lambda e: e.memset(epsb[:], EPS), writes=[epsb])

    def load_w(slot, src2d, c0=0):
        kc = src2d.shape[0] // 128
        ncols = src2d.shape[1]
        src = src2d.rearrange("(kc p) n -> p kc n", p=128)
        P.dma("pool", lambda e: e.dma_start(out=slot[:, 0:kc, c0:c0 + ncols], in_=src), writes=[slot], semtile=slot)

    def norm_to_T(x_dram, n_tiles, dstT, wrow, xin, xb):
        for t in range(n_tiles):
            xi = xin[t % 2]
            P.dma("sp", lambda e, xi=xi, t=t: e.dma_start(out=xi[:], in_=x_dram[t * 128:(t + 1) * 128, :]), writes=[xi], semtile=xi)
            ss = small
            P.op("act", lambda e, xi=xi: e.activation(out=junk[:], in_=xi[:], func=AF.Square, accum_out=small[:, 0:1]),
                 reads=[xi], writes=[junk, small])
            P.op("act", lambda e: e.activation(out=small[:, 1:2], in_=small[:, 0:1], func=AF.Sqrt, bias=epsb[:], scale=1.0 / D),
                 reads=[small, epsb], writes=[small])
            P.op("dve", lambda e: e.reciprocal(out=small[:, 2:3], in_=small[:, 1:2]), reads=[small], writes=[small])
            P.op("dve", lambda e, xi=xi: e.tensor_scalar(out=xb[:], in0=xi[:], scalar1=small[:, 2:3], scalar2=None, op0=ALU.mult),
                 reads=[xi, small], writes=[xb])
            for g in range(2):
                pt = nextT()
                ptb = pt[:].bitcast(BF16)
                for j in range(8):
                    kc = g * 8 + j
                    P.op("pe", lambda e, ptb=ptb, j=j, kc=kc: e.transpose(out=ptb[:, j * 128:(j + 1) * 128], in_=xb[:, kc * 128:(kc + 1) * 128], identity=ident_b[:]),
                         reads=[xb, ident_b], writes=[pt])
                P.op("dve", lambda e, ptb=ptb, g=g, t=t: e.tensor_tensor(
                    out=dstT[:, g * 8:(g + 1) * 8, t * 128:(t + 1) * 128],
                    in0=ptb.rearrange("p (a b) -> p a b", a=8),
                    in1=vec_s[:, wrow, g * 8:(g + 1) * 8].unsqueeze(2).to_broadcast([128, 8, 128]), op=ALU.mult),
                    reads=[pt, vec_s], writes=[dstT])

    def fm_proj(slot, c0, srcT, blks, evac):
        for bi, (t0, t1) in enumerate(blks):
            ps = nextM()
            for kc in range(16):
                P.op("pe", lambda e, ps=ps, kc=kc, t0=t0, t1=t1: e.matmul(ps[:, 0:t1 - t0], lhsT=slot[:, kc, c0:c0 + 128], rhs=srcT[:, kc, t0:t1], start=(kc == 0), stop=(kc == 15)),
                     reads=[slot, srcT], writes=[ps])
            evac(ps, bi, (t0, t1))

    def tm_proj(slot, ncols, srcT, tile, evac):
        ps = nextM()
        for kc in range(16):
            P.op("pe", lambda e, ps=ps, kc=kc: e.matmul(ps[:, 0:ncols], lhsT=srcT[:, kc, tile * 128:(tile + 1) * 128], rhs=slot[:, kc, 0:ncols], start=(kc == 0), stop=(kc == 15)),
                 reads=[slot, srcT], writes=[ps])
        evac(ps)

    def rotary_pair(p1, p2, n, cs, t0, decsel, out_fn, rot):
        cos = cs[:, 0, t0:t0 + n]
        sin = cs[:, 1, t0:t0 + n]
        ta, tb, tc, td = rot
        P.op("dve", lambda e: e.tensor_tensor(out=ta[:, 0:n], in0=p1[:, 0:n], in1=cos, op=ALU.mult), reads=[p1, cs], writes=[ta])
        P.op("dve", lambda e: e.tensor_tensor(out=tb[:, 0:n], in0=p2[:, 0:n], in1=sin, op=ALU.mult), reads=[p2, cs], writes=[tb])
        P.op("dve", lambda e: e.tensor_tensor(out=tc[:, 0:n], in0=p1[:, 0:n], in1=sin, op=ALU.mult), reads=[p1, cs], writes=[tc])
        P.op("dve", lambda e: e.tensor_tensor(out=td[:, 0:n], in0=p2[:, 0:n], in1=cos, op=ALU.mult), reads=[p2, cs], writes=[td])
        P.op("pool", lambda e: e.tensor_tensor(out=ta[:, 0:n], in0=ta[:, 0:n], in1=tb[:, 0:n], op=ALU.subtract), reads=[ta, tb], writes=[ta])
        P.op("pool", lambda e: e.tensor_tensor(out=tc[:, 0:n], in0=tc[:, 0:n], in1=td[:, 0:n], op=ALU.add), reads=[tc, td], writes=[tc])
        out_fn(0, ta)
        out_fn(1, tc)

    rot = [AR.view("rot%d" % i, OFF_ROT + i * 2048, [512], F32) for i in range(4)]
    xin = [AR.view("xin%d" % i, OFF_HEAD + i * 8192, [2048], F32) for i in range(2)]
    xb = AR.view("xb", OFF_HEAD + 16384, [2048], BF16)

    def dec_load(h):
        P.dma("sp", lambda e: e.dma_start(out=dec_h[:], in_=dec_d[h]), writes=[dec_h], semtile=dec_h)

    if "A" in stages:
        hTp = AR.view("hTp", OFF_HT, [16, 1024], BF16)
        cs_prev = AR.view("cs_prev", OFF_S0, [2, 1024], F32)
        ld(cs_prev, cs_prev_d[:])
        norm_to_T(x_prev, 8, hTp, 0, xin, xb)
        P.op("pool", lambda e: e.tensor_copy(out=hTh[:], in_=hTp[:, :, 896:1024]), reads=[hTp], writes=[hTh])
        P.barrier()
        kTa = AR.view("kTa", OFF_HEAD, [2, 1024], BF16)
        ktokA = AR.view("ktokA", OFF_HEAD + 4096, [8, 256], BF16)
        vA = AR.view("vA", OFF_HEAD + 8192, [8, 512], BF16)
        sstage = AR.view("sstageA", OFF_S, [512], F32)
        for h in range(H):
            dec_load(h)
            wk = nextW()
            load_w(wk, w_in[:, K0 + h * 256:K0 + (h + 1) * 256])
            wv = nextW()
            load_w(wv, w_in[:, V0 + h * 512:V0 + (h + 1) * 512])
            for bi, (t0, t1) in enumerate(((0, 512), (512, 1024))):
                pp = []
                for dc in range(2):
                    ps = nextM()
                    for kc in range(16):
                        P.op("pe", lambda e, ps=ps, kc=kc, dc=dc, t0=t0, t1=t1, wk=wk: e.matmul(ps[:, 0:512], lhsT=wk[:, kc, dc * 128:(dc + 1) * 128], rhs=hTp[:, kc, t0:t1], start=(kc == 0), stop=(kc == 15)),
                             reads=[wk, hTp], writes=[ps])
                    pp.append(ps)

                def outk(which, src, t0=t0):
                    P.op("dve", lambda e: e.tensor_tensor(out=kTa[:, which, t0:t0 + 512].rearrange("p (a b) -> p a b", a=4),
                                                          in0=src[:, 0:512].rearrange("p (a b) -> p a b", a=4),
                                                          in1=dec_h[:, 1, :].unsqueeze(1).to_broadcast([128, 4, 128]), op=ALU.mult),
                         reads=[src, dec_h], writes=[kTa])
                rotary_pair(pp[0], pp[1], 512, cs_prev, t0, None, outk, rot)
            for n in range(8):
                pt = nextT()
                ptb = pt[:].bitcast(BF16)
                for dc in range(2):
                    P.op("pe", lambda e, ptb=ptb, dc=dc, n=n: e.transpose(out=ptb[:, dc * 128:(dc + 1) * 128], in_=kTa[:, dc, n * 128:(n + 1) * 128], identity=ident_b[:]),
                         reads=[kTa, ident_b], writes=[pt])
                sc = CDEC_P[h] * float(np.exp(128 * (7 - n) * LOG_G[h]))
                P.op("act", lambda e, ptb=ptb, n=n, sc=sc: e.activation(out=ktokA[:, n, :], in_=ptb[:, 0:256], func=AF.Copy, scale=sc),
                     reads=[pt], writes=[ktokA])
                tm_proj(wv, 512, hTp, n, lambda ps, n=n: P.op("act", lambda e: e.activation(out=vA[:, n, :], in_=ps[:, 0:512], func=AF.Copy), reads=[ps], writes=[vA]))
            for dc in range(2):
                ps = nextS()
                for n in range(8):
                    P.op("pe", lambda e, ps=ps, n=n, dc=dc: e.matmul(ps[:, 0:512], lhsT=ktokA[:, n, dc * 128:(dc + 1) * 128], rhs=vA[:, n, :], start=(n == 0), stop=(n == 7)),
                         reads=[ktokA, vA], writes=[ps])
                P.op("act", lambda e, ps=ps: e.activation(out=sstage[:], in_=ps[:, 0:512], func=AF.Copy), reads=[ps], writes=[sstage])
                P.dma("sp", lambda e, dc=dc, h=h: e.dma_start(out=sinit[h, dc * 128:(dc + 1) * 128, :], in_=sstage[:]), reads=[sstage], writes=[sinit], semtile=sstage)
        P.barrier()

    hT = AR.view("hT", OFF_HT, [16, TOK], BF16)
    if "R" in stages:
        norm_to_T(x_own, NTILE, hT, 0, xin, xb)
        P.barrier()
        qT = AR.view("qT", OFF_HEAD, [2, TOK], BF16)
        kT = AR.view("kT", OFF_HEAD + 4608, [2, TOK], BF16)
        ktok = AR.view("ktok", OFF_HEAD + 9216, [NTILE, 256], BF16)
        vv = AR.view("vv", OFF_HEAD + 13824, [NTILE, 512], BF16)
        sg = AR.view("sg", OFF_HEAD + 23040, [NTILE, 512], BF16)
        Sf = AR.view("Sf", OFF_S, [2, 512], F32)
        Sb = AR.view("Sb", OFF_S + 4096, [2, 512], BF16)
        S0 = [AR.view("S0_%d" % i, OFF_S0 + i * 4096, [2, 512], F32) for i in range(3)]
        So = [AR.view("So_%d" % i, OFF_S0 + 12288 + i * 4096, [2, 512], F32) for i in range(2)]
        S0b = [AR.view("S0b%d" % i, OFF_QK + 3072 + 2048 + i * 2048, [2, 512], BF16) for i in range(2)]
        Qz = [AR.view("Qz%d" % i, OFF_QK + i * 512, [2, 128], BF16) for i in range(2)]
        Kz = [AR.view("Kz%d" % i, OFF_QK + 2048 + i * 512, [256], BF16) for i in range(2)]
        ofs = [AR.view("of%d" % i, OFF_OF + i * 1024, [512], BF16) for i in range(2)]
        attm = [AR.view("attm%d" % i, OFF_END + 12288 + i * 256, [128], BF16) for i in range(2)]
        for h in range(H):
            dec_load(h)
            wqk = nextW()
            load_w(wqk, w_in[:, Q0 + h * 256:Q0 + (h + 1) * 256], 0)
            load_w(wqk, w_in[:, K0 + h * 256:K0 + (h + 1) * 256], 256)
            wv = nextW()
            load_w(wv, w_in[:, V0 + h * 512:V0 + (h + 1) * 512])
            wg = nextW()
            load_w(wg, w_in[:, G0 + h * 512:G0 + (h + 1) * 512])
            if "A" in stages:
                P.dma("sp", lambda e, h=h: e.dma_start(out=Sf[:], in_=sinit[h].rearrange("(dc p) e -> p dc e", p=128)), reads=[sinit], writes=[Sf], semtile=Sf)
            else:
                P.op("dve", lambda e: e.memset(Sf[:], 0.0), writes=[Sf])
            P.op("act", lambda e: e.activation(out=Sb[:], in_=Sf[:], func=AF.Copy), reads=[Sf], writes=[Sb])
            for which, dstT in ((0, qT), (1, kT)):
                for bi, (t0, t1) in enumerate(BLKS):
                    n = t1 - t0
                    pp = []
                    for dc in range(2):
                        ps = nextM()
                        c0 = which * 256 + dc * 128
                        for kc in range(16):
                            P.op("pe", lambda e, ps=ps, kc=kc, c0=c0, t0=t0, t1=t1, wqk=wqk: e.matmul(ps[:, 0:t1 - t0], lhsT=wqk[:, kc, c0:c0 + 128], rhs=hT[:, kc, t0:t1], start=(kc == 0), stop=(kc == 15)),
                                 reads=[wqk, hT], writes=[ps])
                        pp.append(ps)
                    drow = which + (2 if bi == 2 else 0)

                    def outqk(dcw, src, t0=t0, n=n, drow=drow, dstT=dstT, bi=bi, which=which):
                        a = n // 128
                        P.op("dve", lambda e: e.tensor_tensor(out=dstT[:, dcw, t0:t0 + n].rearrange("p (a b) -> p a b", a=a),
                                                              in0=src[:, 0:n].rearrange("p (a b) -> p a b", a=a),
                                                              in1=dec_h[:, drow, :].unsqueeze(1).to_broadcast([128, a, 128]), op=ALU.mult),
                             reads=[src, dec_h], writes=[dstT])
                    rotary_pair(pp[0], pp[1], n, cs_own, t0, None, outqk, rot)
            for t in range(NTILE):
                pt = nextT()
                ptb = pt[:].bitcast(BF16)
                for dc in range(2):
                    P.op("pe", lambda e, ptb=ptb, dc=dc, t=t: e.transpose(out=ptb[:, dc * 128:(dc + 1) * 128], in_=kT[:, dc, t * 128:(t + 1) * 128], identity=ident_b[:]),
                         reads=[kT, ident_b], writes=[pt])
                sc = CDEC_P[h] if t < 8 else CDEC_S[h]
                P.op("act", lambda e, ptb=ptb, t=t, sc=sc: e.activation(out=ktok[:, t, :], in_=ptb[:, 0:256], func=AF.Copy, scale=sc),
                     reads=[pt], writes=[ktok])
                tm_proj(wv, 512, hT, t, lambda ps, t=t: P.op("act", lambda e: e.activation(out=vv[:, t, :], in_=ps[:, 0:512], func=AF.Copy), reads=[ps], writes=[vv]))
                tm_proj(wg, 512, hT, t, lambda ps, t=t: P.op("act", lambda e: e.activation(out=sg[:, t, :], in_=ps[:, 0:512], func=AF.Silu), reads=[ps], writes=[sg]))
            for t in range(NTILE):
                tc0, tc1 = t * 128, (t + 1) * 128
                pa = nextM()
                for dc in range(2):
                    P.op("pe", lambda e, pa=pa, dc=dc, tc0=tc0, tc1=tc1: e.matmul(pa[:, 0:128], lhsT=kT[:, dc, tc0:tc1], rhs=qT[:, dc, tc0:tc1], start=(dc == 0), stop=(dc == 1)),
                         reads=[kT, qT], writes=[pa])
                am = attm[t % 2]
                mrow = 0 if t < 8 else 1
                P.op("dve", lambda e, pa=pa, am=am, mrow=mrow: e.tensor_tensor(out=am[:], in0=pa[:, 0:128], in1=masks[:, mrow, :], op=ALU.mult),
                     reads=[pa, masks], writes=[am])
                po = nextM()
                P.op("pe", lambda e, po=po, am=am, t=t: e.matmul(po[:, 0:512], lhsT=am[:], rhs=vv[:, t, :], start=True, stop=False),
                     reads=[am, vv], writes=[po])
                if t < 8:
                    for dc in range(2):
                        P.op("pe", lambda e, po=po, dc=dc, tc0=tc0, tc1=tc1: e.matmul(po[:, 0:512], lhsT=qT[:, dc, tc0:tc1], rhs=Sb[:, dc, :], start=False, stop=(dc == 1)),
                             reads=[qT, Sb], writes=[po])
                    for dc in range(2):
                        ps = nextS()
                        P.op("pe", lambda e, ps=ps, dc=dc, t=t: e.matmul(ps[:, 0:512], lhsT=ktok[:, t, dc * 128:(dc + 1) * 128], rhs=vv[:, t, :], start=True, stop=True),
                             reads=[ktok, vv], writes=[ps])
                        P.op("dve", lambda e, ps=ps, dc=dc, h=h: e.scalar_tensor_tensor(out=Sf[:, dc, :], in0=Sf[:, dc, :], scalar=CDEC_P[h], in1=ps[:, 0:512], op0=ALU.mult, op1=ALU.add),
                             reads=[Sf, ps], writes=[Sf])
                    if t < 7:
                        P.op("act", lambda e: e.activation(out=Sb[:], in_=Sf[:], func=AF.Copy), reads=[Sf], writes=[Sb])
                    else:
                        P.dma("sp", lambda e, h=h: e.dma_start(out=retp_out[h].rearrange("(dc p) e -> p dc e", p=128), in_=Sf[:]), reads=[Sf], semtile=Sf, is_output=True)
                else:
                    for bb in range(16):
                        s0 = S0[bb % 3]
                        P.dma("sp", lambda e, s0=s0, bb=bb, h=h: e.dma_start(out=s0[:], in_=sret[bb, h].rearrange("(dc p) e -> p dc e", p=128)), writes=[s0], semtile=s0)
                        qz = Qz[bb % 2]
                        s0b = S0b[bb % 2]
                        P.op("act", lambda e, s0=s0, s0b=s0b: e.activation(out=s0b[:], in_=s0[:], func=AF.Copy), reads=[s0], writes=[s0b])
                        P.op("pool", lambda e, qz=qz: e.memset(qz[:], 0.0), writes=[qz])
                        P.op("pool", lambda e, qz=qz, bb=bb: e.tensor_copy(out=qz[:, :, bb * 8:(bb + 1) * 8], in_=qT[:, :, 1024 + bb * 8:1024 + (bb + 1) * 8]), reads=[qT], writes=[qz])
                        for dc in range(2):
                            P.op("pe", lambda e, po=po, dc=dc, s0b=s0b, qz=qz, bb=bb: e.matmul(po[:, 0:512], lhsT=qz[:, dc, :], rhs=s0b[:, dc, :], start=False, stop=(dc == 1 and bb == 15)),
                                 reads=[qz, s0b], writes=[po])
                        kz = Kz[bb % 2]
                        P.op("dve", lambda e, kz=kz, bb=bb, t=t: e.tensor_scalar(out=kz[:], in0=ktok[:, t, :], scalar1=rowmask[:, bb:bb + 1], scalar2=None, op0=ALU.mult),
                             reads=[ktok, rowmask], writes=[kz])
                        so = So[bb % 2]
                        for dc in range(2):
                            ps = nextS()
                            P.op("pe", lambda e, ps=ps, dc=dc, kz=kz, t=t: e.matmul(ps[:, 0:512], lhsT=kz[:, dc * 128:(dc + 1) * 128], rhs=vv[:, t, :], start=True, stop=True),
                                 reads=[kz, vv], writes=[ps])
                            P.op("dve", lambda e, ps=ps, dc=dc, s0=s0, so=so, h=h: e.scalar_tensor_tensor(out=so[:, dc, :], in0=s0[:, dc, :], scalar=CDEC_S[h], in1=ps[:, 0:512], op0=ALU.mult, op1=ALU.add),
                                 reads=[s0, ps], writes=[so])
                        P.dma("sp", lambda e, so=so, bb=bb, h=h: e.dma_start(out=rets_out[bb, h].rearrange("(dc p) e -> p dc e", p=128), in_=so[:]), reads=[so], semtile=so, is_output=True)
                P.op("act", lambda e, po=po: e.activation(out=junk[:, 0:512], in_=po[:, 0:512], func=AF.Square, accum_out=small[:, 4:5]),
                     reads=[po], writes=[junk, small])
                P.op("act", lambda e: e.activation(out=small[:, 5:6], in_=small[:, 4:5], func=AF.Sqrt, bias=epsb[:], scale=1.0 / DV),
                     reads=[small, epsb], writes=[small])
                P.op("dve", lambda e: e.reciprocal(out=small[:, 6:7], in_=small[:, 5:6]), reads=[small], writes=[small])
                of = ofs[t % 2]
                P.op("dve", lambda e, po=po, of=of, t=t: e.scalar_tensor_tensor(out=of[:], in0=po[:, 0:512], scalar=small[:, 6:7], in1=sg[:, t, :], op0=ALU.mult, op1=ALU.mult),
                     reads=[po, small, sg], writes=[of])
                P.dma("sp", lambda e, of=of, t=t, h=h: e.dma_start(out=ospill[t * 128:(t + 1) * 128, h * 512:(h + 1) * 512], in_=of[:]), reads=[of], writes=[ospill], semtile=of, is_output=debug)
        P.barrier()

    OFF_Z = OFF_HEAD
    OFF_Y = OFF_Z + 36864
    OFF_X = OFF_Y + 36864
    dbg_fm = P.dram("dbg_fm", [128, 16, TOK], BF16, OUT) if debug else None
    if "C" in stages:
        cf = AR.view("cf", OFF_Z, [16, TOK], BF16)
        fmY = AR.view("fmY", OFF_Y, [16, TOK], BF16)
        def cset(i):
            b0 = OFF_Y + i * 17984
            return dict(uP=AR.view("uP%d" % i, b0, [1056], F32), uPb=AR.view("uPb%d" % i, b0 + 4224, [1056], BF16),
                        uS=AR.view("uS%d" % i, b0 + 6400, [16, 38], F32), uSb=AR.view("uSb%d" % i, b0 + 8832, [16, 38], BF16),
                        dg=AR.view("dg%d" % i, b0 + 10048, [31, 128], BF16))
        csets = [cset(0), cset(1)]
        cwT = AR.view("cwT", OFF_X, [16, 31], F32)
        sgt = [AR.view("sgt%d" % i, OFF_X + 2048 + i * 2048, [512], F32) for i in range(2)]
        scs = AR.view("scs", OFF_X + 6144, [4, 128], F32)
        cwrow = AR.view("cwrow", OFF_X + 8192, [2048], F32)
        strow = [AR.view("strow%d" % i, OFF_X + 16384 + i * 512, [128], F32) for i in range(2)]
        P.dma("sp", lambda e: e.dma_start(out=cwrow[0:CW, :], in_=conv_w[:]), writes=[cwrow], semtile=cwrow)
        for c in range(16):
            pt = nextT()
            P.op("pe", lambda e, pt=pt, c=c: e.transpose(out=pt[:, 0:CW], in_=cwrow[0:CW, c * 128:(c + 1) * 128], identity=ident_f[0:CW, 0:CW]),
                 reads=[cwrow, ident_f], writes=[pt])
            P.op("act", lambda e, pt=pt, c=c: e.activation(out=cwT[:, c, :], in_=pt[:, 0:CW], func=AF.Copy), reads=[pt], writes=[cwT])
        cpy = Tile("cpy", None)
        P.dma("sp", lambda e: e.dma_start(out=convs_out[:, 0:22, :], in_=sconv[:].rearrange("(b w) d -> b w d", w=30)[:, 8:30, :]), semtile=cpy, is_output=True)
        for c in range(16):
            if c % 4 == 0:
                wa = nextW()
                load_w(wa, w_in[:, CA0 + c * 128:CA0 + (c + 4) * 128])
                wb_ = nextW()
                load_w(wb_, w_in[:, CB0 + c * 128:CB0 + (c + 4) * 128])
            cc = (c % 4) * 128
            cs_ = csets[c % 2]
            uP, uPb, uS, uSb, dg = cs_["uP"], cs_["uPb"], cs_["uS"], cs_["uSb"], cs_["dg"]
            pst = nextT()
            for a in range(4):
                rows = 128 if a < 3 else 96
                st = strow[a % 2]
                P.dma("sp", lambda e, st=st, a=a, rows=rows, c=c: e.dma_start(out=st[0:rows, :], in_=sconv[a * 128:a * 128 + rows, c * 128:(c + 1) * 128]), writes=[st], semtile=st)
                P.op("pe", lambda e, pst=pst, st=st, a=a, rows=rows: e.transpose(out=pst[:, a * 128:a * 128 + rows], in_=st[0:rows, :], identity=ident_f[0:rows, 0:rows]),
                     reads=[st, ident_f], writes=[pst])
            P.op("act", lambda e, pst=pst, uS=uS: e.activation(out=uS[:, :, 0:30], in_=pst[:, 0:480].rearrange("p (b w) -> p b w", w=30), func=AF.Copy), reads=[pst], writes=[uS])
            segs = ((hTh, 0, 128, "h"), (hT, 0, 512, "p0"), (hT, 512, 1024, "p1"), (hT, 1024, 1152, "s"))
            for si, (src, t0, t1, kind) in enumerate(segs):
                n = t1 - t0
                pa_ = nextM()
                pb_ = nextM()
                for (pp_, wsl) in ((pa_, wa), (pb_, wb_)):
                    for kc in range(16):
                        P.op("pe", lambda e, pp_=pp_, wsl=wsl, kc=kc, cc=cc, src=src, t0=t0, t1=t1: e.matmul(pp_[:, 0:t1 - t0], lhsT=wsl[:, kc, cc:cc + 128], rhs=src[:, kc, t0:t1], start=(kc == 0), stop=(kc == 15)),
                             reads=[wsl, src], writes=[pp_])
                sgx = sgt[si % 2]
                P.op("act", lambda e, pb_=pb_, sgx=sgx, n=n: e.activation(out=sgx[:, 0:n], in_=pb_[:, 0:n], func=AF.Sigmoid), reads=[pb_], writes=[sgx])
                if kind == "h":
                    P.op("dve", lambda e, pa_=pa_, sgx=sgx, uP=uP: e.tensor_tensor(out=uP[:, 0:30], in0=pa_[:, 98:128], in1=sgx[:, 98:128], op=ALU.mult), reads=[pa_, sgx], writes=[uP])
                elif kind == "s":
                    P.op("dve", lambda e, pa_=pa_, sgx=sgx, uS=uS: e.tensor_tensor(out=uS[:, :, 30:38], in0=pa_[:, 0:128].rearrange("p (b i) -> p b i", i=8), in1=sgx[:, 0:128].rearrange("p (b i) -> p b i", i=8), op=ALU.mult),
                         reads=[pa_, sgx], writes=[uS])
                else:
                    P.op("dve", lambda e, pa_=pa_, sgx=sgx, t0=t0, uP=uP: e.tensor_tensor(out=uP[:, 30 + t0:30 + t0 + 512], in0=pa_[:, 0:512], in1=sgx[:, 0:512], op=ALU.mult), reads=[pa_, sgx], writes=[uP])
            P.op("act", lambda e, uP=uP, uPb=uPb: e.activation(out=uPb[:, 0:1054], in_=uP[:, 0:1054], func=AF.Copy), reads=[uP], writes=[uPb])
            P.op("dve", lambda e, uS=uS, uSb=uSb: e.tensor_copy(out=uSb[:], in_=uS[:]), reads=[uS], writes=[uSb])
            pt = nextT()
            P.op("pe", lambda e, pt=pt, uP=uP: e.transpose(out=pt[0:30, 0:128], in_=uP[:, 1024:1054], identity=ident_f[:]), reads=[uP, ident_f], writes=[pt])
            P.op("act", lambda e, pt=pt: e.activation(out=scs[0:30, 0, :], in_=pt[0:30, 0:128], func=AF.Copy), reads=[pt], writes=[scs])
            P.dma("sp", lambda e, c=c: e.dma_start(out=convp_out[:, c * 128:(c + 1) * 128], in_=scs[0:30, 0, :]), reads=[scs], semtile=scs, is_output=True)
            P.op("act", lambda e, uS=uS: e.activation(out=scs[:, 1, :].rearrange("p (b i) -> p b i", i=8), in_=uS[:, :, 30:38], func=AF.Copy), reads=[uS], writes=[scs])
            pt2 = nextT()
            P.op("pe", lambda e, pt2=pt2: e.transpose(out=pt2[:, 0:128], in_=scs[:, 1, :], identity=ident_f[:]), reads=[scs, ident_f], writes=[pt2])
            P.op("act", lambda e, pt2=pt2: e.activation(out=scs[:, 2, :], in_=pt2[:, 0:128], func=AF.Copy), reads=[pt2], writes=[scs])
            for bb in range(16):
                P.dma("sp", lambda e, bb=bb, c=c: e.dma_start(out=convs_out[bb, 22:30, c * 128:(c + 1) * 128], in_=scs[bb * 8:(bb + 1) * 8, 2, :]), reads=[scs], semtile=scs, is_output=True)
            for tap in range(CW):
                if tap % 2 == 0:
                    P.op("dve", lambda e, tap=tap, c=c, dg=dg: e.tensor_scalar(out=dg[:, tap, :], in0=ident_f[:], scalar1=cwT[:, c, tap:tap + 1], scalar2=None, op0=ALU.mult),
                         reads=[ident_f, cwT], writes=[dg])
                else:
                    P.op("act", lambda e, tap=tap, c=c, dg=dg: e.activation(out=dg[:, tap, :], in_=ident_f[:], func=AF.Identity, scale=cwT[:, c, tap:tap + 1]),
                         reads=[ident_f, cwT], writes=[dg])
            for (t0, n, kind) in ((0, 512, "p"), (512, 512, "p"), (1024, 128, "s")):
                pc = nextM()
                for tap in range(CW):
                    if kind == "p":
                        P.op("pe", lambda e, pc=pc, tap=tap, t0=t0, dg=dg, uPb=uPb: e.matmul(pc[:, 0:512], lhsT=dg[:, tap, :], rhs=uPb[:, t0 + tap:t0 + tap + 512], start=(tap == 0), stop=(tap == CW - 1)),
                             reads=[dg, uPb], writes=[pc])
                    else:
                        P.op("pe", lambda e, pc=pc, tap=tap, dg=dg, uSb=uSb: e.matmul(pc[:, 0:128], lhsT=dg[:, tap, :], rhs=uSb[:, :, tap:tap + 8], start=(tap == 0), stop=(tap == CW - 1)),
                             reads=[dg, uSb], writes=[pc])
                P.op("act", lambda e, pc=pc, t0=t0, n=n, c=c: e.activation(out=cf[:, c, t0:t0 + n], in_=pc[:, 0:n], func=AF.Identity, bias=vec_s[:, 2, c:c + 1]),
                     reads=[pc, vec_s], writes=[cf])
        P.barrier()
        ones_b = AR.view("ones_b", OFF_Y, [128], BF16)
        sq = [AR.view("sq%d" % i, OFF_Y + 256 + i * 1024, [512], BF16) for i in range(2)]
        mu_t = AR.view("mu_t", OFF_Y + 2304, [TOK], F32)
        rs_t = AR.view("rs_t", OFF_Y + 2304 + 4608, [TOK], F32)
        lt = [AR.view("lt%d" % i, OFF_Y + 11520 + i * 2048, [512], F32) for i in range(2)]
        epsw = AR.view("epsw", OFF_Y + 15616, [1], F32)
        P.op("dve", lambda e: e.memset(ones_b[:], 1.0), writes=[ones_b])
        P.op("dve", lambda e: e.memset(epsw[:], EPS), writes=[epsw])
        for (t0, t1) in BLKS:
            n = t1 - t0
            p1 = nextM()
            p2 = nextM()
            for c in range(16):
                P.op("pe", lambda e, p1=p1, c=c, t0=t0, t1=t1: e.matmul(p1[:, 0:t1 - t0], lhsT=ones_b[:], rhs=cf[:, c, t0:t1], start=(c == 0), stop=(c == 15)),
                     reads=[ones_b, cf], writes=[p1])
                sqx = sq[c % 2]
                P.op("act", lambda e, sqx=sqx, c=c, t0=t0, t1=t1: e.activation(out=sqx[:, 0:t1 - t0], in_=cf[:, c, t0:t1], func=AF.Square), reads=[cf], writes=[sqx])
                P.op("pe", lambda e, p2=p2, sqx=sqx, c=c, n=n: e.matmul(p2[:, 0:n], lhsT=ones_b[:], rhs=sqx[:, 0:n], start=(c == 0), stop=(c == 15)),
                     reads=[ones_b, sqx], writes=[p2])
            P.op("act", lambda e, p1=p1, t0=t0, n=n: e.activation(out=mu_t[:, t0:t0 + n], in_=p1[:, 0:n], func=AF.Copy, scale=1.0 / D), reads=[p1], writes=[mu_t])
            l0 = lt[0]
            P.op("dve", lambda e, t0=t0, n=n, l0=l0: e.tensor_tensor(out=l0[:, 0:n], in0=mu_t[:, t0:t0 + n], in1=mu_t[:, t0:t0 + n], op=ALU.mult), reads=[mu_t], writes=[l0])
            P.op("dve", lambda e, p2=p2, n=n, l0=l0: e.scalar_tensor_tensor(out=l0[:, 0:n], in0=p2[:, 0:n], scalar=1.0 / D, in1=l0[:, 0:n], op0=ALU.mult, op1=ALU.subtract), reads=[p2, l0], writes=[l0])
            P.op("act", lambda e, n=n, l0=l0: e.activation(out=l0[:, 0:n], in_=l0[:, 0:n], func=AF.Sqrt, bias=epsw[:], scale=1.0), reads=[l0, epsw], writes=[l0])
            P.op("dve", lambda e, t0=t0, n=n, l0=l0: e.reciprocal(out=rs_t[:, t0:t0 + n], in_=l0[:, 0:n]), reads=[l0], writes=[rs_t])
        for c in range(16):
            for (t0, t1) in BLKS:
                n = t1 - t0
                lx = lt[(c * 3 + (t0 // 512)) % 2]
                P.op("dve", lambda e, lx=lx, c=c, t0=t0, t1=t1: e.tensor_tensor(out=lx[:, 0:t1 - t0], in0=cf[:, c, t0:t1], in1=mu_t[:, t0:t1], op=ALU.subtract), reads=[cf, mu_t], writes=[lx])
                P.op("pool", lambda e, lx=lx, t0=t0, t1=t1: e.tensor_tensor(out=lx[:, 0:t1 - t0], in0=lx[:, 0:t1 - t0], in1=rs_t[:, t0:t1], op=ALU.mult), reads=[lx, rs_t], writes=[lx])
                P.op("act", lambda e, lx=lx, c=c, t0=t0, t1=t1: e.activation(out=cf[:, c, t0:t1], in_=lx[:, 0:t1 - t0], func=AF.Silu, bias=vec_s[:, 4, c:c + 1], scale=vec_s[:, 3, c:c + 1]),
                     reads=[lx, vec_s], writes=[cf])
        P.barrier()
        for c4 in range(4):
            wsl = nextW()
            load_w(wsl, w_in[:, GC0 + c4 * 512:GC0 + (c4 + 1) * 512])
            for j in range(4):
                c = c4 * 4 + j
                fm_proj(wsl, j * 128, hT, BLKS, lambda ps, bi, tt, c=c: P.op("act", lambda e: e.activation(out=fmY[:, c, tt[0]:tt[1]], in_=ps[:, 0:tt[1] - tt[0]], func=AF.Sigmoid), reads=[ps], writes=[fmY]))
        for c4 in range(4):
            wsl = nextW()
            load_w(wsl, w_conv_o[:, c4 * 512:(c4 + 1) * 512])
            for j in range(4):
                c = c4 * 4 + j
                fm_proj(wsl, j * 128, cf, BLKS, lambda ps, bi, tt, c=c: P.op("dve", lambda e: e.tensor_tensor(out=fmY[:, c, tt[0]:tt[1]], in0=ps[:, 0:tt[1] - tt[0]], in1=fmY[:, c, tt[0]:tt[1]], op=ALU.mult), reads=[ps, fmY], writes=[fmY]))
        P.barrier()
        fmZ = AR.view("fmZ", OFF_Z, [16, TOK], BF16)
        for c4 in range(4):
            wsl = nextW()
            load_w(wsl, w_in[:, GR0 + c4 * 512:GR0 + (c4 + 1) * 512])
            for j in range(4):
                c = c4 * 4 + j
                fm_proj(wsl, j * 128, hT, BLKS, lambda ps, bi, tt, c=c: P.op("act", lambda e: e.activation(out=fmZ[:, c, tt[0]:tt[1]], in_=ps[:, 0:tt[1] - tt[0]], func=AF.Sigmoid), reads=[ps], writes=[fmZ]))
        P.barrier()

    if "M" in stages:
        oT = AR.view("oT", OFF_HT, [32, 512], BF16)
        orow = [AR.view("orow%d" % i, OFF_X + i * 8192, [4096], BF16) for i in range(2)]
        mt = [AR.view("mt%d" % i, OFF_X + 16384 + i * 2048, [512], F32) for i in range(2)]
        Wr = [Tile("Wr%d" % i, W[i][:].rearrange("p a b -> p (a b)").rearrange("p (a b) -> p a b", a=32)) for i in range(3)]
        for bi, (t0, t1) in enumerate(BLKS):
            n = t1 - t0
            for ti in range(n // 128):
                t = t0 // 128 + ti
                orw = orow[t % 2]
                P.dma("sp", lambda e, orw=orw, t=t: e.dma_start(out=orw[:], in_=ospill[t * 128:(t + 1) * 128, :]), reads=[ospill], writes=[orw], semtile=orw)
                for g in range(4):
                    pt = nextT()
                    ptb = pt[:].bitcast(BF16)
                    for j in range(8):
                        kc = g * 8 + j
                        P.op("pe", lambda e, ptb=ptb, j=j, kc=kc, orw=orw: e.transpose(out=ptb[:, j * 128:(j + 1) * 128], in_=orw[:, kc * 128:(kc + 1) * 128], identity=ident_b[:]),
                             reads=[orw, ident_b], writes=[pt])
                    P.op("act", lambda e, ptb=ptb, g=g, ti=ti: e.activation(out=oT[:, g * 8:(g + 1) * 8, ti * 128:(ti + 1) * 128], in_=ptb.rearrange("p (a b) -> p a b", a=8), func=AF.Copy),
                         reads=[pt], writes=[oT])
            for c2 in range(8):
                wsl = Wr[(bi * 8 + c2) % 3]
                src = w_ret_o[:, c2 * 256:(c2 + 1) * 256].rearrange("(kc p) n -> p kc n", p=128)
                P.dma("pool", lambda e, wsl=wsl, src=src: e.dma_start(out=wsl[:], in_=src), writes=[wsl], semtile=wsl)
                for j in range(2):
                    c = c2 * 2 + j
                    ps = nextM()
                    for kc in range(32):
                        P.op("pe", lambda e, ps=ps, kc=kc, wsl=wsl, j=j, n=n: e.matmul(ps[:, 0:n], lhsT=wsl[:, kc, j * 128:(j + 1) * 128], rhs=oT[:, kc, 0:n], start=(kc == 0), stop=(kc == 31)),
                             reads=[wsl, oT], writes=[ps])
                    mx = mt[c % 2]
                    P.op("dve", lambda e, ps=ps, mx=mx, c=c, t0=t0, t1=t1: e.tensor_tensor(out=mx[:, 0:t1 - t0], in0=ps[:, 0:t1 - t0], in1=fmZ[:, c, t0:t1], op=ALU.mult), reads=[ps, fmZ], writes=[mx])
                    P.op("pool", lambda e, mx=mx, c=c, t0=t0, t1=t1: e.tensor_tensor(out=fmY[:, c, t0:t1], in0=mx[:, 0:t1 - t0], in1=fmY[:, c, t0:t1], op=ALU.add), reads=[mx, fmY], writes=[fmY])
        if debug and stages.endswith("M"):
            P.dma("sp", lambda e: e.dma_start(out=dbg_fm[:], in_=fmY[:]), reads=[fmY], semtile=fmY, is_output=True)
        P.barrier()

    if "O" in stages:
        yacc = AR.view("yacc", OFF_HT, [NTILE, D], F32)
        xt = [AR.view("xt%d" % i, OFF_X + i * 2048, [512], F32) for i in range(2)]
        junk2 = AR.view("junk2", OFF_X + 4096, [2048], BF16)
        xb2 = AR.view("xb2", OFF_X + 8192, [2048], BF16)
        for cb in range(4):
            wsl = nextW()
            load_w(wsl, w_o[:, cb * 512:(cb + 1) * 512])
            for t in range(NTILE):
                xx = xt[t % 2]
                P.dma("sp", lambda e, xx=xx, t=t, cb=cb: e.dma_start(out=xx[:], in_=x_own[t * 128:(t + 1) * 128, cb * 512:(cb + 1) * 512]), writes=[xx], semtile=xx)
                ps = nextM()
                for kc in range(16):
                    P.op("pe", lambda e, ps=ps, kc=kc, t=t, wsl=wsl: e.matmul(ps[:, 0:512], lhsT=fmY[:, kc, t * 128:(t + 1) * 128], rhs=wsl[:, kc, :], start=(kc == 0), stop=(kc == 15)),
                         reads=[fmY, wsl], writes=[ps])
                P.op("dve", lambda e, ps=ps, xx=xx, t=t, cb=cb: e.tensor_tensor(out=yacc[:, t, cb * 512:(cb + 1) * 512], in0=ps[:, 0:512], in1=xx[:], op=ALU.add), reads=[ps, xx], writes=[yacc])
        P.barrier()
        if stages.endswith("O"):
            for t in range(NTILE):
                P.dma("sp", lambda e, t=t: e.dma_start(out=y_out[t * 128:(t + 1) * 128, :], in_=yacc[:, t, :]), reads=[yacc], semtile=yacc, is_output=True)
        NBLK = 2 * TOK // 128 + NE
        h2p = AR.view("h2p", OFF_Y, [NTILE, D], BF16)
        h2Tt = AR.view("h2Tt", OFF_X + 12288, [16, 128], BF16)
        XO = OFF_X + 16384
        OH1s = AR.view("OH1s", XO, [NTILE, 32], F32)
        OH2s = AR.view("OH2s", XO + 1152, [NTILE, 32], F32)
        G12 = AR.view("G12", XO + 2304, [2, 16], F32)
        R12 = AR.view("R12", XO + 2432, [2, 16], F32)
        carry = AR.view("carry", XO + 2560, [32], F32)
        rt = AR.view("rt", XO + 2688, [256], F32)
        mconst = AR.view("mconst", XO + 3712, [3, 128], F32)
        ones_f = AR.view("ones_f", XO + 5248, [128], F32)
        wrt = AR.view("wrt", XO + 5760, [16, 36], BF16)
        brt = AR.view("brt", XO + 6912, [36], F32)
        rk = AR.view("rk", XO + 7056, [64], F32)
        x1scr = P.dram("x1_scr", [TOK, D], F32, "Internal")
        P.dma("sp", lambda e: e.dma_start(out=mconst[:], in_=mconst_d[:]), writes=[mconst], semtile=mconst)
        P.dma("pool", lambda e: e.dma_start(out=wrt[:], in_=w_rt[:].rearrange("(kc p) n -> p kc n", p=128)), writes=[wrt], semtile=wrt)
        P.dma("sp", lambda e: e.dma_start(out=brt[:], in_=b_rt[:].partition_broadcast(128)), writes=[brt], semtile=brt)
        P.op("dve", lambda e: e.memset(ones_f[:], 1.0), writes=[ones_f])
        P.op("dve", lambda e: e.memset(carry[:], 0.0), writes=[carry])
        for t in range(NTILE):
            P.dma("sp", lambda e, t=t: e.dma_start(out=x1scr[t * 128:(t + 1) * 128, :], in_=yacc[:, t, :]), reads=[yacc], writes=[x1scr], semtile=yacc)
            P.op("act", lambda e, t=t: e.activation(out=junk2[:], in_=yacc[:, t, :], func=AF.Square, accum_out=small[:, 0:1]), reads=[yacc], writes=[junk2, small])
            P.op("act", lambda e: e.activation(out=small[:, 1:2], in_=small[:, 0:1], func=AF.Sqrt, bias=epsb[:], scale=1.0 / D), reads=[small, epsb], writes=[small])
            P.op("dve", lambda e: e.reciprocal(out=small[:, 2:3], in_=small[:, 1:2]), reads=[small], writes=[small])
            P.op("dve", lambda e, t=t: e.tensor_scalar(out=xb2[:], in0=yacc[:, t, :], scalar1=small[:, 2:3], scalar2=None, op0=ALU.mult), reads=[yacc, small], writes=[xb2])
            for g in range(2):
                pt = nextT()
                ptb = pt[:].bitcast(BF16)
                for j in range(8):
                    kc = g * 8 + j
                    P.op("pe", lambda e, ptb=ptb, j=j, kc=kc: e.transpose(out=ptb[:, j * 128:(j + 1) * 128], in_=xb2[:, kc * 128:(kc + 1) * 128], identity=ident_b[:]), reads=[xb2, ident_b], writes=[pt])
                P.op("dve", lambda e, ptb=ptb, g=g: e.tensor_tensor(out=h2Tt[:, g * 8:(g + 1) * 8, :], in0=ptb.rearrange("p (a b) -> p a b", a=8),
                                                                in1=vec_s[:, 1, g * 8:(g + 1) * 8].unsqueeze(2).to_broadcast([128, 8, 128]), op=ALU.mult), reads=[pt, vec_s], writes=[h2Tt])
            for g in range(2):
                pt = nextT()
                ptb = pt[:].bitcast(BF16)
                for j in range(8):
                    kc = g * 8 + j
                    P.op("pe", lambda e, ptb=ptb, j=j, kc=kc: e.transpose(out=ptb[:, j * 128:(j + 1) * 128], in_=h2Tt[:, kc, :], identity=ident_b[:]), reads=[h2Tt, ident_b], writes=[pt])
                P.op("act", lambda e, ptb=ptb, g=g, t=t: e.activation(
                    out=h2p[:, t, :].rearrange("t (j p) -> t p j", j=16)[:, g * 64:(g + 1) * 64, :],
                    in_=ptb.rearrange("t (p j) -> t p j", j=16), func=AF.Copy), reads=[pt], writes=[h2p])
            ps = nextM()
            for kc in range(16):
                P.op("pe", lambda e, ps=ps, kc=kc: e.matmul(ps[:, 0:36], lhsT=h2Tt[:, kc, :], rhs=wrt[:, kc, :], start=(kc == 0), stop=(kc == 15)), reads=[h2Tt, wrt], writes=[ps])
            dv = lambda fn, rd=(), wr=(): P.op("dve", fn, reads=[rt] + list(rd), writes=[rt] + list(wr))
            dv(lambda e, ps=ps: e.tensor_tensor(out=rt[:, 0:36], in0=ps[:, 0:36], in1=brt[:], op=ALU.add), rd=[ps, brt])
            dv(lambda e: e.tensor_reduce(out=rt[:, 40:41], in_=rt[:, 0:4], axis=mybir.AxisListType.X, op=ALU.max))
            dv(lambda e: e.tensor_scalar(out=rt[:, 44:48], in0=rt[:, 0:4], scalar1=rt[:, 40:41], scalar2=None, op0=ALU.is_equal))
            dv(lambda e: e.tensor_scalar(out=rt[:, 41:42], in0=rt[:, 40:41], scalar1=-1.0, scalar2=None, op0=ALU.mult))
            P.op("act", lambda e: e.activation(out=rt[:, 48:52], in_=rt[:, 0:4], func=AF.Exp, bias=rt[:, 41:42], scale=1.0, accum_out=rt[:, 42:43]), reads=[rt], writes=[rt])
            dv(lambda e: e.reciprocal(out=rt[:, 43:44], in_=rt[:, 42:43]))
            dv(lambda e: e.tensor_scalar(out=rt[:, 52:56], in0=rt[:, 44:48], scalar1=-1.0, scalar2=1e30, op0=ALU.add, op1=ALU.mult))
            dv(lambda e: e.tensor_tensor(out=rt[:, 64:96].rearrange("p (g x) -> p g x", g=4), in0=rt[:, 4:36].rearrange("p (g x) -> p g x", g=4),
                                         in1=rt[:, 52:56].unsqueeze(2).to_broadcast([128, 4, 8]), op=ALU.add))
            dv(lambda e: e.tensor_reduce(out=rt[:, 56:57], in_=rt[:, 64:96], axis=mybir.AxisListType.X, op=ALU.max))
            dv(lambda e, t=t: e.tensor_scalar(out=OH1s[:, t, :], in0=rt[:, 64:96], scalar1=rt[:, 56:57], scalar2=None, op0=ALU.is_equal), wr=[OH1s])
            dv(lambda e, t=t: e.scalar_tensor_tensor(out=rt[:, 128:160], in0=OH1s[:, t, :], scalar=-1e30, in1=rt[:, 64:96], op0=ALU.mult, op1=ALU.add), rd=[OH1s])
            dv(lambda e: e.tensor_reduce(out=rt[:, 57:58], in_=rt[:, 128:160], axis=mybir.AxisListType.X, op=ALU.max))
            dv(lambda e, t=t: e.tensor_scalar(out=OH2s[:, t, :], in0=rt[:, 128:160], scalar1=rt[:, 57:58], scalar2=None, op0=ALU.is_equal), wr=[OH2s])
            dv(lambda e: e.tensor_scalar(out=rt[:, 58:59], in0=rt[:, 56:57], scalar1=-1.0, scalar2=None, op0=ALU.mult))
            P.op("act", lambda e: e.activation(out=rt[:, 59:60], in_=rt[:, 57:58], func=AF.Exp, bias=rt[:, 58:59], scale=1.0), reads=[rt], writes=[rt])
            dv(lambda e: e.tensor_scalar(out=rt[:, 60:61], in0=rt[:, 59:60], scalar1=1.0, scalar2=None, op0=ALU.add))
            dv(lambda e: e.reciprocal(out=rt[:, 61:62], in_=rt[:, 60:61]))
            dv(lambda e, t=t: e.tensor_tensor(out=G12[:, 0, t:t + 1], in0=rt[:, 61:62], in1=rt[:, 43:44], op=ALU.mult), wr=[G12])
            dv(lambda e, t=t: e.tensor_tensor(out=G12[:, 1, t:t + 1], in0=G12[:, 0, t:t + 1], in1=rt[:, 59:60], op=ALU.mult), rd=[G12], wr=[G12])
            for k, OHs in ((0, OH1s), (1, OH2s)):
                pr = nextM()
                P.op("pe", lambda e, pr=pr, OHs=OHs, t=t: e.matmul(pr[:, 0:32], lhsT=mconst[:, 0, :], rhs=OHs[:, t, :], start=True, stop=True), reads=[mconst, OHs], writes=[pr])
                pc_ = nextM()
                P.op("pe", lambda e, pc_=pc_, OHs=OHs, t=t: e.matmul(pc_[:, 0:32], lhsT=ones_f[:], rhs=OHs[:, t, :], start=True, stop=True), reads=[ones_f, OHs], writes=[pc_])
                P.op("dve", lambda e, pr=pr: e.tensor_tensor(out=rk[:, 0:32], in0=pr[:, 0:32], in1=carry[:], op=ALU.add), reads=[pr, carry], writes=[rk])
                P.op("dve", lambda e, OHs=OHs, t=t: e.tensor_tensor(out=rk[:, 32:64], in0=OHs[:, t, :], in1=rk[:, 0:32], op=ALU.mult), reads=[OHs, rk], writes=[rk])
                P.op("dve", lambda e, t=t, k=k: e.tensor_reduce(out=R12[:, k, t:t + 1], in_=rk[:, 32:64], axis=mybir.AxisListType.X, op=ALU.add), reads=[rk], writes=[R12])
                P.op("dve", lambda e, pc_=pc_: e.tensor_tensor(out=carry[:], in0=pc_[:, 0:32], in1=carry[:], op=ALU.add), reads=[pc_, carry], writes=[carry])
        if debug and stages.endswith("O"):
            dbg_s = P.dram("dbg_s", [128, 96], F32, OUT)
            P.dma("sp", lambda e: e.dma_start(out=dbg_s[:, 0:32], in_=R12[:].rearrange("p a b -> p (a b)")), reads=[R12], semtile=R12, is_output=True)
            P.dma("sp", lambda e: e.dma_start(out=dbg_s[:, 32:64], in_=G12[:].rearrange("p a b -> p (a b)")), reads=[G12], semtile=G12, is_output=True)
            P.dma("sp", lambda e: e.dma_start(out=dbg_s[:, 64:96], in_=carry[:]), reads=[carry], semtile=carry, is_output=True)
        P.barrier()

    if "E" in stages:
        EO = XO + 8192
        nblk = AR.view("nblk", EO, [32], F32)
        pst = AR.view("pst", EO + 128, [32], F32)
        pend = AR.view("pend", EO + 256, [32], F32)
        prow = AR.view("prow", EO + 384, [32], F32)
        ebf = AR.view("ebf", EO + 512, [64], F32)
        idxW = AR.view("idxW", EO + 768, [64], I32)
        Ri = AR.view("Ri", EO + 1024, [2, 16], I32)
        ebe = AR.view("ebe", EO + 1152, [64], F32)
        big = AR.view("big", OFF_X, [NBLK, 32], F32)
        big2 = AR.view("big2", OFF_X + 6400, [NBLK, 32], F32)
        P.op("dve", lambda e: e.memset(nblk[:], 0.0), writes=[nblk])
        for m in range(2 * TOK // 128 + 1):
            P.op("dve", lambda e, m=m: e.scalar_tensor_tensor(out=nblk[:], in0=carry[:], scalar=float(128 * m), in1=nblk[:], op0=ALU.is_gt, op1=ALU.add), reads=[carry, nblk], writes=[nblk])
        P.op("dve", lambda e: e.memset(pst[:, 0:1], 0.0), writes=[pst])
        for ei in range(1, 32):
            P.op("dve", lambda e, ei=ei: e.tensor_tensor(out=pst[:, ei:ei + 1], in0=pst[:, ei - 1:ei], in1=nblk[:, ei - 1:ei], op=ALU.add), reads=[pst, nblk], writes=[pst])
        P.op("dve", lambda e: e.tensor_tensor(out=pend[:], in0=pst[:], in1=nblk[:], op=ALU.add), reads=[pst, nblk], writes=[pend])
        P.op("dve", lambda e: e.tensor_scalar(out=prow[:], in0=pst[:], scalar1=128.0, scalar2=None, op0=ALU.mult), reads=[pst], writes=[prow])
        for t in range(NTILE):
            for k, OHs in ((0, OH1s), (1, OH2s)):
                P.op("dve", lambda e, OHs=OHs, t=t: e.tensor_tensor(out=rk[:, 32:64], in0=OHs[:, t, :], in1=prow[:], op=ALU.mult), reads=[OHs, prow], writes=[rk])
                P.op("dve", lambda e: e.tensor_reduce(out=rk[:, 0:1], in_=rk[:, 32:64], axis=mybir.AxisListType.X, op=ALU.add), reads=[rk], writes=[rk])
                P.op("dve", lambda e, t=t, k=k: e.tensor_tensor(out=R12[:, k, t:t + 1], in0=R12[:, k, t:t + 1], in1=rk[:, 0:1], op=ALU.add), reads=[R12, rk], writes=[R12])
        P.op("dve", lambda e: e.tensor_copy(out=Ri[:], in_=R12[:]), reads=[R12], writes=[Ri])
        bio = mconst[:, 1, 0:NBLK].unsqueeze(2).to_broadcast([128, NBLK, 32])
        P.op("dve", lambda e: e.tensor_tensor(out=big[:], in0=pst[:].unsqueeze(1).to_broadcast([128, NBLK, 32]), in1=bio, op=ALU.is_le), reads=[pst, mconst], writes=[big])
        P.op("dve", lambda e: e.tensor_tensor(out=big2[:], in0=pend[:].unsqueeze(1).to_broadcast([128, NBLK, 32]), in1=bio, op=ALU.is_gt), reads=[pend, mconst], writes=[big2])
        P.op("dve", lambda e: e.tensor_tensor(out=big[:], in0=big[:], in1=big2[:], op=ALU.mult), reads=[big, big2], writes=[big])
        P.op("dve", lambda e: e.tensor_reduce(out=ebf[:, 0:NBLK], in_=big[:], axis=mybir.AxisListType.X, op=ALU.add), reads=[big], writes=[ebf])
        P.op("dve", lambda e: e.tensor_tensor(out=big2[:], in0=big[:], in1=mconst[:, 1, 0:32].unsqueeze(1).to_broadcast([128, NBLK, 32]), op=ALU.mult), reads=[big, mconst], writes=[big2])
        P.op("dve", lambda e: e.tensor_reduce(out=ebe[:, 0:NBLK], in_=big2[:], axis=mybir.AxisListType.X, op=ALU.add), reads=[big2], writes=[ebe])
        idx8f = AR.view("idx8f", OFF_X, [NBLK, 8], F32)
        idx8 = AR.view("idx8", EO + 1408, [NBLK, 8], I32)
        base8 = AR.view("base8", EO + 768, [8], F32)
        P.op("dve", lambda e: e.tensor_scalar(out=ebf[:, 0:NBLK], in0=ebf[:, 0:NBLK], scalar1=-1.0, scalar2=-1.0e4, op0=ALU.add, op1=ALU.mult), reads=[ebf], writes=[ebf])
        P.op("dve", lambda e: e.tensor_tensor(out=ebf[:, 0:NBLK], in0=ebf[:, 0:NBLK], in1=ebe[:, 0:NBLK], op=ALU.add), reads=[ebf, ebe], writes=[ebf])
        P.op("dve", lambda e: e.tensor_scalar(out=ebf[:, 0:NBLK], in0=ebf[:, 0:NBLK], scalar1=1024.0, scalar2=None, op0=ALU.mult), reads=[ebf], writes=[ebf])
        P.op("dve", lambda e: e.scalar_tensor_tensor(out=base8[:], in0=mconst[:, 2, 0:8], scalar=8.0, in1=mconst[:, 1, 0:8], op0=ALU.mult, op1=ALU.add), reads=[mconst], writes=[base8])
        P.op("dve", lambda e: e.tensor_tensor(out=idx8f[:], in0=ebf[:, 0:NBLK].unsqueeze(2).to_broadcast([128, NBLK, 8]), in1=base8[:].unsqueeze(1).to_broadcast([128, NBLK, 8]), op=ALU.add),
             reads=[ebf, base8, big], writes=[idx8f])
        P.op("dve", lambda e: e.tensor_copy(out=idx8[:], in_=idx8f[:]), reads=[idx8f], writes=[idx8])
        P.barrier()
        WS = [Tile("WS%d" % i, AR.t[:, i * 32768:(i + 1) * 32768].bitcast(BF16)) for i in range(3)]
        wcnt = {"n": 0}

        def nextWS():
            wcnt["n"] += 1
            return WS[wcnt["n"] % 3]
        MO = 98304
        iob = [AR.view("iob%d" % i, MO + i * 512, [128], F32) for i in range(2)]
        selt = [AR.view("selt%d" % i, MO + 1024 + i * 256, [128], BF16) for i in range(2)]
        Sel = [AR.view("Sel%d" % i, MO + 1536 + i * 2304, [NTILE, 128], BF16) for i in range(2)]
        XbT = [AR.view("XbT%d" % i, MO + 6144 + i * 4096, [16, 128], BF16) for i in range(2)]
        sgm = [AR.view("sgm%d" % i, MO + 14336 + i * 2048, [512], F32) for i in range(2)]
        hperm = AR.view("hperm", MO + 18432, [1024], BF16)
        hTm = AR.view("hTm", MO + 20480, [8, 128], BF16)
        Yst = [AR.view("Yst%d" % i, OFF_X + i * 8192, [D], F32) for i in range(2)]
        yscr = P.dram("y_scr", [NBLK * 128, D], F32, "Internal")
        wg2 = w_gate[:].rearrange("e (k two) n -> (e k) (two n)", two=2)
        wu2 = w_up[:].rearrange("e (k two) n -> (e k) (two n)", two=2)
        wd2 = w_down[:].rearrange("e f n -> (e f) n")
        bound = n_experts * 1024 - 1

        regs = {}

        def breg(e, key, val):
            if key not in regs:
                regs[key] = e.to_reg(val)
            return regs[key]

        def wload(slot, src2, b):
            for j in range(8):
                P.dma("pool", lambda e, j=j: e.indirect_dma_start(out=slot[:, j * 2048:(j + 1) * 2048], out_offset=None, in_=src2, in_offset=bass.IndirectOffsetOnAxis(ap=idx8[:, b, j:j + 1], axis=0),
                                                             bounds_check=breg(e, "w", bound), oob_is_err=False), reads=[idx8], writes=[slot], semtile=slot)
        for b in range(NBLK):
            wg_ = nextWS(); wload(wg_, wg2, b)
            wu_ = nextWS(); wload(wu_, wu2, b)
            wd_ = nextWS(); wload(wd_, wd2, b)
            io_ = iob[b % 2]
            P.op("dve", lambda e, io_=io_, b=b: e.tensor_scalar(out=io_[:], in0=mconst[:, 1, :], scalar1=float(128 * b), scalar2=None, op0=ALU.add), reads=[mconst], writes=[io_])
            sel = Sel[b % 2]
            for t in range(NTILE):
                st_ = selt[t % 2]
                P.op("pool", lambda e, st_=st_, io_=io_, t=t: e.tensor_scalar(out=st_[:], in0=io_[:], scalar1=R12[:, 0, t:t + 1], scalar2=None, op0=ALU.is_equal), reads=[io_, R12], writes=[st_])
                P.op("dve", lambda e, st_=st_, io_=io_, t=t, sel=sel: e.scalar_tensor_tensor(out=sel[:, t, :], in0=io_[:], scalar=R12[:, 1, t:t + 1], in1=st_[:], op0=ALU.is_equal, op1=ALU.add),
                     reads=[io_, R12, st_], writes=[sel])
            xbt = XbT[b % 2]
            for g in range(4):
                pg_ = pM[g]
                for jj in range(4):
                    j = g * 4 + jj
                    for t in range(NTILE):
                        P.op("pe", lambda e, pg_=pg_, jj=jj, j=j, t=t, sel=sel: e.matmul(pg_[:, jj * 128:(jj + 1) * 128], lhsT=h2p[:, t, j * 128:(j + 1) * 128], rhs=sel[:, t, :], start=(t == 0), stop=(t == NTILE - 1)),
                             reads=[h2p, sel], writes=[pg_])
                if g % 2 == 0:
                    P.op("act", lambda e, pg_=pg_, g=g, xbt=xbt: e.activation(out=xbt[:, g * 4:(g + 1) * 4, :], in_=pg_[:].rearrange("p (a b) -> p a b", a=4), func=AF.Copy), reads=[pg_], writes=[xbt])
                else:
                    P.op("dve", lambda e, pg_=pg_, g=g, xbt=xbt: e.tensor_copy(out=xbt[:, g * 4:(g + 1) * 4, :], in_=pg_[:].rearrange("p (a b) -> p a b", a=4)), reads=[pg_], writes=[xbt])
            for hf in range(2):
                pgt, put = pS[0], pS[1]
                for (pp_, wsl) in ((pgt, wg_), (put, wu_)):
                    for j in range(16):
                        P.op("pe", lambda e, pp_=pp_, wsl=wsl, j=j, hf=hf, xbt=xbt: e.matmul(pp_[:, 0:512], lhsT=xbt[:, j, :], rhs=wsl[:].rearrange("p (j n) -> p j n", j=16)[:, j, hf * 512:(hf + 1) * 512], start=(j == 0), stop=(j == 15)),
                             reads=[xbt, wsl], writes=[pp_])
                sx = sgm[hf]
                P.op("act", lambda e, pgt=pgt, sx=sx: e.activation(out=sx[:], in_=pgt[:, 0:512], func=AF.Silu), reads=[pgt], writes=[sx])
                P.op("dve", lambda e, put=put, sx=sx, hf=hf: e.tensor_tensor(out=hperm[:].rearrange("t (j p) -> t p j", j=8)[:, hf * 64:(hf + 1) * 64, :],
                                                                          in0=put[:, 0:512].rearrange("t (p j) -> t p j", j=8), in1=sx[:].rearrange("t (p j) -> t p j", j=8), op=ALU.mult),
                     reads=[put, sx], writes=[hperm])
            ptm = pT[0]
            ptb = ptm[:].bitcast(BF16)
            for j in range(8):
                P.op("pe", lambda e, ptb=ptb, j=j: e.transpose(out=ptb[:, j * 128:(j + 1) * 128], in_=hperm[:, j * 128:(j + 1) * 128], identity=ident_b[:]), reads=[hperm, ident_b], writes=[ptm])
            P.op("act", lambda e, ptb=ptb: e.activation(out=hTm[:], in_=ptb.rearrange("p (a b) -> p a b", a=8), func=AF.Copy), reads=[ptm], writes=[hTm])
            yst = Yst[b % 2]
            for cb in range(4):
                pd = pT[1]
                for j in range(8):
                    P.op("pe", lambda e, pd=pd, j=j, cb=cb, wd_=wd_: e.matmul(pd[:, 0:512], lhsT=hTm[:, j, :], rhs=wd_[:].rearrange("p (j n) -> p j n", j=8)[:, j, cb * 512:(cb + 1) * 512], start=(j == 0), stop=(j == 7)),
                         reads=[hTm, wd_], writes=[pd])
                if cb % 2 == 0:
                    P.op("act", lambda e, pd=pd, cb=cb, yst=yst: e.activation(out=yst[:, cb * 512:(cb + 1) * 512], in_=pd[:, 0:512], func=AF.Copy), reads=[pd], writes=[yst])
                else:
                    P.op("dve", lambda e, pd=pd, cb=cb, yst=yst: e.tensor_copy(out=yst[:, cb * 512:(cb + 1) * 512], in_=pd[:, 0:512]), reads=[pd], writes=[yst])
            P.dma("sp", lambda e, yst=yst, b=b: e.dma_start(out=yscr[b * 128:(b + 1) * 128, :], in_=yst[:]), reads=[yst], writes=[yscr], semtile=yst)
        P.barrier()
        nf = AR.view("nf", 0, [D], F32)
        junk3 = AR.view("junk3", 8192, [2048], BF16)
        xc = [AR.view("xc%d" % i, 16384 + i * 8192, [D], F32) for i in range(2)]
        y1 = [AR.view("y1_%d" % i, 32768 + i * 8192, [D], F32) for i in range(2)]
        y2 = [AR.view("y2_%d" % i, 49152 + i * 8192, [D], F32) for i in range(2)]
        P.dma("sp", lambda e: e.dma_start(out=nf[:], in_=norm_f[:].partition_broadcast(128)), writes=[nf], semtile=nf)
        for t in range(NTILE):
            xx, ya, yb_ = xc[t % 2], y1[t % 2], y2[t % 2]
            P.dma("sp", lambda e, xx=xx, t=t: e.dma_start(out=xx[:], in_=x1scr[t * 128:(t + 1) * 128, :]), reads=[x1scr], writes=[xx], semtile=xx)
            for k, yy in ((0, ya), (1, yb_)):
                P.dma("pool", lambda e, yy=yy, k=k, t=t: e.indirect_dma_start(out=yy[:], out_offset=None, in_=yscr[:], in_offset=bass.IndirectOffsetOnAxis(ap=Ri[:, k, t:t + 1], axis=0),
                                                                       bounds_check=breg(e, "y", NBLK * 128 - 1), oob_is_err=False), reads=[Ri, yscr], writes=[yy], semtile=yy)
            P.op("dve", lambda e, xx=xx, ya=ya, t=t: e.scalar_tensor_tensor(out=xx[:], in0=ya[:], scalar=G12[:, 0, t:t + 1], in1=xx[:], op0=ALU.mult, op1=ALU.add), reads=[ya, G12, xx], writes=[xx])
            P.op("dve", lambda e, xx=xx, yb_=yb_, t=t: e.scalar_tensor_tensor(out=xx[:], in0=yb_[:], scalar=G12[:, 1, t:t + 1], in1=xx[:], op0=ALU.mult, op1=ALU.add), reads=[yb_, G12, xx], writes=[xx])
            P.op("act", lambda e, xx=xx: e.activation(out=junk3[:], in_=xx[:], func=AF.Square, accum_out=small[:, 0:1]), reads=[xx], writes=[junk3, small])
            P.op("act", lambda e: e.activation(out=small[:, 1:2], in_=small[:, 0:1], func=AF.Sqrt, bias=epsb[:], scale=1.0 / D), reads=[small, epsb], writes=[small])
            P.op("dve", lambda e: e.reciprocal(out=small[:, 2:3], in_=small[:, 1:2]), reads=[small], writes=[small])
            P.op("dve", lambda e, xx=xx: e.scalar_tensor_tensor(out=xx[:], in0=xx[:], scalar=small[:, 2:3], in1=nf[:], op0=ALU.mult, op1=ALU.mult), reads=[xx, small, nf], writes=[xx])
            P.dma("sp", lambda e, xx=xx, t=t: e.dma_start(out=y_out[t * 128:(t + 1) * 128, :], in_=xx[:]), reads=[xx], semtile=xx, is_output=True)

    P.emit()
    return nc, P


def _tables(half):
    pos = np.concatenate([half * 1024 + np.arange(1024), np.tile(PAST_LEN + np.arange(8), 16)]).astype(np.float32)
    ppos = np.arange(1024).astype(np.float32)
    inv = (np.float32(10000.0) ** (-np.arange(128, dtype=np.float32) / np.float32(128))).astype(np.float32)

    def cs(p):
        ang = (p[None, :] * inv[:, None]).astype(np.float32)
        return np.stack([np.cos(ang.astype(np.float64)), np.sin(ang.astype(np.float64))], axis=1).astype(np.float32)

    dec = np.zeros((H, 128, 4, 128), np.float32)
    i = np.arange(128, dtype=np.float64)
    i8 = (np.arange(128) % 8).astype(np.float64)
    for h in range(H):
        lg = LOG_G[h]
        dec[h, :, 0, :] = np.exp((i + 1) * lg)[None, :]
        dec[h, :, 1, :] = (np.exp(-(i + 1) * lg) * DK ** -0.5)[None, :]
        dec[h, :, 2, :] = np.exp((i8 + 1) * lg)[None, :]
        dec[h, :, 3, :] = (np.exp(-(i8 + 1) * lg) * DK ** -0.5)[None, :]
    j = np.arange(128)
    mp = (j[:, None] <= j[None, :]).astype(np.float32)
    ms = ((j[:, None] <= j[None, :]) & (j[:, None] // 8 == j[None, :] // 8)).astype(np.float32)
    masks = np.stack([mp, ms], axis=1)
    rowmask = (j[:, None] // 8 == np.arange(16)[None, :]).astype(np.float32)
    mconst = np.zeros((128, 3, 128), np.float32)
    mconst[:, 0, :] = (j[:, None] < j[None, :])
    mconst[:, 1, :] = j[None, :]
    mconst[:, 2, :] = j[:, None]
    return dict(cs_own=cs(pos), cs_prev=cs(ppos), dec=dec, masks=np.ascontiguousarray(masks),
                ident=np.eye(128, dtype=np.float32), rowmask=rowmask, mconst=mconst)


def make_in_maps(inp, n_experts=NE, cores=range(8)):
    f = lambda a: np.ascontiguousarray(np.asarray(a, dtype=np.float32))
    xp = f(inp["x_prompt"])
    xs = f(inp["x_sample"])
    sr = f(inp["state_ret"])[0]
    sc = f(inp["state_conv"])[0]

    def kc_layout(v):
        return f(v).reshape(16, 128).T

    vecs = np.stack([kc_layout(inp["norm_mix"][0]), kc_layout(inp["norm_ffn"][0]), kc_layout(inp["conv_b"][0]),
                     kc_layout(inp["conv_ln_w"][0]), kc_layout(inp["conv_ln_b"][0]), np.zeros((128, 16), np.float32)], axis=1)
    shared = dict(
        w_in=f(inp["w_in"])[0], w_ret_o=f(inp["w_ret_o"])[0], conv_w=f(inp["conv_w"])[0], vecs=np.ascontiguousarray(vecs),
        norm_f=f(inp["norm_f"]).reshape(1, D), w_conv_o=f(inp["w_conv_o"])[0], w_o=f(inp["w_o"])[0],
        w_rt=np.ascontiguousarray(np.concatenate([f(inp["w_coarse"])[0], f(inp["w_fine"])[0]], axis=1)),
        b_rt=np.concatenate([f(inp["b_coarse"])[0], f(inp["b_fine"])[0]]).reshape(1, 36),
        w_gate=f(inp["w_gate"])[0][:n_experts], w_up=f(inp["w_up"])[0][:n_experts], w_down=f(inp["w_down"])[0][:n_experts],
    )
    tabs = [_tables(0), _tables(1)]
    maps = []
    for c in cores:
        b, half = c // 2, c % 2
        m = dict(shared)
        m.update(tabs[half])
        m["x_own"] = np.ascontiguousarray(np.concatenate([xp[b, half * 1024:(half + 1) * 1024], xs[16 * c:16 * (c + 1)].reshape(128, D)], axis=0))
        m["x_prev"] = np.ascontiguousarray(xp[b, 0:1024]) if half == 1 else np.zeros((1024, D), np.float32)
        m["sret"] = np.ascontiguousarray(sr[16 * c:16 * (c + 1)])
        m["sconv"] = np.ascontiguousarray(sc[16 * c:16 * (c + 1)].reshape(16 * 30, D))
        maps.append(m)
    return maps


_CACHE = {}


def kernel(**inputs):
    if "nc" not in _CACHE:
        _CACHE["nc"] = build()[0]
    nc = _CACHE["nc"]
    maps = make_in_maps(inputs)
    res = run_bass_kernel_spmd(nc, maps, core_ids=list(range(8)))
    r = res.results
    yp = np.zeros((4, 2048, D), np.float32)
    ys = np.zeros((128, 8, D), np.float32)
    retp = np.zeros((1, 4, H, DK, DV), np.float32)
    convp = np.zeros((1, 4, 30, D), np.float32)
    rets = np.zeros((1, 128, H, DK, DV), np.float32)
    convs = np.zeros((1, 128, 30, D), np.float32)
    for c in range(8):
        b, half = c // 2, c % 2
        yp[b, half * 1024:(half + 1) * 1024] = r[c]["y_out"][:1024]
        ys[16 * c:16 * (c + 1)] = r[c]["y_out"][1024:].reshape(16, 8, D)
        rets[0, 16 * c:16 * (c + 1)] = r[c]["rets_out"]
        convs[0, 16 * c:16 * (c + 1)] = r[c]["convs_out"]
        if half == 1:
            retp[0, b] = r[c]["retp_out"]
            convp[0, b] = r[c]["convp_out"]
    return (yp, ys, retp, convp, rets, convs)
```

```python
import contextlib
import numpy as np
import concourse.bass as bass
import concourse.mybir as mybir
from concourse.bass_utils import run_bass_kernel_spmd

F32, BF16, I32, U8 = mybir.dt.float32, mybir.dt.bfloat16, mybir.dt.int32, mybir.dt.uint8
F32R = mybir.dt.float32r
ALU = mybir.AluOpType
AF = mybir.ActivationFunctionType

ENGS = ("pe", "act", "dve", "pool", "sp")
DSIZE = {F32: 4, BF16: 2, I32: 4, U8: 1}

D = 2048
H = 8
DK = 256
DV = 512
NTILE = 9
TOK = 1152
PAST_LEN = 16384
CW = 31
NE = 32
DE = 1024
EPS = 1e-6
Q0, K0, V0, G0, CA0, CB0, GR0, GC0 = 0, 2048, 4096, 8192, 12288, 14336, 16384, 18432
IN_COLS = 20480
BLKS = ((0, 512), (512, 1024), (1024, 1152))
LOG_G = [float(np.log1p(-2.0 ** (-5.0 - h))) for h in range(H)]
CDEC_P = [float(np.exp(128 * LOG_G[h])) for h in range(H)]
CDEC_S = [float(np.exp(8 * LOG_G[h])) for h in range(H)]


class Tile:
    def __init__(self, name, h):
        self.name, self.h = name, h
        self.w = None
        self.r = []
        self.sem = None

    def __getitem__(self, idx):
        return self.h[idx]


class Op:
    __slots__ = ("eng", "fn", "deps", "is_dma", "sem", "count", "need_inc", "val")

    def __init__(self, eng, fn, is_dma=False):
        self.eng, self.fn, self.is_dma = eng, fn, is_dma
        self.deps = []
        self.sem = None
        self.count = 0
        self.need_inc = False
        self.val = 0


class Prog:
    def __init__(self, nc):
        self.nc = nc
        self.ops = {e: [] for e in ENGS}
        self.dma_counts = {}
        self.last_dma = {}
        self.stack = contextlib.ExitStack()
        self.out_ops = []
        self.pending = {e: [] for e in ENGS}

    def sb(self, name, shape, dt):
        h = self.stack.enter_context(self.nc.sbuf_tensor(name, list(shape), dt))
        return Tile(name, h)

    def ps(self, name, shape, dt=F32):
        h = self.stack.enter_context(self.nc.psum_tensor(name, list(shape), dt))
        return Tile(name, h)

    def dram(self, name, shape, dt, kind):
        h = self.nc.dram_tensor(name, list(shape), dt, kind=kind)
        return Tile(name, h.ap())

    def _deps(self, o, reads, writes):
        deps = []
        for t in reads:
            if t.w is not None:
                deps.append((t.w, "raw"))
        for t in writes:
            if t.w is not None:
                deps.append((t.w, "waw"))
            for r in t.r:
                deps.append((r, "war"))
        seen = set()
        for d, kind in deps:
            if d is o or id(d) in seen:
                continue
            if (not d.is_dma) and d.eng == o.eng and not o.is_dma:
                if kind != "raw" or o.eng == "pe":
                    continue
            seen.add(id(d))
            o.deps.append(d)
        for d in self.pending[o.eng]:
            if id(d) not in seen and d is not o:
                seen.add(id(d))
                o.deps.append(d)
        self.pending[o.eng] = []
        for t in reads:
            t.r.append(o)
        for t in writes:
            t.w = o
            t.r = []

    def op(self, eng, fn, reads=(), writes=()):
        o = Op(eng, fn)
        self._deps(o, reads, writes)
        self.ops[eng].append(o)
        return o

    def dma(self, eng, fn, reads=(), writes=(), semtile=None, is_output=False):
        o = Op(eng, fn, is_dma=True)
        if semtile.sem is None:
            semtile.sem = "ds_%s" % semtile.name
        o.sem = semtile.sem
        self.dma_counts[o.sem] = self.dma_counts.get(o.sem, 0) + 16
        o.count = self.dma_counts[o.sem]
        self.last_dma[o.sem] = o
        self._deps(o, reads, writes)
        self.ops[eng].append(o)
        if is_output:
            self.out_ops.append(o)
        return o

    def barrier(self):
        deps = []
        for e in ENGS:
            for o in reversed(self.ops[e]):
                if not o.is_dma:
                    deps.append(o)
                    break
        deps += list(self.last_dma.values())
        for e in ENGS:
            self.pending[e] = self.pending[e] + [d for d in deps if d.is_dma or d.eng != e]

    def emit(self):
        nc = self.nc
        for e in ENGS:
            for o in self.ops[e]:
                for d in o.deps:
                    if not d.is_dma:
                        d.need_inc = True
        for e in ENGS:
            c = 0
            for o in self.ops[e]:
                if (not o.is_dma) and o.need_inc:
                    c += 1
                    o.val = c
        sems = {}
        for e in ENGS:
            sems["eng_" + e] = self.stack.enter_context(nc.semaphore("eng_" + e))
        for s in self.dma_counts:
            sems[s] = self.stack.enter_context(nc.semaphore(s))
        self.nsems = len(sems)
        final = {}
        for o in self.out_ops:
            final[o.sem] = max(final.get(o.sem, 0), o.count)

        def emit_eng(ename, eng):
            waited = {}
            for o in self.ops[ename]:
                for d in o.deps:
                    if d.is_dma:
                        s, v = d.sem, d.count
                    else:
                        s, v = "eng_" + d.eng, d.val
                    if waited.get(s, 0) >= v:
                        continue
                    waited[s] = v
                    eng.wait_ge(sems[s], v)
                ins = o.fn(eng)
                if o.is_dma:
                    ins.then_inc(sems[o.sem], 16)
                elif o.need_inc:
                    ins.then_inc(sems["eng_" + ename], 1)
            if ename == "sp":
                for s, v in final.items():
                    if waited.get(s, 0) < v:
                        eng.wait_ge(sems[s], v)

        with nc.Block() as block:
            @block.sync
            def _(e):
                emit_eng("sp", e)

            @block.scalar
            def _(e):
                emit_eng("act", e)

            @block.vector
            def _(e):
                emit_eng("dve", e)

            @block.gpsimd
            def _(e):
                emit_eng("pool", e)

            @block.tensor
            def _(e):
                emit_eng("pe", e)
        self.stack.close()


class Arena:
    def __init__(self, P, nbytes):
        self.P = P
        self.t = P.stack.enter_context(P.nc.sbuf_tensor("arena", [128, nbytes], U8))
        self.nbytes = nbytes

    def view(self, name, off, shape, dt):
        n = int(np.prod(shape)) * DSIZE[dt]
        assert off + n <= self.nbytes, (name, off, n)
        assert off % 4 == 0
        ap = self.t[:, off:off + n].bitcast(dt)
        if len(shape) == 2:
            ap = ap.rearrange("p (a b) -> p a b", a=shape[0])
        elif len(shape) == 3:
            ap = ap.rearrange("p (a b c) -> p a b c", a=shape[0], b=shape[1])
        return Tile(name, ap)


def build(n_experts=NE, stages="ARCMOE", debug=False):
    nc = bass.Bass("TRN2", target_bir_lowering=False)
    P = Prog(nc)
    IN, OUT = "ExternalInput", "ExternalOutput"
    x_own = P.dram("x_own", [TOK, D], F32, IN)
    x_prev = P.dram("x_prev", [1024, D], F32, IN)
    sret = P.dram("sret", [16, H, DK, DV], F32, IN)
    sconv = P.dram("sconv", [16 * 30, D], F32, IN)
    w_in = P.dram("w_in", [D, IN_COLS], F32, IN)
    w_ret_o = P.dram("w_ret_o", [H * DV, D], F32, IN)
    conv_w = P.dram("conv_w", [CW, D], F32, IN)
    vecs = P.dram("vecs", [128, 6, 16], F32, IN)
    norm_f = P.dram("norm_f", [1, D], F32, IN)
    w_conv_o = P.dram("w_conv_o", [D, D], F32, IN)
    w_o = P.dram("w_o", [D, D], F32, IN)
    w_rt = P.dram("w_rt", [D, 36], F32, IN)
    b_rt = P.dram("b_rt", [1, 36], F32, IN)
    w_gate = P.dram("w_gate", [n_experts, D, DE], F32, IN)
    w_up = P.dram("w_up", [n_experts, D, DE], F32, IN)
    w_down = P.dram("w_down", [n_experts, DE, D], F32, IN)
    cs_own_d = P.dram("cs_own", [128, 2, TOK], F32, IN)
    cs_prev_d = P.dram("cs_prev", [128, 2, 1024], F32, IN)
    dec_d = P.dram("dec", [H, 128, 4, 128], F32, IN)
    masks_d = P.dram("masks", [128, 2, 128], F32, IN)
    ident_d = P.dram("ident", [128, 128], F32, IN)
    rowmask_d = P.dram("rowmask", [128, 16], F32, IN)
    mconst_d = P.dram("mconst", [128, 3, 128], F32, IN)

    y_out = P.dram("y_out", [TOK, D], F32, OUT)
    retp_out = P.dram("retp_out", [H, DK, DV], F32, OUT)
    convp_out = P.dram("convp_out", [30, D], F32, OUT)
    rets_out = P.dram("rets_out", [16, H, DK, DV], F32, OUT)
    convs_out = P.dram("convs_out", [16, 30, D], F32, OUT)
    sinit = P.dram("sinit_scr", [H, DK, DV], F32, "Internal")
    ospill = P.dram("ospill_scr", [TOK, H * DV], BF16, OUT if debug else "Internal")

    ident_f = P.sb("ident_f", [128, 128], F32)
    ident_b = P.sb("ident_b", [128, 128], BF16)
    rowmask = P.sb("rowmask_s", [128, 16], F32)
    vec_s = P.sb("vec_s", [128, 6, 16], F32)
    hTh = P.sb("hTh", [128, 16, 128], BF16)
    small = P.sb("small", [128, 16], F32)
    epsb = P.sb("epsb", [128, 1], F32)

    AR = Arena(P, 184 * 1024)
    W = [AR.view("W%d" % i, i * 16384, [16, 512], BF16) for i in range(3)]
    OFF_HT = 49152
    OFF_HEAD = OFF_HT + 36864
    OFF_ROT = OFF_HEAD + 32256
    OFF_S = OFF_ROT + 8192
    OFF_S0 = OFF_S + 6144
    OFF_QK = OFF_S0 + 20480
    OFF_OF = OFF_QK + 3072 + 2048 + 4096
    OFF_END = OFF_OF + 2048
    cs_own = AR.view("cs_own_s", OFF_END, [2, TOK], F32)
    dec_h = AR.view("dec_h", OFF_END + 9216, [4, 128], F32)
    masks = AR.view("masks_s", OFF_END + 11264, [2, 128], F32)
    junk = AR.view("junk", OFF_END + 12800, [2048], BF16)

    pT = [P.ps("pT%d" % i, [128, 512]) for i in range(2)]
    pM = [P.ps("pM%d" % i, [128, 512]) for i in range(4)]
    pS = [P.ps("pS%d" % i, [128, 512]) for i in range(2)]
    cnt = {"m": 0, "t": 0, "s": 0, "w": 0}

    def nextM():
        cnt["m"] += 1
        return pM[cnt["m"] % 4]

    def nextT():
        cnt["t"] += 1
        return pT[cnt["t"] % 2]

    def nextS():
        cnt["s"] += 1
        return pS[cnt["s"] % 2]

    def nextW():
        cnt["w"] += 1
        return W[cnt["w"] % 3]

    def ld(dst, src_ap, eng="sp"):
        P.dma(eng, lambda e: e.dma_start(out=dst[:], in_=src_ap), writes=[dst], semtile=dst)

    ld(ident_f, ident_d[:])
    ld(cs_own, cs_own_d[:])
    ld(masks, masks_d[:])
    ld(rowmask, rowmask_d[:])
    ld(vec_s, vecs[:])
    P.op("dve", lambda e: e.tensor_copy(out=ident_b[:], in_=ident_f[:]), reads=[ident_f], writes=[ident_b])
    P.op("dve", lambda e: e.memset(epsb[:], EPS), writes=[epsb])

    def load_w(slot, src2d, c0=0):
        kc = src2d.shape[0] // 128
        ncols = src2d.shape[1]
        src = src2d.rearrange("(kc p) n -> p kc n", p=128)
        P.dma("pool", lambda e: e.dma_start(out=slot[:, 0:kc, c0:c0 + ncols], in_=src), writes=[slot], semtile=slot)

    def norm_to_T(x_dram, n_tiles, dstT, wrow, xin, xb):
        for t in range(n_tiles):
            xi = xin[t % 2]
            P.dma("sp", lambda e, xi=xi, t=t: e.dma_start(out=xi[:], in_=x_dram[t * 128:(t + 1) * 128, :]), writes=[xi], semtile=xi)
            ss = small
            P.op("act", lambda e, xi=xi: e.activation(out=junk[:], in_=xi[:], func=AF.Square, accum_out=small[:, 0:1]),
                 reads=[xi], writes=[junk, small])
            P.op("act", lambda e: e.activation(out=small[:, 1:2], in_=small[:, 0:1], func=AF.Sqrt, bias=epsb[:], scale=1.0 / D),
                 reads=[small, epsb], writes=[small])
            P.op("dve", lambda e: e.reciprocal(out=small[:, 2:3], in_=small[:, 1:2]), reads=[small], writes=[small])
            P.op("dve", lambda e, xi=xi: e.tensor_scalar(out=xb[:], in0=xi[:], scalar1=small[:, 2:3], scalar2=None, op0=ALU.mult),
                 reads=[xi, small], writes=[xb])
            for g in range(2):
                pt = nextT()
                ptb = pt[:].bitcast(BF16)
                for j in range(8):
                    kc = g * 8 + j
                    P.op("pe", lambda e, ptb=ptb, j=j, kc=kc: e.transpose(out=ptb[:, j * 128:(j + 1) * 128], in_=xb[:, kc * 128:(kc + 1) * 128], identity=ident_b[:]),
                         reads=[xb, ident_b], writes=[pt])
                P.op("dve", lambda e, ptb=ptb, g=g, t=t: e.tensor_tensor(
                    out=dstT[:, g * 8:(g + 1) * 8, t * 128:(t + 1) * 128],
                    in0=ptb.rearrange("p (a b) -> p a b", a=8),
                    in1=vec_s[:, wrow, g * 8:(g + 1) * 8].unsqueeze(2).to_broadcast([128, 8, 128]), op=ALU.mult),
                    reads=[pt, vec_s], writes=[dstT])

    def fm_proj(slot, c0, srcT, blks, evac):
        for bi, (t0, t1) in enumerate(blks):
            ps = nextM()
            for kc in range(16):
                P.op("pe", lambda e, ps=ps, kc=kc, t0=t0, t1=t1: e.matmul(ps[:, 0:t1 - t0], lhsT=slot[:, kc, c0:c0 + 128], rhs=srcT[:, kc, t0:t1], start=(kc == 0), stop=(kc == 15)),
                     reads=[slot, srcT], writes=[ps])
            evac(ps, bi, (t0, t1))

    def tm_proj(slot, ncols, srcT, tile, evac):
        ps = nextM()
        for kc in range(16):
            P.op("pe", lambda e, ps=ps, kc=kc: e.matmul(ps[:, 0:ncols], lhsT=srcT[:, kc, tile * 128:(tile + 1) * 128], rhs=slot[:, kc, 0:ncols], start=(kc == 0), stop=(kc == 15)),
                 reads=[slot, srcT], writes=[ps])
        evac(ps)

    def rotary_pair(p1, p2, n, cs, t0, decsel, out_fn, rot):
        cos = cs[:, 0, t0:t0 + n]
        sin = cs[:, 1, t0:t0 + n]
        ta, tb, tc, td = rot
        P.op("dve", lambda e: e.tensor_tensor(out=ta[:, 0:n], in0=p1[:, 0:n], in1=cos, op=ALU.mult), reads=[p1, cs], writes=[ta])
        P.op("dve", lambda e: e.tensor_tensor(out=tb[:, 0:n], in0=p2[:, 0:n], in1=sin, op=ALU.mult), reads=[p2, cs], writes=[tb])
        P.op("dve", lambda e: e.tensor_tensor(out=tc[:, 0:n], in0=p1[:, 0:n], in1=sin, op=ALU.mult), reads=[p1, cs], writes=[tc])
        P.op("dve", lambda e: e.tensor_tensor(out=td[:, 0:n], in0=p2[:, 0:n], in1=cos, op=ALU.mult), reads=[p2, cs], writes=[td])
        P.op("pool", lambda e: e.tensor_tensor(out=ta[:, 0:n], in0=ta[:, 0:n], in1=tb[:, 0:n], op=ALU.subtract), reads=[ta, tb], writes=[ta])
        P.op("pool", lambda e: e.tensor_tensor(out=tc[:, 0:n], in0=tc[:, 0:n], in1=td[:, 0:n], op=ALU.add), reads=[tc, td], writes=[tc])
        out_fn(0, ta)
        out_fn(1, tc)

    rot = [AR.view("rot%d" % i, OFF_ROT + i * 2048, [512], F32) for i in range(4)]
    xin = [AR.view("xin%d" % i, OFF_HEAD + i * 8192, [2048], F32) for i in range(2)]
    xb = AR.view("xb", OFF_HEAD + 16384, [2048], BF16)

    def dec_load(h):
        P.dma("sp", lambda e: e.dma_start(out=dec_h[:], in_=dec_d[h]), writes=[dec_h], semtile=dec_h)

    if "A" in stages:
        hTp = AR.view("hTp", OFF_HT, [16, 1024], BF16)
        cs_prev = AR.view("cs_prev", OFF_S0, [2, 1024], F32)
        ld(cs_prev, cs_prev_d[:])
        norm_to_T(x_prev, 8, hTp, 0, xin, xb)
        P.op("pool", lambda e: e.tensor_copy(out=hTh[:], in_=hTp[:, :, 896:1024]), reads=[hTp], writes=[hTh])
        P.barrier()
        kTa = AR.view("kTa", OFF_HEAD, [2, 1024], BF16)
        ktokA = AR.view("ktokA", OFF_HEAD + 4096, [8, 256], BF16)
        vA = AR.view("vA", OFF_HEAD + 8192, [8, 512], BF16)
        sstage = AR.view("sstageA", OFF_S, [512], F32)
        for h in range(H):
            dec_load(h)
            wk = nextW()
            load_w(wk, w_in[:, K0 + h * 256:K0 + (h + 1) * 256])
            wv = nextW()
            load_w(wv, w_in[:, V0 + h * 512:V0 + (h + 1) * 512])
            for bi, (t0, t1) in enumerate(((0, 512), (512, 1024))):
                pp = []
                for dc in range(2):
                    ps = nextM()
                    for kc in range(16):
                        P.op("pe", lambda e, ps=ps, kc=kc, dc=dc, t0=t0, t1=t1, wk=wk: e.matmul(ps[:, 0:512], lhsT=wk[:, kc, dc * 128:(dc + 1) * 128], rhs=hTp[:, kc, t0:t1], start=(kc == 0), stop=(kc == 15)),
                             reads=[wk, hTp], writes=[ps])
                    pp.append(ps)

                def outk(which, src, t0=t0):
                    P.op("dve", lambda e: e.tensor_tensor(out=kTa[:, which, t0:t0 + 512].rearrange("p (a b) -> p a b", a=4),
                                                          in0=src[:, 0:512].rearrange("p (a b) -> p a b", a=4),
                                                          in1=dec_h[:, 1, :].unsqueeze(1).to_broadcast([128, 4, 128]), op=ALU.mult),
                         reads=[src, dec_h], writes=[kTa])
                rotary_pair(pp[0], pp[1], 512, cs_prev, t0, None, outk, rot)
            for n in range(8):
                pt = nextT()
                ptb = pt[:].bitcast(BF16)
                for dc in range(2):
                    P.op("pe", lambda e, ptb=ptb, dc=dc, n=n: e.transpose(out=ptb[:, dc * 128:(dc + 1) * 128], in_=kTa[:, dc, n * 128:(n + 1) * 128], identity=ident_b[:]),
                         reads=[kTa, ident_b], writes=[pt])
                sc = CDEC_P[h] * float(np.exp(128 * (7 - n) * LOG_G[h]))
                P.op("act", lambda e, ptb=ptb, n=n, sc=sc: e.activation(out=ktokA[:, n, :], in_=ptb[:, 0:256], func=AF.Copy, scale=sc),
                     reads=[pt], writes=[ktokA])
                tm_proj(wv, 512, hTp, n, lambda ps, n=n: P.op("act", lambda e: e.activation(out=vA[:, n, :], in_=ps[:, 0:512], func=AF.Copy), reads=[ps], writes=[vA]))
            for dc in range(2):
                ps = nextS()
                for n in range(8):
                    P.op("pe", lambda e, ps=ps, n=n, dc=dc: e.matmul(ps[:, 0:512], lhsT=ktokA[:, n, dc * 128:(dc + 1) * 128], rhs=vA[:, n, :], start=(n == 0), stop=(n == 7)),
                         reads=[ktokA, vA], writes=[ps])
                P.op("act", lambda e, ps=ps: e.activation(out=sstage[:], in_=ps[:, 0:512], func=AF.Copy), reads=[ps], writes=[sstage])
                P.dma("sp", lambda e, dc=dc, h=h: e.dma_start(out=sinit[h, dc * 128:(dc + 1) * 128, :], in_=sstage[:]), reads=[sstage], writes=[sinit], semtile=sstage)
        P.barrier()

    hT = AR.view("hT", OFF_HT, [16, TOK], BF16)
    if "R" in stages:
        norm_to_T(x_own, NTILE, hT, 0, xin, xb)
        P.barrier()
        qT = AR.view("qT", OFF_HEAD, [2, TOK], BF16)
        kT = AR.view("kT", OFF_HEAD + 4608, [2, TOK], BF16)
        ktok = AR.view("ktok", OFF_HEAD + 9216, [NTILE, 256], BF16)
        vv = AR.view("vv", OFF_HEAD + 13824, [NTILE, 512], BF16)
        sg = AR.view("sg", OFF_HEAD + 23040, [NTILE, 512], BF16)
        Sf = AR.view("Sf", OFF_S, [2, 512], F32)
        Sb = AR.view("Sb", OFF_S + 4096, [2, 512], BF16)
        S0 = [AR.view("S0_%d" % i, OFF_S0 + i * 4096, [2, 512], F32) for i in range(3)]
        So = [AR.view("So_%d" % i, OFF_S0 + 12288 + i * 4096, [2, 512], F32) for i in range(2)]
        S0b = [AR.view("S0b%d" % i, OFF_QK + 3072 + 2048 + i * 2048, [2, 512], BF16) for i in range(2)]
        Qz = [AR.view("Qz%d" % i, OFF_QK + i * 512, [2, 128], BF16) for i in range(2)]
        Kz = [AR.view("Kz%d" % i, OFF_QK + 2048 + i * 512, [256], BF16) for i in range(2)]
        ofs = [AR.view("of%d" % i, OFF_OF + i * 1024, [512], BF16) for i in range(2)]
        attm = [AR.view("attm%d" % i, OFF_END + 12288 + i * 256, [128], BF16) for i in range(2)]
        for h in range(H):
            dec_load(h)
            wqk = nextW()
            load_w(wqk, w_in[:, Q0 + h * 256:Q0 + (h + 1) * 256], 0)
            load_w(wqk, w_in[:, K0 + h * 256:K0 + (h + 1) * 256], 256)
            wv = nextW()
            load_w(wv, w_in[:, V0 + h * 512:V0 + (h + 1) * 512])
            wg = nextW()
            load_w(wg, w_in[:, G0 + h * 512:G0 + (h + 1) * 512])
            if "A" in stages:
                P.dma("sp", lambda e, h=h: e.dma_start(out=Sf[:], in_=sinit[h].rearrange("(dc p) e -> p dc e", p=128)), reads=[sinit], writes=[Sf], semtile=Sf)
            else:
                P.op("dve", lambda e: e.memset(Sf[:], 0.0), writes=[Sf])
            P.op("act", lambda e: e.activation(out=Sb[:], in_=Sf[:], func=AF.Copy), reads=[Sf], writes=[Sb])
            for which, dstT in ((0, qT), (1, kT)):
                for bi, (t0, t1) in enumerate(BLKS):
                    n = t1 - t0
                    pp = []
                    for dc in range(2):
                        ps = nextM()
                        c0 = which * 256 + dc * 128
                        for kc in range(16):
                            P.op("pe", lambda e, ps=ps, kc=kc, c0=c0, t0=t0, t1=t1, wqk=wqk: e.matmul(ps[:, 0:t1 - t0], lhsT=wqk[:, kc, c0:c0 + 128], rhs=hT[:, kc, t0:t1], start=(kc == 0), stop=(kc == 15)),
                                 reads=[wqk, hT], writes=[ps])
                        pp.append(ps)
                    drow = which + (2 if bi == 2 else 0)

                    def outqk(dcw, src, t0=t0, n=n, drow=drow, dstT=dstT, bi=bi, which=which):
                        a = n // 128
                        P.op("dve", lambda e: e.tensor_tensor(out=dstT[:, dcw, t0:t0 + n].rearrange("p (a b) -> p a b", a=a),
                                                              in0=src[:, 0:n].rearrange("p (a b) -> p a b", a=a),
                                                              in1=dec_h[:, drow, :].unsqueeze(1).to_broadcast([128, a, 128]), op=ALU.mult),
                             reads=[src, dec_h], writes=[dstT])
                    rotary_pair(pp[0], pp[1], n, cs_own, t0, None, outqk, rot)
            for t in range(NTILE):
                pt = nextT()
                ptb = pt[:].bitcast(BF16)
                for dc in range(2):
                    P.op("pe", lambda e, ptb=ptb, dc=dc, t=t: e.transpose(out=ptb[:, dc * 128:(dc + 1) * 128], in_=kT[:, dc, t * 128:(t + 1) * 128], identity=ident_b[:]),
                         reads=[kT, ident_b], writes=[pt])
                sc = CDEC_P[h] if t < 8 else CDEC_S[h]
                P.op("act", lambda e, ptb=ptb, t=t, sc=sc: e.activation(out=ktok[:, t, :], in_=ptb[:, 0:256], func=AF.Copy, scale=sc),
                     reads=[pt], writes=[ktok])
                tm_proj(wv, 512, hT, t, lambda ps, t=t: P.op("act", lambda e: e.activation(out=vv[:, t, :], in_=ps[:, 0:512], func=AF.Copy), reads=[ps], writes=[vv]))
                tm_proj(wg, 512, hT, t, lambda ps, t=t: P.op("act", lambda e: e.activation(out=sg[:, t, :], in_=ps[:, 0:512], func=AF.Silu), reads=[ps], writes=[sg]))
            for t in range(NTILE):
                tc0, tc1 = t * 128, (t + 1) * 128
                pa = nextM()
                for dc in range(2):
                    P.op("pe", lambda e, pa=pa, dc=dc, tc0=tc0, tc1=tc1: e.matmul(pa[:, 0:128], lhsT=kT[:, dc, tc0:tc1], rhs=qT[:, dc, tc0:tc1], start=(dc == 0), stop=(dc == 1)),
                         reads=[kT, qT], writes=[pa])
                am = attm[t % 2]
                mrow = 0 if t < 8 else 1
                P.op("dve", lambda e, pa=pa, am=am, mrow=mrow: e.tensor_tensor(out=am[:], in0=pa[:, 0:128], in1=masks[:, mrow, :], op=ALU.mult),
                     reads=[pa, masks], writes=[am])
                po = nextM()
                P.op("pe", lambda e, po=po, am=am, t=t: e.matmul(po[:, 0:512], lhsT=am[:], rhs=vv[:, t, :], start=True, stop=False),
                     reads=[am, vv], writes=[po])
                if t < 8:
                    for dc in range(2):
                        P.op("pe", lambda e, po=po, dc=dc, tc0=tc0, tc1=tc1: e.matmul(po[:, 0:512], lhsT=qT[:, dc, tc0:tc1], rhs=Sb[:, dc, :], start=False, stop=(dc == 1)),
                             reads=[qT, Sb], writes=[po])
                    for dc in range(2):
                        ps = nextS()
                        P.op("pe", lambda e, ps=ps, dc=dc, t=t: e.matmul(ps[:, 0:512], lhsT=ktok[:, t, dc * 128:(dc + 1) * 128], rhs=vv[:, t, :], start=True, stop=True),
                             reads=[ktok, vv], writes=[ps])
                        P.op("dve", lambda e, ps=ps, dc=dc, h=h: e.scalar_tensor_tensor(out=Sf[:, dc, :], in0=Sf[:, dc, :], scalar=CDEC_P[h], in1=ps[:, 0:512], op0=ALU.mult, op1=ALU.add),
                             reads=[Sf, ps], writes=[Sf])
                    if t < 7:
                        P.op("act", lambda e: e.activation(out=Sb[:], in_=Sf[:], func=AF.Copy), reads=[Sf], writes=[Sb])
                    else:
                        P.dma("sp", lambda e, h=h: e.dma_start(out=retp_out[h].rearrange("(dc p) e -> p dc e", p=128), in_=Sf[:]), reads=[Sf], semtile=Sf, is_output=True)
                else:
                    for bb in range(16):
                        s0 = S0[bb % 3]
                        P.dma("sp", lambda e, s0=s0, bb=bb, h=h: e.dma_start(out=s0[:], in_=sret[bb, h].rearrange("(dc p) e -> p dc e", p=128)), writes=[s0], semtile=s0)
                        qz = Qz[bb % 2]
                        s0b = S0b[bb % 2]
                        P.op("act", lambda e, s0=s0, s0b=s0b: e.activation(out=s0b[:], in_=s0[:], func=AF.Copy), reads=[s0], writes=[s0b])
                        P.op("pool", lambda e, qz=qz: e.memset(qz[:], 0.0), writes=[qz])
                        P.op("pool", lambda e, qz=qz, bb=bb: e.tensor_copy(out=qz[:, :, bb * 8:(bb + 1) * 8], in_=qT[:, :, 1024 + bb * 8:1024 + (bb + 1) * 8]), reads=[qT], writes=[qz])
                        for dc in range(2):
                            P.op("pe", lambda e, po=po, dc=dc, s0b=s0b, qz=qz, bb=bb: e.matmul(po[:, 0:512], lhsT=qz[:, dc, :], rhs=s0b[:, dc, :], start=False, stop=(dc == 1 and bb == 15)),
                                 reads=[qz, s0b], writes=[po])
                        kz = Kz[bb % 2]
                        P.op("dve", lambda e, kz=kz, bb=bb, t=t: e.tensor_scalar(out=kz[:], in0=ktok[:, t, :], scalar1=rowmask[:, bb:bb + 1], scalar2=None, op0=ALU.mult),
                             reads=[ktok, rowmask], writes=[kz])
                        so = So[bb % 2]
                        for dc in range(2):
                            ps = nextS()
                            P.op("pe", lambda e, ps=ps, dc=dc, kz=kz, t=t: e.matmul(ps[:, 0:512], lhsT=kz[:, dc * 128:(dc + 1) * 128], rhs=vv[:, t, :], start=True, stop=True),
                                 reads=[kz, vv], writes=[ps])
                            P.op("dve", lambda e, ps=ps, dc=dc, s0=s0, so=so, h=h: e.scalar_tensor_tensor(out=so[:, dc, :], in0=s0[:, dc, :], scalar=CDEC_S[h], in1=ps[:, 0:512], op0=ALU.mult, op1=ALU.add),
                                 reads=[s0, ps], writes=[so])
                        P.dma("sp", lambda e, so=so, bb=bb, h=h: e.dma_start(out=rets_out[bb, h].rearrange("(dc p) e -> p dc e", p=128), in_=so[:]), reads=[so], semtile=so, is_output=True)
                P.op("act", lambda e, po=po: e.activation(out=junk[:, 0:512], in_=po[:, 0:512], func=AF.Square, accum_out=small[:, 4:5]),
                     reads=[po], writes=[junk, small])
                P.op("act", lambda e: e.activation(out=small[:, 5:6], in_=small[:, 4:5], func=AF.Sqrt, bias=epsb[:], scale=1.0 / DV),
                     reads=[small, epsb], writes=[small])
                P.op("dve", lambda e: e.reciprocal(out=small[:, 6:7], in_=small[:, 5:6]), reads=[small], writes=[small])
                of = ofs[t % 2]
                P.op("dve", lambda e, po=po, of=of, t=t: e.scalar_tensor_tensor(out=of[:], in0=po[:, 0:512], scalar=small[:, 6:7], in1=sg[:, t, :], op0=ALU.mult, op1=ALU.mult),
                     reads=[po, small, sg], writes=[of])
                P.dma("sp", lambda e, of=of, t=t, h=h: e.dma_start(out=ospill[t * 128:(t + 1) * 128, h * 512:(h + 1) * 512], in_=of[:]), reads=[of], writes=[ospill], semtile=of, is_output=debug)
        P.barrier()

    OFF_Z = OFF_HEAD
    OFF_Y = OFF_Z + 36864
    OFF_X = OFF_Y + 36864
    dbg_fm = P.dram("dbg_fm", [128, 16, TOK], BF16, OUT) if debug else None
    if "C" in stages:
        cf = AR.view("cf", OFF_Z, [16, TOK], BF16)
        fmY = AR.view("fmY", OFF_Y, [16, TOK], BF16)
        def cset(i):
            b0 = OFF_Y + i * 17984
            return dict(uP=AR.view("uP%d" % i, b0, [1056], F32), uPb=AR.view("uPb%d" % i, b0 + 4224, [1056], BF16),
                        uS=AR.view("uS%d" % i, b0 + 6400, [16, 38], F32), uSb=AR.view("uSb%d" % i, b0 + 8832, [16, 38], BF16),
                        dg=AR.view("dg%d" % i, b0 + 10048, [31, 128], BF16))
        csets = [cset(0), cset(1)]
        cwT = AR.view("cwT", OFF_X, [16, 31], F32)
        sgt = [AR.view("sgt%d" % i, OFF_X + 2048 + i * 2048, [512], F32) for i in range(2)]
        scs = AR.view("scs", OFF_X + 6144, [4, 128], F32)
        cwrow = AR.view("cwrow", OFF_X + 8192, [2048], F32)
        strow = [AR.view("strow%d" % i, OFF_X + 16384 + i * 512, [128], F32) for i in range(2)]
        P.dma("sp", lambda e: e.dma_start(out=cwrow[0:CW, :], in_=conv_w[:]), writes=[cwrow], semtile=cwrow)
        for c in range(16):
            pt = nextT()
            P.op("pe", lambda e, pt=pt, c=c: e.transpose(out=pt[:, 0:CW], in_=cwrow[0:CW, c * 128:(c + 1) * 128], identity=ident_f[0:CW, 0:CW]),
                 reads=[cwrow, ident_f], writes=[pt])
            P.op("act", lambda e, pt=pt, c=c: e.activation(out=cwT[:, c, :], in_=pt[:, 0:CW], func=AF.Copy), reads=[pt], writes=[cwT])
        cpy = Tile("cpy", None)
        P.dma("sp", lambda e: e.dma_start(out=convs_out[:, 0:22, :], in_=sconv[:].rearrange("(b w) d -> b w d", w=30)[:, 8:30, :]), semtile=cpy, is_output=True)
        for c in range(16):
            if c % 4 == 0:
                wa = nextW()
                load_w(wa, w_in[:, CA0 + c * 128:CA0 + (c + 4) * 128])
                wb_ = nextW()
                load_w(wb_, w_in[:, CB0 + c * 128:CB0 + (c + 4) * 128])
            cc = (c % 4) * 128
            cs_ = csets[c % 2]
            uP, uPb, uS, uSb, dg = cs_["uP"], cs_["uPb"], cs_["uS"], cs_["uSb"], cs_["dg"]
            pst = nextT()
            for a in range(4):
                rows = 128 if a < 3 else 96
                st = strow[a % 2]
                P.dma("sp", lambda e, st=st, a=a, rows=rows, c=c: e.dma_start(out=st[0:rows, :], in_=sconv[a * 128:a * 128 + rows, c * 128:(c + 1) * 128]), writes=[st], semtile=st)
                P.op("pe", lambda e, pst=pst, st=st, a=a, rows=rows: e.transpose(out=pst[:, a * 128:a * 128 + rows], in_=st[0:rows, :], identity=ident_f[0:rows, 0:rows]),
                     reads=[st, ident_f], writes=[pst])
            P.op("act", lambda e, pst=pst, uS=uS: e.activation(out=uS[:, :, 0:30], in_=pst[:, 0:480].rearrange("p (b w) -> p b w", w=30), func=AF.Copy), reads=[pst], writes=[uS])
            segs = ((hTh, 0, 128, "h"), (hT, 0, 512, "p0"), (hT, 512, 1024, "p1"), (hT, 1024, 1152, "s"))
            for si, (src, t0, t1, kind) in enumerate(segs):
                n = t1 - t0
                pa_ = nextM()
                pb_ = nextM()
                for (pp_, wsl) in ((pa_, wa), (pb_, wb_)):
                    for kc in range(16):
                        P.op("pe", lambda e, pp_=pp_, wsl=wsl, kc=kc, cc=cc, src=src, t0=t0, t1=t1: e.matmul(pp_[:, 0:t1 - t0], lhsT=wsl[:, kc, cc:cc + 128], rhs=src[:, kc, t0:t1], start=(kc == 0), stop=(kc == 15)),
                             reads=[wsl, src], writes=[pp_])
                sgx = sgt[si % 2]
                P.op("act", lambda e, pb_=pb_, sgx=sgx, n=n: e.activation(out=sgx[:, 0:n], in_=pb_[:, 0:n], func=AF.Sigmoid), reads=[pb_], writes=[sgx])
                if kind == "h":
                    P.op("dve", lambda e, pa_=pa_, sgx=sgx, uP=uP: e.tensor_tensor(out=uP[:, 0:30], in0=pa_[:, 98:128], in1=sgx[:, 98:128], op=ALU.mult), reads=[pa_, sgx], writes=[uP])
                elif kind == "s":
                    P.op("dve", lambda e, pa_=pa_, sgx=sgx, uS=uS: e.tensor_tensor(out=uS[:, :, 30:38], in0=pa_[:, 0:128].rearrange("p (b i) -> p b i", i=8), in1=sgx[:, 0:128].rearrange("p (b i) -> p b i", i=8), op=ALU.mult),
                         reads=[pa_, sgx], writes=[uS])
                else:
                    P.op("dve", lambda e, pa_=pa_, sgx=sgx, t0=t0, uP=uP: e.tensor_tensor(out=uP[:, 30 + t0:30 + t0 + 512], in0=pa_[:, 0:512], in1=sgx[:, 0:512], op=ALU.mult), reads=[pa_, sgx], writes=[uP])
            P.op("act", lambda e, uP=uP, uPb=uPb: e.activation(out=uPb[:, 0:1054], in_=uP[:, 0:1054], func=AF.Copy), reads=[uP], writes=[uPb])
            P.op("dve", lambda e, uS=uS, uSb=uSb: e.tensor_copy(out=uSb[:], in_=uS[:]), reads=[uS], writes=[uSb])
            pt = nextT()
            P.op("pe", lambda e, pt=pt, uP=uP: e.transpose(out=pt[0:30, 0:128], in_=uP[:, 1024:1054], identity=ident_f[:]), reads=[uP, ident_f], writes=[pt])
            P.op("act", lambda e, pt=pt: e.activation(out=scs[0:30, 0, :], in_=pt[0:30, 0:128], func=AF.Copy), reads=[pt], writes=[scs])
            P.dma("sp", lambda e, c=c: e.dma_start(out=convp_out[:, c * 128:(c + 1) * 128], in_=scs[0:30, 0, :]), reads=[scs], semtile=scs, is_output=True)
            P.op("act", lambda e, uS=uS: e.activation(out=scs[:, 1, :].rearrange("p (b i) -> p b i", i=8), in_=uS[:, :, 30:38], func=AF.Copy), reads=[uS], writes=[scs])
            pt2 = nextT()
            P.op("pe", lambda e, pt2=pt2: e.transpose(out=pt2[:, 0:128], in_=scs[:, 1, :], identity=ident_f[:]), reads=[scs, ident_f], writes=[pt2])
            P.op("act", lambda e, pt2=pt2: e.activation(out=scs[:, 2, :], in_=pt2[:, 0:128], func=AF.Copy), reads=[pt2], writes=[scs])
            for bb in range(16):
                P.dma("sp", lambda e, bb=bb, c=c: e.dma_start(out=convs_out[bb, 22:30, c * 128:(c + 1) * 128], in_=scs[bb * 8:(bb + 1) * 8, 2, :]), reads=[scs], semtile=scs, is_output=True)
            for tap in range(CW):
                if tap % 2 == 0:
                    P.op("dve", lambda e, tap=tap, c=c, dg=dg: e.tensor_scalar(out=dg[:, tap, :], in0=ident_f[:], scalar1=cwT[:, c, tap:tap + 1], scalar2=None, op0=ALU.mult),
                         reads=[ident_f, cwT], writes=[dg])
                else:
                    P.op("act", lambda e, tap=tap, c=c, dg=dg: e.activation(out=dg[:, tap, :], in_=ident_f[:], func=AF.Identity, scale=cwT[:, c, tap:tap + 1]),
                         reads=[ident_f, cwT], writes=[dg])
            for (t0, n, kind) in ((0, 512, "p"), (512, 512, "p"), (1024, 128, "s")):
                pc = nextM()
                for tap in range(CW):
                    if kind == "p":
                        P.op("pe", lambda e, pc=pc, tap=tap, t0=t0, dg=dg, uPb=uPb: e.matmul(pc[:, 0:512], lhsT=dg[:, tap, :], rhs=uPb[:, t0 + tap:t0 + tap + 512], start=(tap == 0), stop=(tap == CW - 1)),
                             reads=[dg, uPb], writes=[pc])
                    else:
                        P.op("pe", lambda e, pc=pc, tap=tap, dg=dg, uSb=uSb: e.matmul(pc[:, 0:128], lhsT=dg[:, tap, :], rhs=uSb[:, :, tap:tap + 8], start=(tap == 0), stop=(tap == CW - 1)),
                             reads=[dg, uSb], writes=[pc])
                P.op("act", lambda e, pc=pc, t0=t0, n=n, c=c: e.activation(out=cf[:, c, t0:t0 + n], in_=pc[:, 0:n], func=AF.Identity, bias=vec_s[:, 2, c:c + 1]),
                     reads=[pc, vec_s], writes=[cf])
        P.barrier()
        ones_b = AR.view("ones_b", OFF_Y, [128], BF16)
        sq = [AR.view("sq%d" % i, OFF_Y + 256 + i * 1024, [512], BF16) for i in range(2)]
        mu_t = AR.view("mu_t", OFF_Y + 2304, [TOK], F32)
        rs_t = AR.view("rs_t", OFF_Y + 2304 + 4608, [TOK], F32)
        lt = [AR.view("lt%d" % i, OFF_Y + 11520 + i * 2048, [512], F32) for i in range(2)]
        epsw = AR.view("epsw", OFF_Y + 15616, [1], F32)
        P.op("dve", lambda e: e.memset(ones_b[:], 1.0), writes=[ones_b])
        P.op("dve", lambda e: e.memset(epsw[:], EPS), writes=[epsw])
        for (t0, t1) in BLKS:
            n = t1 - t0
            p1 = nextM()
            p2 = nextM()
            for c in range(16):
                P.op("pe", lambda e, p1=p1, c=c, t0=t0, t1=t1: e.matmul(p1[:, 0:t1 - t0], lhsT=ones_b[:], rhs=cf[:, c, t0:t1], start=(c == 0), stop=(c == 15)),
                     reads=[ones_b, cf], writes=[p1])
                sqx = sq[c % 2]
                P.op("act", lambda e, sqx=sqx, c=c, t0=t0, t1=t1: e.activation(out=sqx[:, 0:t1 - t0], in_=cf[:, c, t0:t1], func=AF.Square), reads=[cf], writes=[sqx])
                P.op("pe", lambda e, p2=p2, sqx=sqx, c=c, n=n: e.matmul(p2[:, 0:n], lhsT=ones_b[:], rhs=sqx[:, 0:n], start=(c == 0), stop=(c == 15)),
                     reads=[ones_b, sqx], writes=[p2])
            P.op("act", lambda e, p1=p1, t0=t0, n=n: e.activation(out=mu_t[:, t0:t0 + n], in_=p1[:, 0:n], func=AF.Copy, scale=1.0 / D), reads=[p1], writes=[mu_t])
            l0 = lt[0]
            P.op("dve", lambda e, t0=t0, n=n, l0=l0: e.tensor_tensor(out=l0[:, 0:n], in0=mu_t[:, t0:t0 + n], in1=mu_t[:, t0:t0 + n], op=ALU.mult), reads=[mu_t], writes=[l0])
            P.op("dve", lambda e, p2=p2, n=n, l0=l0: e.scalar_tensor_tensor(out=l0[:, 0:n], in0=p2[:, 0:n], scalar=1.0 / D, in1=l0[:, 0:n], op0=ALU.mult, op1=ALU.subtract), reads=[p2, l0], writes=[l0])
            P.op("act", lambda e, n=n, l0=l0: e.activation(out=l0[:, 0:n], in_=l0[:, 0:n], func=AF.Sqrt, bias=epsw[:], scale=1.0), reads=[l0, epsw], writes=[l0])
            P.op("dve", lambda e, t0=t0, n=n, l0=l0: e.reciprocal(out=rs_t[:, t0:t0 + n], in_=l0[:, 0:n]), reads=[l0], writes=[rs_t])
        for c in range(16):
            for (t0, t1) in BLKS:
                n = t1 - t0
                lx = lt[(c * 3 + (t0 // 512)) % 2]
                P.op("dve", lambda e, lx=lx, c=c, t0=t0, t1=t1: e.tensor_tensor(out=lx[:, 0:t1 - t0], in0=cf[:, c, t0:t1], in1=mu_t[:, t0:t1], op=ALU.subtract), reads=[cf, mu_t], writes=[lx])
                P.op("pool", lambda e, lx=lx, t0=t0, t1=t1: e.tensor_tensor(out=lx[:, 0:t1 - t0], in0=lx[:, 0:t1 - t0], in1=rs_t[:, t0:t1], op=ALU.mult), reads=[lx, rs_t], writes=[lx])
                P.op("act", lambda e, lx=lx, c=c, t0=t0, t1=t1: e.activation(out=cf[:, c, t0:t1], in_=lx[:, 0:t1 - t0], func=AF.Silu, bias=vec_s[:, 4, c:c + 1], scale=vec_s[:, 3, c:c + 1]),
                     reads=[lx, vec_s], writes=[cf])
        P.barrier()
        for c4 in range(4):
            wsl = nextW()
            load_w(wsl, w_in[:, GC0 + c4 * 512:GC0 + (c4 + 1) * 512])
            for j in range(4):
                c = c4 * 4 + j
                fm_proj(wsl, j * 128, hT, BLKS, lambda ps, bi, tt, c=c: P.op("act", lambda e: e.activation(out=fmY[:, c, tt[0]:tt[1]], in_=ps[:, 0:tt[1] - tt[0]], func=AF.Sigmoid), reads=[ps], writes=[fmY]))
        for c4 in range(4):
            wsl = nextW()
            load_w(wsl, w_conv_o[:, c4 * 512:(c4 + 1) * 512])
            for j in range(4):
                c = c4 * 4 + j
                fm_proj(wsl, j * 128, cf, BLKS, lambda ps, bi, tt, c=c: P.op("dve", lambda e: e.tensor_tensor(out=fmY[:, c, tt[0]:tt[1]], in0=ps[:, 0:tt[1] - tt[0]], in1=fmY[:, c, tt[0]:tt[1]], op=ALU.mult), reads=[ps, fmY], writes=[fmY]))
        P.barrier()
        fmZ = AR.view("fmZ", OFF_Z, [16, TOK], BF16)
        for c4 in range(4):
            wsl = nextW()
            load_w(wsl, w_in[:, GR0 + c4 * 512:GR0 + (c4 + 1) * 512])
            for j in range(4):
                c = c4 * 4 + j
                fm_proj(wsl, j * 128, hT, BLKS, lambda ps, bi, tt, c=c: P.op("act", lambda e: e.activation(out=fmZ[:, c, tt[0]:tt[1]], in_=ps[:, 0:tt[1] - tt[0]], func=AF.Sigmoid), reads=[ps], writes=[fmZ]))
        P.barrier()

    if "M" in stages:
        oT = AR.view("oT", OFF_HT, [32, 512], BF16)
        orow = [AR.view("orow%d" % i, OFF_X + i * 8192, [4096], BF16) for i in range(2)]
        mt = [AR.view("mt%d" % i, OFF_X + 16384 + i * 2048, [512], F32) for i in range(2)]
        Wr = [Tile("Wr%d" % i, W[i][:].rearrange("p a b -> p (a b)").rearrange("p (a b) -> p a b", a=32)) for i in range(3)]
        for bi, (t0, t1) in enumerate(BLKS):
            n = t1 - t0
            for ti in range(n // 128):
                t = t0 // 128 + ti
                orw = orow[t % 2]
                P.dma("sp", lambda e, orw=orw, t=t: e.dma_start(out=orw[:], in_=ospill[t * 128:(t + 1) * 128, :]), reads=[ospill], writes=[orw], semtile=orw)
                for g in range(4):
                    pt = nextT()
                    ptb = pt[:].bitcast(BF16)
                    for j in range(8):
                        kc = g * 8 + j
                        P.op("pe", lambda e, ptb=ptb, j=j, kc=kc, orw=orw: e.transpose(out=ptb[:, j * 128:(j + 1) * 128], in_=orw[:, kc * 128:(kc + 1) * 128], identity=ident_b[:]),
                             reads=[orw, ident_b], writes=[pt])
                    P.op("act", lambda e, ptb=ptb, g=g, ti=ti: e.activation(out=oT[:, g * 8:(g + 1) * 8, ti * 128:(ti + 1) * 128], in_=ptb.rearrange("p (a b) -> p a b", a=8), func=AF.Copy),
                         reads=[pt], writes=[oT])
            for c2 in range(8):
                wsl = Wr[(bi * 8 + c2) % 3]
                src = w_ret_o[:, c2 * 256:(c2 + 1) * 256].rearrange("(kc p) n -> p kc n", p=128)
                P.dma("pool", lambda e, wsl=wsl, src=src: e.dma_start(out=wsl[:], in_=src), writes=[wsl], semtile=wsl)
                for j in range(2):
                    c = c2 * 2 + j
                    ps = nextM()
                    for kc in range(32):
                        P.op("pe", lambda e, ps=ps, kc=kc, wsl=wsl, j=j, n=n: e.matmul(ps[:, 0:n], lhsT=wsl[:, kc, j * 128:(j + 1) * 128], rhs=oT[:, kc, 0:n], start=(kc == 0), stop=(kc == 31)),
                             reads=[wsl, oT], writes=[ps])
                    mx = mt[c % 2]
                    P.op("dve", lambda e, ps=ps, mx=mx, c=c, t0=t0, t1=t1: e.tensor_tensor(out=mx[:, 0:t1 - t0], in0=ps[:, 0:t1 - t0], in1=fmZ[:, c, t0:t1], op=ALU.mult), reads=[ps, fmZ], writes=[mx])
                    P.op("pool", lambda e, mx=mx, c=c, t0=t0, t1=t1: e.tensor_tensor(out=fmY[:, c, t0:t1], in0=mx[:, 0:t1 - t0], in1=fmY[:, c, t0:t1], op=ALU.add), reads=[mx, fmY], writes=[fmY])
        if debug and stages.endswith("M"):
            P.dma("sp", lambda e: e.dma_start(out=dbg_fm[:], in_=fmY[:]), reads=[fmY], semtile=fmY, is_output=True)
        P.barrier()

    if "O" in stages:
        yacc = AR.view("yacc", OFF_HT, [NTILE, D], F32)
        xt = [AR.view("xt%d" % i, OFF_X + i * 2048, [512], F32) for i in range(2)]
        junk2 = AR.view("junk2", OFF_X + 4096, [2048], BF16)
        xb2 = AR.view("xb2", OFF_X + 8192, [2048], BF16)
        for cb in range(4):
            wsl = nextW()
            load_w(wsl, w_o[:, cb * 512:(cb + 1) * 512])
            for t in range(NTILE):
                xx = xt[t % 2]
                P.dma("sp", lambda e, xx=xx, t=t, cb=cb: e.dma_start(out=xx[:], in_=x_own[t * 128:(t + 1) * 128, cb * 512:(cb + 1) * 512]), writes=[xx], semtile=xx)
                ps = nextM()
                for kc in range(16):
                    P.op("pe", lambda e, ps=ps, kc=kc, t=t, wsl=wsl: e.matmul(ps[:, 0:512], lhsT=fmY[:, kc, t * 128:(t + 1) * 128], rhs=wsl[:, kc, :], start=(kc == 0), stop=(kc == 15)),
                         reads=[fmY, wsl], writes=[ps])
                P.op("dve", lambda e, ps=ps, xx=xx, t=t, cb=cb: e.tensor_tensor(out=yacc[:, t, cb * 512:(cb + 1) * 512], in0=ps[:, 0:512], in1=xx[:], op=ALU.add), reads=[ps, xx], writes=[yacc])
        P.barrier()
        if stages.endswith("O"):
            for t in range(NTILE):
                P.dma("sp", lambda e, t=t: e.dma_start(out=y_out[t * 128:(t + 1) * 128, :], in_=yacc[:, t, :]), reads=[yacc], semtile=yacc, is_output=True)
        NBLK = 2 * TOK // 128 + NE
        h2p = AR.view("h2p", OFF_Y, [NTILE, D], BF16)
        h2Tt = AR.view("h2Tt", OFF_X + 12288, [16, 128], BF16)
        XO = OFF_X + 16384
        OH1s = AR.view("OH1s", XO, [NTILE, 32], F32)
        OH2s = AR.view("OH2s", XO + 1152, [NTILE, 32], F32)
        G12 = AR.view("G12", XO + 2304, [2, 16], F32)
        R12 = AR.view("R12", XO + 2432, [2, 16], F32)
        carry = AR.view("carry", XO + 2560, [32], F32)
        rt = AR.view("rt", XO + 2688, [256], F32)
        mconst = AR.view("mconst", XO + 3712, [3, 128], F32)
        ones_f = AR.view("ones_f", XO + 5248, [128], F32)
        wrt = AR.view("wrt", XO + 5760, [16, 36], BF16)
        brt = AR.view("brt", XO + 6912, [36], F32)
        rk = AR.view("rk", XO + 7056, [64], F32)
        x1scr = P.dram("x1_scr", [TOK, D], F32, "Internal")
        P.dma("sp", lambda e: e.dma_start(out=mconst[:], in_=mconst_d[:]), writes=[mconst], semtile=mconst)
        P.dma("pool", lambda e: e.dma_start(out=wrt[:], in_=w_rt[:].rearrange("(kc p) n -> p kc n", p=128)), writes=[wrt], semtile=wrt)
        P.dma("sp", lambda e: e.dma_start(out=brt[:], in_=b_rt[:].partition_broadcast(128)), writes=[brt], semtile=brt)
        P.op("dve", lambda e: e.memset(ones_f[:], 1.0), writes=[ones_f])
        P.op("dve", lambda e: e.memset(carry[:], 0.0), writes=[carry])
        for t in range(NTILE):
            P.dma("sp", lambda e, t=t: e.dma_start(out=x1scr[t * 128:(t + 1) * 128, :], in_=yacc[:, t, :]), reads=[yacc], writes=[x1scr], semtile=yacc)
            P.op("act", lambda e, t=t: e.activation(out=junk2[:], in_=yacc[:, t, :], func=AF.Square, accum_out=small[:, 0:1]), reads=[yacc], writes=[junk2, small])
            P.op("act", lambda e: e.activation(out=small[:, 1:2], in_=small[:, 0:1], func=AF.Sqrt, bias=epsb[:], scale=1.0 / D), reads=[small, epsb], writes=[small])
            P.op("dve", lambda e: e.reciprocal(out=small[:, 2:3], in_=small[:, 1:2]), reads=[small], writes=[small])
            P.op("dve", lambda e, t=t: e.tensor_scalar(out=xb2[:], in0=yacc[:, t, :], scalar1=small[:, 2:3], scalar2=None, op0=ALU.mult), reads=[yacc, small], writes=[xb2])
            for g in range(2):
                pt = nextT()
                ptb = pt[:].bitcast(BF16)
                for j in range(8):
                    kc = g * 8 + j
                    P.op("pe", lambda e, ptb=ptb, j=j, kc=kc: e.transpose(out=ptb[:, j * 128:(j + 1) * 128], in_=xb2[:, kc * 128:(kc + 1) * 128], identity=ident_b[:]), reads=[xb2, ident_b], writes=[pt])
                P.op("dve", lambda e, ptb=ptb, g=g: e.tensor_tensor(out=h2Tt[:, g * 8:(g + 1) * 8, :], in0=ptb.rearrange("p (a b) -> p a b", a=8),
                                                                in1=vec_s[:, 1, g * 8:(g + 1) * 8].unsqueeze(2).to_broadcast([128, 8, 128]), op=ALU.mult), reads=[pt, vec_s], writes=[h2Tt])
            for g in range(2):
                pt = nextT()
                ptb = pt[:].bitcast(BF16)
                for j in range(8):
                    kc = g * 8 + j
                    P.op("pe", lambda e, ptb=ptb, j=j, kc=kc: e.transpose(out=ptb[:, j * 128:(j + 1) * 128], in_=h2Tt[:, kc, :], identity=ident_b[:]), reads=[h2Tt, ident_b], writes=[pt])
                P.op("act", lambda e, ptb=ptb, g=g, t=t: e.activation(
                    out=h2p[:, t, :].rearrange("t (j p) -> t p j", j=16)[:, g * 64:(g + 1) * 64, :],
                    in_=ptb.rearrange("t (p j) -> t p j", j=16), func=AF.Copy), reads=[pt], writes=[h2p])
            ps = nextM()
            for kc in range(16):
                P.op("pe", lambda e, ps=ps, kc=kc: e.matmul(ps[:, 0:36], lhsT=h2Tt[:, kc, :], rhs=wrt[:, kc, :], start=(kc == 0), stop=(kc == 15)), reads=[h2Tt, wrt], writes=[ps])
            dv = lambda fn, rd=(), wr=(): P.op("dve", fn, reads=[rt] + list(rd), writes=[rt] + list(wr))
            dv(lambda e, ps=ps: e.tensor_tensor(out=rt[:, 0:36], in0=ps[:, 0:36], in1=brt[:], op=ALU.add), rd=[ps, brt])
            dv(lambda e: e.tensor_reduce(out=rt[:, 40:41], in_=rt[:, 0:4], axis=mybir.AxisListType.X, op=ALU.max))
            dv(lambda e: e.tensor_scalar(out=rt[:, 44:48], in0=rt[:, 0:4], scalar1=rt[:, 40:41], scalar2=None, op0=ALU.is_equal))
            dv(lambda e: e.tensor_scalar(out=rt[:, 41:42], in0=rt[:, 40:41], scalar1=-1.0, scalar2=None, op0=ALU.mult))
            P.op("act", lambda e: e.activation(out=rt[:, 48:52], in_=rt[:, 0:4], func=AF.Exp, bias=rt[:, 41:42], scale=1.0, accum_out=rt[:, 42:43]), reads=[rt], writes=[rt])
            dv(lambda e: e.reciprocal(out=rt[:, 43:44], in_=rt[:, 42:43]))
            dv(lambda e: e.tensor_scalar(out=rt[:, 52:56], in0=rt[:, 44:48], scalar1=-1.0, scalar2=1e30, op0=ALU.add, op1=ALU.mult))
            dv(lambda e: e.tensor_tensor(out=rt[:, 64:96].rearrange("p (g x) -> p g x", g=4), in0=rt[:, 4:36].rearrange("p (g x) -> p g x", g=4),
                                         in1=rt[:, 52:56].unsqueeze(2).to_broadcast([128, 4, 8]), op=ALU.add))
            dv(lambda e: e.tensor_reduce(out=rt[:, 56:57], in_=rt[:, 64:96], axis=mybir.AxisListType.X, op=ALU.max))
            dv(lambda e, t=t: e.tensor_scalar(out=OH1s[:, t, :], in0=rt[:, 64:96], scalar1=rt[:, 56:57], scalar2=None, op0=ALU.is_equal), wr=[OH1s])
            dv(lambda e, t=t: e.scalar_tensor_tensor(out=rt[:, 128:160], in0=OH1s[:, t, :], scalar=-1e30, in1=rt[:, 64:96], op0=ALU.mult, op1=ALU.add), rd=[OH1s])
            dv(lambda e: e.tensor_reduce(out=rt[:, 57:58], in_=rt[:, 128:160], axis=mybir.AxisListType.X, op=ALU.max))
            dv(lambda e, t=t: e.tensor_scalar(out=OH2s[:, t, :], in0=rt[:, 128:160], scalar1=rt[:, 57:58], scalar2=None, op0=ALU.is_equal), wr=[OH2s])
            dv(lambda e: e.tensor_scalar(out=rt[:, 58:59], in0=rt[:, 56:57], scalar1=-1.0, scalar2=None, op0=ALU.mult))
            P.op("act", lambda e: e.activation(out=rt[:, 59:60], in_=rt[:, 57:58], func=AF.Exp, bias=rt[:, 58:59], scale=1.0), reads=[rt], writes=[rt])
            dv(lambda e: e.tensor_scalar(out=rt[:, 60:61], in0=rt[:, 59:60], scalar1=1.0, scalar2=None, op0=ALU.add))
            dv(lambda e: e.reciprocal(out=rt[:, 61:62], in_=rt[:, 60:61]))
            dv(lambda e, t=t: e.tensor_tensor(out=G12[:, 0, t:t + 1], in0=rt[:, 61:62], in1=rt[:, 43:44], op=ALU.mult), wr=[G12])
            dv(lambda e, t=t: e.tensor_tensor(out=G12[:, 1, t:t + 1], in0=G12[:, 0, t:t + 1], in1=rt[:, 59:60], op=ALU.mult), rd=[G12], wr=[G12])
            for k, OHs in ((0, OH1s), (1, OH2s)):
                pr = nextM()
                P.op("pe", lambda e, pr=pr, OHs=OHs, t=t: e.matmul(pr[:, 0:32], lhsT=mconst[:, 0, :], rhs=OHs[:, t, :], start=True, stop=True), reads=[mconst, OHs], writes=[pr])
                pc_ = nextM()
                P.op("pe", lambda e, pc_=pc_, OHs=OHs, t=t: e.matmul(pc_[:, 0:32], lhsT=ones_f[:], rhs=OHs[:, t, :], start=True, stop=True), reads=[ones_f, OHs], writes=[pc_])
                P.op("dve", lambda e, pr=pr: e.tensor_tensor(out=rk[:, 0:32], in0=pr[:, 0:32], in1=carry[:], op=ALU.add), reads=[pr, carry], writes=[rk])
                P.op("dve", lambda e, OHs=OHs, t=t: e.tensor_tensor(out=rk[:, 32:64], in0=OHs[:, t, :], in1=rk[:, 0:32], op=ALU.mult), reads=[OHs, rk], writes=[rk])
                P.op("dve", lambda e, t=t, k=k: e.tensor_reduce(out=R12[:, k, t:t + 1], in_=rk[:, 32:64], axis=mybir.AxisListType.X, op=ALU.add), reads=[rk], writes=[R12])
                P.op("dve", lambda e, pc_=pc_: e.tensor_tensor(out=carry[:], in0=pc_[:, 0:32], in1=carry[:], op=ALU.add), reads=[pc_, carry], writes=[carry])
        if debug and stages.endswith("O"):
            dbg_s = P.dram("dbg_s", [128, 96], F32, OUT)
            P.dma("sp", lambda e: e.dma_start(out=dbg_s[:, 0:32], in_=R12[:].rearrange("p a b -> p (a b)")), reads=[R12], semtile=R12, is_output=True)
            P.dma("sp", lambda e: e.dma_start(out=dbg_s[:, 32:64], in_=G12[:].rearrange("p a b -> p (a b)")), reads=[G12], semtile=G12, is_output=True)
            P.dma("sp", lambda e: e.dma_start(out=dbg_s[:, 64:96], in_=carry[:]), reads=[carry], semtile=carry, is_output=True)
        P.barrier()

    if "E" in stages:
        EO = XO + 8192
        nblk = AR.view("nblk", EO, [32], F32)
        pst = AR.view("pst", EO + 128, [32], F32)
        pend = AR.view("pend", EO + 256, [32], F32)
        prow = AR.view("prow", EO + 384, [32], F32)
        ebf = AR.view("ebf", EO + 512, [64], F32)
        idxW = AR.view("idxW", EO + 768, [64], I32)
        Ri = AR.view("Ri", EO + 1024, [2, 16], I32)
        ebe = AR.view("ebe", EO + 1152, [64], F32)
        big = AR.view("big", OFF_X, [NBLK, 32], F32)
        big2 = AR.view("big2", OFF_X + 6400, [NBLK, 32], F32)
        P.op("dve", lambda e: e.memset(nblk[:], 0.0), writes=[nblk])
        for m in range(2 * TOK // 128 + 1):
            P.op("dve", lambda e, m=m: e.scalar_tensor_tensor(out=nblk[:], in0=carry[:], scalar=float(128 * m), in1=nblk[:], op0=ALU.is_gt, op1=ALU.add), reads=[carry, nblk], writes=[nblk])
        P.op("dve", lambda e: e.memset(pst[:, 0:1], 0.0), writes=[pst])
        for ei in range(1, 32):
            P.op("dve", lambda e, ei=ei: e.tensor_tensor(out=pst[:, ei:ei + 1], in0=pst[:, ei - 1:ei], in1=nblk[:, ei - 1:ei], op=ALU.add), reads=[pst, nblk], writes=[pst])
        P.op("dve", lambda e: e.tensor_tensor(out=pend[:], in0=pst[:], in1=nblk[:], op=ALU.add), reads=[pst, nblk], writes=[pend])
        P.op("dve", lambda e: e.tensor_scalar(out=prow[:], in0=pst[:], scalar1=128.0, scalar2=None, op0=ALU.mult), reads=[pst], writes=[prow])
        for t in range(NTILE):
            for k, OHs in ((0, OH1s), (1, OH2s)):
                P.op("dve", lambda e, OHs=OHs, t=t: e.tensor_tensor(out=rk[:, 32:64], in0=OHs[:, t, :], in1=prow[:], op=ALU.mult), reads=[OHs, prow], writes=[rk])
                P.op("dve", lambda e: e.tensor_reduce(out=rk[:, 0:1], in_=rk[:, 32:64], axis=mybir.AxisListType.X, op=ALU.add), reads=[rk], writes=[rk])
                P.op("dve", lambda e, t=t, k=k: e.tensor_tensor(out=R12[:, k, t:t + 1], in0=R12[:, k, t:t + 1], in1=rk[:, 0:1], op=ALU.add), reads=[R12, rk], writes=[R12])
        P.op("dve", lambda e: e.tensor_copy(out=Ri[:], in_=R12[:]), reads=[R12], writes=[Ri])
        bio = mconst[:, 1, 0:NBLK].unsqueeze(2).to_broadcast([128, NBLK, 32])
        P.op("dve", lambda e: e.tensor_tensor(out=big[:], in0=pst[:].unsqueeze(1).to_broadcast([128, NBLK, 32]), in1=bio, op=ALU.is_le), reads=[pst, mconst], writes=[big])
        P.op("dve", lambda e: e.tensor_tensor(out=big2[:], in0=pend[:].unsqueeze(1).to_broadcast([128, NBLK, 32]), in1=bio, op=ALU.is_gt), reads=[pend, mconst], writes=[big2])
        P.op("dve", lambda e: e.tensor_tensor(out=big[:], in0=big[:], in1=big2[:], op=ALU.mult), reads=[big, big2], writes=[big])
        P.op("dve", lambda e: e.tensor_reduce(out=ebf[:, 0:NBLK], in_=big[:], axis=mybir.AxisListType.X, op=ALU.add), reads=[big], writes=[ebf])
        P.op("dve", lambda e: e.tensor_tensor(out=big2[:], in0=big[:], in1=mconst[:, 1, 0:32].unsqueeze(1).to_broadcast([128, NBLK, 32]), op=ALU.mult), reads=[big, mconst], writes=[big2])
        P.op("dve", lambda e: e.tensor_reduce(out=ebe[:, 0:NBLK], in_=big2[:], axis=mybir.AxisListType.X, op=ALU.add), reads=[big2], writes=[ebe])
        idx2f = AR.view("idx2f", OFF_X, [NBLK, 2], F32)
        idx2 = AR.view("idx2", EO + 1408, [NBLK, 2], I32)
        base2 = AR.view("base2", EO + 768, [2], F32)
        P.op("dve", lambda e: e.tensor_scalar(out=ebf[:, 0:NBLK], in0=ebf[:, 0:NBLK], scalar1=-1.0, scalar2=-1.0e4, op0=ALU.add, op1=ALU.mult), reads=[ebf], writes=[ebf])
        P.op("dve", lambda e: e.tensor_tensor(out=ebf[:, 0:NBLK], in0=ebf[:, 0:NBLK], in1=ebe[:, 0:NBLK], op=ALU.add), reads=[ebf, ebe], writes=[ebf])
        P.op("dve", lambda e: e.tensor_scalar(out=ebf[:, 0:NBLK], in0=ebf[:, 0:NBLK], scalar1=256.0, scalar2=None, op0=ALU.mult), reads=[ebf], writes=[ebf])
        P.op("dve", lambda e: e.scalar_tensor_tensor(out=base2[:], in0=mconst[:, 2, 0:2], scalar=2.0, in1=mconst[:, 1, 0:2], op0=ALU.mult, op1=ALU.add), reads=[mconst], writes=[base2])
        P.op("dve", lambda e: e.tensor_tensor(out=idx2f[:], in0=ebf[:, 0:NBLK].unsqueeze(2).to_broadcast([128, NBLK, 2]), in1=base2[:].unsqueeze(1).to_broadcast([128, NBLK, 2]), op=ALU.add),
             reads=[ebf, base2, big], writes=[idx2f])
        P.op("dve", lambda e: e.tensor_copy(out=idx2[:], in_=idx2f[:]), reads=[idx2f], writes=[idx2])
        P.barrier()
        WS = [Tile("WS%d" % i, AR.t[:, i * 32768:(i + 1) * 32768].bitcast(BF16)) for i in range(3)]
        wcnt = {"n": 0}

        def nextWS():
            wcnt["n"] += 1
            return WS[wcnt["n"] % 3]
        MO = 98304
        iob = [AR.view("iob%d" % i, MO + i * 512, [128], F32) for i in range(2)]
        selt = [AR.view("selt%d" % i, MO + 1024 + i * 256, [128], BF16) for i in range(2)]
        Sel = [AR.view("Sel%d" % i, MO + 1536 + i * 2304, [NTILE, 128], BF16) for i in range(2)]
        XbT = [AR.view("XbT%d" % i, MO + 6144 + i * 4096, [16, 128], BF16) for i in range(2)]
        sgm = [AR.view("sgm%d" % i, MO + 14336 + i * 2048, [512], F32) for i in range(2)]
        hperm = AR.view("hperm", MO + 18432, [1024], BF16)
        hTm = AR.view("hTm", MO + 20480, [8, 128], BF16)
        Yst = [AR.view("Yst%d" % i, OFF_X + i * 8192, [D], F32) for i in range(2)]
        yscr = P.dram("y_scr", [NBLK * 128, D], F32, "Internal")
        regs = {}

        def breg(e, key, val):
            if key not in regs:
                regs[key] = e.to_reg(val)
            return regs[key]
        wflat = {id(w_gate): w_gate[:].rearrange("e k n -> (e k n)").rearrange("(r c) -> r c", c=8192),
                 id(w_up): w_up[:].rearrange("e k n -> (e k n)").rearrange("(r c) -> r c", c=8192),
                 id(w_down): w_down[:].rearrange("e k n -> (e k n)").rearrange("(r c) -> r c", c=8192)}
        bound = n_experts * 256 - 1

        def wload(slot, wsrc, b, J):
            src2 = wflat[id(wsrc)]
            for hh in range(2):
                P.dma("pool", lambda e, hh=hh: e.indirect_dma_start(out=slot[:, hh * 8192:(hh + 1) * 8192], out_offset=None, in_=src2, in_offset=bass.IndirectOffsetOnAxis(ap=idx2[:, b, hh:hh + 1], axis=0),
                                                               bounds_check=breg(e, "w", bound), oob_is_err=False), reads=[idx2], writes=[slot], semtile=slot)
        for b in range(NBLK):
            wg_ = nextWS(); wload(wg_, w_gate, b, 16)
            wu_ = nextWS(); wload(wu_, w_up, b, 16)
            wd_ = nextWS(); wload(wd_, w_down, b, 8)
            io_ = iob[b % 2]
            P.op("dve", lambda e, io_=io_, b=b: e.tensor_scalar(out=io_[:], in0=mconst[:, 1, :], scalar1=float(128 * b), scalar2=None, op0=ALU.add), reads=[mconst], writes=[io_])
            sel = Sel[b % 2]
            for t in range(NTILE):
                st_ = selt[t % 2]
                P.op("pool", lambda e, st_=st_, io_=io_, t=t: e.tensor_scalar(out=st_[:], in0=io_[:], scalar1=R12[:, 0, t:t + 1], scalar2=None, op0=ALU.is_equal), reads=[io_, R12], writes=[st_])
                P.op("dve", lambda e, st_=st_, io_=io_, t=t, sel=sel: e.scalar_tensor_tensor(out=sel[:, t, :], in0=io_[:], scalar=R12[:, 1, t:t + 1], in1=st_[:], op0=ALU.is_equal, op1=ALU.add),
                     reads=[io_, R12, st_], writes=[sel])
            xbt = XbT[b % 2]
            for g in range(4):
                pg_ = pM[g]
                for jj in range(4):
                    j = g * 4 + jj
                    for t in range(NTILE):
                        P.op("pe", lambda e, pg_=pg_, jj=jj, j=j, t=t, sel=sel: e.matmul(pg_[:, jj * 128:(jj + 1) * 128], lhsT=h2p[:, t, j * 128:(j + 1) * 128], rhs=sel[:, t, :], start=(t == 0), stop=(t == NTILE - 1)),
                             reads=[h2p, sel], writes=[pg_])
                if g % 2 == 0:
                    P.op("act", lambda e, pg_=pg_, g=g, xbt=xbt: e.activation(out=xbt[:, g * 4:(g + 1) * 4, :], in_=pg_[:].rearrange("p (a b) -> p a b", a=4), func=AF.Copy), reads=[pg_], writes=[xbt])
                else:
                    P.op("dve", lambda e, pg_=pg_, g=g, xbt=xbt: e.tensor_copy(out=xbt[:, g * 4:(g + 1) * 4, :], in_=pg_[:].rearrange("p (a b) -> p a b", a=4)), reads=[pg_], writes=[xbt])
            for hf in range(2):
                pgt, put = pS[0], pS[1]
                for (pp_, wsl) in ((pgt, wg_), (put, wu_)):
                    for j in range(16):
                        P.op("pe", lambda e, pp_=pp_, wsl=wsl, j=j, hf=hf, xbt=xbt: e.matmul(pp_[:, 0:512], lhsT=xbt[:, j, :], rhs=wsl[:].rearrange("p (j n) -> p j n", j=16)[:, j, hf * 512:(hf + 1) * 512], start=(j == 0), stop=(j == 15)),
                             reads=[xbt, wsl], writes=[pp_])
                sx = sgm[hf]
                P.op("act", lambda e, pgt=pgt, sx=sx: e.activation(out=sx[:], in_=pgt[:, 0:512], func=AF.Silu), reads=[pgt], writes=[sx])
                P.op("dve", lambda e, put=put, sx=sx, hf=hf: e.tensor_tensor(out=hperm[:].rearrange("t (j p) -> t p j", j=8)[:, hf * 64:(hf + 1) * 64, :],
                                                                          in0=put[:, 0:512].rearrange("t (p j) -> t p j", j=8), in1=sx[:].rearrange("t (p j) -> t p j", j=8), op=ALU.mult),
                     reads=[put, sx], writes=[hperm])
            ptm = pT[0]
            ptb = ptm[:].bitcast(BF16)
            for j in range(8):
                P.op("pe", lambda e, ptb=ptb, j=j: e.transpose(out=ptb[:, j * 128:(j + 1) * 128], in_=hperm[:, j * 128:(j + 1) * 128], identity=ident_b[:]), reads=[hperm, ident_b], writes=[ptm])
            P.op("act", lambda e, ptb=ptb: e.activation(out=hTm[:], in_=ptb.rearrange("p (a b) -> p a b", a=8), func=AF.Copy), reads=[ptm], writes=[hTm])
            yst = Yst[b % 2]
            for cb in range(4):
                pd = pT[1]
                for j in range(8):
                    P.op("pe", lambda e, pd=pd, j=j, cb=cb, wd_=wd_: e.matmul(pd[:, 0:512], lhsT=hTm[:, j, :], rhs=wd_[:].rearrange("p (j n) -> p j n", j=8)[:, j, cb * 512:(cb + 1) * 512], start=(j == 0), stop=(j == 7)),
                         reads=[hTm, wd_], writes=[pd])
                if cb % 2 == 0:
                    P.op("act", lambda e, pd=pd, cb=cb, yst=yst: e.activation(out=yst[:, cb * 512:(cb + 1) * 512], in_=pd[:, 0:512], func=AF.Copy), reads=[pd], writes=[yst])
                else:
                    P.op("dve", lambda e, pd=pd, cb=cb, yst=yst: e.tensor_copy(out=yst[:, cb * 512:(cb + 1) * 512], in_=pd[:, 0:512]), reads=[pd], writes=[yst])
            P.dma("sp", lambda e, yst=yst, b=b: e.dma_start(out=yscr[b * 128:(b + 1) * 128, :], in_=yst[:]), reads=[yst], writes=[yscr], semtile=yst)
        P.barrier()
        nf = AR.view("nf", 0, [D], F32)
        junk3 = AR.view("junk3", 8192, [2048], BF16)
        xc = [AR.view("xc%d" % i, 16384 + i * 8192, [D], F32) for i in range(2)]
        y1 = [AR.view("y1_%d" % i, 32768 + i * 8192, [D], F32) for i in range(2)]
        y2 = [AR.view("y2_%d" % i, 49152 + i * 8192, [D], F32) for i in range(2)]
        P.dma("sp", lambda e: e.dma_start(out=nf[:], in_=norm_f[:].partition_broadcast(128)), writes=[nf], semtile=nf)
        for t in range(NTILE):
            xx, ya, yb_ = xc[t % 2], y1[t % 2], y2[t % 2]
            P.dma("sp", lambda e, xx=xx, t=t: e.dma_start(out=xx[:], in_=x1scr[t * 128:(t + 1) * 128, :]), reads=[x1scr], writes=[xx], semtile=xx)
            for k, yy in ((0, ya), (1, yb_)):
                P.dma("pool", lambda e, yy=yy, k=k, t=t: e.indirect_dma_start(out=yy[:], out_offset=None, in_=yscr[:], in_offset=bass.IndirectOffsetOnAxis(ap=Ri[:, k, t:t + 1], axis=0),
                                                                       bounds_check=breg(e, "y", NBLK * 128 - 1), oob_is_err=False), reads=[Ri, yscr], writes=[yy], semtile=yy)
            P.op("dve", lambda e, xx=xx, ya=ya, t=t: e.scalar_tensor_tensor(out=xx[:], in0=ya[:], scalar=G12[:, 0, t:t + 1], in1=xx[:], op0=ALU.mult, op1=ALU.add), reads=[ya, G12, xx], writes=[xx])
            P.op("dve", lambda e, xx=xx, yb_=yb_, t=t: e.scalar_tensor_tensor(out=xx[:], in0=yb_[:], scalar=G12[:, 1, t:t + 1], in1=xx[:], op0=ALU.mult, op1=ALU.add), reads=[yb_, G12, xx], writes=[xx])
            P.op("act", lambda e, xx=xx: e.activation(out=junk3[:], in_=xx[:], func=AF.Square, accum_out=small[:, 0:1]), reads=[xx], writes=[junk3, small])
            P.op("act", lambda e: e.activation(out=small[:, 1:2], in_=small[:, 0:1], func=AF.Sqrt, bias=epsb[:], scale=1.0 / D), reads=[small, epsb], writes=[small])
            P.op("dve", lambda e: e.reciprocal(out=small[:, 2:3], in_=small[:, 1:2]), reads=[small], writes=[small])
            P.op("dve", lambda e, xx=xx: e.scalar_tensor_tensor(out=xx[:], in0=xx[:], scalar=small[:, 2:3], in1=nf[:], op0=ALU.mult, op1=ALU.mult), reads=[xx, small, nf], writes=[xx])
            P.dma("sp", lambda e, xx=xx, t=t: e.dma_start(out=y_out[t * 128:(t + 1) * 128, :], in_=xx[:]), reads=[xx], semtile=xx, is_output=True)

    P.emit()
    return nc, P


def _tables(half):
    pos = np.concatenate([half * 1024 + np.arange(1024), np.tile(PAST_LEN + np.arange(8), 16)]).astype(np.float32)
    ppos = np.arange(1024).astype(np.float32)
    inv = (np.float32(10000.0) ** (-np.arange(128, dtype=np.float32) / np.float32(128))).astype(np.float32)

    def cs(p):
        ang = (p[None, :] * inv[:, None]).astype(np.float32)
        return np.stack([np.cos(ang.astype(np.float64)), np.sin(ang.astype(np.float64))], axis=1).astype(np.float32)

    dec = np.zeros((H, 128, 4, 128), np.float32)
    i = np.arange(128, dtype=np.float64)
    i8 = (np.arange(128) % 8).astype(np.float64)
    for h in range(H):
        lg = LOG_G[h]
        dec[h, :, 0, :] = np.exp((i + 1) * lg)[None, :]
        dec[h, :, 1, :] = (np.exp(-(i + 1) * lg) * DK ** -0.5)[None, :]
        dec[h, :, 2, :] = np.exp((i8 + 1) * lg)[None, :]
        dec[h, :, 3, :] = (np.exp(-(i8 + 1) * lg) * DK ** -0.5)[None, :]
    j = np.arange(128)
    mp = (j[:, None] <= j[None, :]).astype(np.float32)
    ms = ((j[:, None] <= j[None, :]) & (j[:, None] // 8 == j[None, :] // 8)).astype(np.float32)
    masks = np.stack([mp, ms], axis=1)
    rowmask = (j[:, None] // 8 == np.arange(16)[None, :]).astype(np.float32)
    mconst = np.zeros((128, 3, 128), np.float32)
    mconst[:, 0, :] = (j[:, None] < j[None, :])
    mconst[:, 1, :] = j[None, :]
    mconst[:, 2, :] = j[:, None]
    return dict(cs_own=cs(pos), cs_prev=cs(ppos), dec=dec, masks=np.ascontiguousarray(masks),
                ident=np.eye(128, dtype=np.float32), rowmask=rowmask, mconst=mconst)


def make_in_maps(inp, n_experts=NE, cores=range(8)):
    f = lambda a: np.ascontiguousarray(np.asarray(a, dtype=np.float32))
    xp = f(inp["x_prompt"])
    xs = f(inp["x_sample"])
    sr = f(inp["state_ret"])[0]
    sc = f(inp["state_conv"])[0]

    def kc_layout(v):
        return f(v).reshape(16, 128).T

    vecs = np.stack([kc_layout(inp["norm_mix"][0]), kc_layout(inp["norm_ffn"][0]), kc_layout(inp["conv_b"][0]),
                     kc_layout(inp["conv_ln_w"][0]), kc_layout(inp["conv_ln_b"][0]), np.zeros((128, 16), np.float32)], axis=1)
    shared = dict(
        w_in=f(inp["w_in"])[0], w_ret_o=f(inp["w_ret_o"])[0], conv_w=f(inp["conv_w"])[0], vecs=np.ascontiguousarray(vecs),
        norm_f=f(inp["norm_f"]).reshape(1, D), w_conv_o=f(inp["w_conv_o"])[0], w_o=f(inp["w_o"])[0],
        w_rt=np.ascontiguousarray(np.concatenate([f(inp["w_coarse"])[0], f(inp["w_fine"])[0]], axis=1)),
        b_rt=np.concatenate([f(inp["b_coarse"])[0], f(inp["b_fine"])[0]]).reshape(1, 36),
        w_gate=f(inp["w_gate"])[0][:n_experts], w_up=f(inp["w_up"])[0][:n_experts], w_down=f(inp["w_down"])[0][:n_experts],
    )
    tabs = [_tables(0), _tables(1)]
    maps = []
    for c in cores:
        b, half = c // 2, c % 2
        m = dict(shared)
        m.update(tabs[half])
        m["x_own"] = np.ascontiguousarray(np.concatenate([xp[b, half * 1024:(half + 1) * 1024], xs[16 * c:16 * (c + 1)].reshape(128, D)], axis=0))
        m["x_prev"] = np.ascontiguousarray(xp[b, 0:1024]) if half == 1 else np.zeros((1024, D), np.float32)
        m["sret"] = np.ascontiguousarray(sr[16 * c:16 * (c + 1)])
        m["sconv"] = np.ascontiguousarray(sc[16 * c:16 * (c + 1)].reshape(16 * 30, D))
        maps.append(m)
    return maps


_CACHE = {}


def kernel(**inputs):
    if "nc" not in _CACHE:
        _CACHE["nc"] = build()[0]
    nc = _CACHE["nc"]
    maps = make_in_maps(inputs)
    res = run_bass_kernel_spmd(nc, maps, core_ids=list(range(8)))
    r = res.results
    yp = np.zeros((4, 2048, D), np.float32)
    ys = np.zeros((128, 8, D), np.float32)
    retp = np.zeros((1, 4, H, DK, DV), np.float32)
    convp = np.zeros((1, 4, 30, D), np.float32)
    rets = np.zeros((1, 128, H, DK, DV), np.float32)
    convs = np.zeros((1, 128, 30, D), np.float32)
    for c in range(8):
        b, half = c // 2, c % 2
        yp[b, half * 1024:(half + 1) * 1024] = r[c]["y_out"][:1024]
        ys[16 * c:16 * (c + 1)] = r[c]["y_out"][1024:].reshape(16, 8, D)
        rets[0, 16 * c:16 * (c + 1)] = r[c]["rets_out"]
        convs[0, 16 * c:16 * (c + 1)] = r[c]["convs_out"]
        if half == 1:
            retp[0, b] = r[c]["retp_out"]
            convp[0, b] = r[c]["convp_out"]
    return (yp, ys, retp, convp, rets, convs)
```

```python
import contextlib
import numpy as np
import concourse.bass as bass
import concourse.mybir as mybir
from concourse.bass_utils import run_bass_kernel_spmd

F32, BF16, I32, U8 = mybir.dt.float32, mybir.dt.bfloat16, mybir.dt.int32, mybir.dt.uint8
F32R = mybir.dt.float32r
ALU = mybir.AluOpType
AF = mybir.ActivationFunctionType

ENGS = ("pe", "act", "dve", "pool", "sp")
DSIZE = {F32: 4, BF16: 2, I32: 4, U8: 1}

D = 2048
H = 8
DK = 256
DV = 512
NTILE = 9
TOK = 1152
PAST_LEN = 16384
CW = 31
NE = 32
DE = 1024
EPS = 1e-6
Q0, K0, V0, G0, CA0, CB0, GR0, GC0 = 0, 2048, 4096, 8192, 12288, 14336, 16384, 18432
IN_COLS = 20480
BLKS = ((0, 512), (512, 1024), (1024, 1152))
LOG_G = [float(np.log1p(-2.0 ** (-5.0 - h))) for h in range(H)]
CDEC_P = [float(np.exp(128 * LOG_G[h])) for h in range(H)]
CDEC_S = [float(np.exp(8 * LOG_G[h])) for h in range(H)]


class Tile:
    def __init__(self, name, h):
        self.name, self.h = name, h
        self.w = None
        self.r = []
        self.sem = None

    def __getitem__(self, idx):
        return self.h[idx]


class Op:
    __slots__ = ("eng", "fn", "deps", "is_dma", "sem", "count", "need_inc", "val")

    def __init__(self, eng, fn, is_dma=False):
        self.eng, self.fn, self.is_dma = eng, fn, is_dma
        self.deps = []
        self.sem = None
        self.count = 0
        self.need_inc = False
        self.val = 0


class Prog:
    def __init__(self, nc):
        self.nc = nc
        self.ops = {e: [] for e in ENGS}
        self.dma_counts = {}
        self.last_dma = {}
        self.stack = contextlib.ExitStack()
        self.out_ops = []
        self.pending = {e: [] for e in ENGS}

    def sb(self, name, shape, dt):
        h = self.stack.enter_context(self.nc.sbuf_tensor(name, list(shape), dt))
        return Tile(name, h)

    def ps(self, name, shape, dt=F32):
        h = self.stack.enter_context(self.nc.psum_tensor(name, list(shape), dt))
        return Tile(name, h)

    def dram(self, name, shape, dt, kind):
        h = self.nc.dram_tensor(name, list(shape), dt, kind=kind)
        return Tile(name, h.ap())

    def _deps(self, o, reads, writes):
        deps = []
        for t in reads:
            if t.w is not None:
                deps.append((t.w, "raw"))
        for t in writes:
            if t.w is not None:
                deps.append((t.w, "waw"))
            for r in t.r:
                deps.append((r, "war"))
        seen = set()
        for d, kind in deps:
            if d is o or id(d) in seen:
                continue
            if (not d.is_dma) and d.eng == o.eng and not o.is_dma:
                if kind != "raw" or o.eng == "pe":
                    continue
            seen.add(id(d))
            o.deps.append(d)
        for d in self.pending[o.eng]:
            if id(d) not in seen and d is not o:
                seen.add(id(d))
                o.deps.append(d)
        self.pending[o.eng] = []
        for t in reads:
            t.r.append(o)
        for t in writes:
            t.w = o
            t.r = []

    def op(self, eng, fn, reads=(), writes=()):
        o = Op(eng, fn)
        self._deps(o, reads, writes)
        self.ops[eng].append(o)
        return o

    def dma(self, eng, fn, reads=(), writes=(), semtile=None, is_output=False):
        o = Op(eng, fn, is_dma=True)
        if semtile.sem is None:
            semtile.sem = "ds_%s" % semtile.name
        o.sem = semtile.sem
        self.dma_counts[o.sem] = self.dma_counts.get(o.sem, 0) + 16
        o.count = self.dma_counts[o.sem]
        self.last_dma[o.sem] = o
        self._deps(o, reads, writes)
        self.ops[eng].append(o)
        if is_output:
            self.out_ops.append(o)
        return o

    def barrier(self):
        deps = []
        for e in ENGS:
            for o in reversed(self.ops[e]):
                if not o.is_dma:
                    deps.append(o)
                    break
        deps += list(self.last_dma.values())
        for e in ENGS:
            self.pending[e] = self.pending[e] + [d for d in deps if d.is_dma or d.eng != e]

    def emit(self):
        nc = self.nc
        for e in ENGS:
            for o in self.ops[e]:
                for d in o.deps:
                    if not d.is_dma:
                        d.need_inc = True
        for e in ENGS:
            c = 0
            for o in self.ops[e]:
                if (not o.is_dma) and o.need_inc:
                    c += 1
                    o.val = c
        sems = {}
        for e in ENGS:
            sems["eng_" + e] = self.stack.enter_context(nc.semaphore("eng_" + e))
        for s in self.dma_counts:
            sems[s] = self.stack.enter_context(nc.semaphore(s))
        self.nsems = len(sems)
        final = {}
        for o in self.out_ops:
            final[o.sem] = max(final.get(o.sem, 0), o.count)

        def emit_eng(ename, eng):
            waited = {}
            for o in self.ops[ename]:
                for d in o.deps:
                    if d.is_dma:
                        s, v = d.sem, d.count
                    else:
                        s, v = "eng_" + d.eng, d.val
                    if waited.get(s, 0) >= v:
                        continue
                    waited[s] = v
                    eng.wait_ge(sems[s], v)
                ins = o.fn(eng)
                if o.is_dma:
                    ins.then_inc(sems[o.sem], 16)
                elif o.need_inc:
                    ins.then_inc(sems["eng_" + ename], 1)
            if ename == "sp":
                for s, v in final.items():
                    if waited.get(s, 0) < v:
                        eng.wait_ge(sems[s], v)

        with nc.Block() as block:
            @block.sync
            def _(e):
                emit_eng("sp", e)

            @block.scalar
            def _(e):
                emit_eng("act", e)

            @block.vector
            def _(e):
                emit_eng("dve", e)

            @block.gpsimd
            def _(e):
                emit_eng("pool", e)

            @block.tensor
            def _(e):
                emit_eng("pe", e)
        self.stack.close()


class Arena:
    def __init__(self, P, nbytes):
        self.P = P
        self.t = P.stack.enter_context(P.nc.sbuf_tensor("arena", [128, nbytes], U8))
        self.nbytes = nbytes

    def view(self, name, off, shape, dt):
        n = int(np.prod(shape)) * DSIZE[dt]
        assert off + n <= self.nbytes, (name, off, n)
        assert off % 4 == 0
        ap = self.t[:, off:off + n].bitcast(dt)
        if len(shape) == 2:
            ap = ap.rearrange("p (a b) -> p a b", a=shape[0])
        elif len(shape) == 3:
            ap = ap.rearrange("p (a b c) -> p a b c", a=shape[0], b=shape[1])
        return Tile(name, ap)


def build(n_experts=NE, stages="ARCMOE", debug=False):
    nc = bass.Bass("TRN2", target_bir_lowering=False)
    P = Prog(nc)
    IN, OUT = "ExternalInput", "ExternalOutput"
    x_own = P.dram("x_own", [TOK, D], F32, IN)
    x_prev = P.dram("x_prev", [1024, D], F32, IN)
    sret = P.dram("sret", [16, H, DK, DV], F32, IN)
    sconv = P.dram("sconv", [16 * 30, D], F32, IN)
    w_in = P.dram("w_in", [D, IN_COLS], F32, IN)
    w_ret_o = P.dram("w_ret_o", [H * DV, D], F32, IN)
    conv_w = P.dram("conv_w", [CW, D], F32, IN)
    vecs = P.dram("vecs", [128, 6, 16], F32, IN)
    norm_f = P.dram("norm_f", [1, D], F32, IN)
    w_conv_o = P.dram("w_conv_o", [D, D], F32, IN)
    w_o = P.dram("w_o", [D, D], F32, IN)
    w_rt = P.dram("w_rt", [D, 36], F32, IN)
    b_rt = P.dram("b_rt", [1, 36], F32, IN)
    w_gate = P.dram("w_gate", [n_experts, D, DE], F32, IN)
    w_up = P.dram("w_up", [n_experts, D, DE], F32, IN)
    w_down = P.dram("w_down", [n_experts, DE, D], F32, IN)
    cs_own_d = P.dram("cs_own", [128, 2, TOK], F32, IN)
    cs_prev_d = P.dram("cs_prev", [128, 2, 1024], F32, IN)
    dec_d = P.dram("dec", [H, 128, 4, 128], F32, IN)
    masks_d = P.dram("masks", [128, 2, 128], F32, IN)
    ident_d = P.dram("ident", [128, 128], F32, IN)
    rowmask_d = P.dram("rowmask", [128, 16], F32, IN)
    mconst_d = P.dram("mconst", [128, 3, 128], F32, IN)

    y_out = P.dram("y_out", [TOK, D], F32, OUT)
    retp_out = P.dram("retp_out", [H, DK, DV], F32, OUT)
    convp_out = P.dram("convp_out", [30, D], F32, OUT)
    rets_out = P.dram("rets_out", [16, H, DK, DV], F32, OUT)
    convs_out = P.dram("convs_out", [16, 30, D], F32, OUT)
    sinit = P.dram("sinit_scr", [H, DK, DV], F32, "Internal")
    ospill = P.dram("ospill_scr", [TOK, H * DV], BF16, OUT if debug else "Internal")

    ident_f = P.sb("ident_f", [128, 128], F32)
    ident_b = P.sb("ident_b", [128, 128], BF16)
    rowmask = P.sb("rowmask_s", [128, 16], F32)
    vec_s = P.sb("vec_s", [128, 6, 16], F32)
    hTh = P.sb("hTh", [128, 16, 128], BF16)
    small = P.sb("small", [128, 16], F32)
    epsb = P.sb("epsb", [128, 1], F32)

    AR = Arena(P, 184 * 1024)
    W = [AR.view("W%d" % i, i * 16384, [16, 512], BF16) for i in range(3)]
    OFF_HT = 49152
    OFF_HEAD = OFF_HT + 36864
    OFF_ROT = OFF_HEAD + 32256
    OFF_S = OFF_ROT + 8192
    OFF_S0 = OFF_S + 6144
    OFF_QK = OFF_S0 + 20480
    OFF_OF = OFF_QK + 3072 + 2048 + 4096
    OFF_END = OFF_OF + 2048
    cs_own = AR.view("cs_own_s", OFF_END, [2, TOK], F32)
    dec_h = AR.view("dec_h", OFF_END + 9216, [4, 128], F32)
    masks = AR.view("masks_s", OFF_END + 11264, [2, 128], F32)
    junk = AR.view("junk", OFF_END + 12800, [2048], BF16)

    pT = [P.ps("pT%d" % i, [128, 512]) for i in range(2)]
    pM = [P.ps("pM%d" % i, [128, 512]) for i in range(4)]
    pS = [P.ps("pS%d" % i, [128, 512]) for i in range(2)]
    cnt = {"m": 0, "t": 0, "s": 0, "w": 0}

    def nextM():
        cnt["m"] += 1
        return pM[cnt["m"] % 4]

    def nextT():
        cnt["t"] += 1
        return pT[cnt["t"] % 2]

    def nextS():
        cnt["s"] += 1
        return pS[cnt["s"] % 2]

    def nextW():
        cnt["w"] += 1
        return W[cnt["w"] % 3]

    def ld(dst, src_ap, eng="sp"):
        P.dma(eng, lambda e: e.dma_start(out=dst[:], in_=src_ap), writes=[dst], semtile=dst)

    ld(ident_f, ident_d[:])
    ld(cs_own, cs_own_d[:])
    ld(masks, masks_d[:])
    ld(rowmask, rowmask_d[:])
    ld(vec_s, vecs[:])
    P.op("dve", lambda e: e.tensor_copy(out=ident_b[:], in_=ident_f[:]), reads=[ident_f], writes=[ident_b])
    P.op("dve", lambda e: e.memset(epsb[:], EPS), writes=[epsb])

    def load_w(slot, src2d, c0=0):
        kc = src2d.shape[0] // 128
        ncols = src2d.shape[1]
        src = src2d.rearrange("(kc p) n -> p kc n", p=128)
        P.dma("pool", lambda e: e.dma_start(out=slot[:, 0:kc, c0:c0 + ncols], in_=src), writes=[slot], semtile=slot)

    def norm_to_T(x_dram, n_tiles, dstT, wrow, xin, xb):
        for t in range(n_tiles):
            xi = xin[t % 2]
            P.dma("sp", lambda e, xi=xi, t=t: e.dma_start(out=xi[:], in_=x_dram[t * 128:(t + 1) * 128, :]), writes=[xi], semtile=xi)
            ss = small
            P.op("act", lambda e, xi=xi: e.activation(out=junk[:], in_=xi[:], func=AF.Square, accum_out=small[:, 0:1]),
                 reads=[xi], writes=[junk, small])
            P.op("act", lambda e: e.activation(out=small[:, 1:2], in_=small[:, 0:1], func=AF.Sqrt, bias=epsb[:], scale=1.0 / D),
                 reads=[small, epsb], writes=[small])
            P.op("dve", lambda e: e.reciprocal(out=small[:, 2:3], in_=small[:, 1:2]), reads=[small], writes=[small])
            P.op("dve", lambda e, xi=xi: e.tensor_scalar(out=xb[:], in0=xi[:], scalar1=small[:, 2:3], scalar2=None, op0=ALU.mult),
                 reads=[xi, small], writes=[xb])
            for g in range(2):
                pt = nextT()
                ptb = pt[:].bitcast(BF16)
                for j in range(8):
                    kc = g * 8 + j
                    P.op("pe", lambda e, ptb=ptb, j=j, kc=kc: e.transpose(out=ptb[:, j * 128:(j + 1) * 128], in_=xb[:, kc * 128:(kc + 1) * 128], identity=ident_b[:]),
                         reads=[xb, ident_b], writes=[pt])
                P.op("dve", lambda e, ptb=ptb, g=g, t=t: e.tensor_tensor(
                    out=dstT[:, g * 8:(g + 1) * 8, t * 128:(t + 1) * 128],
                    in0=ptb.rearrange("p (a b) -> p a b", a=8),
                    in1=vec_s[:, wrow, g * 8:(g + 1) * 8].unsqueeze(2).to_broadcast([128, 8, 128]), op=ALU.mult),
                    reads=[pt, vec_s], writes=[dstT])

    def fm_proj(slot, c0, srcT, blks, evac):
        for bi, (t0, t1) in enumerate(blks):
            ps = nextM()
            for kc in range(16):
                P.op("pe", lambda e, ps=ps, kc=kc, t0=t0, t1=t1: e.matmul(ps[:, 0:t1 - t0], lhsT=slot[:, kc, c0:c0 + 128], rhs=srcT[:, kc, t0:t1], start=(kc == 0), stop=(kc == 15)),
                     reads=[slot, srcT], writes=[ps])
            evac(ps, bi, (t0, t1))

    def tm_proj(slot, ncols, srcT, tile, evac):
        ps = nextM()
        for kc in range(16):
            P.op("pe", lambda e, ps=ps, kc=kc: e.matmul(ps[:, 0:ncols], lhsT=srcT[:, kc, tile * 128:(tile + 1) * 128], rhs=slot[:, kc, 0:ncols], start=(kc == 0), stop=(kc == 15)),
                 reads=[slot, srcT], writes=[ps])
        evac(ps)

    def rotary_pair(p1, p2, n, cs, t0, decsel, out_fn, rot):
        cos = cs[:, 0, t0:t0 + n]
        sin = cs[:, 1, t0:t0 + n]
        ta, tb, tc, td = rot
        P.op("dve", lambda e: e.tensor_tensor(out=ta[:, 0:n], in0=p1[:, 0:n], in1=cos, op=ALU.mult), reads=[p1, cs], writes=[ta])
        P.op("dve", lambda e: e.tensor_tensor(out=tb[:, 0:n], in0=p2[:, 0:n], in1=sin, op=ALU.mult), reads=[p2, cs], writes=[tb])
        P.op("dve", lambda e: e.tensor_tensor(out=tc[:, 0:n], in0=p1[:, 0:n], in1=sin, op=ALU.mult), reads=[p1, cs], writes=[tc])
        P.op("dve", lambda e: e.tensor_tensor(out=td[:, 0:n], in0=p2[:, 0:n], in1=cos, op=ALU.mult), reads=[p2, cs], writes=[td])
        P.op("dve", lambda e: e.tensor_tensor(out=ta[:, 0:n], in0=ta[:, 0:n], in1=tb[:, 0:n], op=ALU.subtract), reads=[ta, tb], writes=[ta])
        P.op("dve", lambda e: e.tensor_tensor(out=tc[:, 0:n], in0=tc[:, 0:n], in1=td[:, 0:n], op=ALU.add), reads=[tc, td], writes=[tc])
        out_fn(0, ta)
        out_fn(1, tc)

    rot = [AR.view("rot%d" % i, OFF_ROT + i * 2048, [512], F32) for i in range(4)]
    xin = [AR.view("xin%d" % i, OFF_HEAD + i * 8192, [2048], F32) for i in range(2)]
    xb = AR.view("xb", OFF_HEAD + 16384, [2048], BF16)

    def dec_load(h):
        P.dma("sp", lambda e: e.dma_start(out=dec_h[:], in_=dec_d[h]), writes=[dec_h], semtile=dec_h)

    if "A" in stages:
        hTp = AR.view("hTp", OFF_HT, [16, 1024], BF16)
        cs_prev = AR.view("cs_prev", OFF_S0, [2, 1024], F32)
        ld(cs_prev, cs_prev_d[:])
        norm_to_T(x_prev, 8, hTp, 0, xin, xb)
        P.op("dve", lambda e: e.tensor_copy(out=hTh[:], in_=hTp[:, :, 896:1024]), reads=[hTp], writes=[hTh])
        P.barrier()
        kTa = AR.view("kTa", OFF_HEAD, [2, 1024], BF16)
        ktokA = AR.view("ktokA", OFF_HEAD + 4096, [8, 256], BF16)
        vA = AR.view("vA", OFF_HEAD + 8192, [8, 512], BF16)
        sstage = AR.view("sstageA", OFF_S, [512], F32)
        for h in range(H):
            dec_load(h)
            wk = nextW()
            load_w(wk, w_in[:, K0 + h * 256:K0 + (h + 1) * 256])
            wv = nextW()
            load_w(wv, w_in[:, V0 + h * 512:V0 + (h + 1) * 512])
            for bi, (t0, t1) in enumerate(((0, 512), (512, 1024))):
                pp = []
                for dc in range(2):
                    ps = nextM()
                    for kc in range(16):
                        P.op("pe", lambda e, ps=ps, kc=kc, dc=dc, t0=t0, t1=t1, wk=wk: e.matmul(ps[:, 0:512], lhsT=wk[:, kc, dc * 128:(dc + 1) * 128], rhs=hTp[:, kc, t0:t1], start=(kc == 0), stop=(kc == 15)),
                             reads=[wk, hTp], writes=[ps])
                    pp.append(ps)

                def outk(which, src, t0=t0):
                    P.op("dve", lambda e: e.tensor_tensor(out=kTa[:, which, t0:t0 + 512].rearrange("p (a b) -> p a b", a=4),
                                                          in0=src[:, 0:512].rearrange("p (a b) -> p a b", a=4),
                                                          in1=dec_h[:, 1, :].unsqueeze(1).to_broadcast([128, 4, 128]), op=ALU.mult),
                         reads=[src, dec_h], writes=[kTa])
                rotary_pair(pp[0], pp[1], 512, cs_prev, t0, None, outk, rot)
            for n in range(8):
                pt = nextT()
                ptb = pt[:].bitcast(BF16)
                for dc in range(2):
                    P.op("pe", lambda e, ptb=ptb, dc=dc, n=n: e.transpose(out=ptb[:, dc * 128:(dc + 1) * 128], in_=kTa[:, dc, n * 128:(n + 1) * 128], identity=ident_b[:]),
                         reads=[kTa, ident_b], writes=[pt])
                sc = CDEC_P[h] * float(np.exp(128 * (7 - n) * LOG_G[h]))
                P.op("act", lambda e, ptb=ptb, n=n, sc=sc: e.activation(out=ktokA[:, n, :], in_=ptb[:, 0:256], func=AF.Copy, scale=sc),
                     reads=[pt], writes=[ktokA])
                tm_proj(wv, 512, hTp, n, lambda ps, n=n: P.op("act", lambda e: e.activation(out=vA[:, n, :], in_=ps[:, 0:512], func=AF.Copy), reads=[ps], writes=[vA]))
            for dc in range(2):
                ps = nextS()
                for n in range(8):
                    P.op("pe", lambda e, ps=ps, n=n, dc=dc: e.matmul(ps[:, 0:512], lhsT=ktokA[:, n, dc * 128:(dc + 1) * 128], rhs=vA[:, n, :], start=(n == 0), stop=(n == 7)),
                         reads=[ktokA, vA], writes=[ps])
                P.op("act", lambda e, ps=ps: e.activation(out=sstage[:], in_=ps[:, 0:512], func=AF.Copy), reads=[ps], writes=[sstage])
                P.dma("sp", lambda e, dc=dc, h=h: e.dma_start(out=sinit[h, dc * 128:(dc + 1) * 128, :], in_=sstage[:]), reads=[sstage], writes=[sinit], semtile=sstage)
        P.barrier()

    hT = AR.view("hT", OFF_HT, [16, TOK], BF16)
    if "R" in stages:
        norm_to_T(x_own, NTILE, hT, 0, xin, xb)
        P.barrier()
        qT = AR.view("qT", OFF_HEAD, [2, TOK], BF16)
        kT = AR.view("kT", OFF_HEAD + 4608, [2, TOK], BF16)
        ktok = AR.view("ktok", OFF_HEAD + 9216, [NTILE, 256], BF16)
        vv = AR.view("vv", OFF_HEAD + 13824, [NTILE, 512], BF16)
        sg = AR.view("sg", OFF_HEAD + 23040, [NTILE, 512], BF16)
        Sf = AR.view("Sf", OFF_S, [2, 512], F32)
        Sb = AR.view("Sb", OFF_S + 4096, [2, 512], BF16)
        S0 = [AR.view("S0_%d" % i, OFF_S0 + i * 4096, [2, 512], F32) for i in range(3)]
        So = [AR.view("So_%d" % i, OFF_S0 + 12288 + i * 4096, [2, 512], F32) for i in range(2)]
        S0b = [AR.view("S0b%d" % i, OFF_QK + 3072 + 2048 + i * 2048, [2, 512], BF16) for i in range(2)]
        Qz = [AR.view("Qz%d" % i, OFF_QK + i * 512, [2, 128], BF16) for i in range(2)]
        Kz = [AR.view("Kz%d" % i, OFF_QK + 2048 + i * 512, [256], BF16) for i in range(2)]
        ofs = [AR.view("of%d" % i, OFF_OF + i * 1024, [512], BF16) for i in range(2)]
        attm = [AR.view("attm%d" % i, OFF_END + 12288 + i * 256, [128], BF16) for i in range(2)]
        for h in range(H):
            dec_load(h)
            wqk = nextW()
            load_w(wqk, w_in[:, Q0 + h * 256:Q0 + (h + 1) * 256], 0)
            load_w(wqk, w_in[:, K0 + h * 256:K0 + (h + 1) * 256], 256)
            wv = nextW()
            load_w(wv, w_in[:, V0 + h * 512:V0 + (h + 1) * 512])
            wg = nextW()
            load_w(wg, w_in[:, G0 + h * 512:G0 + (h + 1) * 512])
            if "A" in stages:
                P.dma("sp", lambda e, h=h: e.dma_start(out=Sf[:], in_=sinit[h].rearrange("(dc p) e -> p dc e", p=128)), reads=[sinit], writes=[Sf], semtile=Sf)
            else:
                P.op("dve", lambda e: e.memset(Sf[:], 0.0), writes=[Sf])
            P.op("act", lambda e: e.activation(out=Sb[:], in_=Sf[:], func=AF.Copy), reads=[Sf], writes=[Sb])
            for which, dstT in ((0, qT), (1, kT)):
                for bi, (t0, t1) in enumerate(BLKS):
                    n = t1 - t0
                    pp = []
                    for dc in range(2):
                        ps = nextM()
                        c0 = which * 256 + dc * 128
                        for kc in range(16):
                            P.op("pe", lambda e, ps=ps, kc=kc, c0=c0, t0=t0, t1=t1, wqk=wqk: e.matmul(ps[:, 0:t1 - t0], lhsT=wqk[:, kc, c0:c0 + 128], rhs=hT[:, kc, t0:t1], start=(kc == 0), stop=(kc == 15)),
                                 reads=[wqk, hT], writes=[ps])
                        pp.append(ps)
                    drow = which + (2 if bi == 2 else 0)

                    def outqk(dcw, src, t0=t0, n=n, drow=drow, dstT=dstT, bi=bi, which=which):
                        a = n // 128
                        P.op("dve", lambda e: e.tensor_tensor(out=dstT[:, dcw, t0:t0 + n].rearrange("p (a b) -> p a b", a=a),
                                                              in0=src[:, 0:n].rearrange("p (a b) -> p a b", a=a),
                                                              in1=dec_h[:, drow, :].unsqueeze(1).to_broadcast([128, a, 128]), op=ALU.mult),
                             reads=[src, dec_h], writes=[dstT])
                    rotary_pair(pp[0], pp[1], n, cs_own, t0, None, outqk, rot)
            for t in range(NTILE):
                pt = nextT()
                ptb = pt[:].bitcast(BF16)
                for dc in range(2):
                    P.op("pe", lambda e, ptb=ptb, dc=dc, t=t: e.transpose(out=ptb[:, dc * 128:(dc + 1) * 128], in_=kT[:, dc, t * 128:(t + 1) * 128], identity=ident_b[:]),
                         reads=[kT, ident_b], writes=[pt])
                sc = CDEC_P[h] if t < 8 else CDEC_S[h]
                P.op("act", lambda e, ptb=ptb, t=t, sc=sc: e.activation(out=ktok[:, t, :], in_=ptb[:, 0:256], func=AF.Copy, scale=sc),
                     reads=[pt], writes=[ktok])
                tm_proj(wv, 512, hT, t, lambda ps, t=t: P.op("act", lambda e: e.activation(out=vv[:, t, :], in_=ps[:, 0:512], func=AF.Copy), reads=[ps], writes=[vv]))
                tm_proj(wg, 512, hT, t, lambda ps, t=t: P.op("act", lambda e: e.activation(out=sg[:, t, :], in_=ps[:, 0:512], func=AF.Silu), reads=[ps], writes=[sg]))
            for t in range(NTILE):
                tc0, tc1 = t * 128, (t + 1) * 128
                pa = nextM()
                for dc in range(2):
                    P.op("pe", lambda e, pa=pa, dc=dc, tc0=tc0, tc1=tc1: e.matmul(pa[:, 0:128], lhsT=kT[:, dc, tc0:tc1], rhs=qT[:, dc, tc0:tc1], start=(dc == 0), stop=(dc == 1)),
                         reads=[kT, qT], writes=[pa])
                am = attm[t % 2]
                mrow = 0 if t < 8 else 1
                P.op("dve", lambda e, pa=pa, am=am, mrow=mrow: e.tensor_tensor(out=am[:], in0=pa[:, 0:128], in1=masks[:, mrow, :], op=ALU.mult),
                     reads=[pa, masks], writes=[am])
                po = nextM()
                P.op("pe", lambda e, po=po, am=am, t=t: e.matmul(po[:, 0:512], lhsT=am[:], rhs=vv[:, t, :], start=True, stop=False),
                     reads=[am, vv], writes=[po])
                if t < 8:
                    for dc in range(2):
                        P.op("pe", lambda e, po=po, dc=dc, tc0=tc0, tc1=tc1: e.matmul(po[:, 0:512], lhsT=qT[:, dc, tc0:tc1], rhs=Sb[:, dc, :], start=False, stop=(dc == 1)),
                             reads=[qT, Sb], writes=[po])
                    for dc in range(2):
                        ps = nextS()
                        P.op("pe", lambda e, ps=ps, dc=dc, t=t: e.matmul(ps[:, 0:512], lhsT=ktok[:, t, dc * 128:(dc + 1) * 128], rhs=vv[:, t, :], start=True, stop=True),
                             reads=[ktok, vv], writes=[ps])
                        P.op("dve", lambda e, ps=ps, dc=dc, h=h: e.scalar_tensor_tensor(out=Sf[:, dc, :], in0=Sf[:, dc, :], scalar=CDEC_P[h], in1=ps[:, 0:512], op0=ALU.mult, op1=ALU.add),
                             reads=[Sf, ps], writes=[Sf])
                    if t < 7:
                        P.op("act", lambda e: e.activation(out=Sb[:], in_=Sf[:], func=AF.Copy), reads=[Sf], writes=[Sb])
                    else:
                        P.dma("sp", lambda e, h=h: e.dma_start(out=retp_out[h].rearrange("(dc p) e -> p dc e", p=128), in_=Sf[:]), reads=[Sf], semtile=Sf, is_output=True)
                else:
                    for bb in range(16):
                        s0 = S0[bb % 3]
                        P.dma("sp", lambda e, s0=s0, bb=bb, h=h: e.dma_start(out=s0[:], in_=sret[bb, h].rearrange("(dc p) e -> p dc e", p=128)), writes=[s0], semtile=s0)
                        qz = Qz[bb % 2]
                        s0b = S0b[bb % 2]
                        P.op("act", lambda e, s0=s0, s0b=s0b: e.activation(out=s0b[:], in_=s0[:], func=AF.Copy), reads=[s0], writes=[s0b])
                        P.op("dve", lambda e, qz=qz: e.memset(qz[:], 0.0), writes=[qz])
                        P.op("dve", lambda e, qz=qz, bb=bb: e.tensor_copy(out=qz[:, :, bb * 8:(bb + 1) * 8], in_=qT[:, :, 1024 + bb * 8:1024 + (bb + 1) * 8]), reads=[qT], writes=[qz])
                        for dc in range(2):
                            P.op("pe", lambda e, po=po, dc=dc, s0b=s0b, qz=qz, bb=bb: e.matmul(po[:, 0:512], lhsT=qz[:, dc, :], rhs=s0b[:, dc, :], start=False, stop=(dc == 1 and bb == 15)),
                                 reads=[qz, s0b], writes=[po])
                        kz = Kz[bb % 2]
                        P.op("dve", lambda e, kz=kz, bb=bb, t=t: e.tensor_scalar(out=kz[:], in0=ktok[:, t, :], scalar1=rowmask[:, bb:bb + 1], scalar2=None, op0=ALU.mult),
                             reads=[ktok, rowmask], writes=[kz])
                        so = So[bb % 2]
                        for dc in range(2):
                            ps = nextS()
                            P.op("pe", lambda e, ps=ps, dc=dc, kz=kz, t=t: e.matmul(ps[:, 0:512], lhsT=kz[:, dc * 128:(dc + 1) * 128], rhs=vv[:, t, :], start=True, stop=True),
                                 reads=[kz, vv], writes=[ps])
                            P.op("dve", lambda e, ps=ps, dc=dc, s0=s0, so=so, h=h: e.scalar_tensor_tensor(out=so[:, dc, :], in0=s0[:, dc, :], scalar=CDEC_S[h], in1=ps[:, 0:512], op0=ALU.mult, op1=ALU.add),
                                 reads=[s0, ps], writes=[so])
                        P.dma("sp", lambda e, so=so, bb=bb, h=h: e.dma_start(out=rets_out[bb, h].rearrange("(dc p) e -> p dc e", p=128), in_=so[:]), reads=[so], semtile=so, is_output=True)
                P.op("act", lambda e, po=po: e.activation(out=junk[:, 0:512], in_=po[:, 0:512], func=AF.Square, accum_out=small[:, 4:5]),
                     reads=[po], writes=[junk, small])
                P.op("act", lambda e: e.activation(out=small[:, 5:6], in_=small[:, 4:5], func=AF.Sqrt, bias=epsb[:], scale=1.0 / DV),
                     reads=[small, epsb], writes=[small])
                P.op("dve", lambda e: e.reciprocal(out=small[:, 6:7], in_=small[:, 5:6]), reads=[small], writes=[small])
                of = ofs[t % 2]
                P.op("dve", lambda e, po=po, of=of, t=t: e.scalar_tensor_tensor(out=of[:], in0=po[:, 0:512], scalar=small[:, 6:7], in1=sg[:, t, :], op0=ALU.mult, op1=ALU.mult),
                     reads=[po, small, sg], writes=[of])
                P.dma("sp", lambda e, of=of, t=t, h=h: e.dma_start(out=ospill[t * 128:(t + 1) * 128, h * 512:(h + 1) * 512], in_=of[:]), reads=[of], writes=[ospill], semtile=of, is_output=debug)
        P.barrier()

    OFF_Z = OFF_HEAD
    OFF_Y = OFF_Z + 36864
    OFF_X = OFF_Y + 36864
    dbg_fm = P.dram("dbg_fm", [128, 16, TOK], BF16, OUT) if debug else None
    if "C" in stages:
        cf = AR.view("cf", OFF_Z, [16, TOK], BF16)
        fmY = AR.view("fmY", OFF_Y, [16, TOK], BF16)
        def cset(i):
            b0 = OFF_Y + i * 17984
            return dict(uP=AR.view("uP%d" % i, b0, [1056], F32), uPb=AR.view("uPb%d" % i, b0 + 4224, [1056], BF16),
                        uS=AR.view("uS%d" % i, b0 + 6400, [16, 38], F32), uSb=AR.view("uSb%d" % i, b0 + 8832, [16, 38], BF16),
                        dg=AR.view("dg%d" % i, b0 + 10048, [31, 128], BF16))
        csets = [cset(0), cset(1)]
        cwT = AR.view("cwT", OFF_X, [16, 31], F32)
        sgt = [AR.view("sgt%d" % i, OFF_X + 2048 + i * 2048, [512], F32) for i in range(2)]
        scs = AR.view("scs", OFF_X + 6144, [4, 128], F32)
        cwrow = AR.view("cwrow", OFF_X + 8192, [2048], F32)
        strow = [AR.view("strow%d" % i, OFF_X + 16384 + i * 512, [128], F32) for i in range(2)]
        P.dma("sp", lambda e: e.dma_start(out=cwrow[0:CW, :], in_=conv_w[:]), writes=[cwrow], semtile=cwrow)
        for c in range(16):
            pt = nextT()
            P.op("pe", lambda e, pt=pt, c=c: e.transpose(out=pt[:, 0:CW], in_=cwrow[0:CW, c * 128:(c + 1) * 128], identity=ident_f[0:CW, 0:CW]),
                 reads=[cwrow, ident_f], writes=[pt])
            P.op("act", lambda e, pt=pt, c=c: e.activation(out=cwT[:, c, :], in_=pt[:, 0:CW], func=AF.Copy), reads=[pt], writes=[cwT])
        cpy = Tile("cpy", None)
        P.dma("sp", lambda e: e.dma_start(out=convs_out[:, 0:22, :], in_=sconv[:].rearrange("(b w) d -> b w d", w=30)[:, 8:30, :]), semtile=cpy, is_output=True)
        for c in range(16):
            if c % 4 == 0:
                wa = nextW()
                load_w(wa, w_in[:, CA0 + c * 128:CA0 + (c + 4) * 128])
                wb_ = nextW()
                load_w(wb_, w_in[:, CB0 + c * 128:CB0 + (c + 4) * 128])
            cc = (c % 4) * 128
            cs_ = csets[c % 2]
            uP, uPb, uS, uSb, dg = cs_["uP"], cs_["uPb"], cs_["uS"], cs_["uSb"], cs_["dg"]
            pst = nextT()
            for a in range(4):
                rows = 128 if a < 3 else 96
                st = strow[a % 2]
                P.dma("sp", lambda e, st=st, a=a, rows=rows, c=c: e.dma_start(out=st[0:rows, :], in_=sconv[a * 128:a * 128 + rows, c * 128:(c + 1) * 128]), writes=[st], semtile=st)
                P.op("pe", lambda e, pst=pst, st=st, a=a, rows=rows: e.transpose(out=pst[:, a * 128:a * 128 + rows], in_=st[0:rows, :], identity=ident_f[0:rows, 0:rows]),
                     reads=[st, ident_f], writes=[pst])
            P.op("act", lambda e, pst=pst, uS=uS: e.activation(out=uS[:, :, 0:30], in_=pst[:, 0:480].rearrange("p (b w) -> p b w", w=30), func=AF.Copy), reads=[pst], writes=[uS])
            segs = ((hTh, 0, 128, "h"), (hT, 0, 512, "p0"), (hT, 512, 1024, "p1"), (hT, 1024, 1152, "s"))
            for si, (src, t0, t1, kind) in enumerate(segs):
                n = t1 - t0
                pa_ = nextM()
                pb_ = nextM()
                for (pp_, wsl) in ((pa_, wa), (pb_, wb_)):
                    for kc in range(16):
                        P.op("pe", lambda e, pp_=pp_, wsl=wsl, kc=kc, cc=cc, src=src, t0=t0, t1=t1: e.matmul(pp_[:, 0:t1 - t0], lhsT=wsl[:, kc, cc:cc + 128], rhs=src[:, kc, t0:t1], start=(kc == 0), stop=(kc == 15)),
                             reads=[wsl, src], writes=[pp_])
                sgx = sgt[si % 2]
                P.op("act", lambda e, pb_=pb_, sgx=sgx, n=n: e.activation(out=sgx[:, 0:n], in_=pb_[:, 0:n], func=AF.Sigmoid), reads=[pb_], writes=[sgx])
                if kind == "h":
                    P.op("dve", lambda e, pa_=pa_, sgx=sgx, uP=uP: e.tensor_tensor(out=uP[:, 0:30], in0=pa_[:, 98:128], in1=sgx[:, 98:128], op=ALU.mult), reads=[pa_, sgx], writes=[uP])
                elif kind == "s":
                    P.op("dve", lambda e, pa_=pa_, sgx=sgx, uS=uS: e.tensor_tensor(out=uS[:, :, 30:38], in0=pa_[:, 0:128].rearrange("p (b i) -> p b i", i=8), in1=sgx[:, 0:128].rearrange("p (b i) -> p b i", i=8), op=ALU.mult),
                         reads=[pa_, sgx], writes=[uS])
                else:
                    P.op("dve", lambda e, pa_=pa_, sgx=sgx, t0=t0, uP=uP: e.tensor_tensor(out=uP[:, 30 + t0:30 + t0 + 512], in0=pa_[:, 0:512], in1=sgx[:, 0:512], op=ALU.mult), reads=[pa_, sgx], writes=[uP])
            P.op("act", lambda e, uP=uP, uPb=uPb: e.activation(out=uPb[:, 0:1054], in_=uP[:, 0:1054], func=AF.Copy), reads=[uP], writes=[uPb])
            P.op("dve", lambda e, uS=uS, uSb=uSb: e.tensor_copy(out=uSb[:], in_=uS[:]), reads=[uS], writes=[uSb])
            pt = nextT()
            P.op("pe", lambda e, pt=pt, uP=uP: e.transpose(out=pt[0:30, 0:128], in_=uP[:, 1024:1054], identity=ident_f[:]), reads=[uP, ident_f], writes=[pt])
            P.op("act", lambda e, pt=pt: e.activation(out=scs[0:30, 0, :], in_=pt[0:30, 0:128], func=AF.Copy), reads=[pt], writes=[scs])
            P.dma("sp", lambda e, c=c: e.dma_start(out=convp_out[:, c * 128:(c + 1) * 128], in_=scs[0:30, 0, :]), reads=[scs], semtile=scs, is_output=True)
            P.op("act", lambda e, uS=uS: e.activation(out=scs[:, 1, :].rearrange("p (b i) -> p b i", i=8), in_=uS[:, :, 30:38], func=AF.Copy), reads=[uS], writes=[scs])
            pt2 = nextT()
            P.op("pe", lambda e, pt2=pt2: e.transpose(out=pt2[:, 0:128], in_=scs[:, 1, :], identity=ident_f[:]), reads=[scs, ident_f], writes=[pt2])
            P.op("act", lambda e, pt2=pt2: e.activation(out=scs[:, 2, :], in_=pt2[:, 0:128], func=AF.Copy), reads=[pt2], writes=[scs])
            for bb in range(16):
                P.dma("sp", lambda e, bb=bb, c=c: e.dma_start(out=convs_out[bb, 22:30, c * 128:(c + 1) * 128], in_=scs[bb * 8:(bb + 1) * 8, 2, :]), reads=[scs], semtile=scs, is_output=True)
            for tap in range(CW):
                if tap % 2 == 0:
                    P.op("dve", lambda e, tap=tap, c=c, dg=dg: e.tensor_scalar(out=dg[:, tap, :], in0=ident_f[:], scalar1=cwT[:, c, tap:tap + 1], scalar2=None, op0=ALU.mult),
                         reads=[ident_f, cwT], writes=[dg])
                else:
                    P.op("act", lambda e, tap=tap, c=c, dg=dg: e.activation(out=dg[:, tap, :], in_=ident_f[:], func=AF.Identity, scale=cwT[:, c, tap:tap + 1]),
                         reads=[ident_f, cwT], writes=[dg])
            for (t0, n, kind) in ((0, 512, "p"), (512, 512, "p"), (1024, 128, "s")):
                pc = nextM()
                for tap in range(CW):
                    if kind == "p":
                        P.op("pe", lambda e, pc=pc, tap=tap, t0=t0, dg=dg, uPb=uPb: e.matmul(pc[:, 0:512], lhsT=dg[:, tap, :], rhs=uPb[:, t0 + tap:t0 + tap + 512], start=(tap == 0), stop=(tap == CW - 1)),
                             reads=[dg, uPb], writes=[pc])
                    else:
                        P.op("pe", lambda e, pc=pc, tap=tap, dg=dg, uSb=uSb: e.matmul(pc[:, 0:128], lhsT=dg[:, tap, :], rhs=uSb[:, :, tap:tap + 8], start=(tap == 0), stop=(tap == CW - 1)),
                             reads=[dg, uSb], writes=[pc])
                P.op("act", lambda e, pc=pc, t0=t0, n=n, c=c: e.activation(out=cf[:, c, t0:t0 + n], in_=pc[:, 0:n], func=AF.Identity, bias=vec_s[:, 2, c:c + 1]),
                     reads=[pc, vec_s], writes=[cf])
        P.barrier()
        ones_b = AR.view("ones_b", OFF_Y, [128], BF16)
        sq = [AR.view("sq%d" % i, OFF_Y + 256 + i * 1024, [512], BF16) for i in range(2)]
        mu_t = AR.view("mu_t", OFF_Y + 2304, [TOK], F32)
        rs_t = AR.view("rs_t", OFF_Y + 2304 + 4608, [TOK], F32)
        lt = [AR.view("lt%d" % i, OFF_Y + 11520 + i * 2048, [512], F32) for i in range(2)]
        epsw = AR.view("epsw", OFF_Y + 15616, [1], F32)
        P.op("dve", lambda e: e.memset(ones_b[:], 1.0), writes=[ones_b])
        P.op("dve", lambda e: e.memset(epsw[:], EPS), writes=[epsw])
        for (t0, t1) in BLKS:
            n = t1 - t0
            p1 = nextM()
            p2 = nextM()
            for c in range(16):
                P.op("pe", lambda e, p1=p1, c=c, t0=t0, t1=t1: e.matmul(p1[:, 0:t1 - t0], lhsT=ones_b[:], rhs=cf[:, c, t0:t1], start=(c == 0), stop=(c == 15)),
                     reads=[ones_b, cf], writes=[p1])
                sqx = sq[c % 2]
                P.op("act", lambda e, sqx=sqx, c=c, t0=t0, t1=t1: e.activation(out=sqx[:, 0:t1 - t0], in_=cf[:, c, t0:t1], func=AF.Square), reads=[cf], writes=[sqx])
                P.op("pe", lambda e, p2=p2, sqx=sqx, c=c, n=n: e.matmul(p2[:, 0:n], lhsT=ones_b[:], rhs=sqx[:, 0:n], start=(c == 0), stop=(c == 15)),
                     reads=[ones_b, sqx], writes=[p2])
            P.op("act", lambda e, p1=p1, t0=t0, n=n: e.activation(out=mu_t[:, t0:t0 + n], in_=p1[:, 0:n], func=AF.Copy, scale=1.0 / D), reads=[p1], writes=[mu_t])
            l0 = lt[0]
            P.op("dve", lambda e, t0=t0, n=n, l0=l0: e.tensor_tensor(out=l0[:, 0:n], in0=mu_t[:, t0:t0 + n], in1=mu_t[:, t0:t0 + n], op=ALU.mult), reads=[mu_t], writes=[l0])
            P.op("dve", lambda e, p2=p2, n=n, l0=l0: e.scalar_tensor_tensor(out=l0[:, 0:n], in0=p2[:, 0:n], scalar=1.0 / D, in1=l0[:, 0:n], op0=ALU.mult, op1=ALU.subtract), reads=[p2, l0], writes=[l0])
            P.op("act", lambda e, n=n, l0=l0: e.activation(out=l0[:, 0:n], in_=l0[:, 0:n], func=AF.Sqrt, bias=epsw[:], scale=1.0), reads=[l0, epsw], writes=[l0])
            P.op("dve", lambda e, t0=t0, n=n, l0=l0: e.reciprocal(out=rs_t[:, t0:t0 + n], in_=l0[:, 0:n]), reads=[l0], writes=[rs_t])
        for c in range(16):
            for (t0, t1) in BLKS:
                n = t1 - t0
                lx = lt[(c * 3 + (t0 // 512)) % 2]
                P.op("dve", lambda e, lx=lx, c=c, t0=t0, t1=t1: e.tensor_tensor(out=lx[:, 0:t1 - t0], in0=cf[:, c, t0:t1], in1=mu_t[:, t0:t1], op=ALU.subtract), reads=[cf, mu_t], writes=[lx])
                P.op("dve", lambda e, lx=lx, t0=t0, t1=t1: e.tensor_tensor(out=lx[:, 0:t1 - t0], in0=lx[:, 0:t1 - t0], in1=rs_t[:, t0:t1], op=ALU.mult), reads=[lx, rs_t], writes=[lx])
                P.op("act", lambda e, lx=lx, c=c, t0=t0, t1=t1: e.activation(out=cf[:, c, t0:t1], in_=lx[:, 0:t1 - t0], func=AF.Silu, bias=vec_s[:, 4, c:c + 1], scale=vec_s[:, 3, c:c + 1]),
                     reads=[lx, vec_s], writes=[cf])
        P.barrier()
        for c4 in range(4):
            wsl = nextW()
            load_w(wsl, w_in[:, GC0 + c4 * 512:GC0 + (c4 + 1) * 512])
            for j in range(4):
                c = c4 * 4 + j
                fm_proj(wsl, j * 128, hT, BLKS, lambda ps, bi, tt, c=c: P.op("act", lambda e: e.activation(out=fmY[:, c, tt[0]:tt[1]], in_=ps[:, 0:tt[1] - tt[0]], func=AF.Sigmoid), reads=[ps], writes=[fmY]))
        for c4 in range(4):
            wsl = nextW()
            load_w(wsl, w_conv_o[:, c4 * 512:(c4 + 1) * 512])
            for j in range(4):
                c = c4 * 4 + j
                fm_proj(wsl, j * 128, cf, BLKS, lambda ps, bi, tt, c=c: P.op("dve", lambda e: e.tensor_tensor(out=fmY[:, c, tt[0]:tt[1]], in0=ps[:, 0:tt[1] - tt[0]], in1=fmY[:, c, tt[0]:tt[1]], op=ALU.mult), reads=[ps, fmY], writes=[fmY]))
        P.barrier()
        fmZ = AR.view("fmZ", OFF_Z, [16, TOK], BF16)
        for c4 in range(4):
            wsl = nextW()
            load_w(wsl, w_in[:, GR0 + c4 * 512:GR0 + (c4 + 1) * 512])
            for j in range(4):
                c = c4 * 4 + j
                fm_proj(wsl, j * 128, hT, BLKS, lambda ps, bi, tt, c=c: P.op("act", lambda e: e.activation(out=fmZ[:, c, tt[0]:tt[1]], in_=ps[:, 0:tt[1] - tt[0]], func=AF.Sigmoid), reads=[ps], writes=[fmZ]))
        P.barrier()

    if "M" in stages:
        oT = AR.view("oT", OFF_HT, [32, 512], BF16)
        orow = [AR.view("orow%d" % i, OFF_X + i * 8192, [4096], BF16) for i in range(2)]
        mt = [AR.view("mt%d" % i, OFF_X + 16384 + i * 2048, [512], F32) for i in range(2)]
        Wr = [Tile("Wr%d" % i, W[i][:].rearrange("p a b -> p (a b)").rearrange("p (a b) -> p a b", a=32)) for i in range(3)]
        for bi, (t0, t1) in enumerate(BLKS):
            n = t1 - t0
            for ti in range(n // 128):
                t = t0 // 128 + ti
                orw = orow[t % 2]
                P.dma("sp", lambda e, orw=orw, t=t: e.dma_start(out=orw[:], in_=ospill[t * 128:(t + 1) * 128, :]), reads=[ospill], writes=[orw], semtile=orw)
                for g in range(4):
                    pt = nextT()
                    ptb = pt[:].bitcast(BF16)
                    for j in range(8):
                        kc = g * 8 + j
                        P.op("pe", lambda e, ptb=ptb, j=j, kc=kc, orw=orw: e.transpose(out=ptb[:, j * 128:(j + 1) * 128], in_=orw[:, kc * 128:(kc + 1) * 128], identity=ident_b[:]),
                             reads=[orw, ident_b], writes=[pt])
                    P.op("act", lambda e, ptb=ptb, g=g, ti=ti: e.activation(out=oT[:, g * 8:(g + 1) * 8, ti * 128:(ti + 1) * 128], in_=ptb.rearrange("p (a b) -> p a b", a=8), func=AF.Copy),
                         reads=[pt], writes=[oT])
            for c2 in range(8):
                wsl = Wr[(bi * 8 + c2) % 3]
                src = w_ret_o[:, c2 * 256:(c2 + 1) * 256].rearrange("(kc p) n -> p kc n", p=128)
                P.dma("pool", lambda e, wsl=wsl, src=src: e.dma_start(out=wsl[:], in_=src), writes=[wsl], semtile=wsl)
                for j in range(2):
                    c = c2 * 2 + j
                    ps = nextM()
                    for kc in range(32):
                        P.op("pe", lambda e, ps=ps, kc=kc, wsl=wsl, j=j, n=n: e.matmul(ps[:, 0:n], lhsT=wsl[:, kc, j * 128:(j + 1) * 128], rhs=oT[:, kc, 0:n], start=(kc == 0), stop=(kc == 31)),
                             reads=[wsl, oT], writes=[ps])
                    mx = mt[c % 2]
                    P.op("dve", lambda e, ps=ps, mx=mx, c=c, t0=t0, t1=t1: e.tensor_tensor(out=mx[:, 0:t1 - t0], in0=ps[:, 0:t1 - t0], in1=fmZ[:, c, t0:t1], op=ALU.mult), reads=[ps, fmZ], writes=[mx])
                    P.op("dve", lambda e, mx=mx, c=c, t0=t0, t1=t1: e.tensor_tensor(out=fmY[:, c, t0:t1], in0=mx[:, 0:t1 - t0], in1=fmY[:, c, t0:t1], op=ALU.add), reads=[mx, fmY], writes=[fmY])
        if debug and stages.endswith("M"):
            P.dma("sp", lambda e: e.dma_start(out=dbg_fm[:], in_=fmY[:]), reads=[fmY], semtile=fmY, is_output=True)
        P.barrier()

    if "O" in stages:
        yacc = AR.view("yacc", OFF_HT, [NTILE, D], F32)
        xt = [AR.view("xt%d" % i, OFF_X + i * 2048, [512], F32) for i in range(2)]
        junk2 = AR.view("junk2", OFF_X + 4096, [2048], BF16)
        xb2 = AR.view("xb2", OFF_X + 8192, [2048], BF16)
        for cb in range(4):
            wsl = nextW()
            load_w(wsl, w_o[:, cb * 512:(cb + 1) * 512])
            for t in range(NTILE):
                xx = xt[t % 2]
                P.dma("sp", lambda e, xx=xx, t=t, cb=cb: e.dma_start(out=xx[:], in_=x_own[t * 128:(t + 1) * 128, cb * 512:(cb + 1) * 512]), writes=[xx], semtile=xx)
                ps = nextM()
                for kc in range(16):
                    P.op("pe", lambda e, ps=ps, kc=kc, t=t, wsl=wsl: e.matmul(ps[:, 0:512], lhsT=fmY[:, kc, t * 128:(t + 1) * 128], rhs=wsl[:, kc, :], start=(kc == 0), stop=(kc == 15)),
                         reads=[fmY, wsl], writes=[ps])
                P.op("dve", lambda e, ps=ps, xx=xx, t=t, cb=cb: e.tensor_tensor(out=yacc[:, t, cb * 512:(cb + 1) * 512], in0=ps[:, 0:512], in1=xx[:], op=ALU.add), reads=[ps, xx], writes=[yacc])
        P.barrier()
        if stages.endswith("O"):
            for t in range(NTILE):
                P.dma("sp", lambda e, t=t: e.dma_start(out=y_out[t * 128:(t + 1) * 128, :], in_=yacc[:, t, :]), reads=[yacc], semtile=yacc, is_output=True)
        NBLK = 2 * TOK // 128 + NE
        h2p = AR.view("h2p", OFF_Y, [NTILE, D], BF16)
        h2Tt = AR.view("h2Tt", OFF_X + 12288, [16, 128], BF16)
        XO = OFF_X + 16384
        OH1s = AR.view("OH1s", XO, [NTILE, 32], F32)
        OH2s = AR.view("OH2s", XO + 1152, [NTILE, 32], F32)
        G12 = AR.view("G12", XO + 2304, [2, 16], F32)
        R12 = AR.view("R12", XO + 2432, [2, 16], F32)
        carry = AR.view("carry", XO + 2560, [32], F32)
        rt = AR.view("rt", XO + 2688, [256], F32)
        mconst = AR.view("mconst", XO + 3712, [3, 128], F32)
        ones_f = AR.view("ones_f", XO + 5248, [128], F32)
        wrt = AR.view("wrt", XO + 5760, [16, 36], BF16)
        brt = AR.view("brt", XO + 6912, [36], F32)
        rk = AR.view("rk", XO + 7056, [64], F32)
        x1scr = P.dram("x1_scr", [TOK, D], F32, "Internal")
        P.dma("sp", lambda e: e.dma_start(out=mconst[:], in_=mconst_d[:]), writes=[mconst], semtile=mconst)
        P.dma("pool", lambda e: e.dma_start(out=wrt[:], in_=w_rt[:].rearrange("(kc p) n -> p kc n", p=128)), writes=[wrt], semtile=wrt)
        P.dma("sp", lambda e: e.dma_start(out=brt[:], in_=b_rt[:].partition_broadcast(128)), writes=[brt], semtile=brt)
        P.op("dve", lambda e: e.memset(ones_f[:], 1.0), writes=[ones_f])
        P.op("dve", lambda e: e.memset(carry[:], 0.0), writes=[carry])
        for t in range(NTILE):
            P.dma("sp", lambda e, t=t: e.dma_start(out=x1scr[t * 128:(t + 1) * 128, :], in_=yacc[:, t, :]), reads=[yacc], writes=[x1scr], semtile=yacc)
            P.op("act", lambda e, t=t: e.activation(out=junk2[:], in_=yacc[:, t, :], func=AF.Square, accum_out=small[:, 0:1]), reads=[yacc], writes=[junk2, small])
            P.op("act", lambda e: e.activation(out=small[:, 1:2], in_=small[:, 0:1], func=AF.Sqrt, bias=epsb[:], scale=1.0 / D), reads=[small, epsb], writes=[small])
            P.op("dve", lambda e: e.reciprocal(out=small[:, 2:3], in_=small[:, 1:2]), reads=[small], writes=[small])
            P.op("dve", lambda e, t=t: e.tensor_scalar(out=xb2[:], in0=yacc[:, t, :], scalar1=small[:, 2:3], scalar2=None, op0=ALU.mult), reads=[yacc, small], writes=[xb2])
            for g in range(2):
                pt = nextT()
                ptb = pt[:].bitcast(BF16)
                for j in range(8):
                    kc = g * 8 + j
                    P.op("pe", lambda e, ptb=ptb, j=j, kc=kc: e.transpose(out=ptb[:, j * 128:(j + 1) * 128], in_=xb2[:, kc * 128:(kc + 1) * 128], identity=ident_b[:]), reads=[xb2, ident_b], writes=[pt])
                P.op("dve", lambda e, ptb=ptb, g=g: e.tensor_tensor(out=h2Tt[:, g * 8:(g + 1) * 8, :], in0=ptb.rearrange("p (a b) -> p a b", a=8),
                                                                in1=vec_s[:, 1, g * 8:(g + 1) * 8].unsqueeze(2).to_broadcast([128, 8, 128]), op=ALU.mult), reads=[pt, vec_s], writes=[h2Tt])
            for g in range(2):
                pt = nextT()
                ptb = pt[:].bitcast(BF16)
                for j in range(8):
                    kc = g * 8 + j
                    P.op("pe", lambda e, ptb=ptb, j=j, kc=kc: e.transpose(out=ptb[:, j * 128:(j + 1) * 128], in_=h2Tt[:, kc, :], identity=ident_b[:]), reads=[h2Tt, ident_b], writes=[pt])
                P.op("act", lambda e, ptb=ptb, g=g, t=t: e.activation(
                    out=h2p[:, t, :].rearrange("t (j p) -> t p j", j=16)[:, g * 64:(g + 1) * 64, :],
                    in_=ptb.rearrange("t (p j) -> t p j", j=16), func=AF.Copy), reads=[pt], writes=[h2p])
            ps = nextM()
            for kc in range(16):
                P.op("pe", lambda e, ps=ps, kc=kc: e.matmul(ps[:, 0:36], lhsT=h2Tt[:, kc, :], rhs=wrt[:, kc, :], start=(kc == 0), stop=(kc == 15)), reads=[h2Tt, wrt], writes=[ps])
            dv = lambda fn, rd=(), wr=(): P.op("dve", fn, reads=[rt] + list(rd), writes=[rt] + list(wr))
            dv(lambda e, ps=ps: e.tensor_tensor(out=rt[:, 0:36], in0=ps[:, 0:36], in1=brt[:], op=ALU.add), rd=[ps, brt])
            dv(lambda e: e.tensor_reduce(out=rt[:, 40:41], in_=rt[:, 0:4], axis=mybir.AxisListType.X, op=ALU.max))
            dv(lambda e: e.tensor_scalar(out=rt[:, 44:48], in0=rt[:, 0:4], scalar1=rt[:, 40:41], scalar2=None, op0=ALU.is_equal))
            dv(lambda e: e.tensor_scalar(out=rt[:, 41:42], in0=rt[:, 40:41], scalar1=-1.0, scalar2=None, op0=ALU.mult))
            P.op("act", lambda e: e.activation(out=rt[:, 48:52], in_=rt[:, 0:4], func=AF.Exp, bias=rt[:, 41:42], scale=1.0, accum_out=rt[:, 42:43]), reads=[rt], writes=[rt])
            dv(lambda e: e.reciprocal(out=rt[:, 43:44], in_=rt[:, 42:43]))
            dv(lambda e: e.tensor_scalar(out=rt[:, 52:56], in0=rt[:, 44:48], scalar1=-1.0, scalar2=1e30, op0=ALU.add, op1=ALU.mult))
            dv(lambda e: e.tensor_tensor(out=rt[:, 64:96].rearrange("p (g x) -> p g x", g=4), in0=rt[:, 4:36].rearrange("p (g x) -> p g x", g=4),
                                         in1=rt[:, 52:56].unsqueeze(2).to_broadcast([128, 4, 8]), op=ALU.add))
            dv(lambda e: e.tensor_reduce(out=rt[:, 56:57], in_=rt[:, 64:96], axis=mybir.AxisListType.X, op=ALU.max))
            dv(lambda e, t=t: e.tensor_scalar(out=OH1s[:, t, :], in0=rt[:, 64:96], scalar1=rt[:, 56:57], scalar2=None, op0=ALU.is_equal), wr=[OH1s])
            dv(lambda e, t=t: e.scalar_tensor_tensor(out=rt[:, 128:160], in0=OH1s[:, t, :], scalar=-1e30, in1=rt[:, 64:96], op0=ALU.mult, op1=ALU.add), rd=[OH1s])
            dv(lambda e: e.tensor_reduce(out=rt[:, 57:58], in_=rt[:, 128:160], axis=mybir.AxisListType.X, op=ALU.max))
            dv(lambda e, t=t: e.tensor_scalar(out=OH2s[:, t, :], in0=rt[:, 128:160], scalar1=rt[:, 57:58], scalar2=None, op0=ALU.is_equal), wr=[OH2s])
            dv(lambda e: e.tensor_scalar(out=rt[:, 58:59], in0=rt[:, 56:57], scalar1=-1.0, scalar2=None, op0=ALU.mult))
            P.op("act", lambda e: e.activation(out=rt[:, 59:60], in_=rt[:, 57:58], func=AF.Exp, bias=rt[:, 58:59], scale=1.0), reads=[rt], writes=[rt])
            dv(lambda e: e.tensor_scalar(out=rt[:, 60:61], in0=rt[:, 59:60], scalar1=1.0, scalar2=None, op0=ALU.add))
            dv(lambda e: e.reciprocal(out=rt[:, 61:62], in_=rt[:, 60:61]))
            dv(lambda e, t=t: e.tensor_tensor(out=G12[:, 0, t:t + 1], in0=rt[:, 61:62], in1=rt[:, 43:44], op=ALU.mult), wr=[G12])
            dv(lambda e, t=t: e.tensor_tensor(out=G12[:, 1, t:t + 1], in0=G12[:, 0, t:t + 1], in1=rt[:, 59:60], op=ALU.mult), rd=[G12], wr=[G12])
            for k, OHs in ((0, OH1s), (1, OH2s)):
                pr = nextM()
                P.op("pe", lambda e, pr=pr, OHs=OHs, t=t: e.matmul(pr[:, 0:32], lhsT=mconst[:, 0, :], rhs=OHs[:, t, :], start=True, stop=True), reads=[mconst, OHs], writes=[pr])
                pc_ = nextM()
                P.op("pe", lambda e, pc_=pc_, OHs=OHs, t=t: e.matmul(pc_[:, 0:32], lhsT=ones_f[:], rhs=OHs[:, t, :], start=True, stop=True), reads=[ones_f, OHs], writes=[pc_])
                P.op("dve", lambda e, pr=pr: e.tensor_tensor(out=rk[:, 0:32], in0=pr[:, 0:32], in1=carry[:], op=ALU.add), reads=[pr, carry], writes=[rk])
                P.op("dve", lambda e, OHs=OHs, t=t: e.tensor_tensor(out=rk[:, 32:64], in0=OHs[:, t, :], in1=rk[:, 0:32], op=ALU.mult), reads=[OHs, rk], writes=[rk])
                P.op("dve", lambda e, t=t, k=k: e.tensor_reduce(out=R12[:, k, t:t + 1], in_=rk[:, 32:64], axis=mybir.AxisListType.X, op=ALU.add), reads=[rk], writes=[R12])
                P.op("dve", lambda e, pc_=pc_: e.tensor_tensor(out=carry[:], in0=pc_[:, 0:32], in1=carry[:], op=ALU.add), reads=[pc_, carry], writes=[carry])
        if debug and stages.endswith("O"):
            dbg_s = P.dram("dbg_s", [128, 96], F32, OUT)
            P.dma("sp", lambda e: e.dma_start(out=dbg_s[:, 0:32], in_=R12[:].rearrange("p a b -> p (a b)")), reads=[R12], semtile=R12, is_output=True)
            P.dma("sp", lambda e: e.dma_start(out=dbg_s[:, 32:64], in_=G12[:].rearrange("p a b -> p (a b)")), reads=[G12], semtile=G12, is_output=True)
            P.dma("sp", lambda e: e.dma_start(out=dbg_s[:, 64:96], in_=carry[:]), reads=[carry], semtile=carry, is_output=True)
        P.barrier()

    if "E" in stages:
        EO = XO + 8192
        nblk = AR.view("nblk", EO, [32], F32)
        pst = AR.view("pst", EO + 128, [32], F32)
        pend = AR.view("pend", EO + 256, [32], F32)
        prow = AR.view("prow", EO + 384, [32], F32)
        ebf = AR.view("ebf", EO + 512, [64], F32)
        idxW = AR.view("idxW", EO + 768, [64], I32)
        Ri = AR.view("Ri", EO + 1024, [2, 16], I32)
        ebe = AR.view("ebe", EO + 1152, [64], F32)
        big = AR.view("big", OFF_X, [NBLK, 32], F32)
        big2 = AR.view("big2", OFF_X + 6400, [NBLK, 32], F32)
        P.op("dve", lambda e: e.memset(nblk[:], 0.0), writes=[nblk])
        for m in range(2 * TOK // 128 + 1):
            P.op("dve", lambda e, m=m: e.scalar_tensor_tensor(out=nblk[:], in0=carry[:], scalar=float(128 * m), in1=nblk[:], op0=ALU.is_gt, op1=ALU.add), reads=[carry, nblk], writes=[nblk])
        P.op("dve", lambda e: e.memset(pst[:, 0:1], 0.0), writes=[pst])
        for ei in range(1, 32):
            P.op("dve", lambda e, ei=ei: e.tensor_tensor(out=pst[:, ei:ei + 1], in0=pst[:, ei - 1:ei], in1=nblk[:, ei - 1:ei], op=ALU.add), reads=[pst, nblk], writes=[pst])
        P.op("dve", lambda e: e.tensor_tensor(out=pend[:], in0=pst[:], in1=nblk[:], op=ALU.add), reads=[pst, nblk], writes=[pend])
        P.op("dve", lambda e: e.tensor_scalar(out=prow[:], in0=pst[:], scalar1=128.0, scalar2=None, op0=ALU.mult), reads=[pst], writes=[prow])
        for t in range(NTILE):
            for k, OHs in ((0, OH1s), (1, OH2s)):
                P.op("dve", lambda e, OHs=OHs, t=t: e.tensor_tensor(out=rk[:, 32:64], in0=OHs[:, t, :], in1=prow[:], op=ALU.mult), reads=[OHs, prow], writes=[rk])
                P.op("dve", lambda e: e.tensor_reduce(out=rk[:, 0:1], in_=rk[:, 32:64], axis=mybir.AxisListType.X, op=ALU.add), reads=[rk], writes=[rk])
                P.op("dve", lambda e, t=t, k=k: e.tensor_tensor(out=R12[:, k, t:t + 1], in0=R12[:, k, t:t + 1], in1=rk[:, 0:1], op=ALU.add), reads=[R12, rk], writes=[R12])
        P.op("dve", lambda e: e.tensor_copy(out=Ri[:], in_=R12[:]), reads=[R12], writes=[Ri])
        bio = mconst[:, 1, 0:NBLK].unsqueeze(2).to_broadcast([128, NBLK, 32])
        P.op("dve", lambda e: e.tensor_tensor(out=big[:], in0=pst[:].unsqueeze(1).to_broadcast([128, NBLK, 32]), in1=bio, op=ALU.is_le), reads=[pst, mconst], writes=[big])
        P.op("dve", lambda e: e.tensor_tensor(out=big2[:], in0=pend[:].unsqueeze(1).to_broadcast([128, NBLK, 32]), in1=bio, op=ALU.is_gt), reads=[pend, mconst], writes=[big2])
        P.op("dve", lambda e: e.tensor_tensor(out=big[:], in0=big[:], in1=big2[:], op=ALU.mult), reads=[big, big2], writes=[big])
        P.op("dve", lambda e: e.tensor_reduce(out=ebf[:, 0:NBLK], in_=big[:], axis=mybir.AxisListType.X, op=ALU.add), reads=[big], writes=[ebf])
        P.op("dve", lambda e: e.tensor_tensor(out=big2[:], in0=big[:], in1=mconst[:, 1, 0:32].unsqueeze(1).to_broadcast([128, NBLK, 32]), op=ALU.mult), reads=[big, mconst], writes=[big2])
        P.op("dve", lambda e: e.tensor_reduce(out=ebe[:, 0:NBLK], in_=big2[:], axis=mybir.AxisListType.X, op=ALU.add), reads=[big2], writes=[ebe])
        idx2f = AR.view("idx2f", OFF_X, [NBLK, 2], F32)
        idx2 = AR.view("idx2", EO + 1408, [NBLK, 2], I32)
        base2 = AR.view("base2", EO + 768, [2], F32)
        P.op("dve", lambda e: e.tensor_scalar(out=ebf[:, 0:NBLK], in0=ebf[:, 0:NBLK], scalar1=-1.0, scalar2=-1.0e4, op0=ALU.add, op1=ALU.mult), reads=[ebf], writes=[ebf])
        P.op("dve", lambda e: e.tensor_tensor(out=ebf[:, 0:NBLK], in0=ebf[:, 0:NBLK], in1=ebe[:, 0:NBLK], op=ALU.add), reads=[ebf, ebe], writes=[ebf])
        P.op("dve", lambda e: e.tensor_scalar(out=ebf[:, 0:NBLK], in0=ebf[:, 0:NBLK], scalar1=256.0, scalar2=None, op0=ALU.mult), reads=[ebf], writes=[ebf])
        P.op("dve", lambda e: e.scalar_tensor_tensor(out=base2[:], in0=mconst[:, 2, 0:2], scalar=2.0, in1=mconst[:, 1, 0:2], op0=ALU.mult, op1=ALU.add), reads=[mconst], writes=[base2])
        P.op("dve", lambda e: e.tensor_tensor(out=idx2f[:], in0=ebf[:, 0:NBLK].unsqueeze(2).to_broadcast([128, NBLK, 2]), in1=base2[:].unsqueeze(1).to_broadcast([128, NBLK, 2]), op=ALU.add),
             reads=[ebf, base2, big], writes=[idx2f])
        P.op("dve", lambda e: e.tensor_copy(out=idx2[:], in_=idx2f[:]), reads=[idx2f], writes=[idx2])
        P.barrier()
        WS = [Tile("WS%d" % i, AR.t[:, i * 32768:(i + 1) * 32768].bitcast(BF16)) for i in range(3)]
        wcnt = {"n": 0}

        def nextWS():
            wcnt["n"] += 1
            return WS[wcnt["n"] % 3]
        MO = 98304
        iob = [AR.view("iob%d" % i, MO + i * 512, [128], F32) for i in range(2)]
        selt = [AR.view("selt%d" % i, MO + 1024 + i * 256, [128], BF16) for i in range(2)]
        Sel = [AR.view("Sel%d" % i, MO + 1536 + i * 2304, [NTILE, 128], BF16) for i in range(2)]
        XbT = [AR.view("XbT%d" % i, MO + 6144 + i * 4096, [16, 128], BF16) for i in range(2)]
        sgm = [AR.view("sgm%d" % i, MO + 14336 + i * 2048, [512], F32) for i in range(2)]
        hperm = AR.view("hperm", MO + 18432, [1024], BF16)
        hTm = AR.view("hTm", MO + 20480, [8, 128], BF16)
        Yst = [AR.view("Yst%d" % i, OFF_X + i * 8192, [D], F32) for i in range(2)]
        yscr = P.dram("y_scr", [NBLK * 128, D], F32, "Internal")
        regs = {}

        def breg(e, key, val):
            if key not in regs:
                regs[key] = e.to_reg(val)
            return regs[key]
        wflat = {id(w_gate): w_gate[:].rearrange("e k n -> (e k n)").rearrange("(r c) -> r c", c=8192),
                 id(w_up): w_up[:].rearrange("e k n -> (e k n)").rearrange("(r c) -> r c", c=8192),
                 id(w_down): w_down[:].rearrange("e k n -> (e k n)").rearrange("(r c) -> r c", c=8192)}
        bound = n_experts * 256 - 1

        def wload(slot, wsrc, b, J):
            src2 = wflat[id(wsrc)]
            for hh in range(2):
                P.dma("pool", lambda e, hh=hh: e.indirect_dma_start(out=slot[:, hh * 8192:(hh + 1) * 8192], out_offset=None, in_=src2, in_offset=bass.IndirectOffsetOnAxis(ap=idx2[:, b, hh:hh + 1], axis=0),
                                                               bounds_check=breg(e, "w", bound), oob_is_err=False), reads=[idx2], writes=[slot], semtile=slot)
        border = []
        for i_ in range(32):
            border.append(i_)
            if 32 + i_ < NBLK:
                border.append(32 + i_)
        assert sorted(border) == list(range(NBLK))
        for b in border:
            wg_ = nextWS(); wload(wg_, w_gate, b, 16)
            wu_ = nextWS(); wload(wu_, w_up, b, 16)
            wd_ = nextWS(); wload(wd_, w_down, b, 8)
            bo = border.index(b)
            io_ = iob[bo % 2]
            P.op("dve", lambda e, io_=io_, b=b: e.tensor_scalar(out=io_[:], in0=mconst[:, 1, :], scalar1=float(128 * b), scalar2=None, op0=ALU.add), reads=[mconst], writes=[io_])
            sel = Sel[bo % 2]
            for t in range(NTILE):
                st_ = selt[t % 2]
                P.op("dve", lambda e, st_=st_, io_=io_, t=t: e.tensor_scalar(out=st_[:], in0=io_[:], scalar1=R12[:, 0, t:t + 1], scalar2=None, op0=ALU.is_equal), reads=[io_, R12], writes=[st_])
                P.op("dve", lambda e, st_=st_, io_=io_, t=t, sel=sel: e.scalar_tensor_tensor(out=sel[:, t, :], in0=io_[:], scalar=R12[:, 1, t:t + 1], in1=st_[:], op0=ALU.is_equal, op1=ALU.add),
                     reads=[io_, R12, st_], writes=[sel])
            xbt = XbT[bo % 2]
            for g in range(4):
                pg_ = pM[g]
                for jj in range(4):
                    j = g * 4 + jj
                    for t in range(NTILE):
                        P.op("pe", lambda e, pg_=pg_, jj=jj, j=j, t=t, sel=sel: e.matmul(pg_[:, jj * 128:(jj + 1) * 128], lhsT=h2p[:, t, j * 128:(j + 1) * 128], rhs=sel[:, t, :], start=(t == 0), stop=(t == NTILE - 1)),
                             reads=[h2p, sel], writes=[pg_])
                if g % 2 == 0:
                    P.op("act", lambda e, pg_=pg_, g=g, xbt=xbt: e.activation(out=xbt[:, g * 4:(g + 1) * 4, :], in_=pg_[:].rearrange("p (a b) -> p a b", a=4), func=AF.Copy), reads=[pg_], writes=[xbt])
                else:
                    P.op("dve", lambda e, pg_=pg_, g=g, xbt=xbt: e.tensor_copy(out=xbt[:, g * 4:(g + 1) * 4, :], in_=pg_[:].rearrange("p (a b) -> p a b", a=4)), reads=[pg_], writes=[xbt])
            for hf in range(2):
                pgt, put = pS[0], pS[1]
                for (pp_, wsl) in ((pgt, wg_), (put, wu_)):
                    for j in range(16):
                        P.op("pe", lambda e, pp_=pp_, wsl=wsl, j=j, hf=hf, xbt=xbt: e.matmul(pp_[:, 0:512], lhsT=xbt[:, j, :], rhs=wsl[:].rearrange("p (j n) -> p j n", j=16)[:, j, hf * 512:(hf + 1) * 512], start=(j == 0), stop=(j == 15)),
                             reads=[xbt, wsl], writes=[pp_])
                sx = sgm[hf]
                P.op("act", lambda e, pgt=pgt, sx=sx: e.activation(out=sx[:], in_=pgt[:, 0:512], func=AF.Silu), reads=[pgt], writes=[sx])
                P.op("dve", lambda e, put=put, sx=sx, hf=hf: e.tensor_tensor(out=hperm[:].rearrange("t (j p) -> t p j", j=8)[:, hf * 64:(hf + 1) * 64, :],
                                                                          in0=put[:, 0:512].rearrange("t (p j) -> t p j", j=8), in1=sx[:].rearrange("t (p j) -> t p j", j=8), op=ALU.mult),
                     reads=[put, sx], writes=[hperm])
            ptm = pT[0]
            ptb = ptm[:].bitcast(BF16)
            for j in range(8):
                P.op("pe", lambda e, ptb=ptb, j=j: e.transpose(out=ptb[:, j * 128:(j + 1) * 128], in_=hperm[:, j * 128:(j + 1) * 128], identity=ident_b[:]), reads=[hperm, ident_b], writes=[ptm])
            P.op("act", lambda e, ptb=ptb: e.activation(out=hTm[:], in_=ptb.rearrange("p (a b) -> p a b", a=8), func=AF.Copy), reads=[ptm], writes=[hTm])
            yst = Yst[bo % 2]
            for cb in range(4):
                pd = pT[1]
                for j in range(8):
                    P.op("pe", lambda e, pd=pd, j=j, cb=cb, wd_=wd_: e.matmul(pd[:, 0:512], lhsT=hTm[:, j, :], rhs=wd_[:].rearrange("p (j n) -> p j n", j=8)[:, j, cb * 512:(cb + 1) * 512], start=(j == 0), stop=(j == 7)),
                         reads=[hTm, wd_], writes=[pd])
                if cb % 2 == 0:
                    P.op("act", lambda e, pd=pd, cb=cb, yst=yst: e.activation(out=yst[:, cb * 512:(cb + 1) * 512], in_=pd[:, 0:512], func=AF.Copy), reads=[pd], writes=[yst])
                else:
                    P.op("dve", lambda e, pd=pd, cb=cb, yst=yst: e.tensor_copy(out=yst[:, cb * 512:(cb + 1) * 512], in_=pd[:, 0:512]), reads=[pd], writes=[yst])
            P.dma("sp", lambda e, yst=yst, b=b: e.dma_start(out=yscr[b * 128:(b + 1) * 128, :], in_=yst[:]), reads=[yst], writes=[yscr], semtile=yst)
        P.barrier()
        nf = AR.view("nf", 0, [D], F32)
        junk3 = AR.view("junk3", 8192, [2048], BF16)
        xc = [AR.view("xc%d" % i, 16384 + i * 8192, [D], F32) for i in range(2)]
        y1 = [AR.view("y1_%d" % i, 32768 + i * 8192, [D], F32) for i in range(2)]
        y2 = [AR.view("y2_%d" % i, 49152 + i * 8192, [D], F32) for i in range(2)]
        P.dma("sp", lambda e: e.dma_start(out=nf[:], in_=norm_f[:].partition_broadcast(128)), writes=[nf], semtile=nf)
        for t in range(NTILE):
            xx, ya, yb_ = xc[t % 2], y1[t % 2], y2[t % 2]
            P.dma("sp", lambda e, xx=xx, t=t: e.dma_start(out=xx[:], in_=x1scr[t * 128:(t + 1) * 128, :]), reads=[x1scr], writes=[xx], semtile=xx)
            for k, yy in ((0, ya), (1, yb_)):
                P.dma("pool", lambda e, yy=yy, k=k, t=t: e.indirect_dma_start(out=yy[:], out_offset=None, in_=yscr[:], in_offset=bass.IndirectOffsetOnAxis(ap=Ri[:, k, t:t + 1], axis=0),
                                                                       bounds_check=breg(e, "y", NBLK * 128 - 1), oob_is_err=False), reads=[Ri, yscr], writes=[yy], semtile=yy)
            P.op("dve", lambda e, xx=xx, ya=ya, t=t: e.scalar_tensor_tensor(out=xx[:], in0=ya[:], scalar=G12[:, 0, t:t + 1], in1=xx[:], op0=ALU.mult, op1=ALU.add), reads=[ya, G12, xx], writes=[xx])
            P.op("dve", lambda e, xx=xx, yb_=yb_, t=t: e.scalar_tensor_tensor(out=xx[:], in0=yb_[:], scalar=G12[:, 1, t:t + 1], in1=xx[:], op0=ALU.mult, op1=ALU.add), reads=[yb_, G12, xx], writes=[xx])
            P.op("act", lambda e, xx=xx: e.activation(out=junk3[:], in_=xx[:], func=AF.Square, accum_out=small[:, 0:1]), reads=[xx], writes=[junk3, small])
            P.op("act", lambda e: e.activation(out=small[:, 1:2], in_=small[:, 0:1], func=AF.Sqrt, bias=epsb[:], scale=1.0 / D), reads=[small, epsb], writes=[small])
            P.op("dve", lambda e: e.reciprocal(out=small[:, 2:3], in_=small[:, 1:2]), reads=[small], writes=[small])
            P.op("dve", lambda e, xx=xx: e.scalar_tensor_tensor(out=xx[:], in0=xx[:], scalar=small[:, 2:3], in1=nf[:], op0=ALU.mult, op1=ALU.mult), reads=[xx, small, nf], writes=[xx])
            P.dma("sp", lambda e, xx=xx, t=t: e.dma_start(out=y_out[t * 128:(t + 1) * 128, :], in_=xx[:]), reads=[xx], semtile=xx, is_output=True)

    P.emit()
    return nc, P


def _tables(half):
    pos = np.concatenate([half * 1024 + np.arange(1024), np.tile(PAST_LEN + np.arange(8), 16)]).astype(np.float32)
    ppos = np.arange(1024).astype(np.float32)
    inv = (np.float32(10000.0) ** (-np.arange(128, dtype=np.float32) / np.float32(128))).astype(np.float32)

    def cs(p):
        ang = (p[None, :] * inv[:, None]).astype(np.float32)
        return np.stack([np.cos(ang.astype(np.float64)), np.sin(ang.astype(np.float64))], axis=1).astype(np.float32)

    dec = np.zeros((H, 128, 4, 128), np.float32)
    i = np.arange(128, dtype=np.float64)
    i8 = (np.arange(128) % 8).astype(np.float64)
    for h in range(H):
        lg = LOG_G[h]
        dec[h, :, 0, :] = np.exp((i + 1) * lg)[None, :]
        dec[h, :, 1, :] = (np.exp(-(i + 1) * lg) * DK ** -0.5)[None, :]
        dec[h, :, 2, :] = np.exp((i8 + 1) * lg)[None, :]
        dec[h, :, 3, :] = (np.exp(-(i8 + 1) * lg) * DK ** -0.5)[None, :]
    j = np.arange(128)
    mp = (j[:, None] <= j[None, :]).astype(np.float32)
    ms = ((j[:, None] <= j[None, :]) & (j[:, None] // 8 == j[None, :] // 8)).astype(np.float32)
    masks = np.stack([mp, ms], axis=1)
    rowmask = (j[:, None] // 8 == np.arange(16)[None, :]).astype(np.float32)
    mconst = np.zeros((128, 3, 128), np.float32)
    mconst[:, 0, :] = (j[:, None] < j[None, :])
    mconst[:, 1, :] = j[None, :]
    mconst[:, 2, :] = j[:, None]
    return dict(cs_own=cs(pos), cs_prev=cs(ppos), dec=dec, masks=np.ascontiguousarray(masks),
                ident=np.eye(128, dtype=np.float32), rowmask=rowmask, mconst=mconst)


def make_in_maps(inp, n_experts=NE, cores=range(8)):
    f = lambda a: np.ascontiguousarray(np.asarray(a, dtype=np.float32))
    xp = f(inp["x_prompt"])
    xs = f(inp["x_sample"])
    sr = f(inp["state_ret"])[0]
    sc = f(inp["state_conv"])[0]

    def kc_layout(v):
        return f(v).reshape(16, 128).T

    vecs = np.stack([kc_layout(inp["norm_mix"][0]), kc_layout(inp["norm_ffn"][0]), kc_layout(inp["conv_b"][0]),
                     kc_layout(inp["conv_ln_w"][0]), kc_layout(inp["conv_ln_b"][0]), np.zeros((128, 16), np.float32)], axis=1)
    shared = dict(
        w_in=f(inp["w_in"])[0], w_ret_o=f(inp["w_ret_o"])[0], conv_w=f(inp["conv_w"])[0], vecs=np.ascontiguousarray(vecs),
        norm_f=f(inp["norm_f"]).reshape(1, D), w_conv_o=f(inp["w_conv_o"])[0], w_o=f(inp["w_o"])[0],
        w_rt=np.ascontiguousarray(np.concatenate([f(inp["w_coarse"])[0], f(inp["w_fine"])[0]], axis=1)),
        b_rt=np.concatenate([f(inp["b_coarse"])[0], f(inp["b_fine"])[0]]).reshape(1, 36),
        w_gate=f(inp["w_gate"])[0][:n_experts], w_up=f(inp["w_up"])[0][:n_experts], w_down=f(inp["w_down"])[0][:n_experts],
    )
    tabs = [_tables(0), _tables(1)]
    maps = []
    for c in cores:
        b, half = c // 2, c % 2
        m = dict(shared)
        m.update(tabs[half])
        m["x_own"] = np.ascontiguousarray(np.concatenate([xp[b, half * 1024:(half + 1) * 1024], xs[16 * c:16 * (c + 1)].reshape(128, D)], axis=0))
        m["x_prev"] = np.ascontiguousarray(xp[b, 0:1024]) if half == 1 else np.zeros((1024, D), np.float32)
        m["sret"] = np.ascontiguousarray(sr[16 * c:16 * (c + 1)])
        m["sconv"] = np.ascontiguousarray(sc[16 * c:16 * (c + 1)].reshape(16 * 30, D))
        maps.append(m)
    return maps


_CACHE = {}


def kernel(**inputs):
    if "nc" not in _CACHE:
        _CACHE["nc"] = build()[0]
    nc = _CACHE["nc"]
    maps = make_in_maps(inputs)
    res = run_bass_kernel_spmd(nc, maps, core_ids=list(range(8)))
    r = res.results
    yp = np.zeros((4, 2048, D), np.float32)
    ys = np.zeros((128, 8, D), np.float32)
    retp = np.zeros((1, 4, H, DK, DV), np.float32)
    convp = np.zeros((1, 4, 30, D), np.float32)
    rets = np.zeros((1, 128, H, DK, DV), np.float32)
    convs = np.zeros((1, 128, 30, D), np.float32)
    for c in range(8):
        b, half = c // 2, c % 2
        yp[b, half * 1024:(half + 1) * 1024] = r[c]["y_out"][:1024]
        ys[16 * c:16 * (c + 1)] = r[c]["y_out"][1024:].reshape(16, 8, D)
        rets[0, 16 * c:16 * (c + 1)] = r[c]["rets_out"]
        convs[0, 16 * c:16 * (c + 1)] = r[c]["convs_out"]
        if half == 1:
            retp[0, b] = r[c]["retp_out"]
            convp[0, b] = r[c]["convp_out"]
    return (yp, ys, retp, convp, rets, convs)
```

```python
import contextlib
import numpy as np
import concourse.bass as bass
import concourse.mybir as mybir
from concourse.bass_utils import run_bass_kernel_spmd

F32, BF16, I32, U8 = mybir.dt.float32, mybir.dt.bfloat16, mybir.dt.int32, mybir.dt.uint8
F32R = mybir.dt.float32r
ALU = mybir.AluOpType
AF = mybir.ActivationFunctionType

ENGS = ("pe", "act", "dve", "pool", "sp")
DSIZE = {F32: 4, BF16: 2, I32: 4, U8: 1}

D = 2048
H = 8
DK = 256
DV = 512
NTILE = 9
TOK = 1152
PAST_LEN = 16384
CW = 31
NE = 32
DE = 1024
EPS = 1e-6
Q0, K0, V0, G0, CA0, CB0, GR0, GC0 = 0, 2048, 4096, 8192, 12288, 14336, 16384, 18432
IN_COLS = 20480
BLKS = ((0, 512), (512, 1024), (1024, 1152))
LOG_G = [float(np.log1p(-2.0 ** (-5.0 - h))) for h in range(H)]
CDEC_P = [float(np.exp(128 * LOG_G[h])) for h in range(H)]
CDEC_S = [float(np.exp(8 * LOG_G[h])) for h in range(H)]


class Tile:
    def __init__(self, name, h):
        self.name, self.h = name, h
        self.w = None
        self.r = []
        self.sem = None

    def __getitem__(self, idx):
        return self.h[idx]


class Op:
    __slots__ = ("eng", "fn", "deps", "is_dma", "sem", "count", "need_inc", "val")

    def __init__(self, eng, fn, is_dma=False):
        self.eng, self.fn, self.is_dma = eng, fn, is_dma
        self.deps = []
        self.sem = None
        self.count = 0
        self.need_inc = False
        self.val = 0


class Prog:
    def __init__(self, nc):
        self.nc = nc
        self.ops = {e: [] for e in ENGS}
        self.dma_counts = {}
        self.last_dma = {}
        self.stack = contextlib.ExitStack()
        self.out_ops = []
        self.pending = {e: [] for e in ENGS}

    def sb(self, name, shape, dt):
        h = self.stack.enter_context(self.nc.sbuf_tensor(name, list(shape), dt))
        return Tile(name, h)

    def ps(self, name, shape, dt=F32):
        h = self.stack.enter_context(self.nc.psum_tensor(name, list(shape), dt))
        return Tile(name, h)

    def dram(self, name, shape, dt, kind):
        h = self.nc.dram_tensor(name, list(shape), dt, kind=kind)
        return Tile(name, h.ap())

    def _deps(self, o, reads, writes):
        deps = []
        for t in reads:
            if t.w is not None:
                deps.append((t.w, "raw"))
        for t in writes:
            if t.w is not None:
                deps.append((t.w, "waw"))
            for r in t.r:
                deps.append((r, "war"))
        seen = set()
        for d, kind in deps:
            if d is o or id(d) in seen:
                continue
            if (not d.is_dma) and d.eng == o.eng and not o.is_dma:
                if kind != "raw" or o.eng == "pe":
                    continue
            seen.add(id(d))
            o.deps.append(d)
        for d in self.pending[o.eng]:
            if id(d) not in seen and d is not o:
                seen.add(id(d))
                o.deps.append(d)
        self.pending[o.eng] = []
        for t in reads:
            t.r.append(o)
        for t in writes:
            t.w = o
            t.r = []

    def op(self, eng, fn, reads=(), writes=()):
        o = Op(eng, fn)
        self._deps(o, reads, writes)
        self.ops[eng].append(o)
        return o

    def dma(self, eng, fn, reads=(), writes=(), semtile=None, is_output=False):
        o = Op(eng, fn, is_dma=True)
        if semtile.sem is None:
            semtile.sem = "ds_%s" % semtile.name
        o.sem = semtile.sem
        self.dma_counts[o.sem] = self.dma_counts.get(o.sem, 0) + 16
        o.count = self.dma_counts[o.sem]
        self.last_dma[o.sem] = o
        self._deps(o, reads, writes)
        self.ops[eng].append(o)
        if is_output:
            self.out_ops.append(o)
        return o

    def barrier(self):
        deps = []
        for e in ENGS:
            for o in reversed(self.ops[e]):
                if not o.is_dma:
                    deps.append(o)
                    break
        deps += list(self.last_dma.values())
        for e in ENGS:
            self.pending[e] = self.pending[e] + [d for d in deps if d.is_dma or d.eng != e]

    def emit(self):
        nc = self.nc
        for e in ENGS:
            for o in self.ops[e]:
                for d in o.deps:
                    if not d.is_dma:
                        d.need_inc = True
        for e in ENGS:
            c = 0
            for o in self.ops[e]:
                if (not o.is_dma) and o.need_inc:
                    c += 1
                    o.val = c
        sems = {}
        for e in ENGS:
            sems["eng_" + e] = self.stack.enter_context(nc.semaphore("eng_" + e))
        for s in self.dma_counts:
            sems[s] = self.stack.enter_context(nc.semaphore(s))
        self.nsems = len(sems)
        final = {}
        for o in self.out_ops:
            final[o.sem] = max(final.get(o.sem, 0), o.count)

        def emit_eng(ename, eng):
            waited = {}
            for o in self.ops[ename]:
                for d in o.deps:
                    if d.is_dma:
                        s, v = d.sem, d.count
                    else:
                        s, v = "eng_" + d.eng, d.val
                    if waited.get(s, 0) >= v:
                        continue
                    waited[s] = v
                    eng.wait_ge(sems[s], v)
                ins = o.fn(eng)
                if o.is_dma:
                    ins.then_inc(sems[o.sem], 16)
                elif o.need_inc:
                    ins.then_inc(sems["eng_" + ename], 1)
            if ename == "sp":
                for s, v in final.items():
                    if waited.get(s, 0) < v:
                        eng.wait_ge(sems[s], v)

        with nc.Block() as block:
            @block.sync
            def _(e):
                emit_eng("sp", e)

            @block.scalar
            def _(e):
                emit_eng("act", e)

            @block.vector
            def _(e):
                emit_eng("dve", e)

            @block.gpsimd
            def _(e):
                emit_eng("pool", e)

            @block.tensor
            def _(e):
                emit_eng("pe", e)
        self.stack.close()


class Arena:
    def __init__(self, P, nbytes):
        self.P = P
        self.t = P.stack.enter_context(P.nc.sbuf_tensor("arena", [128, nbytes], U8))
        self.nbytes = nbytes

    def view(self, name, off, shape, dt):
        n = int(np.prod(shape)) * DSIZE[dt]
        assert off + n <= self.nbytes, (name, off, n)
        assert off % 4 == 0
        ap = self.t[:, off:off + n].bitcast(dt)
        if len(shape) == 2:
            ap = ap.rearrange("p (a b) -> p a b", a=shape[0])
        elif len(shape) == 3:
            ap = ap.rearrange("p (a b c) -> p a b c", a=shape[0], b=shape[1])
        return Tile(name, ap)


def build(n_experts=NE, stages="ARCMOE", debug=False):
    nc = bass.Bass("TRN2", target_bir_lowering=False)
    P = Prog(nc)
    IN, OUT = "ExternalInput", "ExternalOutput"
    x_own = P.dram("x_own", [TOK, D], F32, IN)
    x_prev = P.dram("x_prev", [1024, D], F32, IN)
    sret = P.dram("sret", [16, H, DK, DV], F32, IN)
    sconv = P.dram("sconv", [16 * 30, D], F32, IN)
    w_in = P.dram("w_in", [D, IN_COLS], F32, IN)
    w_ret_o = P.dram("w_ret_o", [H * DV, D], F32, IN)
    conv_w = P.dram("conv_w", [CW, D], F32, IN)
    vecs = P.dram("vecs", [128, 6, 16], F32, IN)
    norm_f = P.dram("norm_f", [1, D], F32, IN)
    w_conv_o = P.dram("w_conv_o", [D, D], F32, IN)
    w_o = P.dram("w_o", [D, D], F32, IN)
    w_rt = P.dram("w_rt", [D, 36], F32, IN)
    b_rt = P.dram("b_rt", [1, 36], F32, IN)
    w_gate = P.dram("w_gate", [n_experts, D, DE], F32, IN)
    w_up = P.dram("w_up", [n_experts, D, DE], F32, IN)
    w_down = P.dram("w_down", [n_experts, DE, D], F32, IN)
    cs_own_d = P.dram("cs_own", [128, 2, TOK], F32, IN)
    cs_prev_d = P.dram("cs_prev", [128, 2, 1024], F32, IN)
    dec_d = P.dram("dec", [H, 128, 4, 128], F32, IN)
    masks_d = P.dram("masks", [128, 2, 128], F32, IN)
    ident_d = P.dram("ident", [128, 128], F32, IN)
    rowmask_d = P.dram("rowmask", [128, 16], F32, IN)
    mconst_d = P.dram("mconst", [128, 3, 128], F32, IN)

    y_out = P.dram("y_out", [TOK, D], F32, OUT)
    retp_out = P.dram("retp_out", [H, DK, DV], F32, OUT)
    convp_out = P.dram("convp_out", [30, D], F32, OUT)
    rets_out = P.dram("rets_out", [16, H, DK, DV], F32, OUT)
    convs_out = P.dram("convs_out", [16, 30, D], F32, OUT)
    sinit = P.dram("sinit_scr", [H, DK, DV], F32, "Internal")
    ospill = P.dram("ospill_scr", [TOK, H * DV], BF16, OUT if debug else "Internal")

    ident_f = P.sb("ident_f", [128, 128], F32)
    ident_b = P.sb("ident_b", [128, 128], BF16)
    rowmask = P.sb("rowmask_s", [128, 16], F32)
    vec_s = P.sb("vec_s", [128, 6, 16], F32)
    hTh = P.sb("hTh", [128, 16, 128], BF16)
    small = P.sb("small", [128, 16], F32)
    epsb = P.sb("epsb", [128, 1], F32)

    AR = Arena(P, 184 * 1024)
    W = [AR.view("W%d" % i, i * 16384, [16, 512], BF16) for i in range(3)]
    OFF_HT = 49152
    OFF_HEAD = OFF_HT + 36864
    OFF_ROT = OFF_HEAD + 32256
    OFF_S = OFF_ROT + 8192
    OFF_S0 = OFF_S + 6144
    OFF_QK = OFF_S0 + 20480
    OFF_OF = OFF_QK + 3072 + 2048 + 4096
    OFF_END = OFF_OF + 2048
    cs_own = AR.view("cs_own_s", OFF_END, [2, TOK], F32)
    dec_h = AR.view("dec_h", OFF_END + 9216, [4, 128], F32)
    masks = AR.view("masks_s", OFF_END + 11264, [2, 128], F32)
    junk = AR.view("junk", OFF_END + 12800, [2048], BF16)

    pT = [P.ps("pT%d" % i, [128, 512]) for i in range(2)]
    pM = [P.ps("pM%d" % i, [128, 512]) for i in range(4)]
    pS = [P.ps("pS%d" % i, [128, 512]) for i in range(2)]
    cnt = {"m": 0, "t": 0, "s": 0, "w": 0}

    def nextM():
        cnt["m"] += 1
        return pM[cnt["m"] % 4]

    def nextT():
        cnt["t"] += 1
        return pT[cnt["t"] % 2]

    def nextS():
        cnt["s"] += 1
        return pS[cnt["s"] % 2]

    def nextW():
        cnt["w"] += 1
        return W[cnt["w"] % 3]

    def ld(dst, src_ap, eng="sp"):
        P.dma(eng, lambda e: e.dma_start(out=dst[:], in_=src_ap), writes=[dst], semtile=dst)

    ld(ident_f, ident_d[:])
    ld(cs_own, cs_own_d[:])
    ld(masks, masks_d[:])
    ld(rowmask, rowmask_d[:])
    ld(vec_s, vecs[:])
    P.op("dve", lambda e: e.tensor_copy(out=ident_b[:], in_=ident_f[:]), reads=[ident_f], writes=[ident_b])
    P.op("dve", lambda e: e.memset(epsb[:], EPS), writes=[epsb])

    def load_w(slot, src2d, c0=0):
        kc = src2d.shape[0] // 128
        ncols = src2d.shape[1]
        src = src2d.rearrange("(kc p) n -> p kc n", p=128)
        P.dma("pool", lambda e: e.dma_start(out=slot[:, 0:kc, c0:c0 + ncols], in_=src), writes=[slot], semtile=slot)

    def norm_to_T(x_dram, n_tiles, dstT, wrow, xin, xb):
        for t in range(n_tiles):
            xi = xin[t % 2]
            P.dma("sp", lambda e, xi=xi, t=t: e.dma_start(out=xi[:], in_=x_dram[t * 128:(t + 1) * 128, :]), writes=[xi], semtile=xi)
            ss = small
            P.op("act", lambda e, xi=xi: e.activation(out=junk[:], in_=xi[:], func=AF.Square, accum_out=small[:, 0:1]),
                 reads=[xi], writes=[junk, small])
            P.op("act", lambda e: e.activation(out=small[:, 1:2], in_=small[:, 0:1], func=AF.Sqrt, bias=epsb[:], scale=1.0 / D),
                 reads=[small, epsb], writes=[small])
            P.op("dve", lambda e: e.reciprocal(out=small[:, 2:3], in_=small[:, 1:2]), reads=[small], writes=[small])
            P.op("dve", lambda e, xi=xi: e.tensor_scalar(out=xb[:], in0=xi[:], scalar1=small[:, 2:3], scalar2=None, op0=ALU.mult),
                 reads=[xi, small], writes=[xb])
            for g in range(2):
                pt = nextT()
                ptb = pt[:].bitcast(BF16)
                for j in range(8):
                    kc = g * 8 + j
                    P.op("pe", lambda e, ptb=ptb, j=j, kc=kc: e.transpose(out=ptb[:, j * 128:(j + 1) * 128], in_=xb[:, kc * 128:(kc + 1) * 128], identity=ident_b[:]),
                         reads=[xb, ident_b], writes=[pt])
                P.op("dve", lambda e, ptb=ptb, g=g, t=t: e.tensor_tensor(
                    out=dstT[:, g * 8:(g + 1) * 8, t * 128:(t + 1) * 128],
                    in0=ptb.rearrange("p (a b) -> p a b", a=8),
                    in1=vec_s[:, wrow, g * 8:(g + 1) * 8].unsqueeze(2).to_broadcast([128, 8, 128]), op=ALU.mult),
                    reads=[pt, vec_s], writes=[dstT])

    def fm_proj(slot, c0, srcT, blks, evac):
        for bi, (t0, t1) in enumerate(blks):
            ps = nextM()
            for kc in range(16):
                P.op("pe", lambda e, ps=ps, kc=kc, t0=t0, t1=t1: e.matmul(ps[:, 0:t1 - t0], lhsT=slot[:, kc, c0:c0 + 128], rhs=srcT[:, kc, t0:t1], start=(kc == 0), stop=(kc == 15)),
                     reads=[slot, srcT], writes=[ps])
            evac(ps, bi, (t0, t1))

    def tm_proj(slot, ncols, srcT, tile, evac):
        ps = nextM()
        for kc in range(16):
            P.op("pe", lambda e, ps=ps, kc=kc: e.matmul(ps[:, 0:ncols], lhsT=srcT[:, kc, tile * 128:(tile + 1) * 128], rhs=slot[:, kc, 0:ncols], start=(kc == 0), stop=(kc == 15)),
                 reads=[slot, srcT], writes=[ps])
        evac(ps)

    def rotary_pair(p1, p2, n, cs, t0, decsel, out_fn, rot):
        cos = cs[:, 0, t0:t0 + n]
        sin = cs[:, 1, t0:t0 + n]
        ta, tb, tc, td = rot
        P.op("dve", lambda e: e.tensor_tensor(out=ta[:, 0:n], in0=p1[:, 0:n], in1=cos, op=ALU.mult), reads=[p1, cs], writes=[ta])
        P.op("dve", lambda e: e.tensor_tensor(out=tb[:, 0:n], in0=p2[:, 0:n], in1=sin, op=ALU.mult), reads=[p2, cs], writes=[tb])
        P.op("dve", lambda e: e.tensor_tensor(out=tc[:, 0:n], in0=p1[:, 0:n], in1=sin, op=ALU.mult), reads=[p1, cs], writes=[tc])
        P.op("dve", lambda e: e.tensor_tensor(out=td[:, 0:n], in0=p2[:, 0:n], in1=cos, op=ALU.mult), reads=[p2, cs], writes=[td])
        P.op("dve", lambda e: e.tensor_tensor(out=ta[:, 0:n], in0=ta[:, 0:n], in1=tb[:, 0:n], op=ALU.subtract), reads=[ta, tb], writes=[ta])
        P.op("dve", lambda e: e.tensor_tensor(out=tc[:, 0:n], in0=tc[:, 0:n], in1=td[:, 0:n], op=ALU.add), reads=[tc, td], writes=[tc])
        out_fn(0, ta)
        out_fn(1, tc)

    rot = [AR.view("rot%d" % i, OFF_ROT + i * 2048, [512], F32) for i in range(4)]
    xin = [AR.view("xin%d" % i, OFF_HEAD + i * 8192, [2048], F32) for i in range(2)]
    xb = AR.view("xb", OFF_HEAD + 16384, [2048], BF16)

    def dec_load(h):
        P.dma("sp", lambda e: e.dma_start(out=dec_h[:], in_=dec_d[h]), writes=[dec_h], semtile=dec_h)

    if "A" in stages:
        hTp = AR.view("hTp", OFF_HT, [16, 1024], BF16)
        cs_prev = AR.view("cs_prev", OFF_S0, [2, 1024], F32)
        ld(cs_prev, cs_prev_d[:])
        norm_to_T(x_prev, 8, hTp, 0, xin, xb)
        P.op("dve", lambda e: e.tensor_copy(out=hTh[:], in_=hTp[:, :, 896:1024]), reads=[hTp], writes=[hTh])
        P.barrier()
        kTa = AR.view("kTa", OFF_HEAD, [2, 1024], BF16)
        ktokA = AR.view("ktokA", OFF_HEAD + 4096, [8, 256], BF16)
        vA = AR.view("vA", OFF_HEAD + 8192, [8, 512], BF16)
        sstage = AR.view("sstageA", OFF_S, [512], F32)
        for h in range(H):
            dec_load(h)
            wk = nextW()
            load_w(wk, w_in[:, K0 + h * 256:K0 + (h + 1) * 256])
            wv = nextW()
            load_w(wv, w_in[:, V0 + h * 512:V0 + (h + 1) * 512])
            for bi, (t0, t1) in enumerate(((0, 512), (512, 1024))):
                pp = []
                for dc in range(2):
                    ps = nextM()
                    for kc in range(16):
                        P.op("pe", lambda e, ps=ps, kc=kc, dc=dc, t0=t0, t1=t1, wk=wk: e.matmul(ps[:, 0:512], lhsT=wk[:, kc, dc * 128:(dc + 1) * 128], rhs=hTp[:, kc, t0:t1], start=(kc == 0), stop=(kc == 15)),
                             reads=[wk, hTp], writes=[ps])
                    pp.append(ps)

                def outk(which, src, t0=t0):
                    P.op("dve", lambda e: e.tensor_tensor(out=kTa[:, which, t0:t0 + 512].rearrange("p (a b) -> p a b", a=4),
                                                          in0=src[:, 0:512].rearrange("p (a b) -> p a b", a=4),
                                                          in1=dec_h[:, 1, :].unsqueeze(1).to_broadcast([128, 4, 128]), op=ALU.mult),
                         reads=[src, dec_h], writes=[kTa])
                rotary_pair(pp[0], pp[1], 512, cs_prev, t0, None, outk, rot)
                for n in range(bi * 4, bi * 4 + 4):
                    tm_proj(wv, 512, hTp, n, lambda ps, n=n: P.op("act", lambda e: e.activation(out=vA[:, n, :], in_=ps[:, 0:512], func=AF.Copy), reads=[ps], writes=[vA]))
            for n in range(8):
                pt = nextT()
                ptb = pt[:].bitcast(BF16)
                for dc in range(2):
                    P.op("pe", lambda e, ptb=ptb, dc=dc, n=n: e.transpose(out=ptb[:, dc * 128:(dc + 1) * 128], in_=kTa[:, dc, n * 128:(n + 1) * 128], identity=ident_b[:]),
                         reads=[kTa, ident_b], writes=[pt])
                sc = CDEC_P[h] * float(np.exp(128 * (7 - n) * LOG_G[h]))
                P.op("act", lambda e, ptb=ptb, n=n, sc=sc: e.activation(out=ktokA[:, n, :], in_=ptb[:, 0:256], func=AF.Copy, scale=sc),
                     reads=[pt], writes=[ktokA])
            for dc in range(2):
                ps = nextS()
                for n in range(8):
                    P.op("pe", lambda e, ps=ps, n=n, dc=dc: e.matmul(ps[:, 0:512], lhsT=ktokA[:, n, dc * 128:(dc + 1) * 128], rhs=vA[:, n, :], start=(n == 0), stop=(n == 7)),
                         reads=[ktokA, vA], writes=[ps])
                P.op("act", lambda e, ps=ps: e.activation(out=sstage[:], in_=ps[:, 0:512], func=AF.Copy), reads=[ps], writes=[sstage])
                P.dma("sp", lambda e, dc=dc, h=h: e.dma_start(out=sinit[h, dc * 128:(dc + 1) * 128, :], in_=sstage[:]), reads=[sstage], writes=[sinit], semtile=sstage)
        P.barrier()

    hT = AR.view("hT", OFF_HT, [16, TOK], BF16)
    if "R" in stages:
        norm_to_T(x_own, NTILE, hT, 0, xin, xb)
        P.barrier()
        qT = AR.view("qT", OFF_HEAD, [2, TOK], BF16)
        kT = AR.view("kT", OFF_HEAD + 4608, [2, TOK], BF16)
        ktok = AR.view("ktok", OFF_HEAD + 9216, [NTILE, 256], BF16)
        vv = AR.view("vv", OFF_HEAD + 13824, [NTILE, 512], BF16)
        sg = AR.view("sg", OFF_HEAD + 23040, [NTILE, 512], BF16)
        Sf = AR.view("Sf", OFF_S, [2, 512], F32)
        Sb = AR.view("Sb", OFF_S + 4096, [2, 512], BF16)
        S0 = [AR.view("S0_%d" % i, OFF_S0 + i * 4096, [2, 512], F32) for i in range(3)]
        So = [AR.view("So_%d" % i, OFF_S0 + 12288 + i * 4096, [2, 512], F32) for i in range(2)]
        S0b = [AR.view("S0b%d" % i, OFF_QK + 3072 + 2048 + i * 2048, [2, 512], BF16) for i in range(2)]
        Qz = [AR.view("Qz%d" % i, OFF_QK + i * 512, [2, 128], BF16) for i in range(2)]
        Kz = [AR.view("Kz%d" % i, OFF_QK + 2048 + i * 512, [256], BF16) for i in range(2)]
        ofs = [AR.view("of%d" % i, OFF_OF + i * 1024, [512], BF16) for i in range(2)]
        attm = [AR.view("attm%d" % i, OFF_END + 12288 + i * 256, [128], BF16) for i in range(2)]
        for h in range(H):
            dec_load(h)
            wqk = nextW()
            load_w(wqk, w_in[:, Q0 + h * 256:Q0 + (h + 1) * 256], 0)
            load_w(wqk, w_in[:, K0 + h * 256:K0 + (h + 1) * 256], 256)
            wv = nextW()
            load_w(wv, w_in[:, V0 + h * 512:V0 + (h + 1) * 512])
            wg = nextW()
            load_w(wg, w_in[:, G0 + h * 512:G0 + (h + 1) * 512])
            if "A" in stages:
                P.dma("sp", lambda e, h=h: e.dma_start(out=Sf[:], in_=sinit[h].rearrange("(dc p) e -> p dc e", p=128)), reads=[sinit], writes=[Sf], semtile=Sf)
            else:
                P.op("dve", lambda e: e.memset(Sf[:], 0.0), writes=[Sf])
            P.op("act", lambda e: e.activation(out=Sb[:], in_=Sf[:], func=AF.Copy), reads=[Sf], writes=[Sb])
            vg_jobs = []
            for t in range(NTILE):
                vg_jobs.append(lambda t=t, wv=wv: tm_proj(wv, 512, hT, t, lambda ps, t=t: P.op("act", lambda e: e.activation(out=vv[:, t, :], in_=ps[:, 0:512], func=AF.Copy), reads=[ps], writes=[vv])))
                vg_jobs.append(lambda t=t, wg=wg: tm_proj(wg, 512, hT, t, lambda ps, t=t: P.op("act", lambda e: e.activation(out=sg[:, t, :], in_=ps[:, 0:512], func=AF.Silu), reads=[ps], writes=[sg])))
            for which, dstT in ((0, qT), (1, kT)):
                for bi, (t0, t1) in enumerate(BLKS):
                    n = t1 - t0
                    pp = []
                    for dc in range(2):
                        ps = nextM()
                        c0 = which * 256 + dc * 128
                        for kc in range(16):
                            P.op("pe", lambda e, ps=ps, kc=kc, c0=c0, t0=t0, t1=t1, wqk=wqk: e.matmul(ps[:, 0:t1 - t0], lhsT=wqk[:, kc, c0:c0 + 128], rhs=hT[:, kc, t0:t1], start=(kc == 0), stop=(kc == 15)),
                                 reads=[wqk, hT], writes=[ps])
                        pp.append(ps)
                    drow = which + (2 if bi == 2 else 0)

                    def outqk(dcw, src, t0=t0, n=n, drow=drow, dstT=dstT, bi=bi, which=which):
                        a = n // 128
                        P.op("dve", lambda e: e.tensor_tensor(out=dstT[:, dcw, t0:t0 + n].rearrange("p (a b) -> p a b", a=a),
                                                              in0=src[:, 0:n].rearrange("p (a b) -> p a b", a=a),
                                                              in1=dec_h[:, drow, :].unsqueeze(1).to_broadcast([128, a, 128]), op=ALU.mult),
                             reads=[src, dec_h], writes=[dstT])
                    rotary_pair(pp[0], pp[1], n, cs_own, t0, None, outqk, rot)
                    for _ in range(3):
                        if vg_jobs:
                            vg_jobs.pop(0)()
            while vg_jobs:
                vg_jobs.pop(0)()
            for t in range(NTILE):
                pt = nextT()
                ptb = pt[:].bitcast(BF16)
                for dc in range(2):
                    P.op("pe", lambda e, ptb=ptb, dc=dc, t=t: e.transpose(out=ptb[:, dc * 128:(dc + 1) * 128], in_=kT[:, dc, t * 128:(t + 1) * 128], identity=ident_b[:]),
                         reads=[kT, ident_b], writes=[pt])
                sc = CDEC_P[h] if t < 8 else CDEC_S[h]
                P.op("act", lambda e, ptb=ptb, t=t, sc=sc: e.activation(out=ktok[:, t, :], in_=ptb[:, 0:256], func=AF.Copy, scale=sc),
                     reads=[pt], writes=[ktok])
            for t in range(NTILE):
                tc0, tc1 = t * 128, (t + 1) * 128
                pa = nextM()
                for dc in range(2):
                    P.op("pe", lambda e, pa=pa, dc=dc, tc0=tc0, tc1=tc1: e.matmul(pa[:, 0:128], lhsT=kT[:, dc, tc0:tc1], rhs=qT[:, dc, tc0:tc1], start=(dc == 0), stop=(dc == 1)),
                         reads=[kT, qT], writes=[pa])
                am = attm[t % 2]
                mrow = 0 if t < 8 else 1
                P.op("dve", lambda e, pa=pa, am=am, mrow=mrow: e.tensor_tensor(out=am[:], in0=pa[:, 0:128], in1=masks[:, mrow, :], op=ALU.mult),
                     reads=[pa, masks], writes=[am])
                po = nextM()
                P.op("pe", lambda e, po=po, am=am, t=t: e.matmul(po[:, 0:512], lhsT=am[:], rhs=vv[:, t, :], start=True, stop=False),
                     reads=[am, vv], writes=[po])
                if t < 8:
                    for dc in range(2):
                        P.op("pe", lambda e, po=po, dc=dc, tc0=tc0, tc1=tc1: e.matmul(po[:, 0:512], lhsT=qT[:, dc, tc0:tc1], rhs=Sb[:, dc, :], start=False, stop=(dc == 1)),
                             reads=[qT, Sb], writes=[po])
                    for dc in range(2):
                        ps = nextS()
                        P.op("pe", lambda e, ps=ps, dc=dc, t=t: e.matmul(ps[:, 0:512], lhsT=ktok[:, t, dc * 128:(dc + 1) * 128], rhs=vv[:, t, :], start=True, stop=True),
                             reads=[ktok, vv], writes=[ps])
                        P.op("dve", lambda e, ps=ps, dc=dc, h=h: e.scalar_tensor_tensor(out=Sf[:, dc, :], in0=Sf[:, dc, :], scalar=CDEC_P[h], in1=ps[:, 0:512], op0=ALU.mult, op1=ALU.add),
                             reads=[Sf, ps], writes=[Sf])
                    if t < 7:
                        P.op("act", lambda e: e.activation(out=Sb[:], in_=Sf[:], func=AF.Copy), reads=[Sf], writes=[Sb])
                    else:
                        P.dma("sp", lambda e, h=h: e.dma_start(out=retp_out[h].rearrange("(dc p) e -> p dc e", p=128), in_=Sf[:]), reads=[Sf], semtile=Sf, is_output=True)
                else:
                    for bb in range(16):
                        s0 = S0[bb % 3]
                        P.dma("sp", lambda e, s0=s0, bb=bb, h=h: e.dma_start(out=s0[:], in_=sret[bb, h].rearrange("(dc p) e -> p dc e", p=128)), writes=[s0], semtile=s0)
                        qz = Qz[bb % 2]
                        s0b = S0b[bb % 2]
                        P.op("act", lambda e, s0=s0, s0b=s0b: e.activation(out=s0b[:], in_=s0[:], func=AF.Copy), reads=[s0], writes=[s0b])
                        P.op("dve", lambda e, qz=qz: e.memset(qz[:], 0.0), writes=[qz])
                        P.op("dve", lambda e, qz=qz, bb=bb: e.tensor_copy(out=qz[:, :, bb * 8:(bb + 1) * 8], in_=qT[:, :, 1024 + bb * 8:1024 + (bb + 1) * 8]), reads=[qT], writes=[qz])
                        for dc in range(2):
                            P.op("pe", lambda e, po=po, dc=dc, s0b=s0b, qz=qz, bb=bb: e.matmul(po[:, 0:512], lhsT=qz[:, dc, :], rhs=s0b[:, dc, :], start=False, stop=(dc == 1 and bb == 15)),
                                 reads=[qz, s0b], writes=[po])
                        kz = Kz[bb % 2]
                        P.op("dve", lambda e, kz=kz, bb=bb, t=t: e.tensor_scalar(out=kz[:], in0=ktok[:, t, :], scalar1=rowmask[:, bb:bb + 1], scalar2=None, op0=ALU.mult),
                             reads=[ktok, rowmask], writes=[kz])
                        so = So[bb % 2]
                        for dc in range(2):
                            ps = nextS()
                            P.op("pe", lambda e, ps=ps, dc=dc, kz=kz, t=t: e.matmul(ps[:, 0:512], lhsT=kz[:, dc * 128:(dc + 1) * 128], rhs=vv[:, t, :], start=True, stop=True),
                                 reads=[kz, vv], writes=[ps])
                            P.op("dve", lambda e, ps=ps, dc=dc, s0=s0, so=so, h=h: e.scalar_tensor_tensor(out=so[:, dc, :], in0=s0[:, dc, :], scalar=CDEC_S[h], in1=ps[:, 0:512], op0=ALU.mult, op1=ALU.add),
                                 reads=[s0, ps], writes=[so])
                        P.dma("pool", lambda e, so=so, bb=bb, h=h: e.dma_start(out=rets_out[bb, h].rearrange("(dc p) e -> p dc e", p=128), in_=so[:]), reads=[so], semtile=so, is_output=True)
                P.op("act", lambda e, po=po: e.activation(out=junk[:, 0:512], in_=po[:, 0:512], func=AF.Square, accum_out=small[:, 4:5]),
                     reads=[po], writes=[junk, small])
                P.op("act", lambda e: e.activation(out=small[:, 5:6], in_=small[:, 4:5], func=AF.Sqrt, bias=epsb[:], scale=1.0 / DV),
                     reads=[small, epsb], writes=[small])
                P.op("dve", lambda e: e.reciprocal(out=small[:, 6:7], in_=small[:, 5:6]), reads=[small], writes=[small])
                of = ofs[t % 2]
                P.op("dve", lambda e, po=po, of=of, t=t: e.scalar_tensor_tensor(out=of[:], in0=po[:, 0:512], scalar=small[:, 6:7], in1=sg[:, t, :], op0=ALU.mult, op1=ALU.mult),
                     reads=[po, small, sg], writes=[of])
                P.dma("sp", lambda e, of=of, t=t, h=h: e.dma_start(out=ospill[t * 128:(t + 1) * 128, h * 512:(h + 1) * 512], in_=of[:]), reads=[of], writes=[ospill], semtile=of, is_output=debug)
        P.barrier()

    OFF_Z = OFF_HEAD
    OFF_Y = OFF_Z + 36864
    OFF_X = OFF_Y + 36864
    dbg_fm = P.dram("dbg_fm", [128, 16, TOK], BF16, OUT) if debug else None
    if "C" in stages:
        cf = AR.view("cf", OFF_Z, [16, TOK], BF16)
        fmY = AR.view("fmY", OFF_Y, [16, TOK], BF16)
        def cset(i):
            b0 = OFF_Y + i * 17984
            return dict(uP=AR.view("uP%d" % i, b0, [1056], F32), uPb=AR.view("uPb%d" % i, b0 + 4224, [1056], BF16),
                        uS=AR.view("uS%d" % i, b0 + 6400, [16, 38], F32), uSb=AR.view("uSb%d" % i, b0 + 8832, [16, 38], BF16),
                        dg=AR.view("dg%d" % i, b0 + 10048, [31, 128], BF16))
        csets = [cset(0), cset(1)]
        cwT = AR.view("cwT", OFF_X, [16, 31], F32)
        sgt = [AR.view("sgt%d" % i, OFF_X + 2048 + i * 2048, [512], F32) for i in range(2)]
        scs = AR.view("scs", OFF_X + 6144, [4, 128], F32)
        cwrow = AR.view("cwrow", OFF_X + 8192, [2048], F32)
        strow = [AR.view("strow%d" % i, OFF_X + 16384 + i * 512, [128], F32) for i in range(2)]
        P.dma("sp", lambda e: e.dma_start(out=cwrow[0:CW, :], in_=conv_w[:]), writes=[cwrow], semtile=cwrow)
        for c in range(16):
            pt = nextT()
            P.op("pe", lambda e, pt=pt, c=c: e.transpose(out=pt[:, 0:CW], in_=cwrow[0:CW, c * 128:(c + 1) * 128], identity=ident_f[0:CW, 0:CW]),
                 reads=[cwrow, ident_f], writes=[pt])
            P.op("act", lambda e, pt=pt, c=c: e.activation(out=cwT[:, c, :], in_=pt[:, 0:CW], func=AF.Copy), reads=[pt], writes=[cwT])
        cpy = Tile("cpy", None)
        P.dma("sp", lambda e: e.dma_start(out=convs_out[:, 0:22, :], in_=sconv[:].rearrange("(b w) d -> b w d", w=30)[:, 8:30, :]), semtile=cpy, is_output=True)
        for c in range(16):
            if c % 4 == 0:
                wa = nextW()
                load_w(wa, w_in[:, CA0 + c * 128:CA0 + (c + 4) * 128])
                wb_ = nextW()
                load_w(wb_, w_in[:, CB0 + c * 128:CB0 + (c + 4) * 128])
            cc = (c % 4) * 128
            cs_ = csets[c % 2]
            uP, uPb, uS, uSb, dg = cs_["uP"], cs_["uPb"], cs_["uS"], cs_["uSb"], cs_["dg"]
            pst = nextT()
            for a in range(4):
                rows = 128 if a < 3 else 96
                st = strow[a % 2]
                P.dma("sp", lambda e, st=st, a=a, rows=rows, c=c: e.dma_start(out=st[0:rows, :], in_=sconv[a * 128:a * 128 + rows, c * 128:(c + 1) * 128]), writes=[st], semtile=st)
                P.op("pe", lambda e, pst=pst, st=st, a=a, rows=rows: e.transpose(out=pst[:, a * 128:a * 128 + rows], in_=st[0:rows, :], identity=ident_f[0:rows, 0:rows]),
                     reads=[st, ident_f], writes=[pst])
            P.op("act", lambda e, pst=pst, uS=uS: e.activation(out=uS[:, :, 0:30], in_=pst[:, 0:480].rearrange("p (b w) -> p b w", w=30), func=AF.Copy), reads=[pst], writes=[uS])
            segs = ((hTh, 0, 128, "h"), (hT, 0, 512, "p0"), (hT, 512, 1024, "p1"), (hT, 1024, 1152, "s"))
            for si, (src, t0, t1, kind) in enumerate(segs):
                n = t1 - t0
                pa_ = nextM()
                pb_ = nextM()
                for (pp_, wsl) in ((pa_, wa), (pb_, wb_)):
                    for kc in range(16):
                        P.op("pe", lambda e, pp_=pp_, wsl=wsl, kc=kc, cc=cc, src=src, t0=t0, t1=t1: e.matmul(pp_[:, 0:t1 - t0], lhsT=wsl[:, kc, cc:cc + 128], rhs=src[:, kc, t0:t1], start=(kc == 0), stop=(kc == 15)),
                             reads=[wsl, src], writes=[pp_])
                sgx = sgt[si % 2]
                P.op("act", lambda e, pb_=pb_, sgx=sgx, n=n: e.activation(out=sgx[:, 0:n], in_=pb_[:, 0:n], func=AF.Sigmoid), reads=[pb_], writes=[sgx])
                if kind == "h":
                    P.op("dve", lambda e, pa_=pa_, sgx=sgx, uP=uP: e.tensor_tensor(out=uP[:, 0:30], in0=pa_[:, 98:128], in1=sgx[:, 98:128], op=ALU.mult), reads=[pa_, sgx], writes=[uP])
                elif kind == "s":
                    P.op("dve", lambda e, pa_=pa_, sgx=sgx, uS=uS: e.tensor_tensor(out=uS[:, :, 30:38], in0=pa_[:, 0:128].rearrange("p (b i) -> p b i", i=8), in1=sgx[:, 0:128].rearrange("p (b i) -> p b i", i=8), op=ALU.mult),
                         reads=[pa_, sgx], writes=[uS])
                else:
                    P.op("dve", lambda e, pa_=pa_, sgx=sgx, t0=t0, uP=uP: e.tensor_tensor(out=uP[:, 30 + t0:30 + t0 + 512], in0=pa_[:, 0:512], in1=sgx[:, 0:512], op=ALU.mult), reads=[pa_, sgx], writes=[uP])
            P.op("act", lambda e, uP=uP, uPb=uPb: e.activation(out=uPb[:, 0:1054], in_=uP[:, 0:1054], func=AF.Copy), reads=[uP], writes=[uPb])
            P.op("dve", lambda e, uS=uS, uSb=uSb: e.tensor_copy(out=uSb[:], in_=uS[:]), reads=[uS], writes=[uSb])
            pt = nextT()
            P.op("pe", lambda e, pt=pt, uP=uP: e.transpose(out=pt[0:30, 0:128], in_=uP[:, 1024:1054], identity=ident_f[:]), reads=[uP, ident_f], writes=[pt])
            P.op("act", lambda e, pt=pt: e.activation(out=scs[0:30, 0, :], in_=pt[0:30, 0:128], func=AF.Copy), reads=[pt], writes=[scs])
            P.dma("pool", lambda e, c=c: e.dma_start(out=convp_out[:, c * 128:(c + 1) * 128], in_=scs[0:30, 0, :]), reads=[scs], semtile=scs, is_output=True)
            P.op("act", lambda e, uS=uS: e.activation(out=scs[:, 1, :].rearrange("p (b i) -> p b i", i=8), in_=uS[:, :, 30:38], func=AF.Copy), reads=[uS], writes=[scs])
            pt2 = nextT()
            P.op("pe", lambda e, pt2=pt2: e.transpose(out=pt2[:, 0:128], in_=scs[:, 1, :], identity=ident_f[:]), reads=[scs, ident_f], writes=[pt2])
            P.op("act", lambda e, pt2=pt2: e.activation(out=scs[:, 2, :], in_=pt2[:, 0:128], func=AF.Copy), reads=[pt2], writes=[scs])
            for bb in range(16):
                P.dma("pool", lambda e, bb=bb, c=c: e.dma_start(out=convs_out[bb, 22:30, c * 128:(c + 1) * 128], in_=scs[bb * 8:(bb + 1) * 8, 2, :]), reads=[scs], semtile=scs, is_output=True)
            for tap in range(CW):
                if tap % 2 == 0:
                    P.op("dve", lambda e, tap=tap, c=c, dg=dg: e.tensor_scalar(out=dg[:, tap, :], in0=ident_f[:], scalar1=cwT[:, c, tap:tap + 1], scalar2=None, op0=ALU.mult),
                         reads=[ident_f, cwT], writes=[dg])
                else:
                    P.op("act", lambda e, tap=tap, c=c, dg=dg: e.activation(out=dg[:, tap, :], in_=ident_f[:], func=AF.Identity, scale=cwT[:, c, tap:tap + 1]),
                         reads=[ident_f, cwT], writes=[dg])
            for (t0, n, kind) in ((0, 512, "p"), (512, 512, "p"), (1024, 128, "s")):
                pc = nextM()
                for tap in range(CW):
                    if kind == "p":
                        P.op("pe", lambda e, pc=pc, tap=tap, t0=t0, dg=dg, uPb=uPb: e.matmul(pc[:, 0:512], lhsT=dg[:, tap, :], rhs=uPb[:, t0 + tap:t0 + tap + 512], start=(tap == 0), stop=(tap == CW - 1)),
                             reads=[dg, uPb], writes=[pc])
                    else:
                        P.op("pe", lambda e, pc=pc, tap=tap, dg=dg, uSb=uSb: e.matmul(pc[:, 0:128], lhsT=dg[:, tap, :], rhs=uSb[:, :, tap:tap + 8], start=(tap == 0), stop=(tap == CW - 1)),
                             reads=[dg, uSb], writes=[pc])
                P.op("act", lambda e, pc=pc, t0=t0, n=n, c=c: e.activation(out=cf[:, c, t0:t0 + n], in_=pc[:, 0:n], func=AF.Identity, bias=vec_s[:, 2, c:c + 1]),
                     reads=[pc, vec_s], writes=[cf])
        P.barrier()
        ones_b = AR.view("ones_b", OFF_Y, [128], BF16)
        sq = [AR.view("sq%d" % i, OFF_Y + 256 + i * 1024, [512], BF16) for i in range(2)]
        mu_t = AR.view("mu_t", OFF_Y + 2304, [TOK], F32)
        rs_t = AR.view("rs_t", OFF_Y + 2304 + 4608, [TOK], F32)
        lt = [AR.view("lt%d" % i, OFF_Y + 11520 + i * 2048, [512], F32) for i in range(2)]
        epsw = AR.view("epsw", OFF_Y + 15616, [1], F32)
        P.op("dve", lambda e: e.memset(ones_b[:], 1.0), writes=[ones_b])
        P.op("dve", lambda e: e.memset(epsw[:], EPS), writes=[epsw])
        for (t0, t1) in BLKS:
            n = t1 - t0
            p1 = nextM()
            p2 = nextM()
            for c in range(16):
                P.op("pe", lambda e, p1=p1, c=c, t0=t0, t1=t1: e.matmul(p1[:, 0:t1 - t0], lhsT=ones_b[:], rhs=cf[:, c, t0:t1], start=(c == 0), stop=(c == 15)),
                     reads=[ones_b, cf], writes=[p1])
                sqx = sq[c % 2]
                P.op("act", lambda e, sqx=sqx, c=c, t0=t0, t1=t1: e.activation(out=sqx[:, 0:t1 - t0], in_=cf[:, c, t0:t1], func=AF.Square), reads=[cf], writes=[sqx])
                P.op("pe", lambda e, p2=p2, sqx=sqx, c=c, n=n: e.matmul(p2[:, 0:n], lhsT=ones_b[:], rhs=sqx[:, 0:n], start=(c == 0), stop=(c == 15)),
                     reads=[ones_b, sqx], writes=[p2])
            P.op("act", lambda e, p1=p1, t0=t0, n=n: e.activation(out=mu_t[:, t0:t0 + n], in_=p1[:, 0:n], func=AF.Copy, scale=1.0 / D), reads=[p1], writes=[mu_t])
            l0 = lt[0]
            P.op("dve", lambda e, t0=t0, n=n, l0=l0: e.tensor_tensor(out=l0[:, 0:n], in0=mu_t[:, t0:t0 + n], in1=mu_t[:, t0:t0 + n], op=ALU.mult), reads=[mu_t], writes=[l0])
            P.op("dve", lambda e, p2=p2, n=n, l0=l0: e.scalar_tensor_tensor(out=l0[:, 0:n], in0=p2[:, 0:n], scalar=1.0 / D, in1=l0[:, 0:n], op0=ALU.mult, op1=ALU.subtract), reads=[p2, l0], writes=[l0])
            P.op("act", lambda e, n=n, l0=l0: e.activation(out=l0[:, 0:n], in_=l0[:, 0:n], func=AF.Sqrt, bias=epsw[:], scale=1.0), reads=[l0, epsw], writes=[l0])
            P.op("dve", lambda e, t0=t0, n=n, l0=l0: e.reciprocal(out=rs_t[:, t0:t0 + n], in_=l0[:, 0:n]), reads=[l0], writes=[rs_t])
        for c in range(16):
            for (t0, t1) in BLKS:
                n = t1 - t0
                lx = lt[(c * 3 + (t0 // 512)) % 2]
                P.op("dve", lambda e, lx=lx, c=c, t0=t0, t1=t1: e.tensor_tensor(out=lx[:, 0:t1 - t0], in0=cf[:, c, t0:t1], in1=mu_t[:, t0:t1], op=ALU.subtract), reads=[cf, mu_t], writes=[lx])
                P.op("dve", lambda e, lx=lx, t0=t0, t1=t1: e.tensor_tensor(out=lx[:, 0:t1 - t0], in0=lx[:, 0:t1 - t0], in1=rs_t[:, t0:t1], op=ALU.mult), reads=[lx, rs_t], writes=[lx])
                P.op("act", lambda e, lx=lx, c=c, t0=t0, t1=t1: e.activation(out=cf[:, c, t0:t1], in_=lx[:, 0:t1 - t0], func=AF.Silu, bias=vec_s[:, 4, c:c + 1], scale=vec_s[:, 3, c:c + 1]),
                     reads=[lx, vec_s], writes=[cf])
        P.barrier()
        for c4 in range(4):
            wsl = nextW()
            load_w(wsl, w_in[:, GC0 + c4 * 512:GC0 + (c4 + 1) * 512])
            for j in range(4):
                c = c4 * 4 + j
                fm_proj(wsl, j * 128, hT, BLKS, lambda ps, bi, tt, c=c: P.op("act", lambda e: e.activation(out=fmY[:, c, tt[0]:tt[1]], in_=ps[:, 0:tt[1] - tt[0]], func=AF.Sigmoid), reads=[ps], writes=[fmY]))
        for c4 in range(4):
            wsl = nextW()
            load_w(wsl, w_conv_o[:, c4 * 512:(c4 + 1) * 512])
            for j in range(4):
                c = c4 * 4 + j
                fm_proj(wsl, j * 128, cf, BLKS, lambda ps, bi, tt, c=c: P.op("dve", lambda e: e.tensor_tensor(out=fmY[:, c, tt[0]:tt[1]], in0=ps[:, 0:tt[1] - tt[0]], in1=fmY[:, c, tt[0]:tt[1]], op=ALU.mult), reads=[ps, fmY], writes=[fmY]))
        P.barrier()
        fmZ = AR.view("fmZ", OFF_Z, [16, TOK], BF16)
        for c4 in range(4):
            wsl = nextW()
            load_w(wsl, w_in[:, GR0 + c4 * 512:GR0 + (c4 + 1) * 512])
            for j in range(4):
                c = c4 * 4 + j
                fm_proj(wsl, j * 128, hT, BLKS, lambda ps, bi, tt, c=c: P.op("act", lambda e: e.activation(out=fmZ[:, c, tt[0]:tt[1]], in_=ps[:, 0:tt[1] - tt[0]], func=AF.Sigmoid), reads=[ps], writes=[fmZ]))
        P.barrier()

    if "M" in stages:
        oT = AR.view("oT", OFF_HT, [32, 512], BF16)
        orow = [AR.view("orow%d" % i, OFF_X + i * 8192, [4096], BF16) for i in range(2)]
        mt = [AR.view("mt%d" % i, OFF_X + 16384 + i * 2048, [512], F32) for i in range(2)]
        Wr = [Tile("Wr%d" % i, W[i][:].rearrange("p a b -> p (a b)").rearrange("p (a b) -> p a b", a=32)) for i in range(3)]
        for bi, (t0, t1) in enumerate(BLKS):
            n = t1 - t0
            for ti in range(n // 128):
                t = t0 // 128 + ti
                orw = orow[t % 2]
                P.dma("sp", lambda e, orw=orw, t=t: e.dma_start(out=orw[:], in_=ospill[t * 128:(t + 1) * 128, :]), reads=[ospill], writes=[orw], semtile=orw)
                for g in range(4):
                    pt = nextT()
                    ptb = pt[:].bitcast(BF16)
                    for j in range(8):
                        kc = g * 8 + j
                        P.op("pe", lambda e, ptb=ptb, j=j, kc=kc, orw=orw: e.transpose(out=ptb[:, j * 128:(j + 1) * 128], in_=orw[:, kc * 128:(kc + 1) * 128], identity=ident_b[:]),
                             reads=[orw, ident_b], writes=[pt])
                    P.op("act", lambda e, ptb=ptb, g=g, ti=ti: e.activation(out=oT[:, g * 8:(g + 1) * 8, ti * 128:(ti + 1) * 128], in_=ptb.rearrange("p (a b) -> p a b", a=8), func=AF.Copy),
                         reads=[pt], writes=[oT])
            for c2 in range(8):
                wsl = Wr[(bi * 8 + c2) % 3]
                src = w_ret_o[:, c2 * 256:(c2 + 1) * 256].rearrange("(kc p) n -> p kc n", p=128)
                P.dma("pool", lambda e, wsl=wsl, src=src: e.dma_start(out=wsl[:], in_=src), writes=[wsl], semtile=wsl)
                for j in range(2):
                    c = c2 * 2 + j
                    ps = nextM()
                    for kc in range(32):
                        P.op("pe", lambda e, ps=ps, kc=kc, wsl=wsl, j=j, n=n: e.matmul(ps[:, 0:n], lhsT=wsl[:, kc, j * 128:(j + 1) * 128], rhs=oT[:, kc, 0:n], start=(kc == 0), stop=(kc == 31)),
                             reads=[wsl, oT], writes=[ps])
                    mx = mt[c % 2]
                    P.op("dve", lambda e, ps=ps, mx=mx, c=c, t0=t0, t1=t1: e.tensor_tensor(out=mx[:, 0:t1 - t0], in0=ps[:, 0:t1 - t0], in1=fmZ[:, c, t0:t1], op=ALU.mult), reads=[ps, fmZ], writes=[mx])
                    P.op("dve", lambda e, mx=mx, c=c, t0=t0, t1=t1: e.tensor_tensor(out=fmY[:, c, t0:t1], in0=mx[:, 0:t1 - t0], in1=fmY[:, c, t0:t1], op=ALU.add), reads=[mx, fmY], writes=[fmY])
        if debug and stages.endswith("M"):
            P.dma("sp", lambda e: e.dma_start(out=dbg_fm[:], in_=fmY[:]), reads=[fmY], semtile=fmY, is_output=True)
        P.barrier()

    if "O" in stages:
        yacc = AR.view("yacc", OFF_HT, [NTILE, D], F32)
        xt = [AR.view("xt%d" % i, OFF_X + i * 2048, [512], F32) for i in range(2)]
        junk2 = AR.view("junk2", OFF_X + 4096, [2048], BF16)
        xb2 = AR.view("xb2", OFF_X + 8192, [2048], BF16)
        for cb in range(4):
            wsl = nextW()
            load_w(wsl, w_o[:, cb * 512:(cb + 1) * 512])
            for t in range(NTILE):
                xx = xt[t % 2]
                P.dma("sp", lambda e, xx=xx, t=t, cb=cb: e.dma_start(out=xx[:], in_=x_own[t * 128:(t + 1) * 128, cb * 512:(cb + 1) * 512]), writes=[xx], semtile=xx)
                ps = nextM()
                for kc in range(16):
                    P.op("pe", lambda e, ps=ps, kc=kc, t=t, wsl=wsl: e.matmul(ps[:, 0:512], lhsT=fmY[:, kc, t * 128:(t + 1) * 128], rhs=wsl[:, kc, :], start=(kc == 0), stop=(kc == 15)),
                         reads=[fmY, wsl], writes=[ps])
                P.op("dve", lambda e, ps=ps, xx=xx, t=t, cb=cb: e.tensor_tensor(out=yacc[:, t, cb * 512:(cb + 1) * 512], in0=ps[:, 0:512], in1=xx[:], op=ALU.add), reads=[ps, xx], writes=[yacc])
        P.barrier()
        if stages.endswith("O"):
            for t in range(NTILE):
                P.dma("sp", lambda e, t=t: e.dma_start(out=y_out[t * 128:(t + 1) * 128, :], in_=yacc[:, t, :]), reads=[yacc], semtile=yacc, is_output=True)
        NBLK = 2 * TOK // 128 + NE
        h2p = AR.view("h2p", OFF_Y, [NTILE, D], BF16)
        h2Tt = AR.view("h2Tt", OFF_X + 12288, [16, 128], BF16)
        XO = OFF_X + 16384
        OH1s = AR.view("OH1s", XO, [NTILE, 32], F32)
        OH2s = AR.view("OH2s", XO + 1152, [NTILE, 32], F32)
        G12 = AR.view("G12", XO + 2304, [2, 16], F32)
        R12 = AR.view("R12", XO + 2432, [2, 16], F32)
        carry = AR.view("carry", XO + 2560, [32], F32)
        rt = AR.view("rt", XO + 2688, [256], F32)
        mconst = AR.view("mconst", XO + 3712, [3, 128], F32)
        ones_f = AR.view("ones_f", XO + 5248, [128], F32)
        wrt = AR.view("wrt", XO + 5760, [16, 36], BF16)
        brt = AR.view("brt", XO + 6912, [36], F32)
        rk = AR.view("rk", XO + 7056, [64], F32)
        x1scr = P.dram("x1_scr", [TOK, D], F32, "Internal")
        P.dma("sp", lambda e: e.dma_start(out=mconst[:], in_=mconst_d[:]), writes=[mconst], semtile=mconst)
        P.dma("pool", lambda e: e.dma_start(out=wrt[:], in_=w_rt[:].rearrange("(kc p) n -> p kc n", p=128)), writes=[wrt], semtile=wrt)
        P.dma("sp", lambda e: e.dma_start(out=brt[:], in_=b_rt[:].partition_broadcast(128)), writes=[brt], semtile=brt)
        P.op("dve", lambda e: e.memset(ones_f[:], 1.0), writes=[ones_f])
        P.op("dve", lambda e: e.memset(carry[:], 0.0), writes=[carry])
        for t in range(NTILE):
            P.dma("sp", lambda e, t=t: e.dma_start(out=x1scr[t * 128:(t + 1) * 128, :], in_=yacc[:, t, :]), reads=[yacc], writes=[x1scr], semtile=yacc)
            P.op("act", lambda e, t=t: e.activation(out=junk2[:], in_=yacc[:, t, :], func=AF.Square, accum_out=small[:, 0:1]), reads=[yacc], writes=[junk2, small])
            P.op("act", lambda e: e.activation(out=small[:, 1:2], in_=small[:, 0:1], func=AF.Sqrt, bias=epsb[:], scale=1.0 / D), reads=[small, epsb], writes=[small])
            P.op("dve", lambda e: e.reciprocal(out=small[:, 2:3], in_=small[:, 1:2]), reads=[small], writes=[small])
            P.op("dve", lambda e, t=t: e.tensor_scalar(out=xb2[:], in0=yacc[:, t, :], scalar1=small[:, 2:3], scalar2=None, op0=ALU.mult), reads=[yacc, small], writes=[xb2])
            for g in range(2):
                pt = nextT()
                ptb = pt[:].bitcast(BF16)
                for j in range(8):
                    kc = g * 8 + j
                    P.op("pe", lambda e, ptb=ptb, j=j, kc=kc: e.transpose(out=ptb[:, j * 128:(j + 1) * 128], in_=xb2[:, kc * 128:(kc + 1) * 128], identity=ident_b[:]), reads=[xb2, ident_b], writes=[pt])
                P.op("dve", lambda e, ptb=ptb, g=g: e.tensor_tensor(out=h2Tt[:, g * 8:(g + 1) * 8, :], in0=ptb.rearrange("p (a b) -> p a b", a=8),
                                                                in1=vec_s[:, 1, g * 8:(g + 1) * 8].unsqueeze(2).to_broadcast([128, 8, 128]), op=ALU.mult), reads=[pt, vec_s], writes=[h2Tt])
            for g in range(2):
                pt = nextT()
                ptb = pt[:].bitcast(BF16)
                for j in range(8):
                    kc = g * 8 + j
                    P.op("pe", lambda e, ptb=ptb, j=j, kc=kc: e.transpose(out=ptb[:, j * 128:(j + 1) * 128], in_=h2Tt[:, kc, :], identity=ident_b[:]), reads=[h2Tt, ident_b], writes=[pt])
                P.op("act", lambda e, ptb=ptb, g=g, t=t: e.activation(
                    out=h2p[:, t, :].rearrange("t (j p) -> t p j", j=16)[:, g * 64:(g + 1) * 64, :],
                    in_=ptb.rearrange("t (p j) -> t p j", j=16), func=AF.Copy), reads=[pt], writes=[h2p])
            ps = nextM()
            for kc in range(16):
                P.op("pe", lambda e, ps=ps, kc=kc: e.matmul(ps[:, 0:36], lhsT=h2Tt[:, kc, :], rhs=wrt[:, kc, :], start=(kc == 0), stop=(kc == 15)), reads=[h2Tt, wrt], writes=[ps])
            dv = lambda fn, rd=(), wr=(): P.op("dve", fn, reads=[rt] + list(rd), writes=[rt] + list(wr))
            dv(lambda e, ps=ps: e.tensor_tensor(out=rt[:, 0:36], in0=ps[:, 0:36], in1=brt[:], op=ALU.add), rd=[ps, brt])
            dv(lambda e: e.tensor_reduce(out=rt[:, 40:41], in_=rt[:, 0:4], axis=mybir.AxisListType.X, op=ALU.max))
            dv(lambda e: e.tensor_scalar(out=rt[:, 44:48], in0=rt[:, 0:4], scalar1=rt[:, 40:41], scalar2=None, op0=ALU.is_equal))
            dv(lambda e: e.tensor_scalar(out=rt[:, 41:42], in0=rt[:, 40:41], scalar1=-1.0, scalar2=None, op0=ALU.mult))
            P.op("act", lambda e: e.activation(out=rt[:, 48:52], in_=rt[:, 0:4], func=AF.Exp, bias=rt[:, 41:42], scale=1.0, accum_out=rt[:, 42:43]), reads=[rt], writes=[rt])
            dv(lambda e: e.reciprocal(out=rt[:, 43:44], in_=rt[:, 42:43]))
            dv(lambda e: e.tensor_scalar(out=rt[:, 52:56], in0=rt[:, 44:48], scalar1=-1.0, scalar2=1e30, op0=ALU.add, op1=ALU.mult))
            dv(lambda e: e.tensor_tensor(out=rt[:, 64:96].rearrange("p (g x) -> p g x", g=4), in0=rt[:, 4:36].rearrange("p (g x) -> p g x", g=4),
                                         in1=rt[:, 52:56].unsqueeze(2).to_broadcast([128, 4, 8]), op=ALU.add))
            dv(lambda e: e.tensor_reduce(out=rt[:, 56:57], in_=rt[:, 64:96], axis=mybir.AxisListType.X, op=ALU.max))
            dv(lambda e, t=t: e.tensor_scalar(out=OH1s[:, t, :], in0=rt[:, 64:96], scalar1=rt[:, 56:57], scalar2=None, op0=ALU.is_equal), wr=[OH1s])
            dv(lambda e, t=t: e.scalar_tensor_tensor(out=rt[:, 128:160], in0=OH1s[:, t, :], scalar=-1e30, in1=rt[:, 64:96], op0=ALU.mult, op1=ALU.add), rd=[OH1s])
            dv(lambda e: e.tensor_reduce(out=rt[:, 57:58], in_=rt[:, 128:160], axis=mybir.AxisListType.X, op=ALU.max))
            dv(lambda e, t=t: e.tensor_scalar(out=OH2s[:, t, :], in0=rt[:, 128:160], scalar1=rt[:, 57:58], scalar2=None, op0=ALU.is_equal), wr=[OH2s])
            dv(lambda e: e.tensor_scalar(out=rt[:, 58:59], in0=rt[:, 56:57], scalar1=-1.0, scalar2=None, op0=ALU.mult))
            P.op("act", lambda e: e.activation(out=rt[:, 59:60], in_=rt[:, 57:58], func=AF.Exp, bias=rt[:, 58:59], scale=1.0), reads=[rt], writes=[rt])
            dv(lambda e: e.tensor_scalar(out=rt[:, 60:61], in0=rt[:, 59:60], scalar1=1.0, scalar2=None, op0=ALU.add))
            dv(lambda e: e.reciprocal(out=rt[:, 61:62], in_=rt[:, 60:61]))
            dv(lambda e, t=t: e.tensor_tensor(out=G12[:, 0, t:t + 1], in0=rt[:, 61:62], in1=rt[:, 43:44], op=ALU.mult), wr=[G12])
            dv(lambda e, t=t: e.tensor_tensor(out=G12[:, 1, t:t + 1], in0=G12[:, 0, t:t + 1], in1=rt[:, 59:60], op=ALU.mult), rd=[G12], wr=[G12])
            for k, OHs in ((0, OH1s), (1, OH2s)):
                pr = nextM()
                P.op("pe", lambda e, pr=pr, OHs=OHs, t=t: e.matmul(pr[:, 0:32], lhsT=mconst[:, 0, :], rhs=OHs[:, t, :], start=True, stop=True), reads=[mconst, OHs], writes=[pr])
                pc_ = nextM()
                P.op("pe", lambda e, pc_=pc_, OHs=OHs, t=t: e.matmul(pc_[:, 0:32], lhsT=ones_f[:], rhs=OHs[:, t, :], start=True, stop=True), reads=[ones_f, OHs], writes=[pc_])
                P.op("dve", lambda e, pr=pr: e.tensor_tensor(out=rk[:, 0:32], in0=pr[:, 0:32], in1=carry[:], op=ALU.add), reads=[pr, carry], writes=[rk])
                P.op("dve", lambda e, OHs=OHs, t=t: e.tensor_tensor(out=rk[:, 32:64], in0=OHs[:, t, :], in1=rk[:, 0:32], op=ALU.mult), reads=[OHs, rk], writes=[rk])
                P.op("dve", lambda e, t=t, k=k: e.tensor_reduce(out=R12[:, k, t:t + 1], in_=rk[:, 32:64], axis=mybir.AxisListType.X, op=ALU.add), reads=[rk], writes=[R12])
                P.op("dve", lambda e, pc_=pc_: e.tensor_tensor(out=carry[:], in0=pc_[:, 0:32], in1=carry[:], op=ALU.add), reads=[pc_, carry], writes=[carry])
        if debug and stages.endswith("O"):
            dbg_s = P.dram("dbg_s", [128, 96], F32, OUT)
            P.dma("sp", lambda e: e.dma_start(out=dbg_s[:, 0:32], in_=R12[:].rearrange("p a b -> p (a b)")), reads=[R12], semtile=R12, is_output=True)
            P.dma("sp", lambda e: e.dma_start(out=dbg_s[:, 32:64], in_=G12[:].rearrange("p a b -> p (a b)")), reads=[G12], semtile=G12, is_output=True)
            P.dma("sp", lambda e: e.dma_start(out=dbg_s[:, 64:96], in_=carry[:]), reads=[carry], semtile=carry, is_output=True)
        P.barrier()

    if "E" in stages:
        EO = XO + 8192
        nblk = AR.view("nblk", EO, [32], F32)
        pst = AR.view("pst", EO + 128, [32], F32)
        pend = AR.view("pend", EO + 256, [32], F32)
        prow = AR.view("prow", EO + 384, [32], F32)
        ebf = AR.view("ebf", EO + 512, [64], F32)
        idxW = AR.view("idxW", EO + 768, [64], I32)
        Ri = AR.view("Ri", EO + 1024, [2, 16], I32)
        ebe = AR.view("ebe", EO + 1152, [64], F32)
        big = AR.view("big", OFF_X, [NBLK, 32], F32)
        big2 = AR.view("big2", OFF_X + 6400, [NBLK, 32], F32)
        P.op("dve", lambda e: e.memset(nblk[:], 0.0), writes=[nblk])
        for m in range(2 * TOK // 128 + 1):
            P.op("dve", lambda e, m=m: e.scalar_tensor_tensor(out=nblk[:], in0=carry[:], scalar=float(128 * m), in1=nblk[:], op0=ALU.is_gt, op1=ALU.add), reads=[carry, nblk], writes=[nblk])
        P.op("dve", lambda e: e.memset(pst[:, 0:1], 0.0), writes=[pst])
        for ei in range(1, 32):
            P.op("dve", lambda e, ei=ei: e.tensor_tensor(out=pst[:, ei:ei + 1], in0=pst[:, ei - 1:ei], in1=nblk[:, ei - 1:ei], op=ALU.add), reads=[pst, nblk], writes=[pst])
        P.op("dve", lambda e: e.tensor_tensor(out=pend[:], in0=pst[:], in1=nblk[:], op=ALU.add), reads=[pst, nblk], writes=[pend])
        P.op("dve", lambda e: e.tensor_scalar(out=prow[:], in0=pst[:], scalar1=128.0, scalar2=None, op0=ALU.mult), reads=[pst], writes=[prow])
        for t in range(NTILE):
            for k, OHs in ((0, OH1s), (1, OH2s)):
                P.op("dve", lambda e, OHs=OHs, t=t: e.tensor_tensor(out=rk[:, 32:64], in0=OHs[:, t, :], in1=prow[:], op=ALU.mult), reads=[OHs, prow], writes=[rk])
                P.op("dve", lambda e: e.tensor_reduce(out=rk[:, 0:1], in_=rk[:, 32:64], axis=mybir.AxisListType.X, op=ALU.add), reads=[rk], writes=[rk])
                P.op("dve", lambda e, t=t, k=k: e.tensor_tensor(out=R12[:, k, t:t + 1], in0=R12[:, k, t:t + 1], in1=rk[:, 0:1], op=ALU.add), reads=[R12, rk], writes=[R12])
        P.op("dve", lambda e: e.tensor_copy(out=Ri[:], in_=R12[:]), reads=[R12], writes=[Ri])
        bio = mconst[:, 1, 0:NBLK].unsqueeze(2).to_broadcast([128, NBLK, 32])
        P.op("dve", lambda e: e.tensor_tensor(out=big[:], in0=pst[:].unsqueeze(1).to_broadcast([128, NBLK, 32]), in1=bio, op=ALU.is_le), reads=[pst, mconst], writes=[big])
        P.op("dve", lambda e: e.tensor_tensor(out=big2[:], in0=pend[:].unsqueeze(1).to_broadcast([128, NBLK, 32]), in1=bio, op=ALU.is_gt), reads=[pend, mconst], writes=[big2])
        P.op("dve", lambda e: e.tensor_tensor(out=big[:], in0=big[:], in1=big2[:], op=ALU.mult), reads=[big, big2], writes=[big])
        P.op("dve", lambda e: e.tensor_reduce(out=ebf[:, 0:NBLK], in_=big[:], axis=mybir.AxisListType.X, op=ALU.add), reads=[big], writes=[ebf])
        P.op("dve", lambda e: e.tensor_tensor(out=big2[:], in0=big[:], in1=mconst[:, 1, 0:32].unsqueeze(1).to_broadcast([128, NBLK, 32]), op=ALU.mult), reads=[big, mconst], writes=[big2])
        P.op("dve", lambda e: e.tensor_reduce(out=ebe[:, 0:NBLK], in_=big2[:], axis=mybir.AxisListType.X, op=ALU.add), reads=[big2], writes=[ebe])
        idx2f = AR.view("idx2f", OFF_X, [NBLK, 2], F32)
        idx2 = AR.view("idx2", EO + 1408, [NBLK, 2], I32)
        base2 = AR.view("base2", EO + 768, [2], F32)
        P.op("dve", lambda e: e.tensor_scalar(out=ebf[:, 0:NBLK], in0=ebf[:, 0:NBLK], scalar1=-1.0, scalar2=-1.0e4, op0=ALU.add, op1=ALU.mult), reads=[ebf], writes=[ebf])
        P.op("dve", lambda e: e.tensor_tensor(out=ebf[:, 0:NBLK], in0=ebf[:, 0:NBLK], in1=ebe[:, 0:NBLK], op=ALU.add), reads=[ebf, ebe], writes=[ebf])
        P.op("dve", lambda e: e.tensor_scalar(out=ebf[:, 0:NBLK], in0=ebf[:, 0:NBLK], scalar1=256.0, scalar2=None, op0=ALU.mult), reads=[ebf], writes=[ebf])
        P.op("dve", lambda e: e.scalar_tensor_tensor(out=base2[:], in0=mconst[:, 2, 0:2], scalar=2.0, in1=mconst[:, 1, 0:2], op0=ALU.mult, op1=ALU.add), reads=[mconst], writes=[base2])
        P.op("dve", lambda e: e.tensor_tensor(out=idx2f[:], in0=ebf[:, 0:NBLK].unsqueeze(2).to_broadcast([128, NBLK, 2]), in1=base2[:].unsqueeze(1).to_broadcast([128, NBLK, 2]), op=ALU.add),
             reads=[ebf, base2, big], writes=[idx2f])
        P.op("dve", lambda e: e.tensor_copy(out=idx2[:], in_=idx2f[:]), reads=[idx2f], writes=[idx2])
        P.barrier()
        WS = [Tile("WS%d" % i, AR.t[:, i * 32768:(i + 1) * 32768].bitcast(BF16)) for i in range(3)]
        wcnt = {"n": 0}

        def nextWS():
            wcnt["n"] += 1
            return WS[wcnt["n"] % 3]
        MO = 98304
        iob = [AR.view("iob%d" % i, MO + i * 512, [128], F32) for i in range(2)]
        selt = [AR.view("selt%d" % i, MO + 1024 + i * 256, [128], BF16) for i in range(2)]
        Sel = [AR.view("Sel%d" % i, MO + 1536 + i * 2304, [NTILE, 128], BF16) for i in range(2)]
        XbT = [AR.view("XbT%d" % i, MO + 6144 + i * 4096, [16, 128], BF16) for i in range(2)]
        sgm = [AR.view("sgm%d" % i, MO + 14336 + i * 2048, [512], F32) for i in range(2)]
        hperm = AR.view("hperm", MO + 18432, [1024], BF16)
        hTm = AR.view("hTm", MO + 20480, [8, 128], BF16)
        Yst = [AR.view("Yst%d" % i, OFF_X + i * 8192, [D], F32) for i in range(2)]
        yscr = P.dram("y_scr", [NBLK * 128, D], F32, "Internal")
        regs = {}

        def breg(e, key, val):
            if key not in regs:
                regs[key] = e.to_reg(val)
            return regs[key]
        wflat = {id(w_gate): w_gate[:].rearrange("e k n -> (e k n)").rearrange("(r c) -> r c", c=8192),
                 id(w_up): w_up[:].rearrange("e k n -> (e k n)").rearrange("(r c) -> r c", c=8192),
                 id(w_down): w_down[:].rearrange("e k n -> (e k n)").rearrange("(r c) -> r c", c=8192)}
        bound = n_experts * 256 - 1

        def wload(slot, wsrc, b, J):
            src2 = wflat[id(wsrc)]
            for hh in range(2):
                P.dma("pool", lambda e, hh=hh: e.indirect_dma_start(out=slot[:, hh * 8192:(hh + 1) * 8192], out_offset=None, in_=src2, in_offset=bass.IndirectOffsetOnAxis(ap=idx2[:, b, hh:hh + 1], axis=0),
                                                               bounds_check=breg(e, "w", bound), oob_is_err=False), reads=[idx2], writes=[slot], semtile=slot)
        border = []
        for i_ in range(32):
            border.append(i_)
            if 32 + i_ < NBLK:
                border.append(32 + i_)
        assert sorted(border) == list(range(NBLK))
        for b in border:
            wg_ = nextWS(); wload(wg_, w_gate, b, 16)
            wu_ = nextWS(); wload(wu_, w_up, b, 16)
            wd_ = nextWS(); wload(wd_, w_down, b, 8)
            bo = border.index(b)
            io_ = iob[bo % 2]
            P.op("dve", lambda e, io_=io_, b=b: e.tensor_scalar(out=io_[:], in0=mconst[:, 1, :], scalar1=float(128 * b), scalar2=None, op0=ALU.add), reads=[mconst], writes=[io_])
            sel = Sel[bo % 2]
            for t in range(NTILE):
                st_ = selt[t % 2]
                P.op("dve", lambda e, st_=st_, io_=io_, t=t: e.tensor_scalar(out=st_[:], in0=io_[:], scalar1=R12[:, 0, t:t + 1], scalar2=None, op0=ALU.is_equal), reads=[io_, R12], writes=[st_])
                P.op("dve", lambda e, st_=st_, io_=io_, t=t, sel=sel: e.scalar_tensor_tensor(out=sel[:, t, :], in0=io_[:], scalar=R12[:, 1, t:t + 1], in1=st_[:], op0=ALU.is_equal, op1=ALU.add),
                     reads=[io_, R12, st_], writes=[sel])
            xbt = XbT[bo % 2]
            for g in range(4):
                pg_ = pM[g]
                for jj in range(4):
                    j = g * 4 + jj
                    for t in range(NTILE):
                        P.op("pe", lambda e, pg_=pg_, jj=jj, j=j, t=t, sel=sel: e.matmul(pg_[:, jj * 128:(jj + 1) * 128], lhsT=h2p[:, t, j * 128:(j + 1) * 128], rhs=sel[:, t, :], start=(t == 0), stop=(t == NTILE - 1)),
                             reads=[h2p, sel], writes=[pg_])
                if g % 2 == 0:
                    P.op("act", lambda e, pg_=pg_, g=g, xbt=xbt: e.activation(out=xbt[:, g * 4:(g + 1) * 4, :], in_=pg_[:].rearrange("p (a b) -> p a b", a=4), func=AF.Copy), reads=[pg_], writes=[xbt])
                else:
                    P.op("dve", lambda e, pg_=pg_, g=g, xbt=xbt: e.tensor_copy(out=xbt[:, g * 4:(g + 1) * 4, :], in_=pg_[:].rearrange("p (a b) -> p a b", a=4)), reads=[pg_], writes=[xbt])
            for hf in range(2):
                pgt, put = pS[0], pS[1]
                for (pp_, wsl) in ((pgt, wg_), (put, wu_)):
                    for j in range(16):
                        P.op("pe", lambda e, pp_=pp_, wsl=wsl, j=j, hf=hf, xbt=xbt: e.matmul(pp_[:, 0:512], lhsT=xbt[:, j, :], rhs=wsl[:].rearrange("p (j n) -> p j n", j=16)[:, j, hf * 512:(hf + 1) * 512], start=(j == 0), stop=(j == 15)),
                             reads=[xbt, wsl], writes=[pp_])
                sx = sgm[hf]
                P.op("act", lambda e, pgt=pgt, sx=sx: e.activation(out=sx[:], in_=pgt[:, 0:512], func=AF.Silu), reads=[pgt], writes=[sx])
                P.op("dve", lambda e, put=put, sx=sx, hf=hf: e.tensor_tensor(out=hperm[:].rearrange("t (j p) -> t p j", j=8)[:, hf * 64:(hf + 1) * 64, :],
                                                                          in0=put[:, 0:512].rearrange("t (p j) -> t p j", j=8), in1=sx[:].rearrange("t (p j) -> t p j", j=8), op=ALU.mult),
                     reads=[put, sx], writes=[hperm])
            ptm = pT[0]
            ptb = ptm[:].bitcast(BF16)
            for j in range(8):
                P.op("pe", lambda e, ptb=ptb, j=j: e.transpose(out=ptb[:, j * 128:(j + 1) * 128], in_=hperm[:, j * 128:(j + 1) * 128], identity=ident_b[:]), reads=[hperm, ident_b], writes=[ptm])
            P.op("act", lambda e, ptb=ptb: e.activation(out=hTm[:], in_=ptb.rearrange("p (a b) -> p a b", a=8), func=AF.Copy), reads=[ptm], writes=[hTm])
            yst = Yst[bo % 2]
            for cb in range(4):
                pd = pT[1]
                for j in range(8):
                    P.op("pe", lambda e, pd=pd, j=j, cb=cb, wd_=wd_: e.matmul(pd[:, 0:512], lhsT=hTm[:, j, :], rhs=wd_[:].rearrange("p (j n) -> p j n", j=8)[:, j, cb * 512:(cb + 1) * 512], start=(j == 0), stop=(j == 7)),
                         reads=[hTm, wd_], writes=[pd])
                if cb % 2 == 0:
                    P.op("act", lambda e, pd=pd, cb=cb, yst=yst: e.activation(out=yst[:, cb * 512:(cb + 1) * 512], in_=pd[:, 0:512], func=AF.Copy), reads=[pd], writes=[yst])
                else:
                    P.op("dve", lambda e, pd=pd, cb=cb, yst=yst: e.tensor_copy(out=yst[:, cb * 512:(cb + 1) * 512], in_=pd[:, 0:512]), reads=[pd], writes=[yst])
            P.dma("sp", lambda e, yst=yst, b=b: e.dma_start(out=yscr[b * 128:(b + 1) * 128, :], in_=yst[:]), reads=[yst], writes=[yscr], semtile=yst)
        P.barrier()
        nf = AR.view("nf", 0, [D], F32)
        junk3 = AR.view("junk3", 8192, [2048], BF16)
        xc = [AR.view("xc%d" % i, 16384 + i * 8192, [D], F32) for i in range(2)]
        y1 = [AR.view("y1_%d" % i, 32768 + i * 8192, [D], F32) for i in range(2)]
        y2 = [AR.view("y2_%d" % i, 49152 + i * 8192, [D], F32) for i in range(2)]
        P.dma("sp", lambda e: e.dma_start(out=nf[:], in_=norm_f[:].partition_broadcast(128)), writes=[nf], semtile=nf)
        for t in range(NTILE):
            xx, ya, yb_ = xc[t % 2], y1[t % 2], y2[t % 2]
            P.dma("pool", lambda e, xx=xx, t=t: e.dma_start(out=xx[:], in_=x1scr[t * 128:(t + 1) * 128, :]), reads=[x1scr], writes=[xx], semtile=xx)
            for k, yy in ((0, ya), (1, yb_)):
                P.dma("pool", lambda e, yy=yy, k=k, t=t: e.indirect_dma_start(out=yy[:], out_offset=None, in_=yscr[:], in_offset=bass.IndirectOffsetOnAxis(ap=Ri[:, k, t:t + 1], axis=0),
                                                                       bounds_check=breg(e, "y", NBLK * 128 - 1), oob_is_err=False), reads=[Ri, yscr], writes=[yy], semtile=yy)
            P.op("dve", lambda e, xx=xx, ya=ya, t=t: e.scalar_tensor_tensor(out=xx[:], in0=ya[:], scalar=G12[:, 0, t:t + 1], in1=xx[:], op0=ALU.mult, op1=ALU.add), reads=[ya, G12, xx], writes=[xx])
            P.op("dve", lambda e, xx=xx, yb_=yb_, t=t: e.scalar_tensor_tensor(out=xx[:], in0=yb_[:], scalar=G12[:, 1, t:t + 1], in1=xx[:], op0=ALU.mult, op1=ALU.add), reads=[yb_, G12, xx], writes=[xx])
            P.op("act", lambda e, xx=xx: e.activation(out=junk3[:], in_=xx[:], func=AF.Square, accum_out=small[:, 0:1]), reads=[xx], writes=[junk3, small])
            P.op("act", lambda e: e.activation(out=small[:, 1:2], in_=small[:, 0:1], func=AF.Sqrt, bias=epsb[:], scale=1.0 / D), reads=[small, epsb], writes=[small])
            P.op("dve", lambda e: e.reciprocal(out=small[:, 2:3], in_=small[:, 1:2]), reads=[small], writes=[small])
            P.op("dve", lambda e, xx=xx: e.scalar_tensor_tensor(out=xx[:], in0=xx[:], scalar=small[:, 2:3], in1=nf[:], op0=ALU.mult, op1=ALU.mult), reads=[xx, small, nf], writes=[xx])
            P.dma("sp", lambda e, xx=xx, t=t: e.dma_start(out=y_out[t * 128:(t + 1) * 128, :], in_=xx[:]), reads=[xx], semtile=xx, is_output=True)

    P.emit()
    return nc, P


def _tables(half):
    pos = np.concatenate([half * 1024 + np.arange(1024), np.tile(PAST_LEN + np.arange(8), 16)]).astype(np.float32)
    ppos = np.arange(1024).astype(np.float32)
    inv = (np.float32(10000.0) ** (-np.arange(128, dtype=np.float32) / np.float32(128))).astype(np.float32)

    def cs(p):
        ang = (p[None, :] * inv[:, None]).astype(np.float32)
        return np.stack([np.cos(ang.astype(np.float64)), np.sin(ang.astype(np.float64))], axis=1).astype(np.float32)

    dec = np.zeros((H, 128, 4, 128), np.float32)
    i = np.arange(128, dtype=np.float64)
    i8 = (np.arange(128) % 8).astype(np.float64)
    for h in range(H):
        lg = LOG_G[h]
        dec[h, :, 0, :] = np.exp((i + 1) * lg)[None, :]
        dec[h, :, 1, :] = (np.exp(-(i + 1) * lg) * DK ** -0.5)[None, :]
        dec[h, :, 2, :] = np.exp((i8 + 1) * lg)[None, :]
        dec[h, :, 3, :] = (np.exp(-(i8 + 1) * lg) * DK ** -0.5)[None, :]
    j = np.arange(128)
    mp = (j[:, None] <= j[None, :]).astype(np.float32)
    ms = ((j[:, None] <= j[None, :]) & (j[:, None] // 8 == j[None, :] // 8)).astype(np.float32)
    masks = np.stack([mp, ms], axis=1)
    rowmask = (j[:, None] // 8 == np.arange(16)[None, :]).astype(np.float32)
    mconst = np.zeros((128, 3, 128), np.float32)
    mconst[:, 0, :] = (j[:, None] < j[None, :])
    mconst[:, 1, :] = j[None, :]
    mconst[:, 2, :] = j[:, None]
    return dict(cs_own=cs(pos), cs_prev=cs(ppos), dec=dec, masks=np.ascontiguousarray(masks),
                ident=np.eye(128, dtype=np.float32), rowmask=rowmask, mconst=mconst)


def make_in_maps(inp, n_experts=NE, cores=range(8)):
    f = lambda a: np.ascontiguousarray(np.asarray(a, dtype=np.float32))
    xp = f(inp["x_prompt"])
    xs = f(inp["x_sample"])
    sr = f(inp["state_ret"])[0]
    sc = f(inp["state_conv"])[0]

    def kc_layout(v):
        return f(v).reshape(16, 128).T

    vecs = np.stack([kc_layout(inp["norm_mix"][0]), kc_layout(inp["norm_ffn"][0]), kc_layout(inp["conv_b"][0]),
                     kc_layout(inp["conv_ln_w"][0]), kc_layout(inp["conv_ln_b"][0]), np.zeros((128, 16), np.float32)], axis=1)
    shared = dict(
        w_in=f(inp["w_in"])[0], w_ret_o=f(inp["w_ret_o"])[0], conv_w=f(inp["conv_w"])[0], vecs=np.ascontiguousarray(vecs),
        norm_f=f(inp["norm_f"]).reshape(1, D), w_conv_o=f(inp["w_conv_o"])[0], w_o=f(inp["w_o"])[0],
        w_rt=np.ascontiguousarray(np.concatenate([f(inp["w_coarse"])[0], f(inp["w_fine"])[0]], axis=1)),
        b_rt=np.concatenate([f(inp["b_coarse"])[0], f(inp["b_fine"])[0]]).reshape(1, 36),
        w_gate=f(inp["w_gate"])[0][:n_experts], w_up=f(inp["w_up"])[0][:n_experts], w_down=f(inp["w_down"])[0][:n_experts],
    )
    tabs = [_tables(0), _tables(1)]
    maps = []
    for c in cores:
        b, half = c // 2, c % 2
        m = dict(shared)
        m.update(tabs[half])
        m["x_own"] = np.ascontiguousarray(np.concatenate([xp[b, half * 1024:(half + 1) * 1024], xs[16 * c:16 * (c + 1)].reshape(128, D)], axis=0))
        m["x_prev"] = np.ascontiguousarray(xp[b, 0:1024]) if half == 1 else np.zeros((1024, D), np.float32)
        m["sret"] = np.ascontiguousarray(sr[16 * c:16 * (c + 1)])
        m["sconv"] = np.ascontiguousarray(sc[16 * c:16 * (c + 1)].reshape(16 * 30, D))
        maps.append(m)
    return maps


_CACHE = {}


def kernel(**inputs):
    if "nc" not in _CACHE:
        _CACHE["nc"] = build()[0]
    nc = _CACHE["nc"]
    maps = make_in_maps(inputs)
    res = run_bass_kernel_spmd(nc, maps, core_ids=list(range(8)))
    r = res.results
    yp = np.zeros((4, 2048, D), np.float32)
    ys = np.zeros((128, 8, D), np.float32)
    retp = np.zeros((1, 4, H, DK, DV), np.float32)
    convp = np.zeros((1, 4, 30, D), np.float32)
    rets = np.zeros((1, 128, H, DK, DV), np.float32)
    convs = np.zeros((1, 128, 30, D), np.float32)
    for c in range(8):
        b, half = c // 2, c % 2
        yp[b, half * 1024:(half + 1) * 1024] = r[c]["y_out"][:1024]
        ys[16 * c:16 * (c + 1)] = r[c]["y_out"][1024:].reshape(16, 8, D)
        rets[0, 16 * c:16 * (c + 1)] = r[c]["rets_out"]
        convs[0, 16 * c:16 * (c + 1)] = r[c]["convs_out"]
        if half == 1:
            retp[0, b] = r[c]["retp_out"]
            convp[0, b] = r[c]["convp_out"]
    return (yp, ys, retp, convp, rets, convs)
```
